# Optimizing a Trainium2 kernel written in Bass

```python
import math
import jax, jax.numpy as jnp
from jax import lax
import numpy as np

D_MODEL = 1024
BATCH = 8
SEQ = 2048
DEPTH = 2

P_DIM = 256
N_GROUPS = 4
GROUP_WIDTH = D_MODEL // N_GROUPS
HEAD_DIM = 64
N_HEADS = GROUP_WIDTH // HEAD_DIM
DIFF_QK_DIM = HEAD_DIM // 2
MOBA_BLOCK = 256
MOBA_TOPK = 3
Q_BLOCK = 128
GMLP_CHUNK = 128
CONV_WIDTH = 31
FFN_CONV_WIDTH = 3
D_FF = 256 * ((8 * D_MODEL // 3 + 255) // 256)
ROPE_THETA = 10000.0
EPS = 1e-6

kernel_name = "hymba_style_moba_gmlp_conformer_diffattn_block"


def rms_norm(x, g, eps=EPS):
    xf = x.astype(jnp.float32)
    y = xf * lax.rsqrt(jnp.mean(xf * xf, axis=-1, keepdims=True) + eps)
    return (y * g.astype(jnp.float32)).astype(x.dtype)


def layer_norm(x, g, b, eps=EPS):
    xf = x.astype(jnp.float32)
    mu = jnp.mean(xf, axis=-1, keepdims=True)
    xc = xf - mu
    y = xc * lax.rsqrt(jnp.mean(xc * xc, axis=-1, keepdims=True) + eps)
    return (y * g.astype(jnp.float32) + b.astype(jnp.float32)).astype(x.dtype)


def rope_tables(positions, dim):
    inv = ROPE_THETA ** (-(jnp.arange(0, dim, 2, dtype=jnp.float32) / dim))
    ang = positions.astype(jnp.float32)[..., None] * inv
    return jnp.cos(ang), jnp.sin(ang)


def apply_rope(x, cos, sin):
    c = cos[:, None].astype(x.dtype)
    s = sin[:, None].astype(x.dtype)
    x1, x2 = jnp.split(x, 2, axis=-1)
    return jnp.concatenate([x1 * c - x2 * s, x1 * s + x2 * c], axis=-1)


def causal_dwconv(x, w, b):
    K, C = w.shape
    y = lax.conv_general_dilated(
        x, w[:, None, :].astype(x.dtype), window_strides=(1,), padding=[(K - 1, 0)],
        dimension_numbers=("NWC", "WIO", "NWC"), feature_group_count=C)
    return y + b


def split_heads(t, n):
    B, S, _ = t.shape
    return t.reshape(B, S, n, -1).transpose(0, 2, 1, 3)


def merge_heads(t):
    B, H, S, d = t.shape
    return t.transpose(0, 2, 1, 3).reshape(B, S, H * d)


def moba_attention(q, k, v):
    B, H, S, Dh = q.shape
    nb = -(-S // MOBA_BLOCK)
    n_sel = min(MOBA_TOPK, nb)
    pad = nb * MOBA_BLOCK - S
    padw = ((0, 0), (0, 0), (0, pad), (0, 0))
    kb = jnp.pad(k, padw).reshape(B, H, nb, MOBA_BLOCK, Dh)
    vb = jnp.pad(v, padw).reshape(B, H, nb, MOBA_BLOCK, Dh)
    k_mean = jnp.mean(kb.astype(jnp.float32), axis=3)
    scale = Dh ** -0.5
    bi = jnp.arange(B)[:, None, None]
    hi = jnp.arange(H)[None, :, None]
    blk_ids = jnp.arange(nb)
    t_off = jnp.arange(Q_BLOCK)
    k_off = jnp.arange(MOBA_BLOCK)

    def query_block(qi):
        q0 = qi * Q_BLOCK
        cur = q0 // MOBA_BLOCK
        qc = lax.dynamic_slice_in_dim(q, q0, Q_BLOCK, axis=2)
        gate = jnp.einsum("bhtd,bhnd->bhtn", qc.astype(jnp.float32), k_mean)
        gate = jnp.where(blk_ids < cur, gate, -jnp.inf)
        _, sel = lax.top_k(gate, n_sel)
        sel_ok = sel < cur
        logits = []
        for s in range(n_sel):
            ks = kb[bi, hi, sel[..., s]]
            l = jnp.einsum("bhtd,bhtkd->bhtk", qc, ks).astype(jnp.float32) * scale
            logits.append(jnp.where(sel_ok[..., s, None], l, -jnp.inf))
        k_own = lax.dynamic_index_in_dim(kb, cur, axis=2, keepdims=False)
        l_own = jnp.einsum("bhtd,bhkd->bhtk", qc, k_own).astype(jnp.float32) * scale
        causal = (cur * MOBA_BLOCK + k_off)[None, :] <= (q0 + t_off)[:, None]
        logits.append(jnp.where(causal, l_own, -jnp.inf))
        probs = jax.nn.softmax(jnp.concatenate(logits, axis=-1), axis=-1).astype(v.dtype)
        probs = jnp.split(probs, n_sel + 1, axis=-1)
        v_own = lax.dynamic_index_in_dim(vb, cur, axis=2, keepdims=False)
        out = jnp.einsum("bhtk,bhkd->bhtd", probs[-1], v_own)
        for s in range(n_sel):
            out = out + jnp.einsum("bhtk,bhtkd->bhtd", probs[s], vb[bi, hi, sel[..., s]])
        return out

    out = lax.map(query_block, jnp.arange(S // Q_BLOCK))
    return out.transpose(1, 2, 0, 3, 4).reshape(B, H, S, Dh)


def diff_attention(q1, q2, k1, k2, v, lam):
    B, H, S, dc = q1.shape
    scale = dc ** -0.5
    kpos = jnp.arange(S)
    t_off = jnp.arange(Q_BLOCK)

    def query_block(qi):
        q0 = qi * Q_BLOCK
        causal = kpos[None, :] <= (q0 + t_off)[:, None]

        def attn_map(q, k):
            qc = lax.dynamic_slice_in_dim(q, q0, Q_BLOCK, axis=2)
            l = jnp.einsum("bhtd,bhsd->bhts", qc, k).astype(jnp.float32) * scale
            return jax.nn.softmax(jnp.where(causal, l, -jnp.inf), axis=-1)

        a = attn_map(q1, k1) - lam * attn_map(q2, k2)
        return jnp.einsum("bhts,bhsd->bhtd", a.astype(v.dtype), v)

    out = lax.map(query_block, jnp.arange(S // Q_BLOCK))
    return out.transpose(1, 2, 0, 3, 4).reshape(B, H, S, v.shape[-1])


def spatial_gating(z, ln_g, ln_b, ws, bs):
    B, S, _ = z.shape
    u, v = jnp.split(jax.nn.gelu(z), 2, axis=-1)
    v = layer_norm(v, ln_g, ln_b)
    nh, c = ws.shape[0], ws.shape[1]
    v = v.reshape(B, S // c, c, nh, -1)
    w = ws * jnp.tril(jnp.ones((c, c), ws.dtype))
    s = jnp.einsum("hij,bnjhd->bnihd", w, v) + bs.T[None, None, :, :, None]
    return u * s.reshape(B, S, -1)


def conformer_conv(z, dw_w, dw_b, ln_g, ln_b, pw_w, pw_b):
    a, g = jnp.split(z, 2, axis=-1)
    y = a * jax.nn.sigmoid(g)
    y = causal_dwconv(y, dw_w, dw_b)
    y = jax.nn.silu(layer_norm(y, ln_g, ln_b))
    return y @ pw_w + pw_b


def setup_inputs(seed: int = 0) -> dict:
    key = jax.random.key(seed)
    ks = iter(list(jax.random.split(key, 48)))
    L, D, GW = DEPTH, D_MODEL, GROUP_WIDTH

    def nrm(shape, scale):
        return scale * jax.random.normal(next(ks), shape, jnp.float32)

    def gain(shape):
        return 1.0 + nrm(shape, 0.02)

    offsets = jax.random.randint(next(ks), (BATCH, 1), 0, 1024, dtype=jnp.int32)
    positions = offsets + jnp.arange(SEQ, dtype=jnp.int32)[None, :]
    gmlp_bs = 1.0 + nrm((L, N_HEADS, GMLP_CHUNK), 0.01)
    return {
        "x": nrm((BATCH, SEQ, D), 1.0),
        "p": nrm((L, BATCH, SEQ, P_DIM), 1.0),
        "positions": positions,
        "pre_mix_norm": gain((L, D)),
        "w_in": nrm((L, D, 10 * GW), D ** -0.5),
        "gmlp_ln_g": gain((L, GW)),
        "gmlp_ln_b": nrm((L, GW), 0.02),
        "gmlp_ws": nrm((L, N_HEADS, GMLP_CHUNK, GMLP_CHUNK), GMLP_CHUNK ** -0.5),
        "gmlp_bs": gmlp_bs,
        "conv_dw_w": nrm((L, CONV_WIDTH, GW), CONV_WIDTH ** -0.5),
        "conv_dw_b": nrm((L, GW), 0.02),
        "conv_ln_g": gain((L, GW)),
        "conv_ln_b": nrm((L, GW), 0.02),
        "conv_pw_w": nrm((L, GW, GW), GW ** -0.5),
        "conv_pw_b": nrm((L, GW), 0.02),
        "diff_lq1": nrm((L, DIFF_QK_DIM), 0.1),
        "diff_lk1": nrm((L, DIFF_QK_DIM), 0.1),
        "diff_lq2": nrm((L, DIFF_QK_DIM), 0.1),
        "diff_lk2": nrm((L, DIFF_QK_DIM), 0.1),
        "diff_subln_g": gain((L, HEAD_DIM)),
        "out_norm_a": gain((L, GW)),
        "out_norm_b": gain((L, GW)),
        "out_norm_c": gain((L, GW)),
        "w_out": nrm((L, N_GROUPS * GW, D), (N_GROUPS * GW) ** -0.5),
        "post_mix_norm": gain((L, D)),
        "pre_ffn_norm": gain((L, D)),
        "w_up": nrm((L, D, 2 * D_FF), D ** -0.5),
        "ffn_conv_w": nrm((L, FFN_CONV_WIDTH, 2 * D_FF), FFN_CONV_WIDTH ** -0.5),
        "ffn_conv_b": nrm((L, 2 * D_FF), 0.02),
        "w_down": nrm((L, D_FF, D), D_FF ** -0.5),
        "post_ffn_norm": gain((L, D)),
        "pe_gate_norm": gain((L, D)),
        "w_pe_gate": nrm((L, D, D), D ** -0.5),
        "w_pe_proj": nrm((L, P_DIM, D), P_DIM ** -0.5),
    }


def reference(x, p, positions, pre_mix_norm, w_in, gmlp_ln_g, gmlp_ln_b, gmlp_ws, gmlp_bs,
              conv_dw_w, conv_dw_b, conv_ln_g, conv_ln_b, conv_pw_w, conv_pw_b,
              diff_lq1, diff_lk1, diff_lq2, diff_lk2, diff_subln_g,
              out_norm_a, out_norm_b, out_norm_c, w_out, post_mix_norm,
              pre_ffn_norm, w_up, ffn_conv_w, ffn_conv_b, w_down, post_ffn_norm,
              pe_gate_norm, w_pe_gate, w_pe_proj):
    GW = GROUP_WIDTH
    cos_a, sin_a = rope_tables(positions, HEAD_DIM)
    cos_d, sin_d = rope_tables(positions, DIFF_QK_DIM)
    for i in range(DEPTH):
        B, S, _ = x.shape
        h = rms_norm(x, pre_mix_norm[i])
        z = h @ w_in[i]
        z_a, z_b, z_c, z_d = jnp.split(z, [3 * GW, 5 * GW, 7 * GW], axis=-1)

        qa, ka, va = jnp.split(z_a, 3, axis=-1)
        qa = apply_rope(split_heads(qa, N_HEADS), cos_a, sin_a)
        ka = apply_rope(split_heads(ka, N_HEADS), cos_a, sin_a)
        out_a = rms_norm(merge_heads(moba_attention(qa, ka, split_heads(va, N_HEADS))), out_norm_a[i])

        out_b = rms_norm(spatial_gating(z_b, gmlp_ln_g[i], gmlp_ln_b[i], gmlp_ws[i], gmlp_bs[i]), out_norm_b[i])

        out_c = rms_norm(conformer_conv(z_c, conv_dw_w[i], conv_dw_b[i], conv_ln_g[i], conv_ln_b[i],
                                        conv_pw_w[i], conv_pw_b[i]), out_norm_c[i])

        qd, kd, vd = jnp.split(z_d, 3, axis=-1)
        qd = qd.reshape(B, S, N_HEADS, 2, DIFF_QK_DIM).transpose(0, 2, 1, 3, 4)
        kd = kd.reshape(B, S, N_HEADS, 2, DIFF_QK_DIM).transpose(0, 2, 1, 3, 4)
        q1 = apply_rope(qd[..., 0, :], cos_d, sin_d)
        q2 = apply_rope(qd[..., 1, :], cos_d, sin_d)
        k1 = apply_rope(kd[..., 0, :], cos_d, sin_d)
        k2 = apply_rope(kd[..., 1, :], cos_d, sin_d)
        lam_init = 0.8 - 0.6 * math.exp(-0.3 * i)
        lam = (jnp.exp(jnp.sum(diff_lq1[i].astype(jnp.float32) * diff_lk1[i].astype(jnp.float32)))
               - jnp.exp(jnp.sum(diff_lq2[i].astype(jnp.float32) * diff_lk2[i].astype(jnp.float32)))
               + lam_init)
        od = diff_attention(q1, q2, k1, k2, split_heads(vd, N_HEADS), lam)
        od = rms_norm(od, diff_subln_g[i], eps=1e-5) * (1.0 - lam_init)
        out_d = merge_heads(od)

        y = jnp.concatenate([out_a, out_b, out_c, out_d], axis=-1) @ w_out[i]
        x = x + rms_norm(y, post_mix_norm[i])

        u = causal_dwconv(rms_norm(x, pre_ffn_norm[i]) @ w_up[i], ffn_conv_w[i], ffn_conv_b[i])
        g, val = jnp.split(u, 2, axis=-1)
        f = (jax.nn.gelu(g) * val) @ w_down[i]
        x = x + rms_norm(f, post_ffn_norm[i])

        gate = jax.nn.sigmoid(rms_norm(x, pe_gate_norm[i]) @ w_pe_gate[i])
        x = x + gate * (p[i] @ w_pe_proj[i])
    return x
```

```python
import math
import numpy as np
from contextlib import ExitStack
import concourse.bass as bass
import concourse.mybir as mybir
from concourse.bass_utils import run_bass_kernel_spmd

F32 = mybir.dt.float32
BF16 = mybir.dt.bfloat16
I32 = mybir.dt.int32
AF = mybir.ActivationFunctionType
ALU = mybir.AluOpType
AX = mybir.AxisListType

L = 2
D = 1024
SEQ = 2048
GW = 256
DFF = 2816
NJ = DFF // 128
NT = 4
TW = 512
MASK = 30000.0
BIGG = 1.0e6
EPS = 1e-6
ENGS = ["pe", "act", "dve", "pool", "sp"]


class Sched:
    def __init__(self, nc, es, same_engine_sync=True):
        self.nc = nc
        self.es = es
        self.same = same_engine_sync
        self.prog = {e: [] for e in ENGS}
        self.cnt = {}
        self.sems = {}
        self.seen = {e: {} for e in ENGS}
        self.state = {}
        for e in ENGS:
            self._sem(e)

    def _sem(self, key):
        if key not in self.sems:
            self.sems[key] = self.es.enter_context(self.nc.semaphore("s_" + str(key)))
            self.cnt[key] = 0
        return self.sems[key]

    def _engobj(self, e):
        nc = self.nc
        return dict(pe=nc.tensor, act=nc.scalar, dve=nc.vector, pool=nc.gpsimd, sp=nc.sync)[e]

    def _deps(self, e, r, w):
        toks = {}
        for k in r:
            st = self.state.get(k)
            if st and st[0] is not None:
                t = st[0]
                toks[t[0]] = max(toks.get(t[0], 0), t[1])
        for k in w:
            st = self.state.get(k)
            if st:
                if st[0] is not None:
                    t = st[0]
                    toks[t[0]] = max(toks.get(t[0], 0), t[1])
                for t in st[1]:
                    toks[t[0]] = max(toks.get(t[0], 0), t[1])
        waits = []
        for sk, v in toks.items():
            if sk == e and (not self.same or e == "pe"):
                continue
            if self.seen[e].get(sk, 0) >= v:
                continue
            self.seen[e][sk] = v
            waits.append((sk, v))
        return waits

    def _record(self, tok, r, w):
        for k in r:
            st = self.state.setdefault(k, [None, []])
            st[1].append(tok)
        for k in w:
            self.state[k] = [tok, []]

    def op(self, e, fn, r=(), w=(), inc=True):
        waits = self._deps(e, r, w)
        tok = (e, self.cnt[e] + 1)
        if inc:
            self.cnt[e] += 1
        self._record(tok, r, w)
        self.prog[e].append((waits, fn, (e, 1) if inc else None))
        return tok

    def dma(self, q, fn, semkey, r=(), w=()):
        self._sem(semkey)
        waits = self._deps(q, r, w)
        self.cnt[semkey] += 16
        tok = (semkey, self.cnt[semkey])
        self._record(tok, r, w)
        self.prog[q].append((waits, fn, (semkey, 16)))
        return tok

    def barrier(self):
        for e in ENGS:
            waits = []
            for sk, v in self.cnt.items():
                if v == 0 or self.seen[e].get(sk, 0) >= v:
                    continue
                if sk == e and e == "pe":
                    continue
                self.seen[e][sk] = v
                waits.append((sk, v))
            self.prog[e].append((waits, None, None))

    def simulate(self):
        val = {k: 0 for k in self.sems}
        pc = {e: 0 for e in ENGS}
        progress = True
        while progress:
            progress = False
            for e in ENGS:
                while pc[e] < len(self.prog[e]):
                    waits, fn, inc = self.prog[e][pc[e]]
                    if any(val[sk] < v for sk, v in waits):
                        break
                    if inc is not None:
                        val[inc[0]] += inc[1]
                    pc[e] += 1
                    progress = True
        stuck = {e: (pc[e], len(self.prog[e])) for e in ENGS if pc[e] < len(self.prog[e])}
        if stuck:
            msg = []
            for e, (i, n) in stuck.items():
                waits = self.prog[e][i][0]
                msg.append("%s@%d/%d waits %s" % (e, i, n, [(sk, v, val[sk]) for sk, v in waits if val[sk] < v]))
            raise RuntimeError("DEADLOCK in schedule: " + "; ".join(msg))
        for k in self.sems:
            assert val[k] == self.cnt[k], (k, val[k], self.cnt[k])

    def emit(self):
        self.simulate()
        nc = self.nc
        with nc.Block() as block:
            def run(e):
                eng = self._engobj(e)
                for waits, fn, inc in self.prog[e]:
                    for sk, v in waits:
                        eng.wait_ge(self.sems[sk], v)
                    if fn is None:
                        continue
                    ins = fn()
                    if inc is not None:
                        ins.then_inc(self.sems[inc[0]], inc[1])

            @block.tensor
            def _(e):
                run("pe")

            @block.scalar
            def _(e):
                run("act")

            @block.vector
            def _(e):
                run("dve")

            @block.gpsimd
            def _(e):
                run("pool")

            @block.sync
            def _(e):
                run("sp")


class Arena:
    def __init__(self, t, nbytes):
        self.t = t
        self.n = nbytes
        self.off = 0
        self.hi = 0

    def mark(self):
        return self.off

    def release(self, m):
        self.off = m

    def _take(self, nbytes):
        nbytes = (nbytes + 31) // 32 * 32
        o = self.off
        self.off += nbytes
        self.hi = max(self.hi, self.off)
        assert self.off <= self.n, ("arena overflow", self.off, self.n)
        return o

    def f32(self, n):
        o = self._take(4 * n)
        return self.t[:, o // 4: o // 4 + n]

    def bf16(self, n):
        o = self._take(2 * n)
        return self.t[:, o // 4: o // 4 + (n + 1) // 2].bitcast(BF16)

    def i32(self, n):
        o = self._take(4 * n)
        return self.t[:, o // 4: o // 4 + n].bitcast(I32)


class Ring:
    def __init__(self, items):
        self.items = items
        self.i = 0

    def next(self):
        it = self.items[self.i % len(self.items)]
        self.i += 1
        return it


def _host_consts():
    ident = np.eye(128, dtype=np.float32)
    ones = np.ones((128, 128), np.float32)
    blk2 = np.zeros((128, 128), np.float32)
    blk2[0:64, 0:64] = 1
    blk2[64:128, 64:128] = 1

    def rotT(block):
        h = block // 2
        m = np.zeros((128, 128), np.float32)
        for b0 in range(0, 128, block):
            for i in range(h):
                m[b0 + i + h, b0 + i] = -1.0
                m[b0 + i, b0 + i + h] = 1.0
        return m

    tri = np.where(np.arange(128)[None, :] >= np.arange(128)[:, None], 0.0, -MASK).astype(np.float32)
    cmat = np.concatenate([ident, ones, blk2, rotT(64), rotT(32), tri], axis=1)
    tril = (np.arange(128)[:, None] <= np.arange(128)[None, :]).astype(np.float32)
    pm = np.zeros((16, 8), np.float32)
    for qs in range(16):
        cur = qs // 2
        for n in range(8):
            pm[qs, n] = 0.0 if n < cur else (BIGG if n == cur else -2 * BIGG)
    pmask = np.broadcast_to(pm.reshape(1, 128), (128, 128))
    p = np.arange(128)
    invf64 = (10000.0 ** (-(2.0 * (p % 32)) / 64.0)) / (2 * math.pi)
    invf32 = (10000.0 ** (-(2.0 * (p % 16)) / 32.0)) / (2 * math.pi)
    cf = np.concatenate([tril, pmask, invf64[:, None], invf32[:, None]], axis=1).astype(np.float32)
    ind = (np.arange(SEQ)[None, :] // 256 == np.arange(8)[:, None]).astype(np.float32)
    return np.ascontiguousarray(cmat), np.ascontiguousarray(cf), np.ascontiguousarray(ind)


def _fm(v, c):
    return np.ascontiguousarray(np.asarray(v, np.float32).reshape(c, 128).T)


def _host_params(inp):
    vd = np.stack([np.stack([_fm(inp[k][l], 8) for k in ("pre_mix_norm", "post_mix_norm", "pre_ffn_norm", "post_ffn_norm", "pe_gate_norm")], 1) for l in range(L)], 1)
    vg = np.stack([np.stack([_fm(inp[k][l], 2) for k in ("conv_dw_b", "conv_ln_g", "conv_ln_b", "conv_pw_b", "out_norm_a", "out_norm_b", "out_norm_c")], 1) for l in range(L)], 1)
    wdw = np.stack([np.asarray(inp["conv_dw_w"][l], np.float32).reshape(31, 2, 128).transpose(2, 1, 0) for l in range(L)], 1)
    fcw = np.stack([np.asarray(inp["ffn_conv_w"][l], np.float32).reshape(3, 44, 128).transpose(2, 1, 0) for l in range(L)], 1)
    fcb = np.stack([_fm(inp["ffn_conv_b"][l], 44) for l in range(L)], 1)
    gsub = np.stack([np.asarray(inp["diff_subln_g"][l], np.float32)[np.arange(128) % 64] for l in range(L)], 1)
    lamv = np.stack([np.stack([np.asarray(inp[k][l], np.float32) for k in ("diff_lq1", "diff_lk1", "diff_lq2", "diff_lk2")], 0) for l in range(L)], 0)
    lamv = np.broadcast_to(lamv.reshape(1, L * 4 * 32), (128, L * 4 * 32))
    pv = np.concatenate([vd.reshape(128, -1), vg.reshape(128, -1), wdw.reshape(128, -1), fcw.reshape(128, -1), fcb.reshape(128, -1), gsub.reshape(128, -1), lamv], axis=1)
    glnb = np.stack([np.stack([np.broadcast_to(np.asarray(inp[k][l], np.float32)[None, :], (128, 256)) for k in ("gmlp_ln_g", "gmlp_ln_b")], 1) for l in range(L)], 0)
    bs = np.asarray(inp["gmlp_bs"], np.float32)
    gb = np.zeros((L, 2, 128, 512), np.float32)
    for l in range(L):
        for cc in range(2):
            for hh in range(2):
                gb[l, cc, hh * 64:(hh + 1) * 64, :] = np.tile(bs[l, 2 * cc + hh], 4)[None, :]
    wsT = np.ascontiguousarray(np.asarray(inp["gmlp_ws"], np.float32).transpose(0, 1, 3, 2))
    return np.ascontiguousarray(pv.astype(np.float32)), np.ascontiguousarray(glnb), gb, wsT


PV_OFF = {}


def _pv_layout():
    o = 0
    for name, n in (("vd", L * 5 * 8), ("vg", L * 7 * 2), ("wdw", L * 2 * 31), ("fcw", L * 44 * 3), ("fcb", L * 44), ("gsub", L), ("lamv", L * 4 * 32)):
        PV_OFF[name] = o
        o += n
    return o


PV_N = _pv_layout()


def build(dbg=None, nlayers=L):
    dbg = dbg or []
    nc = bass.Bass("TRN2", target_bir_lowering=False)

    def din(name, shape, dt=F32):
        return nc.dram_tensor(name, list(shape), dt, kind="ExternalInput").ap()

    xT_d = din("xT", [D, SEQ])
    pT_d = din("pT", [L, GW, SEQ])
    pos_d = din("pos", [1, SEQ], I32)
    cmat_d = din("cmat", [128, 768])
    cf_d = din("cf", [128, 258])
    ind_d = din("ind", [8, SEQ])
    pv_d = din("pv", [128, PV_N])
    glnb_d = din("glnb", [L, 128, 2, 256])
    gb_d = din("gb", [L, 2, 128, 512])
    wsT_d = din("wsT", [L, 4, 128, 128])
    w_in_d = din("w_in", [L, D, 10 * GW])
    w_out_d = din("w_out", [L, D, D])
    w_up_d = din("w_up", [L, D, 2 * DFF])
    w_down_d = din("w_down", [L, DFF, D])
    w_gate_d = din("w_pe_gate", [L, D, D])
    w_proj_d = din("w_pe_proj", [L, GW, D])
    w_pw_d = din("conv_pw_w", [L, GW, GW])
    out_d = nc.dram_tensor("outT", [D, SEQ], F32, kind="ExternalOutput").ap()
    dbg_d = {n: nc.dram_tensor("dbg_" + n, [128, w], F32, kind="ExternalOutput").ap() for n, w in dbg}

    es = ExitStack()
    with es:
        S = Sched(nc, es)
        NB = 200 * 1024
        big = es.enter_context(nc.sbuf_tensor("arena", [128, NB // 4], F32))
        A = Arena(big, NB)
        psb = [es.enter_context(nc.psum_tensor("ps%d" % i, [128, 512], F32)) for i in range(8)]
        PS = [(psb[i][:, :], ("ps", i)) for i in range(8)]
        ringA = Ring(PS[0:4])
        ringB = Ring(PS[4:8])

        def MM(out, lhsT, rhs, start, stop, r, w, inc=True, tp=None):
            kw = {}
            if tp is not None:
                kw["tile_position"] = tp
            S.op("pe", lambda: nc.tensor.matmul(out, lhsT=lhsT, rhs=rhs, start=start, stop=stop, **kw), r=r, w=w, inc=inc)

        def ACT(out, in_, func, r, w, scale=None, bias=None):
            kw = {}
            if scale is not None:
                kw["scale"] = scale
            if bias is not None:
                kw["bias"] = bias
            S.op("act", lambda: nc.scalar.activation(out=out, in_=in_, func=func, **kw), r=r, w=w)

        def TTo(eng, out, in0, in1, op, r, w):
            e = nc.vector if eng == "dve" else nc.gpsimd
            S.op(eng, lambda: e.tensor_tensor(out=out, in0=in0, in1=in1, op=op), r=r, w=w)

        def TS(eng, out, in0, s1, s2, op0, op1, r, w):
            e = nc.vector if eng == "dve" else nc.gpsimd
            if op1 is None:
                S.op(eng, lambda: e.tensor_scalar(out=out, in0=in0, scalar1=s1, scalar2=None, op0=op0), r=r, w=w)
            else:
                S.op(eng, lambda: e.tensor_scalar(out=out, in0=in0, scalar1=s1, scalar2=s2, op0=op0, op1=op1), r=r, w=w)

        def STT(out, in0, scalar, in1, op0, op1, r, w):
            S.op("dve", lambda: nc.vector.scalar_tensor_tensor(out=out, in0=in0, scalar=scalar, in1=in1, op0=op0, op1=op1), r=r, w=w)

        def CP(eng, out, in_, r, w):
            e = nc.vector if eng == "dve" else nc.gpsimd
            S.op(eng, lambda: e.tensor_copy(out=out, in_=in_), r=r, w=w)

        def MSET(eng, ap, val, w):
            e = nc.vector if eng == "dve" else nc.gpsimd
            S.op(eng, lambda: e.memset(ap, val), w=w)

        def DMA(q, out, in_, semkey, r, w):
            e = dict(sp=nc.sync, pool=nc.gpsimd, act=nc.scalar)[q]
            S.dma(q, lambda: e.dma_start(out=out, in_=in_), semkey, r=r, w=w)

        def dump(name, ap, keys):
            if name in dbg_d:
                w_ = ap.shape[-1]
                stg = A.f32(w_)
                CP("dve", stg[0:ap.shape[0], :], ap, r=keys, w=[("dbgs", name)])
                DMA("sp", dbg_d[name][0:ap.shape[0], 0:w_], stg[0:ap.shape[0], :], "dbg", r=[("dbgs", name)], w=[("dbgo", name)])

        xT = A.f32(8 * SEQ).rearrange("p (c s) -> p c s", c=8)
        hT = A.bf16(8 * SEQ).rearrange("p (c s) -> p c s", c=8)
        cmat = A.bf16(768)
        IDENT, ONES, BLK2, R64, R32, TRI = [cmat[:, i * 128:(i + 1) * 128] for i in range(6)]
        cf = A.f32(258)
        TRIL = cf[:, 0:128]
        PMASK = cf[:, 128:256].rearrange("p (q n) -> p q n", n=8)
        INVF = [cf[:, 256:257], cf[:, 257:258]]
        pv = A.f32(PV_N)

        def PVv(name, n):
            return pv[:, PV_OFF[name]:PV_OFF[name] + n]

        vd = PVv("vd", L * 5 * 8).rearrange("p (l v c) -> p l v c", l=L, v=5)
        vg = PVv("vg", L * 7 * 2).rearrange("p (l v c) -> p l v c", l=L, v=7)
        wdw = PVv("wdw", L * 2 * 31).rearrange("p (l c k) -> p l c k", l=L, c=2)
        fcw = PVv("fcw", L * 44 * 3).rearrange("p (l j k) -> p l j k", l=L, j=44)
        fcb = PVv("fcb", L * 44).rearrange("p (l j) -> p l j", l=L)
        gsub = PVv("gsub", L)
        lamv = PVv("lamv", L * 4 * 32).rearrange("p (l v k) -> p l v k", l=L, v=4)
        small = A.f32(64)
        lnv = A.f32(512)
        rstd = A.f32(512)
        sq = [A.bf16(512) for _ in range(3)]
        sqring = Ring([(sq[i], ("sq", i)) for i in range(3)])
        PH = A.mark()

        DMA("pool", cmat, cmat_d[:, :], "ld_c", r=[], w=["cmat"])
        DMA("sp", cf, cf_d[:, :], "ld_c2", r=[], w=["cf"])
        DMA("sp", pv, pv_d[:, :], "ld_c3", r=[], w=["pv"])
        for c in range(8):
            DMA("sp", xT[:, c, :], xT_d[c * 128:(c + 1) * 128, :], ("ld_x", c), r=[], w=[("xT", c, t) for t in range(NT)])

        def wload(dst, src, key):
            DMA("pool", dst, src, ("wsem",) + key, r=[], w=[key])

        def rstd_from(ps_ap, ps_key, inv_n, eps, extra_r=()):
            ACT(lnv, ps_ap, AF.Ln, r=[ps_key] + list(extra_r), w=["lnv"], scale=inv_n, bias=eps)
            ACT(rstd, lnv, AF.Exp, r=["lnv"], w=["rstd"], scale=-0.5)

        def rmsnorm_tile(srcs, skeys, dsts, dkeys, gains, n_feat, eps=EPS, lhs=None, lkey="cmat"):
            ps, pk = ringB.next()
            C = len(srcs)
            for c in range(C):
                q, qk = sqring.next()
                ACT(q, srcs[c], AF.Square, r=[skeys[c]], w=[qk])
                MM(ps, lhs if lhs is not None else ONES, q, c == 0, c == C - 1, r=[qk, lkey], w=[pk])
            rstd_from(ps, pk, 1.0 / n_feat, eps)
            for c in range(C):
                STT(dsts[c], srcs[c], gains[c], rstd, ALU.mult, ALU.mult, r=[skeys[c], "rstd", "pv"], w=[dkeys[c]])

        def sl(t):
            return slice(t * TW, (t + 1) * TW)

        def norm_x_to_h(l, vidx):
            for t in range(NT):
                rmsnorm_tile([xT[:, c, sl(t)] for c in range(8)], [("xT", c, t) for c in range(8)],
                             [hT[:, c, sl(t)] for c in range(8)], [("hT", c, t) for c in range(8)],
                             [vd[:, l, vidx, c:c + 1] for c in range(8)], D)

        def proj(ps, pk, wv, wkey, cs, rhs_fn, rkeys_fn, nk=8):
            for kc in range(nk):
                MM(ps, wv[:, kc, cs], rhs_fn(kc), kc == 0, kc == nk - 1, r=[wkey] + rkeys_fn(kc), w=[pk], inc=(kc == nk - 1))

        def hrhs(t):
            return (lambda kc: hT[:, kc, sl(t)]), (lambda kc: [("hT", kc, t)])

        for l in range(nlayers):
            S.barrier()
            A.release(PH)
            catT = A.bf16(8 * SEQ).rearrange("p (c s) -> p c s", c=8)
            ropeC = A.bf16(SEQ)
            ropeS = A.bf16(SEQ)
            wslots = [A.bf16(8 * 256).rearrange("p (k n) -> p k n", k=8) for _ in range(3)]
            wring = Ring([(wslots[i], ("ws", i)) for i in range(3)])
            MX = A.mark()

            def win_block(bi):
                wv, wk = wring.next()
                wload(wv, w_in_d[l].rearrange("(k p) n -> p k n", p=128)[:, :, bi * 256:(bi + 1) * 256], wk)
                return wv, wk

            def rope_tables(which):
                m = A.mark()
                posi = A.i32(SEQ)
                v = A.f32(SEQ)
                ki = A.i32(SEQ)
                DMA("sp", posi, pos_d[0:1, :].broadcast_to([128, SEQ]), "ld_pos", r=[], w=["posi"])
                for tab, shift, key in ((ropeS, 0.0, "ropeS"), (ropeC, 0.25, "ropeC")):
                    TS("dve", v, posi, INVF[which], shift, ALU.mult, ALU.add, r=["posi", "cf"], w=["ropev"])
                    CP("dve", ki, v, r=["ropev"], w=["ropek"])
                    TTo("dve", v, v, ki, ALU.subtract, r=["ropev", "ropek"], w=["ropev"])
                    ACT(tab, v, AF.Sin, r=["ropev"], w=[key], scale=2 * math.pi * (1 - 1e-6))
                S.barrier()
                A.release(m)

            def rope_apply(ps, pk, t, RM, zbr, t1, t2):
                zb, zk = zbr.next()
                ACT(zb, ps, AF.Copy, r=[pk], w=[zk])
                ps2, pk2 = ringA.next()
                MM(ps2, RM, zb, True, True, r=[zk, "cmat"], w=[pk2])
                TTo("pool", t1[0], zb, ropeC[:, sl(t)], ALU.mult, r=[zk, "ropeC"], w=[t1[1]])
                TTo("dve", t2[0], ps2, ropeS[:, sl(t)], ALU.mult, r=[pk2, "ropeS"], w=[t2[1]])

            def attn_finalize_recip(O, ok, dst_rden):
                ACT(dst_rden[0][0:64, :], O[64:128, :], AF.Ln, r=[ok], w=[dst_rden[1]])
                ACT(dst_rden[0][0:64, :], dst_rden[0][0:64, :], AF.Exp, r=[dst_rden[1]], w=[dst_rden[1]], scale=-1.0)

            def vproj(wv, wk, cs, vaug):
                for g in range(4):
                    ps, pk = ringA.next()
                    for s4 in range(4):
                        st = g * 4 + s4
                        for kc in range(8):
                            MM(ps[:, s4 * 128:(s4 + 1) * 128], hT[:, kc, st * 128:(st + 1) * 128], wv[:, kc, cs], kc == 0, kc == 7,
                               r=[wk, ("hT", kc, st // 4)], w=[pk], inc=(kc == 7 and s4 == 3))
                    ACT(vaug[:, g * 4:(g + 1) * 4, :, 0:64], ps.rearrange("p (s h d) -> p s h d", s=4, h=2), AF.Copy, r=[pk], w=[("V", g)])

            norm_x_to_h(l, 0)
            if l == 0:
                dump("hT0", hT[:, 0, 0:512], [("hT", 0, 0)])

            rope_tables(0)
            m0 = A.mark()
            Kaug = [A.bf16(SEQ) for _ in range(2)]
            vaug = A.bf16(16 * 2 * 128).rearrange("p (s h d) -> p s h d", s=16, h=2)
            Qa = [[A.bf16(512) for _ in range(2)] for _ in range(2)]
            Pr = Ring([(A.bf16(512), ("P", i)) for i in range(4)])
            zbr = Ring([(A.bf16(512), ("zb", i)) for i in range(2)])
            t1 = (A.f32(512), "t1")
            t2 = (A.f32(512), "t2")
            rden = (A.f32(512), "rden")
            bstg = A.bf16(128)
            gm = A.f32(64)
            top8 = A.f32(8)
            kmf = A.f32(16)
            kmb = A.bf16(16)
            MSET("pool", vaug[:, :, :, 64:128], 1.0, w=[("V", g) for g in range(4)])
            MSET("pool", bstg, 0.0, w=["bstg"])
            for hh in range(2):
                DMA("pool", Kaug[hh][64:72, :], ind_d[:, :], "ld_ind", r=[], w=[("Kind", hh)])
            wq, wqk = win_block(0)
            wk_, wkk = win_block(1)
            wv_, wvk = win_block(2)
            for c in range(2):
                cs = slice(c * 128, (c + 1) * 128)
                for t in range(NT):
                    ps, pk = ringA.next()
                    rf, kf = hrhs(t)
                    proj(ps, pk, wk_, wkk, cs, rf, kf)
                    rope_apply(ps, pk, t, R64, zbr, t1, t2)
                    for hh in range(2):
                        TTo("pool", Kaug[hh][0:64, sl(t)], t1[0][hh * 64:(hh + 1) * 64, :], t2[0][hh * 64:(hh + 1) * 64, :], ALU.add,
                            r=[t1[1], t2[1]], w=[("K", hh, t)])
                for hh in range(2):
                    S.op("dve", lambda hh=hh: nc.vector.tensor_reduce(out=kmf[0:64, hh * 8:(hh + 1) * 8], in_=Kaug[hh][0:64, :].rearrange("p (n k) -> p n k", k=256), axis=AX.X, op=ALU.add),
                         r=[("K", hh, t) for t in range(NT)], w=[("kmf", hh)])
                    TS("dve", kmb[0:64, hh * 8:(hh + 1) * 8], kmf[0:64, hh * 8:(hh + 1) * 8], 1.0 / 256, None, ALU.mult, None, r=[("kmf", hh)], w=[("kmb", hh)])
                vproj(wv_, wvk, cs, vaug)
                for qt in range(NT):
                    qb = Qa[qt % 2]
                    ps, pk = ringA.next()
                    rf, kf = hrhs(qt)
                    proj(ps, pk, wq, wqk, cs, rf, kf)
                    rope_apply(ps, pk, qt, R64, zbr, t1, t2)
                    for hh in range(2):
                        TTo("pool", qb[hh][0:64, :], t1[0][hh * 64:(hh + 1) * 64, :], t2[0][hh * 64:(hh + 1) * 64, :], ALU.add,
                            r=[t1[1], t2[1]], w=[("Qa", qt % 2, hh)])
                    for hh in range(2):
                        gps, gk = ringA.next()
                        for j in range(4):
                            MM(gps[:, j * 8:(j + 1) * 8], qb[hh][0:64, j * 128:(j + 1) * 128], kmb[0:64, hh * 8:(hh + 1) * 8], True, True,
                               r=[("Qa", qt % 2, hh), ("kmb", hh)], w=[gk], inc=(j == 3))
                        gmv = gm[:, 0:32].rearrange("p (j n) -> p j n", n=8)
                        TTo("dve", gmv, gps[:, 0:32].rearrange("p (j n) -> p j n", n=8), PMASK[:, qt * 4:(qt + 1) * 4, :], ALU.add, r=[gk, "cf"], w=["gm"])
                        tps, tk = ringA.next()
                        tpsb = tps.bitcast(BF16)
                        for j in range(4):
                            S.op("dve", lambda j=j: nc.vector.max(out=top8, in_=gm[:, j * 8:(j + 1) * 8]), r=["gm"], w=["top8"])
                            TS("dve", top8[:, 3:4], top8[:, 3:4], -BIGG, None, ALU.max, None, r=["top8"], w=["top8"])
                            TS("dve", bstg[:, 64:72], gm[:, j * 8:(j + 1) * 8], top8[:, 3:4], -MASK, ALU.is_lt, ALU.mult, r=["gm", "top8"], w=["bstg"])
                            S.op("pe", lambda j=j, tpsb=tpsb: nc.tensor.transpose(tpsb[0:72, j * 128:(j + 1) * 128], bstg[:, 0:72], IDENT), r=["bstg", "cmat"], w=[tk])
                        ACT(qb[hh][64:72, :], tpsb[64:72, 0:512], AF.Copy, r=[tk], w=[("Qb", qt % 2, hh)])
                    for hh in range(2):
                        O, ok = ringB.next()
                        nk = 4 * qt + 4
                        pend = None
                        for kt in range(nk):
                            jd = kt - 4 * qt
                            c0 = 128 * jd if jd > 0 else 0
                            sp_, sk = ringA.next()
                            MM(sp_[:, c0:512], Kaug[hh][0:72, kt * 128:(kt + 1) * 128], qb[hh][0:72, c0:512], True, jd < 0,
                               r=[("K", hh, kt // 4), ("Kind", hh), ("Qa", qt % 2, hh), ("Qb", qt % 2, hh)], w=[sk], inc=(jd < 0))
                            if jd >= 0:
                                MM(sp_[:, c0:c0 + 128], IDENT, TRI, False, True, r=["cmat"], w=[sk])
                            if pend is not None:
                                pend()
                            P, Pk = Pr.next()
                            ACT(P[:, c0:512], sp_[:, c0:512], AF.Exp, r=[sk], w=[Pk], scale=0.125)

                            def pv_(kt=kt, c0=c0, P=P, Pk=Pk, O=O, ok=ok, hh=hh, nk=nk):
                                MM(O[:, c0:512], vaug[:, kt, hh, :], P[:, c0:512], kt == 0, kt == nk - 1, r=[Pk, ("V", kt // 4)], w=[ok])
                            pend = pv_
                        pend()
                        attn_finalize_recip(O, ok, rden)
                        TTo("dve", catT[hh * 64:(hh + 1) * 64, c, sl(qt)], O[0:64, :], rden[0][0:64, :], ALU.mult, r=[ok, rden[1]], w=[("cat", c, qt)])
            if l == 0:
                dump("oa_pre", catT[:, 0, 0:512], [("cat", 0, 0)])
            for t in range(NT):
                rmsnorm_tile([catT[:, c, sl(t)] for c in range(2)], [("cat", c, t) for c in range(2)],
                             [catT[:, c, sl(t)] for c in range(2)], [("cat", c, t) for c in range(2)],
                             [vg[:, l, 4, c:c + 1] for c in range(2)], GW)
            if l == 0:
                dump("oa", catT[:, 0, 0:512], [("cat", 0, 0)])
            S.barrier()
            A.release(m0)

            rope_tables(1)
            lam_init = 0.8 - 0.6 * math.exp(-0.3 * l)
            lt = small[:, 0:8]
            prod = A.f32(64)
            TTo("dve", prod[:, 0:32], lamv[:, l, 0, :], lamv[:, l, 1, :], ALU.mult, r=["pv"], w=["prod"])
            S.op("dve", lambda: nc.vector.tensor_reduce(out=lt[:, 0:1], in_=prod[:, 0:32], axis=AX.X, op=ALU.add), r=["prod"], w=["lt"])
            TTo("dve", prod[:, 32:64], lamv[:, l, 2, :], lamv[:, l, 3, :], ALU.mult, r=["pv"], w=["prod2"])
            S.op("dve", lambda: nc.vector.tensor_reduce(out=lt[:, 1:2], in_=prod[:, 32:64], axis=AX.X, op=ALU.add), r=["prod2"], w=["lt"])
            ACT(lt[:, 2:4], lt[:, 0:2], AF.Exp, r=["lt"], w=["lt"])
            TTo("dve", lt[:, 4:5], lt[:, 3:4], lt[:, 2:3], ALU.subtract, r=["lt"], w=["lt"])
            TS("dve", lt[:, 5:6], lt[:, 4:5], -lam_init, None, ALU.add, None, r=["lt"], w=["lt"])
            TS("dve", lt[:, 6:7], gsub[:, l:l + 1], 1.0 - lam_init, None, ALU.mult, None, r=["pv", "lt"], w=["lt"])
            NEGLAM = lt[:, 5:6]
            GS = lt[:, 6:7]

            m0 = A.mark()
            Kd = A.bf16(SEQ)
            vaug = A.bf16(16 * 2 * 128).rearrange("p (s h d) -> p s h d", s=16, h=2)
            Qd = [A.bf16(512) for _ in range(2)]
            Pr = Ring([(A.bf16(512), ("P", i)) for i in range(4)])
            zbr = Ring([(A.bf16(512), ("zb", i)) for i in range(2)])
            t1 = (A.f32(512), "t1")
            t2 = (A.f32(512), "t2")
            rden = (A.f32(512), "rden")
            od = A.f32(512)
            MSET("pool", vaug[:, :, :, 64:128], 1.0, w=[("V", g) for g in range(4)])
            wq, wqk = win_block(7)
            wk_, wkk = win_block(8)
            wv_, wvk = win_block(9)
            dscale = 32.0 ** -0.5
            for c in range(2):
                cs = slice(c * 128, (c + 1) * 128)
                for t in range(NT):
                    ps, pk = ringA.next()
                    rf, kf = hrhs(t)
                    proj(ps, pk, wk_, wkk, cs, rf, kf)
                    rope_apply(ps, pk, t, R32, zbr, t1, t2)
                    TTo("pool", Kd[:, sl(t)], t1[0], t2[0], ALU.add, r=[t1[1], t2[1]], w=[("Kd", t)])
                vproj(wv_, wvk, cs, vaug)
                for qt in range(NT):
                    qd = Qd[qt % 2]
                    ps, pk = ringA.next()
                    rf, kf = hrhs(qt)
                    proj(ps, pk, wq, wqk, cs, rf, kf)
                    rope_apply(ps, pk, qt, R32, zbr, t1, t2)
                    TTo("pool", qd, t1[0], t2[0], ALU.add, r=[t1[1], t2[1]], w=[("Qd", qt % 2)])
                    for hh in range(2):
                        Os = [ringB.next(), ringB.next()]
                        nk = 4 * qt + 4
                        pend = []
                        for kt in range(nk):
                            jd = kt - 4 * qt
                            c0 = 128 * jd if jd > 0 else 0
                            cur = []
                            for m_ in range(2):
                                g = hh * 2 + m_
                                sp_, sk = ringA.next()
                                MM(sp_[:, c0:512], Kd[32 * g:32 * g + 32, kt * 128:(kt + 1) * 128], qd[32 * g:32 * g + 32, c0:512], True, jd < 0,
                                   r=[("Kd", kt // 4), ("Qd", qt % 2)], w=[sk], inc=(jd < 0), tp=(32 * g, 0))
                                if jd >= 0:
                                    MM(sp_[:, c0:c0 + 128], IDENT, TRI, False, True, r=["cmat"], w=[sk])
                                cur.append((sp_, sk))
                            for f in pend:
                                f()
                            pend = []
                            for m_ in range(2):
                                sp_, sk = cur[m_]
                                P, Pk = Pr.next()
                                ACT(P[:, c0:512], sp_[:, c0:512], AF.Exp, r=[sk], w=[Pk], scale=dscale)

                                def pv_(kt=kt, c0=c0, P=P, Pk=Pk, Oo=Os[m_], hh=hh, nk=nk):
                                    MM(Oo[0][:, c0:512], vaug[:, kt, hh, :], P[:, c0:512], kt == 0, kt == nk - 1, r=[Pk, ("V", kt // 4)], w=[Oo[1]])
                                pend.append(pv_)
                        for f in pend:
                            f()
                        hs = slice(hh * 64, (hh + 1) * 64)
                        attn_finalize_recip(Os[0][0], Os[0][1], rden)
                        TTo("dve", t1[0][0:64, :], Os[0][0][0:64, :], rden[0][0:64, :], ALU.mult, r=[Os[0][1], rden[1]], w=[t1[1]])
                        attn_finalize_recip(Os[1][0], Os[1][1], rden)
                        TTo("dve", t2[0][0:64, :], Os[1][0][0:64, :], rden[0][0:64, :], ALU.mult, r=[Os[1][1], rden[1]], w=[t2[1]])
                        STT(od[hs, :], t2[0][0:64, :], NEGLAM[0:64, :], t1[0][0:64, :], ALU.mult, ALU.add, r=[t1[1], t2[1], "lt"], w=[("od", hh)])
                    ps, pk = ringB.next()
                    q_, qk_ = sqring.next()
                    ACT(q_, od, AF.Square, r=[("od", 0), ("od", 1)], w=[qk_])
                    MM(ps, BLK2, q_, True, True, r=[qk_, "cmat"], w=[pk])
                    rstd_from(ps, pk, 1.0 / 64, 1e-5)
                    STT(catT[:, 6 + c, sl(qt)], od, GS, rstd, ALU.mult, ALU.mult, r=[("od", 0), ("od", 1), "rstd", "lt"], w=[("cat", 6 + c, qt)])
            if l == 0:
                dump("od", catT[:, 6, 0:512], [("cat", 6, 0)])
            S.barrier()
            A.release(m0)

            m0 = A.mark()
            uT = A.bf16(2 * SEQ).rearrange("p (c s) -> p c s", c=2)
            vgl = A.bf16(16 * 256).rearrange("p (n d) -> p n d", n=16)
            glnb = A.f32(512).rearrange("p (v d) -> p v d", v=2)
            gb = A.f32(1024).rearrange("p (c s) -> p c s", c=2)
            wsf = A.f32(512).rearrange("p (h i) -> p h i", h=4)
            wsb = A.bf16(512).rearrange("p (h i) -> p h i", h=4)
            stats = A.f32(16 * 6)
            mv = A.f32(16 * 2).rearrange("p (n k) -> p n k", k=2)
            rsd = A.f32(16)
            vtmp = A.f32(256)
            vln = [A.bf16(256) for _ in range(2)]
            stmp = A.f32(512)
            DMA("sp", glnb, glnb_d[l], "ld_g1", r=[], w=["glnb"])
            DMA("sp", gb, gb_d[l].rearrange("c p s -> p c s"), "ld_g2", r=[], w=["gb"])
            DMA("sp", wsf, wsT_d[l].rearrange("h j i -> j h i"), "ld_g3", r=[], w=["wsf"])
            for h in range(4):
                TTo("pool", wsb[:, h, :], wsf[:, h, :], TRIL, ALU.mult, r=["wsf", "cf"], w=["wsb"])
            wu, wuk = win_block(3)
            wvv, wvvk = win_block(4)
            for t in range(NT):
                for cc in range(2):
                    ps, pk = ringA.next()
                    rf, kf = hrhs(t)
                    proj(ps, pk, wu, wuk, slice(cc * 128, (cc + 1) * 128), rf, kf)
                    ACT(uT[:, cc, sl(t)], ps, AF.Gelu_apprx_tanh, r=[pk], w=[("uT", cc, t)])
            for n2 in range(8):
                ps, pk = ringA.next()
                for s2 in range(2):
                    n = n2 * 2 + s2
                    for kc in range(8):
                        MM(ps[:, s2 * 256:(s2 + 1) * 256], hT[:, kc, n * 128:(n + 1) * 128], wvv[:, kc, :], kc == 0, kc == 7,
                           r=[wvvk, ("hT", kc, n // 4)], w=[pk], inc=(kc == 7 and s2 == 1))
                ACT(vgl[:, n2 * 2:(n2 + 1) * 2, :], ps.rearrange("p (s d) -> p s d", s=2), AF.Gelu_apprx_tanh, r=[pk], w=[("vgl", n2)])
            for n in range(16):
                S.op("dve", lambda n=n: nc.vector.bn_stats(out=stats[:, n * 6:(n + 1) * 6], in_=vgl[:, n, :]), r=[("vgl", n // 2)], w=[("st", n)])
                S.op("dve", lambda n=n: nc.vector.bn_aggr(out=mv[:, n, :], in_=stats[:, n * 6:(n + 1) * 6]), r=[("st", n)], w=["mv"])
            ACT(rsd, mv[:, :, 1], AF.Ln, r=["mv"], w=["rsd"], bias=EPS)
            ACT(rsd, rsd, AF.Exp, r=["rsd"], w=["rsd"], scale=-0.5)
            for t in range(NT):
                pss = [ringA.next(), ringA.next()]
                for s4 in range(4):
                    n = t * 4 + s4
                    vl = vln[n % 2]
                    vk = ("vln", n % 2)
                    TS("dve", vtmp, vgl[:, n, :], mv[:, n, 0:1], rsd[:, n:n + 1], ALU.subtract, ALU.mult, r=[("vgl", n // 2), "mv", "rsd"], w=["vtmp"])
                    TTo("pool", vtmp, vtmp, glnb[:, 0, :], ALU.mult, r=["vtmp", "glnb"], w=["vtmp"])
                    TTo("pool", vl, vtmp, glnb[:, 1, :], ALU.add, r=["vtmp", "glnb"], w=[vk])
                    for cc in range(2):
                        for hh in range(2):
                            h = 2 * cc + hh
                            MM(pss[cc][0][hh * 64:(hh + 1) * 64, s4 * 128:(s4 + 1) * 128], vl[:, h * 64:(h + 1) * 64], wsb[:, h, :], True, True,
                               r=[vk, "wsb"], w=[pss[cc][1]], tp=(0, hh * 64))
                for cc in range(2):
                    TTo("dve", stmp, pss[cc][0], gb[:, cc, :], ALU.add, r=[pss[cc][1], "gb"], w=["stmp"])
                    TTo("pool", catT[:, 2 + cc, sl(t)], stmp, uT[:, cc, sl(t)], ALU.mult, r=["stmp", ("uT", cc, t)], w=[("cat", 2 + cc, t)])
            if l == 0:
                dump("ob_pre", catT[:, 2, 0:512], [("cat", 2, 0)])
            for t in range(NT):
                rmsnorm_tile([catT[:, 2 + c, sl(t)] for c in range(2)], [("cat", 2 + c, t) for c in range(2)],
                             [catT[:, 2 + c, sl(t)] for c in range(2)], [("cat", 2 + c, t) for c in range(2)],
                             [vg[:, l, 5, c:c + 1] for c in range(2)], GW)
            S.barrier()
            A.release(m0)

            m0 = A.mark()
            ybuf = A.bf16(32 + SEQ)
            diag = A.bf16(31 * 128).rearrange("p (k n) -> p k n", k=31)
            cy = A.bf16(2 * SEQ).rearrange("p (c s) -> p c s", c=2)
            sg = A.f32(512)
            wpw = A.bf16(2 * 256).rearrange("p (k n) -> p k n", k=2)
            mstat = A.f32(512)
            m2 = A.f32(512)
            yn = A.f32(512)
            sil = A.bf16(1024).rearrange("p (c s) -> p c s", c=2)
            Y0 = 2
            wa, wak = win_block(5)
            wg_, wgk = win_block(6)
            wload(wpw, w_pw_d[l].rearrange("(k p) n -> p k n", p=128), ("wpw",))
            MSET("pool", ybuf[:, 0:32], 0.0, w=["ypad"])
            for cc in range(2):
                cs = slice(cc * 128, (cc + 1) * 128)
                for k in range(31):
                    TS("pool", diag[:, k, :], IDENT, wdw[:, l, cc, k:k + 1], None, ALU.mult, None, r=["cmat", "pv"], w=[("diag", k)])
                for t in range(NT):
                    pa, pak = ringA.next()
                    rf, kf = hrhs(t)
                    proj(pa, pak, wa, wak, cs, rf, kf)
                    pg, pgk = ringA.next()
                    proj(pg, pgk, wg_, wgk, cs, rf, kf)
                    ACT(sg, pg, AF.Sigmoid, r=[pgk], w=["sg"])
                    TTo("dve", ybuf[:, 32 + t * TW:32 + (t + 1) * TW], pa, sg, ALU.mult, r=[pak, "sg"], w=[("yb", t)])
                for t in range(NT):
                    ps, pk = ringB.next()
                    for k in range(31):
                        o = Y0 + t * TW + k
                        rk = ["ypad", ("diag", k), ("yb", t)] + ([("yb", t - 1)] if t > 0 else [])
                        MM(ps, diag[:, k, :], ybuf[:, o:o + TW], k == 0, k == 30, r=rk, w=[pk], inc=(k == 30))
                    ACT(cy[:, cc, sl(t)], ps, AF.Identity, r=[pk, "pv"], w=[("cy", cc, t)], bias=vg[:, l, 0, cc:cc + 1])
            if l == 0:
                dump("cy", cy[:, 0, 0:512], [("cy", 0, 0)])
            for t in range(NT):
                ps1, pk1 = ringB.next()
                ps2, pk2 = ringB.next()
                for cc in range(2):
                    MM(ps1, ONES, cy[:, cc, sl(t)], cc == 0, cc == 1, r=[("cy", cc, t), "cmat"], w=[pk1])
                for cc in range(2):
                    q_, qk_ = sqring.next()
                    ACT(q_, cy[:, cc, sl(t)], AF.Square, r=[("cy", cc, t)], w=[qk_])
                    MM(ps2, ONES, q_, cc == 0, cc == 1, r=[qk_, "cmat"], w=[pk2])
                TS("dve", mstat, ps1, 1.0 / GW, None, ALU.mult, None, r=[pk1], w=["mstat"])
                TTo("pool", m2, mstat, mstat, ALU.mult, r=["mstat"], w=["m2"])
                STT(m2, ps2, 1.0 / GW, m2, ALU.mult, ALU.subtract, r=[pk2, "m2"], w=["m2"])
                ACT(lnv, m2, AF.Ln, r=["m2"], w=["lnv"], bias=EPS)
                ACT(rstd, lnv, AF.Exp, r=["lnv"], w=["rstd"], scale=-0.5)
                for cc in range(2):
                    TTo("dve", yn, cy[:, cc, sl(t)], mstat, ALU.subtract, r=[("cy", cc, t), "mstat"], w=["yn"])
                    TTo("pool", yn, yn, rstd, ALU.mult, r=["yn", "rstd"], w=["yn"])
                    ACT(sil[:, cc, :], yn, AF.Silu, r=["yn", "pv"], w=[("sil", cc)], scale=vg[:, l, 1, cc:cc + 1], bias=vg[:, l, 2, cc:cc + 1])
                for co in range(2):
                    ps, pk = ringA.next()
                    for ci in range(2):
                        MM(ps, wpw[:, ci, co * 128:(co + 1) * 128], sil[:, ci, :], ci == 0, ci == 1, r=[("wpw",), ("sil", ci)], w=[pk])
                    ACT(catT[:, 4 + co, sl(t)], ps, AF.Identity, r=[pk, "pv"], w=[("cat", 4 + co, t)], bias=vg[:, l, 3, co:co + 1])
            if l == 0:
                dump("oc_pre", catT[:, 4, 0:512], [("cat", 4, 0)])
            for t in range(NT):
                rmsnorm_tile([catT[:, 4 + c, sl(t)] for c in range(2)], [("cat", 4 + c, t) for c in range(2)],
                             [catT[:, 4 + c, sl(t)] for c in range(2)], [("cat", 4 + c, t) for c in range(2)],
                             [vg[:, l, 6, c:c + 1] for c in range(2)], GW)
            S.barrier()
            A.release(m0)

            def out_proj_norm_res(nkc, wsrc, rhs_fn, rkeys_fn, vidx, wr, ytile):
                for t in range(NT):
                    for db in range(4):
                        wv, wk = wr.next()
                        wload(wv[:, 0:nkc, :], wsrc[:, :, db * 256:(db + 1) * 256], wk)
                        for dd in range(2):
                            dc = db * 2 + dd
                            ps, pk = ringA.next()
                            for kc in range(nkc):
                                MM(ps, wv[:, kc, dd * 128:(dd + 1) * 128], rhs_fn(kc, t), kc == 0, kc == nkc - 1, r=[wk] + rkeys_fn(kc, t), w=[pk], inc=(kc == nkc - 1))
                            if dc % 2 == 0:
                                ACT(ytile[:, dc, :], ps, AF.Copy, r=[pk], w=[("yt", dc)])
                            else:
                                CP("dve", ytile[:, dc, :], ps, r=[pk], w=[("yt", dc)])
                    rmsnorm_tile([ytile[:, dc, :] for dc in range(8)], [("yt", dc) for dc in range(8)],
                                 [ytile[:, dc, :] for dc in range(8)], [("yt", dc) for dc in range(8)],
                                 [vd[:, l, vidx, dc:dc + 1] for dc in range(8)], D)
                    for dc in range(8):
                        TTo("pool", xT[:, dc, sl(t)], xT[:, dc, sl(t)], ytile[:, dc, :], ALU.add, r=[("xT", dc, t), ("yt", dc)], w=[("xT", dc, t)])

            m0 = A.mark()
            ytile = A.f32(8 * 512).rearrange("p (c s) -> p c s", c=8)
            out_proj_norm_res(8, w_out_d[l].rearrange("(k p) n -> p k n", p=128), lambda kc, t: catT[:, kc, sl(t)], lambda kc, t: [("cat", kc, t)], 1, wring, ytile)
            if l == 0:
                dump("x1", xT[:, 0, 0:512], [("xT", 0, 0)])
            S.barrier()
            A.release(MX)
            A.release(PH)

            norm_x_to_h(l, 2)
            ytile = A.f32(8 * 512).rearrange("p (c s) -> p c s", c=8)
            fT = A.bf16(NJ * 512).rearrange("p (j s) -> p j s", j=NJ)
            ub = [[A.f32(2 + 512) for _ in range(2)] for _ in range(2)]
            halo = A.f32(44 * 2).rearrange("p (j k) -> p j k", k=2)
            ta = A.f32(512)
            tb = A.f32(512)
            cg = A.f32(512)
            cv = A.f32(512)
            wups = [A.bf16(8 * 2 * 128).rearrange("p (k g n) -> p k g n", k=8, g=2) for _ in range(3)]
            wur = Ring([(wups[i], ("wu", i)) for i in range(3)])
            wdns = [A.bf16(NJ * 128).rearrange("p (j n) -> p j n", j=NJ) for _ in range(2)]
            wdr = Ring([(wdns[i], ("wd", i)) for i in range(2)])
            MSET("pool", halo, 0.0, w=["halo"])
            wupv = w_up_d[l].rearrange("(k p) (g n) -> p k g n", p=128, g=2)
            wdnv = w_down_d[l].rearrange("(j p) n -> p j n", p=128)
            for t in range(NT):
                for j in range(NJ):
                    wv, wk = wur.next()
                    for gi_ in range(2):
                        wload(wv[:, :, gi_, :], wupv[:, :, gi_, j * 128:(j + 1) * 128], wk)
                    u = ub[j % 2]
                    conv = []
                    for gi in range(2):
                        jj = j + gi * NJ
                        ps, pk = ringA.next()
                        for kc in range(8):
                            MM(ps, wv[:, kc, gi, :], hT[:, kc, sl(t)], kc == 0, kc == 7, r=[wk, ("hT", kc, t)], w=[pk], inc=(kc == 7))
                        uk = ("ub", j % 2, gi)
                        ACT(u[gi][:, 2:514], ps, AF.Copy, r=[pk], w=[uk])
                        CP("pool", u[gi][:, 0:2], halo[:, jj, :], r=["halo", ("halo", jj)], w=[uk])
                        tmp = ta if gi == 0 else tb
                        tk_ = "ta" if gi == 0 else "tb"
                        ACT(tmp, ps, AF.Identity, r=[pk, "pv"], w=[tk_], scale=fcw[:, l, jj, 2:3], bias=fcb[:, l, jj:jj + 1])
                        STT(tmp, u[gi][:, 1:513], fcw[:, l, jj, 1:2], tmp, ALU.mult, ALU.add, r=[uk, tk_, "pv"], w=[tk_])
                        dst = cg if gi == 0 else cv
                        dk = "cg" if gi == 0 else "cv"
                        STT(dst, u[gi][:, 0:512], fcw[:, l, jj, 0:1], tmp, ALU.mult, ALU.add, r=[uk, tk_, "pv"], w=[dk])
                        CP("pool", halo[:, jj, :], u[gi][:, 512:514], r=[uk], w=[("halo", jj)])
                    ACT(cg, cg, AF.Gelu_apprx_tanh, r=["cg"], w=["cg"])
                    TTo("pool", fT[:, j, :], cg, cv, ALU.mult, r=["cg", "cv"], w=[("fT", j)])
                if l == 0 and t == 0:
                    dump("fT", fT[:, 0, :], [("fT", 0)])
                for dc in range(8):
                    wv, wk = wdr.next()
                    wload(wv, wdnv[:, :, dc * 128:(dc + 1) * 128], wk)
                    ps, pk = ringB.next()
                    for j in range(NJ):
                        MM(ps, wv[:, j, :], fT[:, j, :], j == 0, j == NJ - 1, r=[wk, ("fT", j)], w=[pk], inc=(j == NJ - 1))
                    if dc % 2 == 0:
                        ACT(ytile[:, dc, :], ps, AF.Copy, r=[pk], w=[("yt", dc)])
                    else:
                        CP("dve", ytile[:, dc, :], ps, r=[pk], w=[("yt", dc)])
                rmsnorm_tile([ytile[:, dc, :] for dc in range(8)], [("yt", dc) for dc in range(8)],
                             [ytile[:, dc, :] for dc in range(8)], [("yt", dc) for dc in range(8)],
                             [vd[:, l, 3, dc:dc + 1] for dc in range(8)], D)
                for dc in range(8):
                    TTo("pool", xT[:, dc, sl(t)], xT[:, dc, sl(t)], ytile[:, dc, :], ALU.add, r=[("xT", dc, t), ("yt", dc)], w=[("xT", dc, t)])
            if l == 0:
                dump("x2", xT[:, 0, 0:512], [("xT", 0, 0)])
            S.barrier()
            A.release(PH)

            norm_x_to_h(l, 4)
            pTb = A.bf16(2 * SEQ).rearrange("p (c s) -> p c s", c=2)
            wgs = [A.bf16(8 * 256).rearrange("p (k n) -> p k n", k=8) for _ in range(2)]
            wgr = Ring([(wgs[i], ("wg", i)) for i in range(2)])
            wpj = A.bf16(2 * D).rearrange("p (k n) -> p k n", k=2)
            sgt = [A.f32(512) for _ in range(2)]
            pjt = [A.f32(512) for _ in range(2)]
            wload(pTb, pT_d[l].rearrange("(k p) s -> p k s", p=128), ("pTb",))
            wload(wpj, w_proj_d[l].rearrange("(k p) n -> p k n", p=128), ("wpj",))
            wgv = w_gate_d[l].rearrange("(k p) n -> p k n", p=128)
            it = 0
            for db in range(4):
                wv, wk = wgr.next()
                wload(wv, wgv[:, :, db * 256:(db + 1) * 256], wk)
                for dd in range(2):
                    dc = db * 2 + dd
                    for t in range(NT):
                        b = it % 2
                        it += 1
                        ps, pk = ringA.next()
                        rf, kf = hrhs(t)
                        proj(ps, pk, wv, wk, slice(dd * 128, (dd + 1) * 128), rf, kf)
                        ACT(sgt[b], ps, AF.Sigmoid, r=[pk], w=[("sgt", b)])
                        ps2, pk2 = ringB.next()
                        for kc in range(2):
                            MM(ps2, wpj[:, kc, dc * 128:(dc + 1) * 128], pTb[:, kc, sl(t)], kc == 0, kc == 1, r=[("wpj",), ("pTb",)], w=[pk2], inc=(kc == 1))
                        TTo("dve", pjt[b], ps2, sgt[b], ALU.mult, r=[pk2, ("sgt", b)], w=[("pjt", b)])
                        TTo("pool", xT[:, dc, sl(t)], xT[:, dc, sl(t)], pjt[b], ALU.add, r=[("xT", dc, t), ("pjt", b)], w=[("xT", dc, t)])
            if l == 0:
                dump("x3", xT[:, 0, 0:512], [("xT", 0, 0)])

        for c in range(8):
            DMA("sp", out_d[c * 128:(c + 1) * 128, :], xT[:, c, :], "st_out", r=[("xT", c, t) for t in range(NT)], w=[("out", c)])
        S.barrier()
        S.emit()
    return nc


_CACHE = {}


def _prep_inputs(inp):
    cmat, cf, ind = _host_consts()
    pv, glnb, gb, wsT = _host_params(inp)
    x = np.asarray(inp["x"], np.float32)
    p = np.asarray(inp["p"], np.float32)
    pos = np.asarray(inp["positions"], np.int32)
    shared = dict(cmat=cmat, cf=cf, ind=ind, pv=pv, glnb=glnb, gb=gb, wsT=wsT)
    for k in ("w_in", "w_out", "w_up", "w_down", "w_pe_gate", "w_pe_proj", "conv_pw_w"):
        shared[k] = np.ascontiguousarray(np.asarray(inp[k], np.float32))
    maps = []
    for b in range(8):
        m = dict(shared)
        m["xT"] = np.ascontiguousarray(x[b].T)
        m["pT"] = np.ascontiguousarray(p[:, b].transpose(0, 2, 1))
        m["pos"] = np.ascontiguousarray(pos[b][None, :])
        maps.append(m)
    return maps


def kernel(**inputs):
    if "nc" not in _CACHE:
        _CACHE["nc"] = build()
    nc = _CACHE["nc"]
    maps = _prep_inputs(inputs)
    res = run_bass_kernel_spmd(nc, maps, core_ids=list(range(8)))
    out = np.stack([np.asarray(r["outT"], np.float32).T for r in res.results], axis=0)
    return np.ascontiguousarray(out)
```

```python
import math
import numpy as np
from contextlib import ExitStack
import concourse.bass as bass
import concourse.mybir as mybir
from concourse.bass_utils import run_bass_kernel_spmd

F32 = mybir.dt.float32
BF16 = mybir.dt.bfloat16
I32 = mybir.dt.int32
AF = mybir.ActivationFunctionType
ALU = mybir.AluOpType
AX = mybir.AxisListType

L = 2
D = 1024
SEQ = 2048
GW = 256
DFF = 2816
NJ = DFF // 128
NT = 4
TW = 512
MASK = 30000.0
BIGG = 1.0e6
EPS = 1e-6
ENGS = ["pe", "act", "dve", "pool", "sp"]


class Sched:
    def __init__(self, nc, es, same_engine_sync=True):
        self.nc = nc
        self.es = es
        self.same = same_engine_sync
        self.prog = {e: [] for e in ENGS}
        self.cnt = {}
        self.sems = {}
        self.seen = {e: {} for e in ENGS}
        self.state = {}
        for e in ENGS:
            self._sem(e)

    def _sem(self, key):
        if key not in self.sems:
            self.sems[key] = self.es.enter_context(self.nc.semaphore("s_" + str(key)))
            self.cnt[key] = 0
        return self.sems[key]

    def _engobj(self, e):
        nc = self.nc
        return dict(pe=nc.tensor, act=nc.scalar, dve=nc.vector, pool=nc.gpsimd, sp=nc.sync)[e]

    def _deps(self, e, r, w):
        toks = {}
        for k in r:
            st = self.state.get(k)
            if st and st[0] is not None:
                t = st[0]
                toks[t[0]] = max(toks.get(t[0], 0), t[1])
        for k in w:
            st = self.state.get(k)
            if st:
                if st[0] is not None:
                    t = st[0]
                    toks[t[0]] = max(toks.get(t[0], 0), t[1])
                for t in st[1]:
                    toks[t[0]] = max(toks.get(t[0], 0), t[1])
        waits = []
        for sk, v in toks.items():
            if sk == e and (not self.same or e == "pe"):
                continue
            if self.seen[e].get(sk, 0) >= v:
                continue
            self.seen[e][sk] = v
            waits.append((sk, v))
        return waits

    def _record(self, tok, r, w):
        for k in r:
            st = self.state.setdefault(k, [None, []])
            st[1].append(tok)
        for k in w:
            self.state[k] = [tok, []]

    def op(self, e, fn, r=(), w=(), inc=True):
        waits = self._deps(e, r, w)
        tok = (e, self.cnt[e] + 1)
        if inc:
            self.cnt[e] += 1
        self._record(tok, r, w)
        self.prog[e].append((waits, fn, (e, 1) if inc else None))
        return tok

    def dma(self, q, fn, semkey, r=(), w=()):
        self._sem(semkey)
        waits = self._deps(q, r, w)
        self.cnt[semkey] += 16
        tok = (semkey, self.cnt[semkey])
        self._record(tok, r, w)
        self.prog[q].append((waits, fn, (semkey, 16)))
        return tok

    def barrier(self):
        for e in ENGS:
            waits = []
            for sk, v in self.cnt.items():
                if v == 0 or self.seen[e].get(sk, 0) >= v:
                    continue
                if sk == e and e == "pe":
                    continue
                self.seen[e][sk] = v
                waits.append((sk, v))
            self.prog[e].append((waits, None, None))

    def simulate(self):
        val = {k: 0 for k in self.sems}
        pc = {e: 0 for e in ENGS}
        progress = True
        while progress:
            progress = False
            for e in ENGS:
                while pc[e] < len(self.prog[e]):
                    waits, fn, inc = self.prog[e][pc[e]]
                    if any(val[sk] < v for sk, v in waits):
                        break
                    if inc is not None:
                        val[inc[0]] += inc[1]
                    pc[e] += 1
                    progress = True
        stuck = {e: (pc[e], len(self.prog[e])) for e in ENGS if pc[e] < len(self.prog[e])}
        if stuck:
            msg = []
            for e, (i, n) in stuck.items():
                waits = self.prog[e][i][0]
                msg.append("%s@%d/%d waits %s" % (e, i, n, [(sk, v, val[sk]) for sk, v in waits if val[sk] < v]))
            raise RuntimeError("DEADLOCK in schedule: " + "; ".join(msg))
        for k in self.sems:
            assert val[k] == self.cnt[k], (k, val[k], self.cnt[k])

    def emit(self):
        self.simulate()
        nc = self.nc
        with nc.Block() as block:
            def run(e):
                eng = self._engobj(e)
                for waits, fn, inc in self.prog[e]:
                    for sk, v in waits:
                        eng.wait_ge(self.sems[sk], v)
                    if fn is None:
                        continue
                    ins = fn()
                    if inc is not None:
                        ins.then_inc(self.sems[inc[0]], inc[1])

            @block.tensor
            def _(e):
                run("pe")

            @block.scalar
            def _(e):
                run("act")

            @block.vector
            def _(e):
                run("dve")

            @block.gpsimd
            def _(e):
                run("pool")

            @block.sync
            def _(e):
                run("sp")


class Arena:
    def __init__(self, t, nbytes):
        self.t = t
        self.n = nbytes
        self.off = 0
        self.hi = 0

    def mark(self):
        return self.off

    def release(self, m):
        self.off = m

    def _take(self, nbytes):
        nbytes = (nbytes + 31) // 32 * 32
        o = self.off
        self.off += nbytes
        self.hi = max(self.hi, self.off)
        assert self.off <= self.n, ("arena overflow", self.off, self.n)
        return o

    def f32(self, n):
        o = self._take(4 * n)
        return self.t[:, o // 4: o // 4 + n]

    def bf16(self, n):
        o = self._take(2 * n)
        return self.t[:, o // 4: o // 4 + (n + 1) // 2].bitcast(BF16)

    def i32(self, n):
        o = self._take(4 * n)
        return self.t[:, o // 4: o // 4 + n].bitcast(I32)


class Ring:
    def __init__(self, items):
        self.items = items
        self.i = 0

    def next(self):
        it = self.items[self.i % len(self.items)]
        self.i += 1
        return it


class WStream:
    def __init__(self, slots, plan, auto=True):
        self.slots = slots
        self.plan = plan
        self.auto = auto
        self.issued = 0
        self.used = 0
        self.freed = 0

    def top(self):
        while self.issued < len(self.plan) and self.issued - self.freed < len(self.slots):
            i = self.issued
            v, k = self.slots[i % len(self.slots)]
            self.plan[i](v, k)
            self.issued += 1

    def done(self):
        self.freed = self.used
        self.top()

    def get(self):
        i = self.used
        if self.auto:
            self.freed = i
        self.top()
        assert self.issued > i, "weight stream: block not issued (slots exhausted)"
        self.used += 1
        return self.slots[i % len(self.slots)]


def _host_consts():
    ident = np.eye(128, dtype=np.float32)
    ones = np.ones((128, 128), np.float32)
    blk2 = np.zeros((128, 128), np.float32)
    blk2[0:64, 0:64] = 1
    blk2[64:128, 64:128] = 1

    def rotT(block):
        h = block // 2
        m = np.zeros((128, 128), np.float32)
        for b0 in range(0, 128, block):
            for i in range(h):
                m[b0 + i + h, b0 + i] = -1.0
                m[b0 + i, b0 + i + h] = 1.0
        return m

    tri = np.where(np.arange(128)[None, :] >= np.arange(128)[:, None], 0.0, -MASK).astype(np.float32)
    cmat = np.concatenate([ident, ones, blk2, rotT(64), rotT(32), tri], axis=1)
    tril = (np.arange(128)[:, None] <= np.arange(128)[None, :]).astype(np.float32)
    pm = np.zeros((16, 8), np.float32)
    for qs in range(16):
        cur = qs // 2
        for n in range(8):
            pm[qs, n] = 0.0 if n < cur else (BIGG if n == cur else -2 * BIGG)
    pmask = np.broadcast_to(pm.reshape(1, 128), (128, 128))
    p = np.arange(128)
    invf64 = (10000.0 ** (-(2.0 * (p % 32)) / 64.0)) / (2 * math.pi)
    invf32 = (10000.0 ** (-(2.0 * (p % 16)) / 32.0)) / (2 * math.pi)
    cf = np.concatenate([tril, pmask, invf64[:, None], invf32[:, None]], axis=1).astype(np.float32)
    ind = (np.arange(SEQ)[None, :] // 256 == np.arange(8)[:, None]).astype(np.float32)
    return np.ascontiguousarray(cmat), np.ascontiguousarray(cf), np.ascontiguousarray(ind)


def _fm(v, c):
    return np.ascontiguousarray(np.asarray(v, np.float32).reshape(c, 128).T)


def _host_params(inp):
    vd = np.stack([np.stack([_fm(inp[k][l], 8) for k in ("pre_mix_norm", "post_mix_norm", "pre_ffn_norm", "post_ffn_norm", "pe_gate_norm")], 1) for l in range(L)], 1)
    vg = np.stack([np.stack([_fm(inp[k][l], 2) for k in ("conv_dw_b", "conv_ln_g", "conv_ln_b", "conv_pw_b", "out_norm_a", "out_norm_b", "out_norm_c")], 1) for l in range(L)], 1)
    wdw = np.stack([np.asarray(inp["conv_dw_w"][l], np.float32).reshape(31, 2, 128).transpose(2, 1, 0) for l in range(L)], 1)
    fcw = np.stack([np.asarray(inp["ffn_conv_w"][l], np.float32).reshape(3, 44, 128).transpose(2, 1, 0) for l in range(L)], 1)
    fcb = np.stack([_fm(inp["ffn_conv_b"][l], 44) for l in range(L)], 1)
    gsub = np.stack([np.asarray(inp["diff_subln_g"][l], np.float32)[np.arange(128) % 64] for l in range(L)], 1)
    lamv = np.stack([np.stack([np.asarray(inp[k][l], np.float32) for k in ("diff_lq1", "diff_lk1", "diff_lq2", "diff_lk2")], 0) for l in range(L)], 0)
    lamv = np.broadcast_to(lamv.reshape(1, L * 4 * 32), (128, L * 4 * 32))
    pv = np.concatenate([vd.reshape(128, -1), vg.reshape(128, -1), wdw.reshape(128, -1), fcw.reshape(128, -1), fcb.reshape(128, -1), gsub.reshape(128, -1), lamv], axis=1)
    glnb = np.stack([np.stack([np.broadcast_to(np.asarray(inp[k][l], np.float32)[None, :], (128, 256)) for k in ("gmlp_ln_g", "gmlp_ln_b")], 1) for l in range(L)], 0)
    bs = np.asarray(inp["gmlp_bs"], np.float32)
    gb = np.zeros((L, 2, 128, 512), np.float32)
    for l in range(L):
        for cc in range(2):
            for hh in range(2):
                gb[l, cc, hh * 64:(hh + 1) * 64, :] = np.tile(bs[l, 2 * cc + hh], 4)[None, :]
    wsT = np.ascontiguousarray(np.asarray(inp["gmlp_ws"], np.float32).transpose(0, 1, 3, 2))
    return np.ascontiguousarray(pv.astype(np.float32)), np.ascontiguousarray(glnb), gb, wsT


PV_OFF = {}


def _pv_layout():
    o = 0
    for name, n in (("vd", L * 5 * 8), ("vg", L * 7 * 2), ("wdw", L * 2 * 31), ("fcw", L * 44 * 3), ("fcb", L * 44), ("gsub", L), ("lamv", L * 4 * 32)):
        PV_OFF[name] = o
        o += n
    return o


PV_N = _pv_layout()


def build(dbg=None, nlayers=L):
    dbg = dbg or []
    nc = bass.Bass("TRN2", target_bir_lowering=False)

    def din(name, shape, dt=F32):
        return nc.dram_tensor(name, list(shape), dt, kind="ExternalInput").ap()

    xT_d = din("xT", [D, SEQ])
    pT_d = din("pT", [L, GW, SEQ])
    pos_d = din("pos", [1, SEQ], I32)
    cmat_d = din("cmat", [128, 768])
    cf_d = din("cf", [128, 258])
    ind_d = din("ind", [8, SEQ])
    pv_d = din("pv", [128, PV_N])
    glnb_d = din("glnb", [L, 128, 2, 256])
    gb_d = din("gb", [L, 2, 128, 512])
    wsT_d = din("wsT", [L, 4, 128, 128])
    w_in_d = din("w_in", [L, D, 10 * GW])
    w_out_d = din("w_out", [L, D, D])
    w_up_d = din("w_up", [L, D, 2 * DFF])
    w_down_d = din("w_down", [L, DFF, D])
    w_gate_d = din("w_pe_gate", [L, D, D])
    w_proj_d = din("w_pe_proj", [L, GW, D])
    w_pw_d = din("conv_pw_w", [L, GW, GW])
    out_d = nc.dram_tensor("outT", [D, SEQ], F32, kind="ExternalOutput").ap()
    dbg_d = {n: nc.dram_tensor("dbg_" + n, [128, w], F32, kind="ExternalOutput").ap() for n, w in dbg}

    es = ExitStack()
    with es:
        S = Sched(nc, es)
        NB = 204 * 1024
        big = es.enter_context(nc.sbuf_tensor("arena", [128, NB // 4], F32))
        A = Arena(big, NB)
        psb = [es.enter_context(nc.psum_tensor("ps%d" % i, [128, 512], F32)) for i in range(8)]
        PS = [(psb[i][:, :], ("ps", i)) for i in range(8)]
        ringA = Ring(PS[0:4])
        ringB = Ring(PS[4:8])

        def MM(out, lhsT, rhs, start, stop, r, w, inc=True, tp=None):
            kw = {}
            if tp is not None:
                kw["tile_position"] = tp
            S.op("pe", lambda: nc.tensor.matmul(out, lhsT=lhsT, rhs=rhs, start=start, stop=stop, **kw), r=r, w=w, inc=inc)

        def ACT(out, in_, func, r, w, scale=None, bias=None):
            kw = {}
            if scale is not None:
                kw["scale"] = scale
            if bias is not None:
                kw["bias"] = bias
            S.op("act", lambda: nc.scalar.activation(out=out, in_=in_, func=func, **kw), r=r, w=w)

        def TTo(eng, out, in0, in1, op, r, w):
            e = nc.vector if eng == "dve" else nc.gpsimd
            S.op(eng, lambda: e.tensor_tensor(out=out, in0=in0, in1=in1, op=op), r=r, w=w)

        def TS(eng, out, in0, s1, s2, op0, op1, r, w):
            e = nc.vector if eng == "dve" else nc.gpsimd
            if op1 is None:
                S.op(eng, lambda: e.tensor_scalar(out=out, in0=in0, scalar1=s1, scalar2=None, op0=op0), r=r, w=w)
            else:
                S.op(eng, lambda: e.tensor_scalar(out=out, in0=in0, scalar1=s1, scalar2=s2, op0=op0, op1=op1), r=r, w=w)

        def STT(out, in0, scalar, in1, op0, op1, r, w):
            S.op("dve", lambda: nc.vector.scalar_tensor_tensor(out=out, in0=in0, scalar=scalar, in1=in1, op0=op0, op1=op1), r=r, w=w)

        def CP(eng, out, in_, r, w):
            e = nc.vector if eng == "dve" else nc.gpsimd
            S.op(eng, lambda: e.tensor_copy(out=out, in_=in_), r=r, w=w)

        def MSET(eng, ap, val, w):
            e = nc.vector if eng == "dve" else nc.gpsimd
            S.op(eng, lambda: e.memset(ap, val), w=w)

        def DMA(q, out, in_, semkey, r, w):
            e = dict(sp=nc.sync, pool=nc.gpsimd, act=nc.scalar)[q]
            S.dma(q, lambda: e.dma_start(out=out, in_=in_), semkey, r=r, w=w)

        def dump(name, ap, keys):
            if name in dbg_d:
                w_ = ap.shape[-1]
                stg = A.f32(w_)
                CP("dve", stg[0:ap.shape[0], :], ap, r=keys, w=[("dbgs", name)])
                DMA("sp", dbg_d[name][0:ap.shape[0], 0:w_], stg[0:ap.shape[0], :], "dbg", r=[("dbgs", name)], w=[("dbgo", name)])

        xT = A.f32(8 * SEQ).rearrange("p (c s) -> p c s", c=8)
        hT = A.bf16(8 * SEQ).rearrange("p (c s) -> p c s", c=8)
        cmat = A.bf16(768)
        IDENT, ONES, BLK2, R64, R32, TRI = [cmat[:, i * 128:(i + 1) * 128] for i in range(6)]
        cf = A.f32(258)
        TRIL = cf[:, 0:128]
        PMASK = cf[:, 128:256].rearrange("p (q n) -> p q n", n=8)
        INVF = [cf[:, 256:257], cf[:, 257:258]]
        pv = A.f32(PV_N)

        def PVv(name, n):
            return pv[:, PV_OFF[name]:PV_OFF[name] + n]

        vd = PVv("vd", L * 5 * 8).rearrange("p (l v c) -> p l v c", l=L, v=5)
        vg = PVv("vg", L * 7 * 2).rearrange("p (l v c) -> p l v c", l=L, v=7)
        wdw = PVv("wdw", L * 2 * 31).rearrange("p (l c k) -> p l c k", l=L, c=2)
        fcw = PVv("fcw", L * 44 * 3).rearrange("p (l j k) -> p l j k", l=L, j=44)
        fcb = PVv("fcb", L * 44).rearrange("p (l j) -> p l j", l=L)
        gsub = PVv("gsub", L)
        lamv = PVv("lamv", L * 4 * 32).rearrange("p (l v k) -> p l v k", l=L, v=4)
        small = A.f32(64)
        lnv = A.f32(512)
        rstd = A.f32(512)
        sq = [A.bf16(512) for _ in range(3)]
        sqring = Ring([(sq[i], ("sq", i)) for i in range(3)])
        PH = A.mark()

        DMA("pool", cmat, cmat_d[:, :], "ld_c", r=[], w=["cmat"])
        DMA("sp", cf, cf_d[:, :], "ld_c2", r=[], w=["cf"])
        DMA("sp", pv, pv_d[:, :], "ld_c3", r=[], w=["pv"])
        for c in range(8):
            DMA("sp", xT[:, c, :], xT_d[c * 128:(c + 1) * 128, :], ("ld_x", c), r=[], w=[("xT", c, t) for t in range(NT)])

        def wload(dst, src, key):
            DMA("pool", dst, src, ("wsem",) + key, r=[], w=[key])

        def rstd_from(ps_ap, ps_key, inv_n, eps, extra_r=()):
            ACT(lnv, ps_ap, AF.Ln, r=[ps_key] + list(extra_r), w=["lnv"], scale=inv_n, bias=eps)
            ACT(rstd, lnv, AF.Exp, r=["lnv"], w=["rstd"], scale=-0.5)

        def rmsnorm_tile(srcs, skeys, dsts, dkeys, gains, n_feat, eps=EPS, lhs=None, lkey="cmat"):
            ps, pk = ringB.next()
            C = len(srcs)
            for c in range(C):
                q, qk = sqring.next()
                ACT(q, srcs[c], AF.Square, r=[skeys[c]], w=[qk])
                MM(ps, lhs if lhs is not None else ONES, q, c == 0, c == C - 1, r=[qk, lkey], w=[pk])
            rstd_from(ps, pk, 1.0 / n_feat, eps)
            for c in range(C):
                STT(dsts[c], srcs[c], gains[c], rstd, ALU.mult, ALU.mult, r=[skeys[c], "rstd", "pv"], w=[dkeys[c]])

        def sl(t):
            return slice(t * TW, (t + 1) * TW)

        def norm_x_to_h(l, vidx):
            for t in range(NT):
                rmsnorm_tile([xT[:, c, sl(t)] for c in range(8)], [("xT", c, t) for c in range(8)],
                             [hT[:, c, sl(t)] for c in range(8)], [("hT", c, t) for c in range(8)],
                             [vd[:, l, vidx, c:c + 1] for c in range(8)], D)

        def proj(ps, pk, wv, wkey, cs, rhs_fn, rkeys_fn, nk=8):
            for kc in range(nk):
                MM(ps, wv[:, kc, cs], rhs_fn(kc), kc == 0, kc == nk - 1, r=[wkey] + rkeys_fn(kc), w=[pk], inc=(kc == nk - 1))

        def hrhs(t):
            return (lambda kc: hT[:, kc, sl(t)]), (lambda kc: [("hT", kc, t)])

        for l in range(nlayers):
            S.barrier()
            A.release(PH)
            catT = A.bf16(8 * SEQ).rearrange("p (c s) -> p c s", c=8)
            ropeC = A.bf16(SEQ)
            ropeS = A.bf16(SEQ)
            wslots = [A.bf16(8 * 256).rearrange("p (k n) -> p k n", k=8) for _ in range(4)]
            MX = A.mark()
            winv = w_in_d[l].rearrange("(k p) n -> p k n", p=128)
            woutv = w_out_d[l].rearrange("(k p) n -> p k n", p=128)
            mplan = [(lambda v, k, bi=bi: wload(v, winv[:, :, bi * 256:(bi + 1) * 256], k)) for bi in (0, 1, 2, 7, 8, 9, 3, 4, 5, 6)]
            mplan += [(lambda v, k, db=db: wload(v, woutv[:, :, db * 256:(db + 1) * 256], k)) for _t in range(NT) for db in range(4)]
            mstream = WStream([(wslots[i], ("ws", i)) for i in range(4)], mplan, auto=False)

            def win_block(bi):
                return mstream.get()

            def rope_tables(which):
                m = A.mark()
                posi = A.i32(SEQ)
                v = A.f32(SEQ)
                ki = A.i32(SEQ)
                DMA("sp", posi, pos_d[0:1, :].broadcast_to([128, SEQ]), "ld_pos", r=[], w=["posi"])
                for tab, shift, key in ((ropeS, 0.0, "ropeS"), (ropeC, 0.25, "ropeC")):
                    TS("dve", v, posi, INVF[which], shift, ALU.mult, ALU.add, r=["posi", "cf"], w=["ropev"])
                    CP("dve", ki, v, r=["ropev"], w=["ropek"])
                    TTo("dve", v, v, ki, ALU.subtract, r=["ropev", "ropek"], w=["ropev"])
                    ACT(tab, v, AF.Sin, r=["ropev"], w=[key], scale=2 * math.pi * (1 - 1e-6))
                S.barrier()
                A.release(m)

            def rope_apply(ps, pk, t, RM, zbr, t1, t2):
                zb, zk = zbr.next()
                ACT(zb, ps, AF.Copy, r=[pk], w=[zk])
                ps2, pk2 = ringA.next()
                MM(ps2, RM, zb, True, True, r=[zk, "cmat"], w=[pk2])
                TTo("pool", t1[0], zb, ropeC[:, sl(t)], ALU.mult, r=[zk, "ropeC"], w=[t1[1]])
                TTo("dve", t2[0], ps2, ropeS[:, sl(t)], ALU.mult, r=[pk2, "ropeS"], w=[t2[1]])

            def attn_finalize_recip(O, ok, dst_rden):
                ACT(dst_rden[0][0:64, :], O[64:128, :], AF.Ln, r=[ok], w=[dst_rden[1]])
                ACT(dst_rden[0][0:64, :], dst_rden[0][0:64, :], AF.Exp, r=[dst_rden[1]], w=[dst_rden[1]], scale=-1.0)

            def vproj(wv, wk, cs, vaug):
                for g in range(4):
                    ps, pk = ringA.next()
                    for s4 in range(4):
                        st = g * 4 + s4
                        for kc in range(8):
                            MM(ps[:, s4 * 128:(s4 + 1) * 128], hT[:, kc, st * 128:(st + 1) * 128], wv[:, kc, cs], kc == 0, kc == 7,
                               r=[wk, ("hT", kc, st // 4)], w=[pk], inc=(kc == 7 and s4 == 3))
                    ACT(vaug[:, g * 4:(g + 1) * 4, :, 0:64], ps.rearrange("p (s h d) -> p s h d", s=4, h=2), AF.Copy, r=[pk], w=[("V", g)])

            norm_x_to_h(l, 0)
            if l == 0:
                dump("hT0", hT[:, 0, 0:512], [("hT", 0, 0)])

            rope_tables(0)
            m0 = A.mark()
            Kaug = [A.bf16(SEQ) for _ in range(2)]
            vaug = A.bf16(16 * 2 * 128).rearrange("p (s h d) -> p s h d", s=16, h=2)
            Qa = [[A.bf16(512) for _ in range(2)] for _ in range(2)]
            Pr = Ring([(A.bf16(512), ("P", i)) for i in range(4)])
            zbr = Ring([(A.bf16(512), ("zb", i)) for i in range(2)])
            t1 = (A.f32(512), "t1")
            t2 = (A.f32(512), "t2")
            rden = (A.f32(512), "rden")
            bstg = A.bf16(128)
            gm = A.f32(64)
            top8 = A.f32(8)
            kmf = A.f32(16)
            kmb = A.bf16(16)
            MSET("pool", vaug[:, :, :, 64:128], 1.0, w=[("V", g) for g in range(4)])
            MSET("pool", bstg, 0.0, w=["bstg"])
            for hh in range(2):
                DMA("pool", Kaug[hh][64:72, :], ind_d[:, :], "ld_ind", r=[], w=[("Kind", hh)])
            wq, wqk = win_block(0)
            wk_, wkk = win_block(1)
            wv_, wvk = win_block(2)
            for c in range(2):
                cs = slice(c * 128, (c + 1) * 128)
                for t in range(NT):
                    ps, pk = ringA.next()
                    rf, kf = hrhs(t)
                    proj(ps, pk, wk_, wkk, cs, rf, kf)
                    rope_apply(ps, pk, t, R64, zbr, t1, t2)
                    for hh in range(2):
                        TTo("pool", Kaug[hh][0:64, sl(t)], t1[0][hh * 64:(hh + 1) * 64, :], t2[0][hh * 64:(hh + 1) * 64, :], ALU.add,
                            r=[t1[1], t2[1]], w=[("K", hh, t)])
                for hh in range(2):
                    S.op("dve", lambda hh=hh: nc.vector.tensor_reduce(out=kmf[0:64, hh * 8:(hh + 1) * 8], in_=Kaug[hh][0:64, :].rearrange("p (n k) -> p n k", k=256), axis=AX.X, op=ALU.add),
                         r=[("K", hh, t) for t in range(NT)], w=[("kmf", hh)])
                    TS("dve", kmb[0:64, hh * 8:(hh + 1) * 8], kmf[0:64, hh * 8:(hh + 1) * 8], 1.0 / 256, None, ALU.mult, None, r=[("kmf", hh)], w=[("kmb", hh)])
                vproj(wv_, wvk, cs, vaug)
                for qt in range(NT):
                    qb = Qa[qt % 2]
                    ps, pk = ringA.next()
                    rf, kf = hrhs(qt)
                    proj(ps, pk, wq, wqk, cs, rf, kf)
                    rope_apply(ps, pk, qt, R64, zbr, t1, t2)
                    for hh in range(2):
                        TTo("pool", qb[hh][0:64, :], t1[0][hh * 64:(hh + 1) * 64, :], t2[0][hh * 64:(hh + 1) * 64, :], ALU.add,
                            r=[t1[1], t2[1]], w=[("Qa", qt % 2, hh)])
                    for hh in range(2):
                        gps, gk = ringA.next()
                        for j in range(4):
                            MM(gps[:, j * 8:(j + 1) * 8], qb[hh][0:64, j * 128:(j + 1) * 128], kmb[0:64, hh * 8:(hh + 1) * 8], True, True,
                               r=[("Qa", qt % 2, hh), ("kmb", hh)], w=[gk], inc=(j == 3))
                        gmv = gm[:, 0:32].rearrange("p (j n) -> p j n", n=8)
                        TTo("dve", gmv, gps[:, 0:32].rearrange("p (j n) -> p j n", n=8), PMASK[:, qt * 4:(qt + 1) * 4, :], ALU.add, r=[gk, "cf"], w=["gm"])
                        tps, tk = ringA.next()
                        tpsb = tps.bitcast(BF16)
                        for j in range(4):
                            S.op("dve", lambda j=j: nc.vector.max(out=top8, in_=gm[:, j * 8:(j + 1) * 8]), r=["gm"], w=["top8"])
                            TS("dve", top8[:, 3:4], top8[:, 3:4], -BIGG, None, ALU.max, None, r=["top8"], w=["top8"])
                            TS("dve", bstg[:, 64:72], gm[:, j * 8:(j + 1) * 8], top8[:, 3:4], -MASK, ALU.is_lt, ALU.mult, r=["gm", "top8"], w=["bstg"])
                            S.op("pe", lambda j=j, tpsb=tpsb: nc.tensor.transpose(tpsb[0:72, j * 128:(j + 1) * 128], bstg[:, 0:72], IDENT), r=["bstg", "cmat"], w=[tk])
                        ACT(qb[hh][64:72, :], tpsb[64:72, 0:512], AF.Copy, r=[tk], w=[("Qb", qt % 2, hh)])
                    for hh in range(2):
                        O, ok = ringB.next()
                        nk = 4 * qt + 4
                        pend = None
                        for kt in range(nk):
                            jd = kt - 4 * qt
                            c0 = 128 * jd if jd > 0 else 0
                            sp_, sk = ringA.next()
                            MM(sp_[:, c0:512], Kaug[hh][0:72, kt * 128:(kt + 1) * 128], qb[hh][0:72, c0:512], True, jd < 0,
                               r=[("K", hh, kt // 4), ("Kind", hh), ("Qa", qt % 2, hh), ("Qb", qt % 2, hh)], w=[sk], inc=(jd < 0))
                            if jd >= 0:
                                MM(sp_[:, c0:c0 + 128], IDENT, TRI, False, True, r=["cmat"], w=[sk])
                            if pend is not None:
                                pend()
                            P, Pk = Pr.next()
                            ACT(P[:, c0:512], sp_[:, c0:512], AF.Exp, r=[sk], w=[Pk], scale=0.125)

                            def pv_(kt=kt, c0=c0, P=P, Pk=Pk, O=O, ok=ok, hh=hh, nk=nk):
                                MM(O[:, c0:512], vaug[:, kt, hh, :], P[:, c0:512], kt == 0, kt == nk - 1, r=[Pk, ("V", kt // 4)], w=[ok])
                            pend = pv_
                        pend()
                        attn_finalize_recip(O, ok, rden)
                        TTo("dve", catT[hh * 64:(hh + 1) * 64, c, sl(qt)], O[0:64, :], rden[0][0:64, :], ALU.mult, r=[ok, rden[1]], w=[("cat", c, qt)])
            if l == 0:
                dump("oa_pre", catT[:, 0, 0:512], [("cat", 0, 0)])
            for t in range(NT):
                rmsnorm_tile([catT[:, c, sl(t)] for c in range(2)], [("cat", c, t) for c in range(2)],
                             [catT[:, c, sl(t)] for c in range(2)], [("cat", c, t) for c in range(2)],
                             [vg[:, l, 4, c:c + 1] for c in range(2)], GW)
            if l == 0:
                dump("oa", catT[:, 0, 0:512], [("cat", 0, 0)])
            mstream.done()
            S.barrier()
            A.release(m0)

            rope_tables(1)
            lam_init = 0.8 - 0.6 * math.exp(-0.3 * l)
            lt = small[:, 0:8]
            prod = A.f32(64)
            TTo("dve", prod[:, 0:32], lamv[:, l, 0, :], lamv[:, l, 1, :], ALU.mult, r=["pv"], w=["prod"])
            S.op("dve", lambda: nc.vector.tensor_reduce(out=lt[:, 0:1], in_=prod[:, 0:32], axis=AX.X, op=ALU.add), r=["prod"], w=["lt"])
            TTo("dve", prod[:, 32:64], lamv[:, l, 2, :], lamv[:, l, 3, :], ALU.mult, r=["pv"], w=["prod2"])
            S.op("dve", lambda: nc.vector.tensor_reduce(out=lt[:, 1:2], in_=prod[:, 32:64], axis=AX.X, op=ALU.add), r=["prod2"], w=["lt"])
            ACT(lt[:, 2:4], lt[:, 0:2], AF.Exp, r=["lt"], w=["lt"])
            TTo("dve", lt[:, 4:5], lt[:, 3:4], lt[:, 2:3], ALU.subtract, r=["lt"], w=["lt"])
            TS("dve", lt[:, 5:6], lt[:, 4:5], -lam_init, None, ALU.add, None, r=["lt"], w=["lt"])
            TS("dve", lt[:, 6:7], gsub[:, l:l + 1], 1.0 - lam_init, None, ALU.mult, None, r=["pv", "lt"], w=["lt"])
            NEGLAM = lt[:, 5:6]
            GS = lt[:, 6:7]

            m0 = A.mark()
            Kd = A.bf16(SEQ)
            vaug = A.bf16(16 * 2 * 128).rearrange("p (s h d) -> p s h d", s=16, h=2)
            Qd = [A.bf16(512) for _ in range(2)]
            Pr = Ring([(A.bf16(512), ("P", i)) for i in range(4)])
            zbr = Ring([(A.bf16(512), ("zb", i)) for i in range(2)])
            t1 = (A.f32(512), "t1")
            t2 = (A.f32(512), "t2")
            rden = (A.f32(512), "rden")
            od = A.f32(512)
            MSET("pool", vaug[:, :, :, 64:128], 1.0, w=[("V", g) for g in range(4)])
            wq, wqk = win_block(7)
            wk_, wkk = win_block(8)
            wv_, wvk = win_block(9)
            dscale = 32.0 ** -0.5
            for c in range(2):
                cs = slice(c * 128, (c + 1) * 128)
                for t in range(NT):
                    ps, pk = ringA.next()
                    rf, kf = hrhs(t)
                    proj(ps, pk, wk_, wkk, cs, rf, kf)
                    rope_apply(ps, pk, t, R32, zbr, t1, t2)
                    TTo("pool", Kd[:, sl(t)], t1[0], t2[0], ALU.add, r=[t1[1], t2[1]], w=[("Kd", t)])
                vproj(wv_, wvk, cs, vaug)
                for qt in range(NT):
                    qd = Qd[qt % 2]
                    ps, pk = ringA.next()
                    rf, kf = hrhs(qt)
                    proj(ps, pk, wq, wqk, cs, rf, kf)
                    rope_apply(ps, pk, qt, R32, zbr, t1, t2)
                    TTo("pool", qd, t1[0], t2[0], ALU.add, r=[t1[1], t2[1]], w=[("Qd", qt % 2)])
                    for hh in range(2):
                        Os = [ringB.next(), ringB.next()]
                        nk = 4 * qt + 4
                        pend = []
                        for kt in range(nk):
                            jd = kt - 4 * qt
                            c0 = 128 * jd if jd > 0 else 0
                            cur = []
                            for m_ in range(2):
                                g = hh * 2 + m_
                                sp_, sk = ringA.next()
                                MM(sp_[:, c0:512], Kd[32 * g:32 * g + 32, kt * 128:(kt + 1) * 128], qd[32 * g:32 * g + 32, c0:512], True, jd < 0,
                                   r=[("Kd", kt // 4), ("Qd", qt % 2)], w=[sk], inc=(jd < 0), tp=(32 * g, 0))
                                if jd >= 0:
                                    MM(sp_[:, c0:c0 + 128], IDENT, TRI, False, True, r=["cmat"], w=[sk])
                                cur.append((sp_, sk))
                            for f in pend:
                                f()
                            pend = []
                            for m_ in range(2):
                                sp_, sk = cur[m_]
                                P, Pk = Pr.next()
                                ACT(P[:, c0:512], sp_[:, c0:512], AF.Exp, r=[sk], w=[Pk], scale=dscale)

                                def pv_(kt=kt, c0=c0, P=P, Pk=Pk, Oo=Os[m_], hh=hh, nk=nk):
                                    MM(Oo[0][:, c0:512], vaug[:, kt, hh, :], P[:, c0:512], kt == 0, kt == nk - 1, r=[Pk, ("V", kt // 4)], w=[Oo[1]])
                                pend.append(pv_)
                        for f in pend:
                            f()
                        hs = slice(hh * 64, (hh + 1) * 64)
                        attn_finalize_recip(Os[0][0], Os[0][1], rden)
                        TTo("dve", t1[0][0:64, :], Os[0][0][0:64, :], rden[0][0:64, :], ALU.mult, r=[Os[0][1], rden[1]], w=[t1[1]])
                        attn_finalize_recip(Os[1][0], Os[1][1], rden)
                        TTo("dve", t2[0][0:64, :], Os[1][0][0:64, :], rden[0][0:64, :], ALU.mult, r=[Os[1][1], rden[1]], w=[t2[1]])
                        STT(od[hs, :], t2[0][0:64, :], NEGLAM[0:64, :], t1[0][0:64, :], ALU.mult, ALU.add, r=[t1[1], t2[1], "lt"], w=[("od", hh)])
                    ps, pk = ringB.next()
                    q_, qk_ = sqring.next()
                    ACT(q_, od, AF.Square, r=[("od", 0), ("od", 1)], w=[qk_])
                    MM(ps, BLK2, q_, True, True, r=[qk_, "cmat"], w=[pk])
                    rstd_from(ps, pk, 1.0 / 64, 1e-5)
                    STT(catT[:, 6 + c, sl(qt)], od, GS, rstd, ALU.mult, ALU.mult, r=[("od", 0), ("od", 1), "rstd", "lt"], w=[("cat", 6 + c, qt)])
            if l == 0:
                dump("od", catT[:, 6, 0:512], [("cat", 6, 0)])
            mstream.done()
            S.barrier()
            A.release(m0)

            m0 = A.mark()
            uT = A.bf16(2 * SEQ).rearrange("p (c s) -> p c s", c=2)
            vgl = A.bf16(16 * 256).rearrange("p (n d) -> p n d", n=16)
            glnb = A.f32(512).rearrange("p (v d) -> p v d", v=2)
            gb = A.f32(1024).rearrange("p (c s) -> p c s", c=2)
            wsf = A.f32(512).rearrange("p (h i) -> p h i", h=4)
            wsb = A.bf16(512).rearrange("p (h i) -> p h i", h=4)
            stats = A.f32(16 * 6)
            mv = A.f32(16 * 2).rearrange("p (n k) -> p n k", k=2)
            rsd = A.f32(16)
            vtmp = A.f32(256)
            vln = [A.bf16(256) for _ in range(2)]
            stmp = A.f32(512)
            DMA("sp", glnb, glnb_d[l], "ld_g1", r=[], w=["glnb"])
            DMA("sp", gb, gb_d[l].rearrange("c p s -> p c s"), "ld_g2", r=[], w=["gb"])
            DMA("sp", wsf, wsT_d[l].rearrange("h j i -> j h i"), "ld_g3", r=[], w=["wsf"])
            for h in range(4):
                TTo("pool", wsb[:, h, :], wsf[:, h, :], TRIL, ALU.mult, r=["wsf", "cf"], w=["wsb"])
            wu, wuk = win_block(3)
            wvv, wvvk = win_block(4)
            for t in range(NT):
                for cc in range(2):
                    ps, pk = ringA.next()
                    rf, kf = hrhs(t)
                    proj(ps, pk, wu, wuk, slice(cc * 128, (cc + 1) * 128), rf, kf)
                    ACT(uT[:, cc, sl(t)], ps, AF.Gelu_apprx_tanh, r=[pk], w=[("uT", cc, t)])
            for n2 in range(8):
                ps, pk = ringA.next()
                for s2 in range(2):
                    n = n2 * 2 + s2
                    for kc in range(8):
                        MM(ps[:, s2 * 256:(s2 + 1) * 256], hT[:, kc, n * 128:(n + 1) * 128], wvv[:, kc, :], kc == 0, kc == 7,
                           r=[wvvk, ("hT", kc, n // 4)], w=[pk], inc=(kc == 7 and s2 == 1))
                ACT(vgl[:, n2 * 2:(n2 + 1) * 2, :], ps.rearrange("p (s d) -> p s d", s=2), AF.Gelu_apprx_tanh, r=[pk], w=[("vgl", n2)])
            for n in range(16):
                S.op("dve", lambda n=n: nc.vector.bn_stats(out=stats[:, n * 6:(n + 1) * 6], in_=vgl[:, n, :]), r=[("vgl", n // 2)], w=[("st", n)])
                S.op("dve", lambda n=n: nc.vector.bn_aggr(out=mv[:, n, :], in_=stats[:, n * 6:(n + 1) * 6]), r=[("st", n)], w=["mv"])
            ACT(rsd, mv[:, :, 1], AF.Ln, r=["mv"], w=["rsd"], bias=EPS)
            ACT(rsd, rsd, AF.Exp, r=["rsd"], w=["rsd"], scale=-0.5)
            for t in range(NT):
                pss = [ringA.next(), ringA.next()]
                for s4 in range(4):
                    n = t * 4 + s4
                    vl = vln[n % 2]
                    vk = ("vln", n % 2)
                    TS("dve", vtmp, vgl[:, n, :], mv[:, n, 0:1], rsd[:, n:n + 1], ALU.subtract, ALU.mult, r=[("vgl", n // 2), "mv", "rsd"], w=["vtmp"])
                    TTo("pool", vtmp, vtmp, glnb[:, 0, :], ALU.mult, r=["vtmp", "glnb"], w=["vtmp"])
                    TTo("pool", vl, vtmp, glnb[:, 1, :], ALU.add, r=["vtmp", "glnb"], w=[vk])
                    for cc in range(2):
                        for hh in range(2):
                            h = 2 * cc + hh
                            MM(pss[cc][0][hh * 64:(hh + 1) * 64, s4 * 128:(s4 + 1) * 128], vl[:, h * 64:(h + 1) * 64], wsb[:, h, :], True, True,
                               r=[vk, "wsb"], w=[pss[cc][1]], tp=(0, hh * 64))
                for cc in range(2):
                    TTo("dve", stmp, pss[cc][0], gb[:, cc, :], ALU.add, r=[pss[cc][1], "gb"], w=["stmp"])
                    TTo("pool", catT[:, 2 + cc, sl(t)], stmp, uT[:, cc, sl(t)], ALU.mult, r=["stmp", ("uT", cc, t)], w=[("cat", 2 + cc, t)])
            if l == 0:
                dump("ob_pre", catT[:, 2, 0:512], [("cat", 2, 0)])
            for t in range(NT):
                rmsnorm_tile([catT[:, 2 + c, sl(t)] for c in range(2)], [("cat", 2 + c, t) for c in range(2)],
                             [catT[:, 2 + c, sl(t)] for c in range(2)], [("cat", 2 + c, t) for c in range(2)],
                             [vg[:, l, 5, c:c + 1] for c in range(2)], GW)
            mstream.done()
            S.barrier()
            A.release(m0)

            m0 = A.mark()
            ybuf = A.bf16(32 + SEQ)
            diag = A.bf16(31 * 128).rearrange("p (k n) -> p k n", k=31)
            cy = A.bf16(2 * SEQ).rearrange("p (c s) -> p c s", c=2)
            sg = A.f32(512)
            wpw = A.bf16(2 * 256).rearrange("p (k n) -> p k n", k=2)
            mstat = A.f32(512)
            m2 = A.f32(512)
            yn = A.f32(512)
            sil = A.bf16(1024).rearrange("p (c s) -> p c s", c=2)
            Y0 = 2
            wa, wak = win_block(5)
            wg_, wgk = win_block(6)
            wload(wpw, w_pw_d[l].rearrange("(k p) n -> p k n", p=128), ("wpw",))
            MSET("pool", ybuf[:, 0:32], 0.0, w=["ypad"])
            for cc in range(2):
                cs = slice(cc * 128, (cc + 1) * 128)
                for k in range(31):
                    TS("pool", diag[:, k, :], IDENT, wdw[:, l, cc, k:k + 1], None, ALU.mult, None, r=["cmat", "pv"], w=[("diag", k)])
                for t in range(NT):
                    pa, pak = ringA.next()
                    rf, kf = hrhs(t)
                    proj(pa, pak, wa, wak, cs, rf, kf)
                    pg, pgk = ringA.next()
                    proj(pg, pgk, wg_, wgk, cs, rf, kf)
                    ACT(sg, pg, AF.Sigmoid, r=[pgk], w=["sg"])
                    TTo("dve", ybuf[:, 32 + t * TW:32 + (t + 1) * TW], pa, sg, ALU.mult, r=[pak, "sg"], w=[("yb", t)])
                for t in range(NT):
                    ps, pk = ringB.next()
                    for k in range(31):
                        o = Y0 + t * TW + k
                        rk = ["ypad", ("diag", k), ("yb", t)] + ([("yb", t - 1)] if t > 0 else [])
                        MM(ps, diag[:, k, :], ybuf[:, o:o + TW], k == 0, k == 30, r=rk, w=[pk], inc=(k == 30))
                    ACT(cy[:, cc, sl(t)], ps, AF.Identity, r=[pk, "pv"], w=[("cy", cc, t)], bias=vg[:, l, 0, cc:cc + 1])
            if l == 0:
                dump("cy", cy[:, 0, 0:512], [("cy", 0, 0)])
            for t in range(NT):
                ps1, pk1 = ringB.next()
                ps2, pk2 = ringB.next()
                for cc in range(2):
                    MM(ps1, ONES, cy[:, cc, sl(t)], cc == 0, cc == 1, r=[("cy", cc, t), "cmat"], w=[pk1])
                for cc in range(2):
                    q_, qk_ = sqring.next()
                    ACT(q_, cy[:, cc, sl(t)], AF.Square, r=[("cy", cc, t)], w=[qk_])
                    MM(ps2, ONES, q_, cc == 0, cc == 1, r=[qk_, "cmat"], w=[pk2])
                TS("dve", mstat, ps1, 1.0 / GW, None, ALU.mult, None, r=[pk1], w=["mstat"])
                TTo("pool", m2, mstat, mstat, ALU.mult, r=["mstat"], w=["m2"])
                STT(m2, ps2, 1.0 / GW, m2, ALU.mult, ALU.subtract, r=[pk2, "m2"], w=["m2"])
                ACT(lnv, m2, AF.Ln, r=["m2"], w=["lnv"], bias=EPS)
                ACT(rstd, lnv, AF.Exp, r=["lnv"], w=["rstd"], scale=-0.5)
                for cc in range(2):
                    TTo("dve", yn, cy[:, cc, sl(t)], mstat, ALU.subtract, r=[("cy", cc, t), "mstat"], w=["yn"])
                    TTo("pool", yn, yn, rstd, ALU.mult, r=["yn", "rstd"], w=["yn"])
                    ACT(sil[:, cc, :], yn, AF.Silu, r=["yn", "pv"], w=[("sil", cc)], scale=vg[:, l, 1, cc:cc + 1], bias=vg[:, l, 2, cc:cc + 1])
                for co in range(2):
                    ps, pk = ringA.next()
                    for ci in range(2):
                        MM(ps, wpw[:, ci, co * 128:(co + 1) * 128], sil[:, ci, :], ci == 0, ci == 1, r=[("wpw",), ("sil", ci)], w=[pk])
                    ACT(catT[:, 4 + co, sl(t)], ps, AF.Identity, r=[pk, "pv"], w=[("cat", 4 + co, t)], bias=vg[:, l, 3, co:co + 1])
            if l == 0:
                dump("oc_pre", catT[:, 4, 0:512], [("cat", 4, 0)])
            for t in range(NT):
                rmsnorm_tile([catT[:, 4 + c, sl(t)] for c in range(2)], [("cat", 4 + c, t) for c in range(2)],
                             [catT[:, 4 + c, sl(t)] for c in range(2)], [("cat", 4 + c, t) for c in range(2)],
                             [vg[:, l, 6, c:c + 1] for c in range(2)], GW)
            mstream.done()
            S.barrier()
            A.release(m0)

            def out_proj_norm_res(nkc, wsrc, rhs_fn, rkeys_fn, vidx, wr, ytile):
                for t in range(NT):
                    for db in range(4):
                        wv, wk = wr.get()
                        for dd in range(2):
                            dc = db * 2 + dd
                            ps, pk = ringA.next()
                            for kc in range(nkc):
                                MM(ps, wv[:, kc, dd * 128:(dd + 1) * 128], rhs_fn(kc, t), kc == 0, kc == nkc - 1, r=[wk] + rkeys_fn(kc, t), w=[pk], inc=(kc == nkc - 1))
                            if dc % 2 == 0:
                                ACT(ytile[:, dc, :], ps, AF.Copy, r=[pk], w=[("yt", dc)])
                            else:
                                CP("dve", ytile[:, dc, :], ps, r=[pk], w=[("yt", dc)])
                        wr.done()
                    rmsnorm_tile([ytile[:, dc, :] for dc in range(8)], [("yt", dc) for dc in range(8)],
                                 [ytile[:, dc, :] for dc in range(8)], [("yt", dc) for dc in range(8)],
                                 [vd[:, l, vidx, dc:dc + 1] for dc in range(8)], D)
                    for dc in range(8):
                        TTo("dve" if dc % 2 else "pool", xT[:, dc, sl(t)], xT[:, dc, sl(t)], ytile[:, dc, :], ALU.add, r=[("xT", dc, t), ("yt", dc)], w=[("xT", dc, t)])

            m0 = A.mark()
            ytile = A.f32(8 * 512).rearrange("p (c s) -> p c s", c=8)
            out_proj_norm_res(8, w_out_d[l].rearrange("(k p) n -> p k n", p=128), lambda kc, t: catT[:, kc, sl(t)], lambda kc, t: [("cat", kc, t)], 1, mstream, ytile)
            if l == 0:
                dump("x1", xT[:, 0, 0:512], [("xT", 0, 0)])
            S.barrier()
            A.release(MX)
            A.release(PH)

            norm_x_to_h(l, 2)
            ytile = A.f32(8 * 512).rearrange("p (c s) -> p c s", c=8)
            fT = A.bf16(NJ * 512).rearrange("p (j s) -> p j s", j=NJ)
            ub = [[A.f32(2 + 512) for _ in range(2)] for _ in range(2)]
            halo = A.f32(44 * 2).rearrange("p (j k) -> p j k", k=2)
            ta = A.f32(512)
            tb = A.f32(512)
            cg = A.f32(512)
            cv = A.f32(512)
            wups = [A.bf16(8 * 2 * 128).rearrange("p (k g n) -> p k g n", k=8, g=2) for _ in range(3)]
            wdns = [A.bf16(NJ * 128).rearrange("p (j n) -> p j n", j=NJ) for _ in range(3)]
            MSET("dve", halo, 0.0, w=["halo"])
            wupv = w_up_d[l].rearrange("(k p) (g n) -> p k g n", p=128, g=2)
            wdnv = w_down_d[l].rearrange("(j p) n -> p j n", p=128)

            def ld_up(v, k, j):
                for gi_ in range(2):
                    wload(v[:, :, gi_, :], wupv[:, :, gi_, j * 128:(j + 1) * 128], k)

            wur = WStream([(wups[i], ("wu", i)) for i in range(3)], [(lambda v, k, j=j: ld_up(v, k, j)) for _t in range(NT) for j in range(NJ)])
            wdr = WStream([(wdns[i], ("wd", i)) for i in range(3)], [(lambda v, k, dc=dc: wload(v, wdnv[:, :, dc * 128:(dc + 1) * 128], k)) for _t in range(NT) for dc in range(8)])
            wur.top()
            wdr.top()
            for t in range(NT):
                for j in range(NJ):
                    wv, wk = wur.get()
                    u = ub[j % 2]
                    conv = []
                    for gi in range(2):
                        jj = j + gi * NJ
                        ps, pk = ringA.next()
                        for kc in range(8):
                            MM(ps, wv[:, kc, gi, :], hT[:, kc, sl(t)], kc == 0, kc == 7, r=[wk, ("hT", kc, t)], w=[pk], inc=(kc == 7))
                        uk = ("ub", j % 2, gi)
                        ACT(u[gi][:, 2:514], ps, AF.Copy, r=[pk], w=[uk])
                        CP("dve", u[gi][:, 0:2], halo[:, jj, :], r=["halo", ("halo", jj)], w=[uk])
                        tmp = ta if gi == 0 else tb
                        tk_ = "ta" if gi == 0 else "tb"
                        ACT(tmp, ps, AF.Identity, r=[pk, "pv"], w=[tk_], scale=fcw[:, l, jj, 2:3], bias=fcb[:, l, jj:jj + 1])
                        STT(tmp, u[gi][:, 1:513], fcw[:, l, jj, 1:2], tmp, ALU.mult, ALU.add, r=[uk, tk_, "pv"], w=[tk_])
                        dst = cg if gi == 0 else cv
                        dk = "cg" if gi == 0 else "cv"
                        STT(dst, u[gi][:, 0:512], fcw[:, l, jj, 0:1], tmp, ALU.mult, ALU.add, r=[uk, tk_, "pv"], w=[dk])
                        ACT(halo[:, jj, :], ps[:, 510:512], AF.Copy, r=[pk], w=[("halo", jj)])
                    ACT(cg, cg, AF.Gelu_apprx_tanh, r=["cg"], w=["cg"])
                    TTo("dve", fT[:, j, :], cg, cv, ALU.mult, r=["cg", "cv"], w=[("fT", j)])
                if l == 0 and t == 0:
                    dump("fT", fT[:, 0, :], [("fT", 0)])
                for dc in range(8):
                    wv, wk = wdr.get()
                    ps, pk = ringB.next()
                    for j in range(NJ):
                        MM(ps, wv[:, j, :], fT[:, j, :], j == 0, j == NJ - 1, r=[wk, ("fT", j)], w=[pk], inc=(j == NJ - 1))
                    if dc % 2 == 0:
                        ACT(ytile[:, dc, :], ps, AF.Copy, r=[pk], w=[("yt", dc)])
                    else:
                        CP("dve", ytile[:, dc, :], ps, r=[pk], w=[("yt", dc)])
                rmsnorm_tile([ytile[:, dc, :] for dc in range(8)], [("yt", dc) for dc in range(8)],
                             [ytile[:, dc, :] for dc in range(8)], [("yt", dc) for dc in range(8)],
                             [vd[:, l, 3, dc:dc + 1] for dc in range(8)], D)
                for dc in range(8):
                    TTo("dve", xT[:, dc, sl(t)], xT[:, dc, sl(t)], ytile[:, dc, :], ALU.add, r=[("xT", dc, t), ("yt", dc)], w=[("xT", dc, t)])
            if l == 0:
                dump("x2", xT[:, 0, 0:512], [("xT", 0, 0)])
            S.barrier()
            A.release(PH)

            norm_x_to_h(l, 4)
            pTb = A.bf16(2 * SEQ).rearrange("p (c s) -> p c s", c=2)
            wgs = [A.bf16(8 * 256).rearrange("p (k n) -> p k n", k=8) for _ in range(3)]
            wpj = A.bf16(2 * D).rearrange("p (k n) -> p k n", k=2)
            sgt = [A.f32(512) for _ in range(2)]
            pjt = [A.f32(512) for _ in range(2)]
            wload(pTb, pT_d[l].rearrange("(k p) s -> p k s", p=128), ("pTb",))
            wload(wpj, w_proj_d[l].rearrange("(k p) n -> p k n", p=128), ("wpj",))
            wgv = w_gate_d[l].rearrange("(k p) n -> p k n", p=128)
            wgr = WStream([(wgs[i], ("wg", i)) for i in range(3)], [(lambda v, k, db=db: wload(v, wgv[:, :, db * 256:(db + 1) * 256], k)) for db in range(4)])
            wgr.top()
            it = 0
            for db in range(4):
                wv, wk = wgr.get()
                for dd in range(2):
                    dc = db * 2 + dd
                    for t in range(NT):
                        b = it % 2
                        it += 1
                        ps, pk = ringA.next()
                        rf, kf = hrhs(t)
                        proj(ps, pk, wv, wk, slice(dd * 128, (dd + 1) * 128), rf, kf)
                        ACT(sgt[b], ps, AF.Sigmoid, r=[pk], w=[("sgt", b)])
                        ps2, pk2 = ringB.next()
                        for kc in range(2):
                            MM(ps2, wpj[:, kc, dc * 128:(dc + 1) * 128], pTb[:, kc, sl(t)], kc == 0, kc == 1, r=[("wpj",), ("pTb",)], w=[pk2], inc=(kc == 1))
                        TTo("dve", pjt[b], ps2, sgt[b], ALU.mult, r=[pk2, ("sgt", b)], w=[("pjt", b)])
                        TTo("pool", xT[:, dc, sl(t)], xT[:, dc, sl(t)], pjt[b], ALU.add, r=[("xT", dc, t), ("pjt", b)], w=[("xT", dc, t)])
            if l == 0:
                dump("x3", xT[:, 0, 0:512], [("xT", 0, 0)])

        for c in range(8):
            DMA("sp", out_d[c * 128:(c + 1) * 128, :], xT[:, c, :], "st_out", r=[("xT", c, t) for t in range(NT)], w=[("out", c)])
        S.barrier()
        S.emit()
    return nc


_CACHE = {}


def _prep_inputs(inp):
    cmat, cf, ind = _host_consts()
    pv, glnb, gb, wsT = _host_params(inp)
    x = np.asarray(inp["x"], np.float32)
    p = np.asarray(inp["p"], np.float32)
    pos = np.asarray(inp["positions"], np.int32)
    shared = dict(cmat=cmat, cf=cf, ind=ind, pv=pv, glnb=glnb, gb=gb, wsT=wsT)
    for k in ("w_in", "w_out", "w_up", "w_down", "w_pe_gate", "w_pe_proj", "conv_pw_w"):
        shared[k] = np.ascontiguousarray(np.asarray(inp[k], np.float32))
    maps = []
    for b in range(8):
        m = dict(shared)
        m["xT"] = np.ascontiguousarray(x[b].T)
        m["pT"] = np.ascontiguousarray(p[:, b].transpose(0, 2, 1))
        m["pos"] = np.ascontiguousarray(pos[b][None, :])
        maps.append(m)
    return maps


def kernel(**inputs):
    if "nc" not in _CACHE:
        _CACHE["nc"] = build()
    nc = _CACHE["nc"]
    maps = _prep_inputs(inputs)
    res = run_bass_kernel_spmd(nc, maps, core_ids=list(range(8)))
    out = np.stack([np.asarray(r["outT"], np.float32).T for r in res.results], axis=0)
    return np.ascontiguousarray(out)
```

```python
import math
import numpy as np
from contextlib import ExitStack
import concourse.bass as bass
import concourse.mybir as mybir
from concourse.bass_utils import run_bass_kernel_spmd

F32 = mybir.dt.float32
BF16 = mybir.dt.bfloat16
I32 = mybir.dt.int32
AF = mybir.ActivationFunctionType
ALU = mybir.AluOpType
AX = mybir.AxisListType

L = 2
D = 1024
SEQ = 2048
GW = 256
DFF = 2816
NJ = DFF // 128
NT = 4
TW = 512
MASK = 30000.0
BIGG = 1.0e6
EPS = 1e-6
ENGS = ["pe", "act", "dve", "pool", "sp"]


class Sched:
    def __init__(self, nc, es, same_engine_sync=True):
        self.nc = nc
        self.es = es
        self.same = same_engine_sync
        self.prog = {e: [] for e in ENGS}
        self.cnt = {}
        self.sems = {}
        self.seen = {e: {} for e in ENGS}
        self.state = {}
        for e in ENGS:
            self._sem(e)

    def _sem(self, key):
        if key not in self.sems:
            self.sems[key] = self.es.enter_context(self.nc.semaphore("s_" + str(key)))
            self.cnt[key] = 0
        return self.sems[key]

    def _engobj(self, e):
        nc = self.nc
        return dict(pe=nc.tensor, act=nc.scalar, dve=nc.vector, pool=nc.gpsimd, sp=nc.sync)[e]

    def _deps(self, e, r, w):
        toks = {}
        for k in r:
            st = self.state.get(k)
            if st and st[0] is not None:
                t = st[0]
                toks[t[0]] = max(toks.get(t[0], 0), t[1])
        for k in w:
            st = self.state.get(k)
            if st:
                if st[0] is not None:
                    t = st[0]
                    toks[t[0]] = max(toks.get(t[0], 0), t[1])
                for t in st[1]:
                    toks[t[0]] = max(toks.get(t[0], 0), t[1])
        waits = []
        for sk, v in toks.items():
            if sk == e and (not self.same or e == "pe"):
                continue
            if self.seen[e].get(sk, 0) >= v:
                continue
            self.seen[e][sk] = v
            waits.append((sk, v))
        return waits

    def _record(self, tok, r, w):
        for k in r:
            st = self.state.setdefault(k, [None, []])
            st[1].append(tok)
        for k in w:
            self.state[k] = [tok, []]

    def op(self, e, fn, r=(), w=(), inc=True):
        waits = self._deps(e, r, w)
        tok = (e, self.cnt[e] + 1)
        if inc:
            self.cnt[e] += 1
        self._record(tok, r, w)
        self.prog[e].append((waits, fn, (e, 1) if inc else None))
        return tok

    def dma(self, q, fn, semkey, r=(), w=()):
        self._sem(semkey)
        waits = self._deps(q, r, w)
        self.cnt[semkey] += 16
        tok = (semkey, self.cnt[semkey])
        self._record(tok, r, w)
        self.prog[q].append((waits, fn, (semkey, 16)))
        return tok

    def barrier(self):
        for e in ENGS:
            waits = []
            for sk, v in self.cnt.items():
                if v == 0 or self.seen[e].get(sk, 0) >= v:
                    continue
                if sk == e and e == "pe":
                    continue
                self.seen[e][sk] = v
                waits.append((sk, v))
            self.prog[e].append((waits, None, None))

    def simulate(self):
        val = {k: 0 for k in self.sems}
        pc = {e: 0 for e in ENGS}
        progress = True
        while progress:
            progress = False
            for e in ENGS:
                while pc[e] < len(self.prog[e]):
                    waits, fn, inc = self.prog[e][pc[e]]
                    if any(val[sk] < v for sk, v in waits):
                        break
                    if inc is not None:
                        val[inc[0]] += inc[1]
                    pc[e] += 1
                    progress = True
        stuck = {e: (pc[e], len(self.prog[e])) for e in ENGS if pc[e] < len(self.prog[e])}
        if stuck:
            msg = []
            for e, (i, n) in stuck.items():
                waits = self.prog[e][i][0]
                msg.append("%s@%d/%d waits %s" % (e, i, n, [(sk, v, val[sk]) for sk, v in waits if val[sk] < v]))
            raise RuntimeError("DEADLOCK in schedule: " + "; ".join(msg))
        for k in self.sems:
            assert val[k] == self.cnt[k], (k, val[k], self.cnt[k])

    def emit(self):
        self.simulate()
        nc = self.nc
        with nc.Block() as block:
            def run(e):
                eng = self._engobj(e)
                for waits, fn, inc in self.prog[e]:
                    for sk, v in waits:
                        eng.wait_ge(self.sems[sk], v)
                    if fn is None:
                        continue
                    ins = fn()
                    if inc is not None:
                        ins.then_inc(self.sems[inc[0]], inc[1])

            @block.tensor
            def _(e):
                run("pe")

            @block.scalar
            def _(e):
                run("act")

            @block.vector
            def _(e):
                run("dve")

            @block.gpsimd
            def _(e):
                run("pool")

            @block.sync
            def _(e):
                run("sp")


class Arena:
    def __init__(self, t, nbytes):
        self.t = t
        self.n = nbytes
        self.off = 0
        self.hi = 0

    def mark(self):
        return self.off

    def release(self, m):
        self.off = m

    def _take(self, nbytes):
        nbytes = (nbytes + 31) // 32 * 32
        o = self.off
        self.off += nbytes
        self.hi = max(self.hi, self.off)
        assert self.off <= self.n, ("arena overflow", self.off, self.n)
        return o

    def f32(self, n):
        o = self._take(4 * n)
        return self.t[:, o // 4: o // 4 + n]

    def bf16(self, n):
        o = self._take(2 * n)
        return self.t[:, o // 4: o // 4 + (n + 1) // 2].bitcast(BF16)

    def i32(self, n):
        o = self._take(4 * n)
        return self.t[:, o // 4: o // 4 + n].bitcast(I32)


class Ring:
    def __init__(self, items):
        self.items = items
        self.i = 0

    def next(self):
        it = self.items[self.i % len(self.items)]
        self.i += 1
        return it


class WStream:
    def __init__(self, slots, plan, auto=True):
        self.slots = slots
        self.plan = plan
        self.auto = auto
        self.issued = 0
        self.used = 0
        self.freed = 0

    def top(self):
        while self.issued < len(self.plan) and self.issued - self.freed < len(self.slots):
            i = self.issued
            v, k = self.slots[i % len(self.slots)]
            self.plan[i](v, k)
            self.issued += 1

    def done(self):
        self.freed = self.used
        self.top()

    def get(self):
        i = self.used
        if self.auto:
            self.freed = i
        self.top()
        assert self.issued > i, "weight stream: block not issued (slots exhausted)"
        self.used += 1
        return self.slots[i % len(self.slots)]


def _host_consts():
    ident = np.eye(128, dtype=np.float32)
    ones = np.ones((128, 128), np.float32)
    blk2 = np.zeros((128, 128), np.float32)
    blk2[0:64, 0:64] = 1
    blk2[64:128, 64:128] = 1

    def rotT(block):
        h = block // 2
        m = np.zeros((128, 128), np.float32)
        for b0 in range(0, 128, block):
            for i in range(h):
                m[b0 + i + h, b0 + i] = -1.0
                m[b0 + i, b0 + i + h] = 1.0
        return m

    tri = np.where(np.arange(128)[None, :] >= np.arange(128)[:, None], 0.0, -MASK).astype(np.float32)
    cmat = np.concatenate([ident, ones, blk2, rotT(64), rotT(32), tri], axis=1)
    tril = (np.arange(128)[:, None] <= np.arange(128)[None, :]).astype(np.float32)
    pm = np.zeros((16, 8), np.float32)
    for qs in range(16):
        cur = qs // 2
        for n in range(8):
            pm[qs, n] = 0.0 if n < cur else (BIGG if n == cur else -2 * BIGG)
    pmask = np.broadcast_to(pm.reshape(1, 128), (128, 128))
    p = np.arange(128)
    invf64 = (10000.0 ** (-(2.0 * (p % 32)) / 64.0)) / (2 * math.pi)
    invf32 = (10000.0 ** (-(2.0 * (p % 16)) / 32.0)) / (2 * math.pi)
    cf = np.concatenate([tril, pmask, invf64[:, None], invf32[:, None]], axis=1).astype(np.float32)
    ind = (np.arange(SEQ)[None, :] // 256 == np.arange(8)[:, None]).astype(np.float32)
    return np.ascontiguousarray(cmat), np.ascontiguousarray(cf), np.ascontiguousarray(ind)


def _fm(v, c):
    return np.ascontiguousarray(np.asarray(v, np.float32).reshape(c, 128).T)


def _host_params(inp):
    vd = np.stack([np.stack([_fm(inp[k][l], 8) for k in ("pre_mix_norm", "post_mix_norm", "pre_ffn_norm", "post_ffn_norm", "pe_gate_norm")], 1) for l in range(L)], 1)
    vg = np.stack([np.stack([_fm(inp[k][l], 2) for k in ("conv_dw_b", "conv_ln_g", "conv_ln_b", "conv_pw_b", "out_norm_a", "out_norm_b", "out_norm_c")], 1) for l in range(L)], 1)
    wdw = np.stack([np.asarray(inp["conv_dw_w"][l], np.float32).reshape(31, 2, 128).transpose(2, 1, 0) for l in range(L)], 1)
    fcw = np.stack([np.asarray(inp["ffn_conv_w"][l], np.float32).reshape(3, 44, 128).transpose(2, 1, 0) for l in range(L)], 1)
    fcb = np.stack([_fm(inp["ffn_conv_b"][l], 44) for l in range(L)], 1)
    gsub = np.stack([np.asarray(inp["diff_subln_g"][l], np.float32)[np.arange(128) % 64] for l in range(L)], 1)
    lamv = np.stack([np.stack([np.asarray(inp[k][l], np.float32) for k in ("diff_lq1", "diff_lk1", "diff_lq2", "diff_lk2")], 0) for l in range(L)], 0)
    lamv = np.broadcast_to(lamv.reshape(1, L * 4 * 32), (128, L * 4 * 32))
    pv = np.concatenate([vd.reshape(128, -1), vg.reshape(128, -1), wdw.reshape(128, -1), fcw.reshape(128, -1), fcb.reshape(128, -1), gsub.reshape(128, -1), lamv], axis=1)
    glnb = np.stack([np.stack([np.broadcast_to(np.asarray(inp[k][l], np.float32)[None, :], (128, 256)) for k in ("gmlp_ln_g", "gmlp_ln_b")], 1) for l in range(L)], 0)
    bs = np.asarray(inp["gmlp_bs"], np.float32)
    gb = np.zeros((L, 2, 128, 512), np.float32)
    for l in range(L):
        for cc in range(2):
            for hh in range(2):
                gb[l, cc, hh * 64:(hh + 1) * 64, :] = np.tile(bs[l, 2 * cc + hh], 4)[None, :]
    wsT = np.ascontiguousarray(np.asarray(inp["gmlp_ws"], np.float32).transpose(0, 1, 3, 2))
    return np.ascontiguousarray(pv.astype(np.float32)), np.ascontiguousarray(glnb), gb, wsT


PV_OFF = {}


def _pv_layout():
    o = 0
    for name, n in (("vd", L * 5 * 8), ("vg", L * 7 * 2), ("wdw", L * 2 * 31), ("fcw", L * 44 * 3), ("fcb", L * 44), ("gsub", L), ("lamv", L * 4 * 32)):
        PV_OFF[name] = o
        o += n
    return o


PV_N = _pv_layout()


def build(dbg=None, nlayers=L):
    dbg = dbg or []
    nc = bass.Bass("TRN2", target_bir_lowering=False)

    def din(name, shape, dt=F32):
        return nc.dram_tensor(name, list(shape), dt, kind="ExternalInput").ap()

    xT_d = din("xT", [D, SEQ])
    pT_d = din("pT", [L, GW, SEQ])
    pos_d = din("pos", [1, SEQ], I32)
    cmat_d = din("cmat", [128, 768])
    cf_d = din("cf", [128, 258])
    ind_d = din("ind", [8, SEQ])
    pv_d = din("pv", [128, PV_N])
    glnb_d = din("glnb", [L, 128, 2, 256])
    gb_d = din("gb", [L, 2, 128, 512])
    wsT_d = din("wsT", [L, 4, 128, 128])
    w_in_d = din("w_in", [L, D, 10 * GW])
    w_out_d = din("w_out", [L, D, D])
    w_up_d = din("w_up", [L, D, 2 * DFF])
    w_down_d = din("w_down", [L, DFF, D])
    w_gate_d = din("w_pe_gate", [L, D, D])
    w_proj_d = din("w_pe_proj", [L, GW, D])
    w_pw_d = din("conv_pw_w", [L, GW, GW])
    out_d = nc.dram_tensor("outT", [D, SEQ], F32, kind="ExternalOutput").ap()
    dbg_d = {n: nc.dram_tensor("dbg_" + n, [128, w], F32, kind="ExternalOutput").ap() for n, w in dbg}

    es = ExitStack()
    with es:
        S = Sched(nc, es)
        NB = 204 * 1024
        big = es.enter_context(nc.sbuf_tensor("arena", [128, NB // 4], F32))
        A = Arena(big, NB)
        psb = [es.enter_context(nc.psum_tensor("ps%d" % i, [128, 512], F32)) for i in range(8)]
        PS = [(psb[i][:, :], ("ps", i)) for i in range(8)]
        ringA = Ring(PS[0:4])
        ringB = Ring(PS[4:8])

        def MM(out, lhsT, rhs, start, stop, r, w, inc=True, tp=None):
            kw = {}
            if tp is not None:
                kw["tile_position"] = tp
            S.op("pe", lambda: nc.tensor.matmul(out, lhsT=lhsT, rhs=rhs, start=start, stop=stop, **kw), r=r, w=w, inc=inc)

        def ACT(out, in_, func, r, w, scale=None, bias=None):
            kw = {}
            if scale is not None:
                kw["scale"] = scale
            if bias is not None:
                kw["bias"] = bias
            S.op("act", lambda: nc.scalar.activation(out=out, in_=in_, func=func, **kw), r=r, w=w)

        def TTo(eng, out, in0, in1, op, r, w):
            e = nc.vector if eng == "dve" else nc.gpsimd
            S.op(eng, lambda: e.tensor_tensor(out=out, in0=in0, in1=in1, op=op), r=r, w=w)

        def TS(eng, out, in0, s1, s2, op0, op1, r, w):
            e = nc.vector if eng == "dve" else nc.gpsimd
            if op1 is None:
                S.op(eng, lambda: e.tensor_scalar(out=out, in0=in0, scalar1=s1, scalar2=None, op0=op0), r=r, w=w)
            else:
                S.op(eng, lambda: e.tensor_scalar(out=out, in0=in0, scalar1=s1, scalar2=s2, op0=op0, op1=op1), r=r, w=w)

        def STT(out, in0, scalar, in1, op0, op1, r, w):
            S.op("dve", lambda: nc.vector.scalar_tensor_tensor(out=out, in0=in0, scalar=scalar, in1=in1, op0=op0, op1=op1), r=r, w=w)

        def CP(eng, out, in_, r, w):
            e = nc.vector if eng == "dve" else nc.gpsimd
            S.op(eng, lambda: e.tensor_copy(out=out, in_=in_), r=r, w=w)

        def MSET(eng, ap, val, w):
            e = nc.vector if eng == "dve" else nc.gpsimd
            S.op(eng, lambda: e.memset(ap, val), w=w)

        def DMA(q, out, in_, semkey, r, w):
            e = dict(sp=nc.sync, pool=nc.gpsimd, act=nc.scalar)[q]
            S.dma(q, lambda: e.dma_start(out=out, in_=in_), semkey, r=r, w=w)

        def dump(name, ap, keys):
            if name in dbg_d:
                w_ = ap.shape[-1]
                stg = A.f32(w_)
                CP("dve", stg[0:ap.shape[0], :], ap, r=keys, w=[("dbgs", name)])
                DMA("sp", dbg_d[name][0:ap.shape[0], 0:w_], stg[0:ap.shape[0], :], "dbg", r=[("dbgs", name)], w=[("dbgo", name)])

        xT = A.f32(8 * SEQ).rearrange("p (c s) -> p c s", c=8)
        hT = A.bf16(8 * SEQ).rearrange("p (c s) -> p c s", c=8)
        cmat = A.bf16(768)
        IDENT, ONES, BLK2, R64, R32, TRI = [cmat[:, i * 128:(i + 1) * 128] for i in range(6)]
        cf = A.f32(258)
        TRIL = cf[:, 0:128]
        PMASK = cf[:, 128:256].rearrange("p (q n) -> p q n", n=8)
        INVF = [cf[:, 256:257], cf[:, 257:258]]
        pv = A.f32(PV_N)

        def PVv(name, n):
            return pv[:, PV_OFF[name]:PV_OFF[name] + n]

        vd = PVv("vd", L * 5 * 8).rearrange("p (l v c) -> p l v c", l=L, v=5)
        vg = PVv("vg", L * 7 * 2).rearrange("p (l v c) -> p l v c", l=L, v=7)
        wdw = PVv("wdw", L * 2 * 31).rearrange("p (l c k) -> p l c k", l=L, c=2)
        fcw = PVv("fcw", L * 44 * 3).rearrange("p (l j k) -> p l j k", l=L, j=44)
        fcb = PVv("fcb", L * 44).rearrange("p (l j) -> p l j", l=L)
        gsub = PVv("gsub", L)
        lamv = PVv("lamv", L * 4 * 32).rearrange("p (l v k) -> p l v k", l=L, v=4)
        small = A.f32(64)
        lnv = A.f32(512)
        rstd = A.f32(512)
        sq = [A.bf16(512) for _ in range(3)]
        sqring = Ring([(sq[i], ("sq", i)) for i in range(3)])
        PH = A.mark()

        DMA("pool", cmat, cmat_d[:, :], "ld_c", r=[], w=["cmat"])
        DMA("sp", cf, cf_d[:, :], "ld_c2", r=[], w=["cf"])
        DMA("sp", pv, pv_d[:, :], "ld_c3", r=[], w=["pv"])
        for c in range(8):
            DMA("sp", xT[:, c, :], xT_d[c * 128:(c + 1) * 128, :], ("ld_x", c), r=[], w=[("xT", c, t) for t in range(NT)])

        def wload(dst, src, key):
            DMA("pool", dst, src, ("wsem",) + key, r=[], w=[key])

        def rstd_from(ps_ap, ps_key, inv_n, eps, extra_r=()):
            ACT(lnv, ps_ap, AF.Ln, r=[ps_key] + list(extra_r), w=["lnv"], scale=inv_n, bias=eps)
            ACT(rstd, lnv, AF.Exp, r=["lnv"], w=["rstd"], scale=-0.5)

        def rmsnorm_tile(srcs, skeys, dsts, dkeys, gains, n_feat, eps=EPS, lhs=None, lkey="cmat"):
            ps, pk = ringB.next()
            C = len(srcs)
            for c in range(C):
                q, qk = sqring.next()
                ACT(q, srcs[c], AF.Square, r=[skeys[c]], w=[qk])
                MM(ps, lhs if lhs is not None else ONES, q, c == 0, c == C - 1, r=[qk, lkey], w=[pk])
            rstd_from(ps, pk, 1.0 / n_feat, eps)
            for c in range(C):
                STT(dsts[c], srcs[c], gains[c], rstd, ALU.mult, ALU.mult, r=[skeys[c], "rstd", "pv"], w=[dkeys[c]])

        def sl(t):
            return slice(t * TW, (t + 1) * TW)

        def norm_x_to_h(l, vidx):
            for t in range(NT):
                rmsnorm_tile([xT[:, c, sl(t)] for c in range(8)], [("xT", c, t) for c in range(8)],
                             [hT[:, c, sl(t)] for c in range(8)], [("hT", c, t) for c in range(8)],
                             [vd[:, l, vidx, c:c + 1] for c in range(8)], D)

        def proj(ps, pk, wv, wkey, cs, rhs_fn, rkeys_fn, nk=8):
            for kc in range(nk):
                MM(ps, wv[:, kc, cs], rhs_fn(kc), kc == 0, kc == nk - 1, r=[wkey] + rkeys_fn(kc), w=[pk], inc=(kc == nk - 1))

        def hrhs(t):
            return (lambda kc: hT[:, kc, sl(t)]), (lambda kc: [("hT", kc, t)])

        for l in range(nlayers):
            S.barrier()
            A.release(PH)
            catT = A.bf16(8 * SEQ).rearrange("p (c s) -> p c s", c=8)
            ropeC = A.bf16(SEQ)
            ropeS = A.bf16(SEQ)
            wslots = [A.bf16(8 * 256).rearrange("p (k n) -> p k n", k=8) for _ in range(4)]
            MX = A.mark()
            winv = w_in_d[l].rearrange("(k p) n -> p k n", p=128)
            woutv = w_out_d[l].rearrange("(k p) n -> p k n", p=128)
            mplan = [(lambda v, k, bi=bi: wload(v, winv[:, :, bi * 256:(bi + 1) * 256], k)) for bi in (0, 1, 2, 7, 8, 9, 3, 4, 5, 6)]
            mplan += [(lambda v, k, db=db: wload(v, woutv[:, :, db * 256:(db + 1) * 256], k)) for _t in range(NT) for db in range(4)]
            mstream = WStream([(wslots[i], ("ws", i)) for i in range(4)], mplan, auto=False)

            def win_block(bi):
                return mstream.get()

            def rope_tables(which):
                m = A.mark()
                posi = A.i32(SEQ)
                v = A.f32(SEQ)
                ki = A.i32(SEQ)
                DMA("sp", posi, pos_d[0:1, :].broadcast_to([128, SEQ]), "ld_pos", r=[], w=["posi"])
                for tab, shift, key in ((ropeS, 0.0, "ropeS"), (ropeC, 0.25, "ropeC")):
                    TS("dve", v, posi, INVF[which], shift, ALU.mult, ALU.add, r=["posi", "cf"], w=["ropev"])
                    CP("dve", ki, v, r=["ropev"], w=["ropek"])
                    TTo("dve", v, v, ki, ALU.subtract, r=["ropev", "ropek"], w=["ropev"])
                    ACT(tab, v, AF.Sin, r=["ropev"], w=[key], scale=2 * math.pi * (1 - 1e-6))
                S.barrier()
                A.release(m)

            def rope_apply(ps, pk, t, RM, zbr, t1, t2):
                zb, zk = zbr.next()
                ACT(zb, ps, AF.Copy, r=[pk], w=[zk])
                ps2, pk2 = ringA.next()
                MM(ps2, RM, zb, True, True, r=[zk, "cmat"], w=[pk2])
                TTo("dve", t1[0], zb, ropeC[:, sl(t)], ALU.mult, r=[zk, "ropeC"], w=[t1[1]])
                TTo("dve", t2[0], ps2, ropeS[:, sl(t)], ALU.mult, r=[pk2, "ropeS"], w=[t2[1]])

            def attn_finalize_recip(O, ok, dst_rden):
                ACT(dst_rden[0][0:64, :], O[64:128, :], AF.Ln, r=[ok], w=[dst_rden[1]])
                ACT(dst_rden[0][0:64, :], dst_rden[0][0:64, :], AF.Exp, r=[dst_rden[1]], w=[dst_rden[1]], scale=-1.0)

            def vproj(wv, wk, cs, vaug):
                for g in range(4):
                    ps, pk = ringA.next()
                    for s4 in range(4):
                        st = g * 4 + s4
                        for kc in range(8):
                            MM(ps[:, s4 * 128:(s4 + 1) * 128], hT[:, kc, st * 128:(st + 1) * 128], wv[:, kc, cs], kc == 0, kc == 7,
                               r=[wk, ("hT", kc, st // 4)], w=[pk], inc=(kc == 7 and s4 == 3))
                    ACT(vaug[:, g * 4:(g + 1) * 4, :, 0:64], ps.rearrange("p (s h d) -> p s h d", s=4, h=2), AF.Copy, r=[pk], w=[("V", g)])

            norm_x_to_h(l, 0)
            if l == 0:
                dump("hT0", hT[:, 0, 0:512], [("hT", 0, 0)])

            rope_tables(0)
            m0 = A.mark()
            Kaug = [A.bf16(SEQ) for _ in range(2)]
            vaug = A.bf16(16 * 2 * 128).rearrange("p (s h d) -> p s h d", s=16, h=2)
            Qa = [[A.bf16(512) for _ in range(2)] for _ in range(2)]
            Pr = Ring([(A.bf16(512), ("P", i)) for i in range(4)])
            zbr = Ring([(A.bf16(512), ("zb", i)) for i in range(2)])
            t1 = (A.f32(512), "t1")
            t2 = (A.f32(512), "t2")
            rden = (A.f32(512), "rden")
            bstg = A.bf16(128)
            gm = A.f32(64)
            top8 = A.f32(8)
            kmf = A.f32(16)
            kmb = A.bf16(16)
            MSET("dve", vaug[:, :, :, 64:128], 1.0, w=[("V", g) for g in range(4)])
            MSET("dve", bstg, 0.0, w=["bstg"])
            for hh in range(2):
                DMA("pool", Kaug[hh][64:72, :], ind_d[:, :], "ld_ind", r=[], w=[("Kind", hh)])
            wq, wqk = win_block(0)
            wk_, wkk = win_block(1)
            wv_, wvk = win_block(2)
            for c in range(2):
                cs = slice(c * 128, (c + 1) * 128)
                for t in range(NT):
                    ps, pk = ringA.next()
                    rf, kf = hrhs(t)
                    proj(ps, pk, wk_, wkk, cs, rf, kf)
                    rope_apply(ps, pk, t, R64, zbr, t1, t2)
                    for hh in range(2):
                        TTo("dve", Kaug[hh][0:64, sl(t)], t1[0][hh * 64:(hh + 1) * 64, :], t2[0][hh * 64:(hh + 1) * 64, :], ALU.add,
                            r=[t1[1], t2[1]], w=[("K", hh, t)])
                for hh in range(2):
                    S.op("dve", lambda hh=hh: nc.vector.tensor_reduce(out=kmf[0:64, hh * 8:(hh + 1) * 8], in_=Kaug[hh][0:64, :].rearrange("p (n k) -> p n k", k=256), axis=AX.X, op=ALU.add),
                         r=[("K", hh, t) for t in range(NT)], w=[("kmf", hh)])
                    TS("dve", kmb[0:64, hh * 8:(hh + 1) * 8], kmf[0:64, hh * 8:(hh + 1) * 8], 1.0 / 256, None, ALU.mult, None, r=[("kmf", hh)], w=[("kmb", hh)])
                vproj(wv_, wvk, cs, vaug)
                for qt in range(NT):
                    qb = Qa[qt % 2]
                    ps, pk = ringA.next()
                    rf, kf = hrhs(qt)
                    proj(ps, pk, wq, wqk, cs, rf, kf)
                    rope_apply(ps, pk, qt, R64, zbr, t1, t2)
                    for hh in range(2):
                        TTo("dve", qb[hh][0:64, :], t1[0][hh * 64:(hh + 1) * 64, :], t2[0][hh * 64:(hh + 1) * 64, :], ALU.add,
                            r=[t1[1], t2[1]], w=[("Qa", qt % 2, hh)])
                    for hh in range(2):
                        gps, gk = ringA.next()
                        for j in range(4):
                            MM(gps[:, j * 8:(j + 1) * 8], qb[hh][0:64, j * 128:(j + 1) * 128], kmb[0:64, hh * 8:(hh + 1) * 8], True, True,
                               r=[("Qa", qt % 2, hh), ("kmb", hh)], w=[gk], inc=(j == 3))
                        gmv = gm[:, 0:32].rearrange("p (j n) -> p j n", n=8)
                        TTo("dve", gmv, gps[:, 0:32].rearrange("p (j n) -> p j n", n=8), PMASK[:, qt * 4:(qt + 1) * 4, :], ALU.add, r=[gk, "cf"], w=["gm"])
                        tps, tk = ringA.next()
                        tpsb = tps.bitcast(BF16)
                        for j in range(4):
                            S.op("dve", lambda j=j: nc.vector.max(out=top8, in_=gm[:, j * 8:(j + 1) * 8]), r=["gm"], w=["top8"])
                            TS("dve", top8[:, 3:4], top8[:, 3:4], -BIGG, None, ALU.max, None, r=["top8"], w=["top8"])
                            TS("dve", bstg[:, 64:72], gm[:, j * 8:(j + 1) * 8], top8[:, 3:4], -MASK, ALU.is_lt, ALU.mult, r=["gm", "top8"], w=["bstg"])
                            S.op("pe", lambda j=j, tpsb=tpsb: nc.tensor.transpose(tpsb[0:72, j * 128:(j + 1) * 128], bstg[:, 0:72], IDENT), r=["bstg", "cmat"], w=[tk])
                        ACT(qb[hh][64:72, :], tpsb[64:72, 0:512], AF.Copy, r=[tk], w=[("Qb", qt % 2, hh)])
                    for hh in range(2):
                        O, ok = ringB.next()
                        nk = 4 * qt + 4
                        pend = None
                        for kt in range(nk):
                            jd = kt - 4 * qt
                            c0 = 128 * jd if jd > 0 else 0
                            sp_, sk = ringA.next()
                            MM(sp_[:, c0:512], Kaug[hh][0:72, kt * 128:(kt + 1) * 128], qb[hh][0:72, c0:512], True, jd < 0,
                               r=[("K", hh, kt // 4), ("Kind", hh), ("Qa", qt % 2, hh), ("Qb", qt % 2, hh)], w=[sk], inc=(jd < 0))
                            if jd >= 0:
                                MM(sp_[:, c0:c0 + 128], IDENT, TRI, False, True, r=["cmat"], w=[sk])
                            if pend is not None:
                                pend()
                            P, Pk = Pr.next()
                            ACT(P[:, c0:512], sp_[:, c0:512], AF.Exp, r=[sk], w=[Pk], scale=0.125)

                            def pv_(kt=kt, c0=c0, P=P, Pk=Pk, O=O, ok=ok, hh=hh, nk=nk):
                                MM(O[:, c0:512], vaug[:, kt, hh, :], P[:, c0:512], kt == 0, kt == nk - 1, r=[Pk, ("V", kt // 4)], w=[ok])
                            pend = pv_
                        pend()
                        attn_finalize_recip(O, ok, rden)
                        TTo("dve", catT[hh * 64:(hh + 1) * 64, c, sl(qt)], O[0:64, :], rden[0][0:64, :], ALU.mult, r=[ok, rden[1]], w=[("cat", c, qt)])
            if l == 0:
                dump("oa_pre", catT[:, 0, 0:512], [("cat", 0, 0)])
            for t in range(NT):
                rmsnorm_tile([catT[:, c, sl(t)] for c in range(2)], [("cat", c, t) for c in range(2)],
                             [catT[:, c, sl(t)] for c in range(2)], [("cat", c, t) for c in range(2)],
                             [vg[:, l, 4, c:c + 1] for c in range(2)], GW)
            if l == 0:
                dump("oa", catT[:, 0, 0:512], [("cat", 0, 0)])
            mstream.done()
            S.barrier()
            A.release(m0)

            rope_tables(1)
            lam_init = 0.8 - 0.6 * math.exp(-0.3 * l)
            lt = small[:, 0:8]
            prod = A.f32(64)
            TTo("dve", prod[:, 0:32], lamv[:, l, 0, :], lamv[:, l, 1, :], ALU.mult, r=["pv"], w=["prod"])
            S.op("dve", lambda: nc.vector.tensor_reduce(out=lt[:, 0:1], in_=prod[:, 0:32], axis=AX.X, op=ALU.add), r=["prod"], w=["lt"])
            TTo("dve", prod[:, 32:64], lamv[:, l, 2, :], lamv[:, l, 3, :], ALU.mult, r=["pv"], w=["prod2"])
            S.op("dve", lambda: nc.vector.tensor_reduce(out=lt[:, 1:2], in_=prod[:, 32:64], axis=AX.X, op=ALU.add), r=["prod2"], w=["lt"])
            ACT(lt[:, 2:4], lt[:, 0:2], AF.Exp, r=["lt"], w=["lt"])
            TTo("dve", lt[:, 4:5], lt[:, 3:4], lt[:, 2:3], ALU.subtract, r=["lt"], w=["lt"])
            TS("dve", lt[:, 5:6], lt[:, 4:5], -lam_init, None, ALU.add, None, r=["lt"], w=["lt"])
            TS("dve", lt[:, 6:7], gsub[:, l:l + 1], 1.0 - lam_init, None, ALU.mult, None, r=["pv", "lt"], w=["lt"])
            NEGLAM = lt[:, 5:6]
            GS = lt[:, 6:7]

            m0 = A.mark()
            Kd = A.bf16(SEQ)
            vaug = A.bf16(16 * 2 * 128).rearrange("p (s h d) -> p s h d", s=16, h=2)
            Qd = [A.bf16(512) for _ in range(2)]
            Pr = Ring([(A.bf16(512), ("P", i)) for i in range(4)])
            zbr = Ring([(A.bf16(512), ("zb", i)) for i in range(2)])
            t1 = (A.f32(512), "t1")
            t2 = (A.f32(512), "t2")
            rden = (A.f32(512), "rden")
            od = A.f32(512)
            MSET("dve", vaug[:, :, :, 64:128], 1.0, w=[("V", g) for g in range(4)])
            wq, wqk = win_block(7)
            wk_, wkk = win_block(8)
            wv_, wvk = win_block(9)
            dscale = 32.0 ** -0.5
            for c in range(2):
                cs = slice(c * 128, (c + 1) * 128)
                for t in range(NT):
                    ps, pk = ringA.next()
                    rf, kf = hrhs(t)
                    proj(ps, pk, wk_, wkk, cs, rf, kf)
                    rope_apply(ps, pk, t, R32, zbr, t1, t2)
                    TTo("dve", Kd[:, sl(t)], t1[0], t2[0], ALU.add, r=[t1[1], t2[1]], w=[("Kd", t)])
                vproj(wv_, wvk, cs, vaug)
                for qt in range(NT):
                    qd = Qd[qt % 2]
                    ps, pk = ringA.next()
                    rf, kf = hrhs(qt)
                    proj(ps, pk, wq, wqk, cs, rf, kf)
                    rope_apply(ps, pk, qt, R32, zbr, t1, t2)
                    TTo("dve", qd, t1[0], t2[0], ALU.add, r=[t1[1], t2[1]], w=[("Qd", qt % 2)])
                    for hh in range(2):
                        Os = [ringB.next(), ringB.next()]
                        nk = 4 * qt + 4
                        pend = []
                        for kt in range(nk):
                            jd = kt - 4 * qt
                            c0 = 128 * jd if jd > 0 else 0
                            cur = []
                            for m_ in range(2):
                                g = hh * 2 + m_
                                sp_, sk = ringA.next()
                                MM(sp_[:, c0:512], Kd[32 * g:32 * g + 32, kt * 128:(kt + 1) * 128], qd[32 * g:32 * g + 32, c0:512], True, jd < 0,
                                   r=[("Kd", kt // 4), ("Qd", qt % 2)], w=[sk], inc=(jd < 0), tp=(32 * g, 0))
                                if jd >= 0:
                                    MM(sp_[:, c0:c0 + 128], IDENT, TRI, False, True, r=["cmat"], w=[sk])
                                cur.append((sp_, sk))
                            for f in pend:
                                f()
                            pend = []
                            for m_ in range(2):
                                sp_, sk = cur[m_]
                                P, Pk = Pr.next()
                                ACT(P[:, c0:512], sp_[:, c0:512], AF.Exp, r=[sk], w=[Pk], scale=dscale)

                                def pv_(kt=kt, c0=c0, P=P, Pk=Pk, Oo=Os[m_], hh=hh, nk=nk):
                                    MM(Oo[0][:, c0:512], vaug[:, kt, hh, :], P[:, c0:512], kt == 0, kt == nk - 1, r=[Pk, ("V", kt // 4)], w=[Oo[1]])
                                pend.append(pv_)
                        for f in pend:
                            f()
                        hs = slice(hh * 64, (hh + 1) * 64)
                        attn_finalize_recip(Os[0][0], Os[0][1], rden)
                        TTo("dve", t1[0][0:64, :], Os[0][0][0:64, :], rden[0][0:64, :], ALU.mult, r=[Os[0][1], rden[1]], w=[t1[1]])
                        attn_finalize_recip(Os[1][0], Os[1][1], rden)
                        TTo("dve", t2[0][0:64, :], Os[1][0][0:64, :], rden[0][0:64, :], ALU.mult, r=[Os[1][1], rden[1]], w=[t2[1]])
                        STT(od[hs, :], t2[0][0:64, :], NEGLAM[0:64, :], t1[0][0:64, :], ALU.mult, ALU.add, r=[t1[1], t2[1], "lt"], w=[("od", hh)])
                    ps, pk = ringB.next()
                    q_, qk_ = sqring.next()
                    ACT(q_, od, AF.Square, r=[("od", 0), ("od", 1)], w=[qk_])
                    MM(ps, BLK2, q_, True, True, r=[qk_, "cmat"], w=[pk])
                    rstd_from(ps, pk, 1.0 / 64, 1e-5)
                    STT(catT[:, 6 + c, sl(qt)], od, GS, rstd, ALU.mult, ALU.mult, r=[("od", 0), ("od", 1), "rstd", "lt"], w=[("cat", 6 + c, qt)])
            if l == 0:
                dump("od", catT[:, 6, 0:512], [("cat", 6, 0)])
            mstream.done()
            S.barrier()
            A.release(m0)

            m0 = A.mark()
            uT = A.bf16(2 * SEQ).rearrange("p (c s) -> p c s", c=2)
            vgl = A.bf16(16 * 256).rearrange("p (n d) -> p n d", n=16)
            glnb = A.f32(512).rearrange("p (v d) -> p v d", v=2)
            gb = A.f32(1024).rearrange("p (c s) -> p c s", c=2)
            wsf = A.f32(512).rearrange("p (h i) -> p h i", h=4)
            wsb = A.bf16(512).rearrange("p (h i) -> p h i", h=4)
            stats = A.f32(16 * 6)
            mv = A.f32(16 * 2).rearrange("p (n k) -> p n k", k=2)
            rsd = A.f32(16)
            vtmp = A.f32(256)
            vln = [A.bf16(256) for _ in range(2)]
            stmp = A.f32(512)
            DMA("sp", glnb, glnb_d[l], "ld_g1", r=[], w=["glnb"])
            DMA("sp", gb, gb_d[l].rearrange("c p s -> p c s"), "ld_g2", r=[], w=["gb"])
            DMA("sp", wsf, wsT_d[l].rearrange("h j i -> j h i"), "ld_g3", r=[], w=["wsf"])
            for h in range(4):
                TTo("dve", wsb[:, h, :], wsf[:, h, :], TRIL, ALU.mult, r=["wsf", "cf"], w=["wsb"])
            wu, wuk = win_block(3)
            wvv, wvvk = win_block(4)
            for t in range(NT):
                for cc in range(2):
                    ps, pk = ringA.next()
                    rf, kf = hrhs(t)
                    proj(ps, pk, wu, wuk, slice(cc * 128, (cc + 1) * 128), rf, kf)
                    ACT(uT[:, cc, sl(t)], ps, AF.Gelu_apprx_tanh, r=[pk], w=[("uT", cc, t)])
            for n2 in range(8):
                ps, pk = ringA.next()
                for s2 in range(2):
                    n = n2 * 2 + s2
                    for kc in range(8):
                        MM(ps[:, s2 * 256:(s2 + 1) * 256], hT[:, kc, n * 128:(n + 1) * 128], wvv[:, kc, :], kc == 0, kc == 7,
                           r=[wvvk, ("hT", kc, n // 4)], w=[pk], inc=(kc == 7 and s2 == 1))
                ACT(vgl[:, n2 * 2:(n2 + 1) * 2, :], ps.rearrange("p (s d) -> p s d", s=2), AF.Gelu_apprx_tanh, r=[pk], w=[("vgl", n2)])
            for n in range(16):
                S.op("dve", lambda n=n: nc.vector.bn_stats(out=stats[:, n * 6:(n + 1) * 6], in_=vgl[:, n, :]), r=[("vgl", n // 2)], w=[("st", n)])
                S.op("dve", lambda n=n: nc.vector.bn_aggr(out=mv[:, n, :], in_=stats[:, n * 6:(n + 1) * 6]), r=[("st", n)], w=["mv"])
            ACT(rsd, mv[:, :, 1], AF.Ln, r=["mv"], w=["rsd"], bias=EPS)
            ACT(rsd, rsd, AF.Exp, r=["rsd"], w=["rsd"], scale=-0.5)
            for t in range(NT):
                pss = [ringA.next(), ringA.next()]
                for s4 in range(4):
                    n = t * 4 + s4
                    vl = vln[n % 2]
                    vk = ("vln", n % 2)
                    TS("dve", vtmp, vgl[:, n, :], mv[:, n, 0:1], rsd[:, n:n + 1], ALU.subtract, ALU.mult, r=[("vgl", n // 2), "mv", "rsd"], w=["vtmp"])
                    TTo("dve", vtmp, vtmp, glnb[:, 0, :], ALU.mult, r=["vtmp", "glnb"], w=["vtmp"])
                    TTo("dve", vl, vtmp, glnb[:, 1, :], ALU.add, r=["vtmp", "glnb"], w=[vk])
                    for cc in range(2):
                        for hh in range(2):
                            h = 2 * cc + hh
                            MM(pss[cc][0][hh * 64:(hh + 1) * 64, s4 * 128:(s4 + 1) * 128], vl[:, h * 64:(h + 1) * 64], wsb[:, h, :], True, True,
                               r=[vk, "wsb"], w=[pss[cc][1]], tp=(0, hh * 64))
                for cc in range(2):
                    TTo("dve", stmp, pss[cc][0], gb[:, cc, :], ALU.add, r=[pss[cc][1], "gb"], w=["stmp"])
                    TTo("dve", catT[:, 2 + cc, sl(t)], stmp, uT[:, cc, sl(t)], ALU.mult, r=["stmp", ("uT", cc, t)], w=[("cat", 2 + cc, t)])
            if l == 0:
                dump("ob_pre", catT[:, 2, 0:512], [("cat", 2, 0)])
            for t in range(NT):
                rmsnorm_tile([catT[:, 2 + c, sl(t)] for c in range(2)], [("cat", 2 + c, t) for c in range(2)],
                             [catT[:, 2 + c, sl(t)] for c in range(2)], [("cat", 2 + c, t) for c in range(2)],
                             [vg[:, l, 5, c:c + 1] for c in range(2)], GW)
            mstream.done()
            S.barrier()
            A.release(m0)

            m0 = A.mark()
            ybuf = A.bf16(32 + SEQ)
            diag = A.bf16(31 * 128).rearrange("p (k n) -> p k n", k=31)
            cy = A.bf16(2 * SEQ).rearrange("p (c s) -> p c s", c=2)
            sg = A.f32(512)
            wpw = A.bf16(2 * 256).rearrange("p (k n) -> p k n", k=2)
            mstat = A.f32(512)
            m2 = A.f32(512)
            yn = A.f32(512)
            sil = A.bf16(1024).rearrange("p (c s) -> p c s", c=2)
            Y0 = 2
            wa, wak = win_block(5)
            wg_, wgk = win_block(6)
            wload(wpw, w_pw_d[l].rearrange("(k p) n -> p k n", p=128), ("wpw",))
            MSET("dve", ybuf[:, 0:32], 0.0, w=["ypad"])
            for cc in range(2):
                cs = slice(cc * 128, (cc + 1) * 128)
                for k in range(31):
                    TS("dve", diag[:, k, :], IDENT, wdw[:, l, cc, k:k + 1], None, ALU.mult, None, r=["cmat", "pv"], w=[("diag", k)])
                for t in range(NT):
                    pa, pak = ringA.next()
                    rf, kf = hrhs(t)
                    proj(pa, pak, wa, wak, cs, rf, kf)
                    pg, pgk = ringA.next()
                    proj(pg, pgk, wg_, wgk, cs, rf, kf)
                    ACT(sg, pg, AF.Sigmoid, r=[pgk], w=["sg"])
                    TTo("dve", ybuf[:, 32 + t * TW:32 + (t + 1) * TW], pa, sg, ALU.mult, r=[pak, "sg"], w=[("yb", t)])
                for t in range(NT):
                    ps, pk = ringB.next()
                    for k in range(31):
                        o = Y0 + t * TW + k
                        rk = ["ypad", ("diag", k), ("yb", t)] + ([("yb", t - 1)] if t > 0 else [])
                        MM(ps, diag[:, k, :], ybuf[:, o:o + TW], k == 0, k == 30, r=rk, w=[pk], inc=(k == 30))
                    ACT(cy[:, cc, sl(t)], ps, AF.Identity, r=[pk, "pv"], w=[("cy", cc, t)], bias=vg[:, l, 0, cc:cc + 1])
            if l == 0:
                dump("cy", cy[:, 0, 0:512], [("cy", 0, 0)])
            for t in range(NT):
                ps1, pk1 = ringB.next()
                ps2, pk2 = ringB.next()
                for cc in range(2):
                    MM(ps1, ONES, cy[:, cc, sl(t)], cc == 0, cc == 1, r=[("cy", cc, t), "cmat"], w=[pk1])
                for cc in range(2):
                    q_, qk_ = sqring.next()
                    ACT(q_, cy[:, cc, sl(t)], AF.Square, r=[("cy", cc, t)], w=[qk_])
                    MM(ps2, ONES, q_, cc == 0, cc == 1, r=[qk_, "cmat"], w=[pk2])
                TS("dve", mstat, ps1, 1.0 / GW, None, ALU.mult, None, r=[pk1], w=["mstat"])
                TTo("dve", m2, mstat, mstat, ALU.mult, r=["mstat"], w=["m2"])
                STT(m2, ps2, 1.0 / GW, m2, ALU.mult, ALU.subtract, r=[pk2, "m2"], w=["m2"])
                ACT(lnv, m2, AF.Ln, r=["m2"], w=["lnv"], bias=EPS)
                ACT(rstd, lnv, AF.Exp, r=["lnv"], w=["rstd"], scale=-0.5)
                for cc in range(2):
                    TTo("dve", yn, cy[:, cc, sl(t)], mstat, ALU.subtract, r=[("cy", cc, t), "mstat"], w=["yn"])
                    TTo("dve", yn, yn, rstd, ALU.mult, r=["yn", "rstd"], w=["yn"])
                    ACT(sil[:, cc, :], yn, AF.Silu, r=["yn", "pv"], w=[("sil", cc)], scale=vg[:, l, 1, cc:cc + 1], bias=vg[:, l, 2, cc:cc + 1])
                for co in range(2):
                    ps, pk = ringA.next()
                    for ci in range(2):
                        MM(ps, wpw[:, ci, co * 128:(co + 1) * 128], sil[:, ci, :], ci == 0, ci == 1, r=[("wpw",), ("sil", ci)], w=[pk])
                    ACT(catT[:, 4 + co, sl(t)], ps, AF.Identity, r=[pk, "pv"], w=[("cat", 4 + co, t)], bias=vg[:, l, 3, co:co + 1])
            if l == 0:
                dump("oc_pre", catT[:, 4, 0:512], [("cat", 4, 0)])
            for t in range(NT):
                rmsnorm_tile([catT[:, 4 + c, sl(t)] for c in range(2)], [("cat", 4 + c, t) for c in range(2)],
                             [catT[:, 4 + c, sl(t)] for c in range(2)], [("cat", 4 + c, t) for c in range(2)],
                             [vg[:, l, 6, c:c + 1] for c in range(2)], GW)
            mstream.done()
            S.barrier()
            A.release(m0)

            def out_proj_norm_res(nkc, wsrc, rhs_fn, rkeys_fn, vidx, wr, ytile):
                for t in range(NT):
                    for db in range(4):
                        wv, wk = wr.get()
                        for dd in range(2):
                            dc = db * 2 + dd
                            ps, pk = ringA.next()
                            for kc in range(nkc):
                                MM(ps, wv[:, kc, dd * 128:(dd + 1) * 128], rhs_fn(kc, t), kc == 0, kc == nkc - 1, r=[wk] + rkeys_fn(kc, t), w=[pk], inc=(kc == nkc - 1))
                            if dc % 2 == 0:
                                ACT(ytile[:, dc, :], ps, AF.Copy, r=[pk], w=[("yt", dc)])
                            else:
                                CP("dve", ytile[:, dc, :], ps, r=[pk], w=[("yt", dc)])
                        wr.done()
                    rmsnorm_tile([ytile[:, dc, :] for dc in range(8)], [("yt", dc) for dc in range(8)],
                                 [ytile[:, dc, :] for dc in range(8)], [("yt", dc) for dc in range(8)],
                                 [vd[:, l, vidx, dc:dc + 1] for dc in range(8)], D)
                    for dc in range(8):
                        TTo("dve", xT[:, dc, sl(t)], xT[:, dc, sl(t)], ytile[:, dc, :], ALU.add, r=[("xT", dc, t), ("yt", dc)], w=[("xT", dc, t)])

            m0 = A.mark()
            ytile = A.f32(8 * 512).rearrange("p (c s) -> p c s", c=8)
            out_proj_norm_res(8, w_out_d[l].rearrange("(k p) n -> p k n", p=128), lambda kc, t: catT[:, kc, sl(t)], lambda kc, t: [("cat", kc, t)], 1, mstream, ytile)
            if l == 0:
                dump("x1", xT[:, 0, 0:512], [("xT", 0, 0)])
            S.barrier()
            A.release(MX)
            A.release(PH)

            norm_x_to_h(l, 2)
            ytile = A.f32(8 * 512).rearrange("p (c s) -> p c s", c=8)
            fT = A.bf16(NJ * 512).rearrange("p (j s) -> p j s", j=NJ)
            ub = [[A.f32(2 + 512) for _ in range(2)] for _ in range(2)]
            halo = A.f32(44 * 2).rearrange("p (j k) -> p j k", k=2)
            tas = [A.f32(512) for _ in range(2)]
            tbs = [A.f32(512) for _ in range(2)]
            cgs = [A.f32(512) for _ in range(2)]
            cvs = [A.f32(512) for _ in range(2)]
            wups = [A.bf16(8 * 2 * 128).rearrange("p (k g n) -> p k g n", k=8, g=2) for _ in range(3)]
            wdns = [A.bf16(NJ * 128).rearrange("p (j n) -> p j n", j=NJ) for _ in range(3)]
            MSET("dve", halo, 0.0, w=["halo"])
            wupv = w_up_d[l].rearrange("(k p) (g n) -> p k g n", p=128, g=2)
            wdnv = w_down_d[l].rearrange("(j p) n -> p j n", p=128)

            def ld_up(v, k, j):
                for gi_ in range(2):
                    wload(v[:, :, gi_, :], wupv[:, :, gi_, j * 128:(j + 1) * 128], k)

            wur = WStream([(wups[i], ("wu", i)) for i in range(3)], [(lambda v, k, j=j: ld_up(v, k, j)) for _t in range(NT) for j in range(NJ)])
            wdr = WStream([(wdns[i], ("wd", i)) for i in range(3)], [(lambda v, k, dc=dc: wload(v, wdnv[:, :, dc * 128:(dc + 1) * 128], k)) for _t in range(NT) for dc in range(8)])
            wur.top()
            wdr.top()
            for t in range(NT):
                for j in range(NJ):
                    wv, wk = wur.get()
                    u = ub[j % 2]
                    b2 = j % 2
                    ta, tb, cg, cv = tas[b2], tbs[b2], cgs[b2], cvs[b2]
                    for gi in range(2):
                        jj = j + gi * NJ
                        ps, pk = ringA.next()
                        for kc in range(8):
                            MM(ps, wv[:, kc, gi, :], hT[:, kc, sl(t)], kc == 0, kc == 7, r=[wk, ("hT", kc, t)], w=[pk], inc=(kc == 7))
                        uk = ("ub", b2, gi)
                        ACT(u[gi][:, 2:514], ps, AF.Copy, r=[pk], w=[uk])
                        CP("dve", u[gi][:, 0:2], halo[:, jj, :], r=["halo", ("halo", jj)], w=[uk])
                        tmp = ta if gi == 0 else tb
                        tk_ = ("ta", b2) if gi == 0 else ("tb", b2)
                        ACT(tmp, ps, AF.Identity, r=[pk, "pv"], w=[tk_], scale=fcw[:, l, jj, 2:3], bias=fcb[:, l, jj:jj + 1])
                        ACT(halo[:, jj, :], ps[:, 510:512], AF.Copy, r=[pk], w=[("halo", jj)])
                        dst = cg if gi == 0 else cv
                        dk = ("cg", b2) if gi == 0 else ("cv", b2)
                        STT(tmp, u[gi][:, 1:513], fcw[:, l, jj, 1:2], tmp, ALU.mult, ALU.add, r=[uk, tk_, "pv"], w=[tk_])
                        STT(dst, u[gi][:, 0:512], fcw[:, l, jj, 0:1], tmp, ALU.mult, ALU.add, r=[uk, tk_, "pv"], w=[dk])
                    ACT(cg, cg, AF.Gelu_apprx_tanh, r=[("cg", b2)], w=[("cg", b2)])
                    TTo("dve", fT[:, j, :], cg, cv, ALU.mult, r=[("cg", b2), ("cv", b2)], w=[("fT", j)])
                if l == 0 and t == 0:
                    dump("fT", fT[:, 0, :], [("fT", 0)])
                for dc in range(8):
                    wv, wk = wdr.get()
                    ps, pk = ringB.next()
                    for j in range(NJ):
                        MM(ps, wv[:, j, :], fT[:, j, :], j == 0, j == NJ - 1, r=[wk, ("fT", j)], w=[pk], inc=(j == NJ - 1))
                    if dc % 2 == 0:
                        ACT(ytile[:, dc, :], ps, AF.Copy, r=[pk], w=[("yt", dc)])
                    else:
                        CP("dve", ytile[:, dc, :], ps, r=[pk], w=[("yt", dc)])
                rmsnorm_tile([ytile[:, dc, :] for dc in range(8)], [("yt", dc) for dc in range(8)],
                             [ytile[:, dc, :] for dc in range(8)], [("yt", dc) for dc in range(8)],
                             [vd[:, l, 3, dc:dc + 1] for dc in range(8)], D)
                for dc in range(8):
                    TTo("dve", xT[:, dc, sl(t)], xT[:, dc, sl(t)], ytile[:, dc, :], ALU.add, r=[("xT", dc, t), ("yt", dc)], w=[("xT", dc, t)])
            if l == 0:
                dump("x2", xT[:, 0, 0:512], [("xT", 0, 0)])
            S.barrier()
            A.release(PH)

            norm_x_to_h(l, 4)
            pTb = A.bf16(2 * SEQ).rearrange("p (c s) -> p c s", c=2)
            wgs = [A.bf16(8 * 256).rearrange("p (k n) -> p k n", k=8) for _ in range(3)]
            wpj = A.bf16(2 * D).rearrange("p (k n) -> p k n", k=2)
            sgt = [A.f32(512) for _ in range(2)]
            pjt = [A.f32(512) for _ in range(2)]
            wload(pTb, pT_d[l].rearrange("(k p) s -> p k s", p=128), ("pTb",))
            wload(wpj, w_proj_d[l].rearrange("(k p) n -> p k n", p=128), ("wpj",))
            wgv = w_gate_d[l].rearrange("(k p) n -> p k n", p=128)
            wgr = WStream([(wgs[i], ("wg", i)) for i in range(3)], [(lambda v, k, db=db: wload(v, wgv[:, :, db * 256:(db + 1) * 256], k)) for db in range(4)])
            wgr.top()
            it = 0
            for db in range(4):
                wv, wk = wgr.get()
                for dd in range(2):
                    dc = db * 2 + dd
                    for t in range(NT):
                        b = it % 2
                        it += 1
                        ps, pk = ringA.next()
                        rf, kf = hrhs(t)
                        proj(ps, pk, wv, wk, slice(dd * 128, (dd + 1) * 128), rf, kf)
                        ACT(sgt[b], ps, AF.Sigmoid, r=[pk], w=[("sgt", b)])
                        ps2, pk2 = ringB.next()
                        for kc in range(2):
                            MM(ps2, wpj[:, kc, dc * 128:(dc + 1) * 128], pTb[:, kc, sl(t)], kc == 0, kc == 1, r=[("wpj",), ("pTb",)], w=[pk2], inc=(kc == 1))
                        TTo("dve", pjt[b], ps2, sgt[b], ALU.mult, r=[pk2, ("sgt", b)], w=[("pjt", b)])
                        TTo("dve", xT[:, dc, sl(t)], xT[:, dc, sl(t)], pjt[b], ALU.add, r=[("xT", dc, t), ("pjt", b)], w=[("xT", dc, t)])
            if l == 0:
                dump("x3", xT[:, 0, 0:512], [("xT", 0, 0)])

        for c in range(8):
            DMA("sp", out_d[c * 128:(c + 1) * 128, :], xT[:, c, :], "st_out", r=[("xT", c, t) for t in range(NT)], w=[("out", c)])
        S.barrier()
        S.emit()
        build.arena_hi = A.hi
    return nc


_CACHE = {}


def _prep_inputs(inp):
    cmat, cf, ind = _host_consts()
    pv, glnb, gb, wsT = _host_params(inp)
    x = np.asarray(inp["x"], np.float32)
    p = np.asarray(inp["p"], np.float32)
    pos = np.asarray(inp["positions"], np.int32)
    shared = dict(cmat=cmat, cf=cf, ind=ind, pv=pv, glnb=glnb, gb=gb, wsT=wsT)
    for k in ("w_in", "w_out", "w_up", "w_down", "w_pe_gate", "w_pe_proj", "conv_pw_w"):
        shared[k] = np.ascontiguousarray(np.asarray(inp[k], np.float32))
    maps = []
    for b in range(8):
        m = dict(shared)
        m["xT"] = np.ascontiguousarray(x[b].T)
        m["pT"] = np.ascontiguousarray(p[:, b].transpose(0, 2, 1))
        m["pos"] = np.ascontiguousarray(pos[b][None, :])
        maps.append(m)
    return maps


def kernel(**inputs):
    if "nc" not in _CACHE:
        _CACHE["nc"] = build()
    nc = _CACHE["nc"]
    maps = _prep_inputs(inputs)
    res = run_bass_kernel_spmd(nc, maps, core_ids=list(range(8)))
    out = np.stack([np.asarray(r["outT"], np.float32).T for r in res.results], axis=0)
    return np.ascontiguousarray(out)
```

```python
import math
import numpy as np
from contextlib import ExitStack
import concourse.bass as bass
import concourse.mybir as mybir
from concourse.bass_utils import run_bass_kernel_spmd

F32 = mybir.dt.float32
BF16 = mybir.dt.bfloat16
I32 = mybir.dt.int32
AF = mybir.ActivationFunctionType
ALU = mybir.AluOpType
AX = mybir.AxisListType

L = 2
D = 1024
SEQ = 2048
GW = 256
DFF = 2816
NJ = DFF // 128
NT = 4
TW = 512
MASK = 30000.0
BIGG = 1.0e6
EPS = 1e-6
ENGS = ["pe", "act", "dve", "pool", "sp"]


class Sched:
    def __init__(self, nc, es, same_engine_sync=True):
        self.nc = nc
        self.es = es
        self.same = same_engine_sync
        self.prog = {e: [] for e in ENGS}
        self.cnt = {}
        self.sems = {}
        self.seen = {e: {} for e in ENGS}
        self.state = {}
        for e in ENGS:
            self._sem(e)

    def _sem(self, key):
        if key not in self.sems:
            self.sems[key] = self.es.enter_context(self.nc.semaphore("s_" + str(key)))
            self.cnt[key] = 0
        return self.sems[key]

    def _engobj(self, e):
        nc = self.nc
        return dict(pe=nc.tensor, act=nc.scalar, dve=nc.vector, pool=nc.gpsimd, sp=nc.sync)[e]

    def _deps(self, e, r, w):
        toks = {}
        for k in r:
            st = self.state.get(k)
            if st and st[0] is not None:
                t = st[0]
                toks[t[0]] = max(toks.get(t[0], 0), t[1])
        for k in w:
            st = self.state.get(k)
            if st:
                if st[0] is not None:
                    t = st[0]
                    toks[t[0]] = max(toks.get(t[0], 0), t[1])
                for t in st[1]:
                    toks[t[0]] = max(toks.get(t[0], 0), t[1])
        waits = []
        for sk, v in toks.items():
            if sk == e and (not self.same or e == "pe"):
                continue
            if self.seen[e].get(sk, 0) >= v:
                continue
            self.seen[e][sk] = v
            waits.append((sk, v))
        return waits

    def _record(self, tok, r, w):
        for k in r:
            st = self.state.setdefault(k, [None, []])
            st[1].append(tok)
        for k in w:
            self.state[k] = [tok, []]

    def op(self, e, fn, r=(), w=(), inc=True):
        waits = self._deps(e, r, w)
        tok = (e, self.cnt[e] + 1)
        if inc:
            self.cnt[e] += 1
        self._record(tok, r, w)
        self.prog[e].append((waits, fn, (e, 1) if inc else None))
        return tok

    def dma(self, q, fn, semkey, r=(), w=()):
        self._sem(semkey)
        waits = self._deps(q, r, w)
        self.cnt[semkey] += 16
        tok = (semkey, self.cnt[semkey])
        self._record(tok, r, w)
        self.prog[q].append((waits, fn, (semkey, 16)))
        return tok

    def barrier(self):
        for e in ENGS:
            waits = []
            for sk, v in self.cnt.items():
                if v == 0 or self.seen[e].get(sk, 0) >= v:
                    continue
                if sk == e and e == "pe":
                    continue
                self.seen[e][sk] = v
                waits.append((sk, v))
            self.prog[e].append((waits, None, None))

    def simulate(self):
        val = {k: 0 for k in self.sems}
        pc = {e: 0 for e in ENGS}
        progress = True
        while progress:
            progress = False
            for e in ENGS:
                while pc[e] < len(self.prog[e]):
                    waits, fn, inc = self.prog[e][pc[e]]
                    if any(val[sk] < v for sk, v in waits):
                        break
                    if inc is not None:
                        val[inc[0]] += inc[1]
                    pc[e] += 1
                    progress = True
        stuck = {e: (pc[e], len(self.prog[e])) for e in ENGS if pc[e] < len(self.prog[e])}
        if stuck:
            msg = []
            for e, (i, n) in stuck.items():
                waits = self.prog[e][i][0]
                msg.append("%s@%d/%d waits %s" % (e, i, n, [(sk, v, val[sk]) for sk, v in waits if val[sk] < v]))
            raise RuntimeError("DEADLOCK in schedule: " + "; ".join(msg))
        for k in self.sems:
            assert val[k] == self.cnt[k], (k, val[k], self.cnt[k])

    def emit(self):
        self.simulate()
        nc = self.nc
        with nc.Block() as block:
            def run(e):
                eng = self._engobj(e)
                for waits, fn, inc in self.prog[e]:
                    for sk, v in waits:
                        eng.wait_ge(self.sems[sk], v)
                    if fn is None:
                        continue
                    ins = fn()
                    if inc is not None:
                        ins.then_inc(self.sems[inc[0]], inc[1])

            @block.tensor
            def _(e):
                run("pe")

            @block.scalar
            def _(e):
                run("act")

            @block.vector
            def _(e):
                run("dve")

            @block.gpsimd
            def _(e):
                run("pool")

            @block.sync
            def _(e):
                run("sp")


class Arena:
    def __init__(self, t, nbytes):
        self.t = t
        self.n = nbytes
        self.off = 0
        self.hi = 0

    def mark(self):
        return self.off

    def release(self, m):
        self.off = m

    def _take(self, nbytes):
        nbytes = (nbytes + 31) // 32 * 32
        o = self.off
        self.off += nbytes
        self.hi = max(self.hi, self.off)
        assert self.off <= self.n, ("arena overflow", self.off, self.n)
        return o

    def f32(self, n):
        o = self._take(4 * n)
        return self.t[:, o // 4: o // 4 + n]

    def bf16(self, n):
        o = self._take(2 * n)
        return self.t[:, o // 4: o // 4 + (n + 1) // 2].bitcast(BF16)

    def i32(self, n):
        o = self._take(4 * n)
        return self.t[:, o // 4: o // 4 + n].bitcast(I32)


class Ring:
    def __init__(self, items):
        self.items = items
        self.i = 0

    def next(self):
        it = self.items[self.i % len(self.items)]
        self.i += 1
        return it


class WStream:
    def __init__(self, slots, plan, auto=True):
        self.slots = slots
        self.plan = plan
        self.auto = auto
        self.issued = 0
        self.used = 0
        self.freed = 0

    def top(self):
        while self.issued < len(self.plan) and self.issued - self.freed < len(self.slots):
            i = self.issued
            v, k = self.slots[i % len(self.slots)]
            self.plan[i](v, k)
            self.issued += 1

    def done(self):
        self.freed = self.used
        self.top()

    def get(self):
        i = self.used
        if self.auto:
            self.freed = i
        self.top()
        assert self.issued > i, "weight stream: block not issued (slots exhausted)"
        self.used += 1
        return self.slots[i % len(self.slots)]


def _host_consts():
    ident = np.eye(128, dtype=np.float32)
    ones = np.ones((128, 128), np.float32)
    blk2 = np.zeros((128, 128), np.float32)
    blk2[0:64, 0:64] = 1
    blk2[64:128, 64:128] = 1

    def rotT(block):
        h = block // 2
        m = np.zeros((128, 128), np.float32)
        for b0 in range(0, 128, block):
            for i in range(h):
                m[b0 + i + h, b0 + i] = -1.0
                m[b0 + i, b0 + i + h] = 1.0
        return m

    tri = np.where(np.arange(128)[None, :] >= np.arange(128)[:, None], 0.0, -MASK).astype(np.float32)
    cmat = np.concatenate([ident, ones, blk2, rotT(64), rotT(32), tri], axis=1)
    tril = (np.arange(128)[:, None] <= np.arange(128)[None, :]).astype(np.float32)
    pm = np.zeros((16, 8), np.float32)
    for qs in range(16):
        cur = qs // 2
        for n in range(8):
            pm[qs, n] = 0.0 if n < cur else (BIGG if n == cur else -2 * BIGG)
    pmask = np.broadcast_to(pm.reshape(1, 128), (128, 128))
    p = np.arange(128)
    invf64 = (10000.0 ** (-(2.0 * (p % 32)) / 64.0)) / (2 * math.pi)
    invf32 = (10000.0 ** (-(2.0 * (p % 16)) / 32.0)) / (2 * math.pi)
    cf = np.concatenate([tril, pmask, invf64[:, None], invf32[:, None]], axis=1).astype(np.float32)
    ind = (-MASK) * (np.arange(SEQ)[None, :] // 256 == np.arange(8)[:, None]).astype(np.float32)
    return np.ascontiguousarray(cmat), np.ascontiguousarray(cf), np.ascontiguousarray(ind)


def _fm(v, c):
    return np.ascontiguousarray(np.asarray(v, np.float32).reshape(c, 128).T)


def _host_params(inp):
    vd = np.stack([np.stack([_fm(inp[k][l], 8) for k in ("pre_mix_norm", "post_mix_norm", "pre_ffn_norm", "post_ffn_norm", "pe_gate_norm")], 1) for l in range(L)], 1)
    vg = np.stack([np.stack([_fm(inp[k][l], 2) for k in ("conv_dw_b", "conv_ln_g", "conv_ln_b", "conv_pw_b", "out_norm_a", "out_norm_b", "out_norm_c")], 1) for l in range(L)], 1)
    wdw = np.stack([np.asarray(inp["conv_dw_w"][l], np.float32).reshape(31, 2, 128).transpose(2, 1, 0) for l in range(L)], 1)
    fcw = np.stack([np.asarray(inp["ffn_conv_w"][l], np.float32).reshape(3, 44, 128).transpose(2, 1, 0) for l in range(L)], 1)
    fcb = np.stack([_fm(inp["ffn_conv_b"][l], 44) for l in range(L)], 1)
    gsub = np.stack([np.asarray(inp["diff_subln_g"][l], np.float32)[np.arange(128) % 64] for l in range(L)], 1)
    lamv = np.stack([np.stack([np.asarray(inp[k][l], np.float32) for k in ("diff_lq1", "diff_lk1", "diff_lq2", "diff_lk2")], 0) for l in range(L)], 0)
    lamv = np.broadcast_to(lamv.reshape(1, L * 4 * 32), (128, L * 4 * 32))
    pv = np.concatenate([vd.reshape(128, -1), vg.reshape(128, -1), wdw.reshape(128, -1), fcw.reshape(128, -1), fcb.reshape(128, -1), gsub.reshape(128, -1), lamv], axis=1)
    glnb = np.stack([np.stack([np.broadcast_to(np.asarray(inp[k][l], np.float32)[None, :], (128, 256)) for k in ("gmlp_ln_g", "gmlp_ln_b")], 1) for l in range(L)], 0)
    bs = np.asarray(inp["gmlp_bs"], np.float32)
    gb = np.zeros((L, 2, 128, 512), np.float32)
    for l in range(L):
        for cc in range(2):
            for hh in range(2):
                gb[l, cc, hh * 64:(hh + 1) * 64, :] = np.tile(bs[l, 2 * cc + hh], 4)[None, :]
    wsT = np.ascontiguousarray(np.asarray(inp["gmlp_ws"], np.float32).transpose(0, 1, 3, 2))
    return np.ascontiguousarray(pv.astype(np.float32)), np.ascontiguousarray(glnb), gb, wsT


PV_OFF = {}


def _pv_layout():
    o = 0
    for name, n in (("vd", L * 5 * 8), ("vg", L * 7 * 2), ("wdw", L * 2 * 31), ("fcw", L * 44 * 3), ("fcb", L * 44), ("gsub", L), ("lamv", L * 4 * 32)):
        PV_OFF[name] = o
        o += n
    return o


PV_N = _pv_layout()


def build(dbg=None, nlayers=L):
    dbg = dbg or []
    nc = bass.Bass("TRN2", target_bir_lowering=False)

    def din(name, shape, dt=F32):
        return nc.dram_tensor(name, list(shape), dt, kind="ExternalInput").ap()

    xT_d = din("xT", [D, SEQ])
    pT_d = din("pT", [L, GW, SEQ])
    pos_d = din("pos", [1, SEQ], I32)
    cmat_d = din("cmat", [128, 768])
    cf_d = din("cf", [128, 258])
    ind_d = din("ind", [8, SEQ])
    pv_d = din("pv", [128, PV_N])
    glnb_d = din("glnb", [L, 128, 2, 256])
    gb_d = din("gb", [L, 2, 128, 512])
    wsT_d = din("wsT", [L, 4, 128, 128])
    w_in_d = din("w_in", [L, D, 10 * GW])
    w_out_d = din("w_out", [L, D, D])
    w_up_d = din("w_up", [L, D, 2 * DFF])
    w_down_d = din("w_down", [L, DFF, D])
    w_gate_d = din("w_pe_gate", [L, D, D])
    w_proj_d = din("w_pe_proj", [L, GW, D])
    w_pw_d = din("conv_pw_w", [L, GW, GW])
    out_d = nc.dram_tensor("outT", [D, SEQ], F32, kind="ExternalOutput").ap()
    dbg_d = {n: nc.dram_tensor("dbg_" + n, [128, w], F32, kind="ExternalOutput").ap() for n, w in dbg}

    es = ExitStack()
    with es:
        S = Sched(nc, es)
        NB = 204 * 1024
        big = es.enter_context(nc.sbuf_tensor("arena", [128, NB // 4], F32))
        A = Arena(big, NB)
        psb = [es.enter_context(nc.psum_tensor("ps%d" % i, [128, 512], F32)) for i in range(8)]
        PS = [(psb[i][:, :], ("ps", i)) for i in range(8)]
        ringA = Ring(PS[0:4])
        ringB = Ring(PS[4:8])

        def MM(out, lhsT, rhs, start, stop, r, w, inc=True, tp=None):
            kw = {}
            if tp is not None:
                kw["tile_position"] = tp
            S.op("pe", lambda: nc.tensor.matmul(out, lhsT=lhsT, rhs=rhs, start=start, stop=stop, **kw), r=r, w=w, inc=inc)

        def ACT(out, in_, func, r, w, scale=None, bias=None):
            kw = {}
            if scale is not None:
                kw["scale"] = scale
            if bias is not None:
                kw["bias"] = bias
            S.op("act", lambda: nc.scalar.activation(out=out, in_=in_, func=func, **kw), r=r, w=w)

        def TTo(eng, out, in0, in1, op, r, w):
            e = nc.vector if eng == "dve" else nc.gpsimd
            S.op(eng, lambda: e.tensor_tensor(out=out, in0=in0, in1=in1, op=op), r=r, w=w)

        def TS(eng, out, in0, s1, s2, op0, op1, r, w):
            e = nc.vector if eng == "dve" else nc.gpsimd
            if op1 is None:
                S.op(eng, lambda: e.tensor_scalar(out=out, in0=in0, scalar1=s1, scalar2=None, op0=op0), r=r, w=w)
            else:
                S.op(eng, lambda: e.tensor_scalar(out=out, in0=in0, scalar1=s1, scalar2=s2, op0=op0, op1=op1), r=r, w=w)

        def STT(out, in0, scalar, in1, op0, op1, r, w):
            S.op("dve", lambda: nc.vector.scalar_tensor_tensor(out=out, in0=in0, scalar=scalar, in1=in1, op0=op0, op1=op1), r=r, w=w)

        def CP(eng, out, in_, r, w):
            e = nc.vector if eng == "dve" else nc.gpsimd
            S.op(eng, lambda: e.tensor_copy(out=out, in_=in_), r=r, w=w)

        def MSET(eng, ap, val, w):
            e = nc.vector if eng == "dve" else nc.gpsimd
            S.op(eng, lambda: e.memset(ap, val), w=w)

        def DMA(q, out, in_, semkey, r, w):
            e = dict(sp=nc.sync, pool=nc.gpsimd, act=nc.scalar)[q]
            S.dma(q, lambda: e.dma_start(out=out, in_=in_), semkey, r=r, w=w)

        def dump(name, ap, keys):
            if name in dbg_d:
                w_ = ap.shape[-1]
                stg = A.f32(w_)
                CP("dve", stg[0:ap.shape[0], :], ap, r=keys, w=[("dbgs", name)])
                DMA("sp", dbg_d[name][0:ap.shape[0], 0:w_], stg[0:ap.shape[0], :], "dbg", r=[("dbgs", name)], w=[("dbgo", name)])

        xT = A.f32(8 * SEQ).rearrange("p (c s) -> p c s", c=8)
        hT = A.bf16(8 * SEQ).rearrange("p (c s) -> p c s", c=8)
        cmat = A.bf16(768)
        IDENT, ONES, BLK2, R64, R32, TRI = [cmat[:, i * 128:(i + 1) * 128] for i in range(6)]
        cf = A.f32(258)
        TRIL = cf[:, 0:128]
        PMASK = cf[:, 128:256].rearrange("p (q n) -> p q n", n=8)
        INVF = [cf[:, 256:257], cf[:, 257:258]]
        pv = A.f32(PV_N)

        def PVv(name, n):
            return pv[:, PV_OFF[name]:PV_OFF[name] + n]

        vd = PVv("vd", L * 5 * 8).rearrange("p (l v c) -> p l v c", l=L, v=5)
        vg = PVv("vg", L * 7 * 2).rearrange("p (l v c) -> p l v c", l=L, v=7)
        wdw = PVv("wdw", L * 2 * 31).rearrange("p (l c k) -> p l c k", l=L, c=2)
        fcw = PVv("fcw", L * 44 * 3).rearrange("p (l j k) -> p l j k", l=L, j=44)
        fcb = PVv("fcb", L * 44).rearrange("p (l j) -> p l j", l=L)
        gsub = PVv("gsub", L)
        lamv = PVv("lamv", L * 4 * 32).rearrange("p (l v k) -> p l v k", l=L, v=4)
        small = A.f32(64)
        lnv = A.f32(512)
        rstd = A.f32(512)
        sq = [A.bf16(512) for _ in range(3)]
        sqring = Ring([(sq[i], ("sq", i)) for i in range(3)])
        PH = A.mark()

        DMA("pool", cmat, cmat_d[:, :], "ld_c", r=[], w=["cmat"])
        DMA("sp", cf, cf_d[:, :], "ld_c2", r=[], w=["cf"])
        DMA("sp", pv, pv_d[:, :], "ld_c3", r=[], w=["pv"])
        for c in range(8):
            DMA("sp", xT[:, c, :], xT_d[c * 128:(c + 1) * 128, :], ("ld_x", c), r=[], w=[("xT", c, t) for t in range(NT)])

        def wload(dst, src, key):
            DMA("pool", dst, src, ("wsem",) + key, r=[], w=[key])

        def rstd_from(ps_ap, ps_key, inv_n, eps, extra_r=()):
            ACT(lnv, ps_ap, AF.Ln, r=[ps_key] + list(extra_r), w=["lnv"], scale=inv_n, bias=eps)
            ACT(rstd, lnv, AF.Exp, r=["lnv"], w=["rstd"], scale=-0.5)

        def rmsnorm_tile(srcs, skeys, dsts, dkeys, gains, n_feat, eps=EPS, lhs=None, lkey="cmat"):
            ps, pk = ringB.next()
            C = len(srcs)
            for c in range(C):
                q, qk = sqring.next()
                ACT(q, srcs[c], AF.Square, r=[skeys[c]], w=[qk])
                MM(ps, lhs if lhs is not None else ONES, q, c == 0, c == C - 1, r=[qk, lkey], w=[pk])
            rstd_from(ps, pk, 1.0 / n_feat, eps)
            for c in range(C):
                STT(dsts[c], srcs[c], gains[c], rstd, ALU.mult, ALU.mult, r=[skeys[c], "rstd", "pv"], w=[dkeys[c]])

        def sl(t):
            return slice(t * TW, (t + 1) * TW)

        def norm_x_to_h(l, vidx):
            for t in range(NT):
                rmsnorm_tile([xT[:, c, sl(t)] for c in range(8)], [("xT", c, t) for c in range(8)],
                             [hT[:, c, sl(t)] for c in range(8)], [("hT", c, t) for c in range(8)],
                             [vd[:, l, vidx, c:c + 1] for c in range(8)], D)

        def proj(ps, pk, wv, wkey, cs, rhs_fn, rkeys_fn, nk=8):
            for kc in range(nk):
                MM(ps, wv[:, kc, cs], rhs_fn(kc), kc == 0, kc == nk - 1, r=[wkey] + rkeys_fn(kc), w=[pk], inc=(kc == nk - 1))

        def hrhs(t):
            return (lambda kc: hT[:, kc, sl(t)]), (lambda kc: [("hT", kc, t)])

        for l in range(nlayers):
            S.barrier()
            A.release(PH)
            catT = A.bf16(8 * SEQ).rearrange("p (c s) -> p c s", c=8)
            ropeC = A.bf16(SEQ)
            ropeS = A.bf16(SEQ)
            wslots = [A.bf16(8 * 256).rearrange("p (k n) -> p k n", k=8) for _ in range(4)]
            MX = A.mark()
            winv = w_in_d[l].rearrange("(k p) n -> p k n", p=128)
            woutv = w_out_d[l].rearrange("(k p) n -> p k n", p=128)
            mplan = [(lambda v, k, bi=bi: wload(v, winv[:, :, bi * 256:(bi + 1) * 256], k)) for bi in (0, 1, 2, 7, 8, 9, 3, 4, 5, 6)]
            mplan += [(lambda v, k, db=db: wload(v, woutv[:, :, db * 256:(db + 1) * 256], k)) for _t in range(NT) for db in range(4)]
            mstream = WStream([(wslots[i], ("ws", i)) for i in range(4)], mplan, auto=False)

            def win_block(bi):
                return mstream.get()

            def rope_tables(which):
                m = A.mark()
                posi = A.i32(SEQ)
                v = A.f32(SEQ)
                ki = A.i32(SEQ)
                DMA("sp", posi, pos_d[0:1, :].broadcast_to([128, SEQ]), "ld_pos", r=[], w=["posi"])
                for tab, shift, key in ((ropeS, 0.0, "ropeS"), (ropeC, 0.25, "ropeC")):
                    TS("dve", v, posi, INVF[which], shift, ALU.mult, ALU.add, r=["posi", "cf"], w=["ropev"])
                    CP("dve", ki, v, r=["ropev"], w=["ropek"])
                    TTo("dve", v, v, ki, ALU.subtract, r=["ropev", "ropek"], w=["ropev"])
                    ACT(tab, v, AF.Sin, r=["ropev"], w=[key], scale=2 * math.pi * (1 - 1e-6))
                S.barrier()
                A.release(m)

            def rope_apply(ps, pk, t, RM, zbr, t1, t2):
                zb, zk = zbr.next()
                ACT(zb, ps, AF.Copy, r=[pk], w=[zk])
                ps2, pk2 = ringA.next()
                MM(ps2, RM, zb, True, True, r=[zk, "cmat"], w=[pk2])
                TTo("dve", t1[0], zb, ropeC[:, sl(t)], ALU.mult, r=[zk, "ropeC"], w=[t1[1]])
                TTo("dve", t2[0], ps2, ropeS[:, sl(t)], ALU.mult, r=[pk2, "ropeS"], w=[t2[1]])

            def attn_finalize_recip(O, ok, dst_rden):
                ACT(dst_rden[0][0:64, :], O[64:128, :], AF.Ln, r=[ok], w=[dst_rden[1]])
                ACT(dst_rden[0][0:64, :], dst_rden[0][0:64, :], AF.Exp, r=[dst_rden[1]], w=[dst_rden[1]], scale=-1.0)

            def vproj(wv, wk, cs, vaug):
                for g in range(4):
                    ps, pk = ringA.next()
                    for s4 in range(4):
                        st = g * 4 + s4
                        for kc in range(8):
                            MM(ps[:, s4 * 128:(s4 + 1) * 128], hT[:, kc, st * 128:(st + 1) * 128], wv[:, kc, cs], kc == 0, kc == 7,
                               r=[wk, ("hT", kc, st // 4)], w=[pk], inc=(kc == 7 and s4 == 3))
                    ACT(vaug[:, g * 4:(g + 1) * 4, :, 0:64], ps.rearrange("p (s h d) -> p s h d", s=4, h=2), AF.Copy, r=[pk], w=[("V", g)])

            norm_x_to_h(l, 0)
            if l == 0:
                dump("hT0", hT[:, 0, 0:512], [("hT", 0, 0)])

            rope_tables(0)
            m0 = A.mark()
            Kaug = [A.bf16(SEQ) for _ in range(2)]
            vaug = A.bf16(16 * 2 * 128).rearrange("p (s h d) -> p s h d", s=16, h=2)
            Qa = [[A.bf16(512) for _ in range(2)] for _ in range(2)]
            Pr = Ring([(A.bf16(512), ("P", i)) for i in range(4)])
            zbr = Ring([(A.bf16(512), ("zb", i)) for i in range(2)])
            t1 = (A.f32(512), "t1")
            t2 = (A.f32(512), "t2")
            rden = (A.f32(512), "rden")
            bstg4 = A.bf16(4 * 72).rearrange("p (j n) -> p j n", j=4)
            gm = A.f32(64)
            top8 = A.f32(32)
            kmf = A.f32(16)
            kmb = A.bf16(16)
            MSET("dve", vaug[:, :, :, 64:128], 1.0, w=[("V", g) for g in range(4)])
            MSET("dve", bstg4, 0.0, w=["bstg"])
            for hh in range(2):
                DMA("pool", Kaug[hh][64:72, :], ind_d[:, :], "ld_ind", r=[], w=[("Kind", hh)])
            wq, wqk = win_block(0)
            wk_, wkk = win_block(1)
            wv_, wvk = win_block(2)
            for c in range(2):
                cs = slice(c * 128, (c + 1) * 128)
                for t in range(NT):
                    ps, pk = ringA.next()
                    rf, kf = hrhs(t)
                    proj(ps, pk, wk_, wkk, cs, rf, kf)
                    rope_apply(ps, pk, t, R64, zbr, t1, t2)
                    for hh in range(2):
                        TTo("dve", Kaug[hh][0:64, sl(t)], t1[0][hh * 64:(hh + 1) * 64, :], t2[0][hh * 64:(hh + 1) * 64, :], ALU.add,
                            r=[t1[1], t2[1]], w=[("K", hh, t)])
                for hh in range(2):
                    S.op("dve", lambda hh=hh: nc.vector.tensor_reduce(out=kmf[0:64, hh * 8:(hh + 1) * 8], in_=Kaug[hh][0:64, :].rearrange("p (n k) -> p n k", k=256), axis=AX.X, op=ALU.add),
                         r=[("K", hh, t) for t in range(NT)], w=[("kmf", hh)])
                    TS("dve", kmb[0:64, hh * 8:(hh + 1) * 8], kmf[0:64, hh * 8:(hh + 1) * 8], 1.0 / 256, None, ALU.mult, None, r=[("kmf", hh)], w=[("kmb", hh)])
                vproj(wv_, wvk, cs, vaug)
                for qt in range(NT):
                    qb = Qa[qt % 2]
                    ps, pk = ringA.next()
                    rf, kf = hrhs(qt)
                    proj(ps, pk, wq, wqk, cs, rf, kf)
                    rope_apply(ps, pk, qt, R64, zbr, t1, t2)
                    for hh in range(2):
                        TTo("dve", qb[hh][0:64, :], t1[0][hh * 64:(hh + 1) * 64, :], t2[0][hh * 64:(hh + 1) * 64, :], ALU.add,
                            r=[t1[1], t2[1]], w=[("Qa", qt % 2, hh)])
                    for hh in range(2):
                        gps, gk = ringA.next()
                        for j in range(4):
                            MM(gps[:, j * 8:(j + 1) * 8], qb[hh][0:64, j * 128:(j + 1) * 128], kmb[0:64, hh * 8:(hh + 1) * 8], True, True,
                               r=[("Qa", qt % 2, hh), ("kmb", hh)], w=[gk], inc=(j == 3))
                        gmv = gm[:, 0:32].rearrange("p (j n) -> p j n", n=8)
                        TTo("dve", gmv, gps[:, 0:32].rearrange("p (j n) -> p j n", n=8), PMASK[:, qt * 4:(qt + 1) * 4, :], ALU.add, r=[gk, "cf"], w=["gm"])
                        tps, tk = ringA.next()
                        tpsb = tps.bitcast(BF16)
                        top8v = top8.rearrange("p (j n) -> p j n", n=8)
                        for j in range(4):
                            S.op("dve", lambda j=j: nc.vector.max(out=top8[:, j * 8:(j + 1) * 8], in_=gm[:, j * 8:(j + 1) * 8]), r=["gm"], w=["top8"])
                        TS("dve", top8v[:, :, 3:4], top8v[:, :, 3:4], -BIGG, None, ALU.max, None, r=["top8"], w=["top8"])
                        TTo("dve", bstg4[:, :, 64:72], gmv, top8v[:, :, 3:4].broadcast_to([128, 4, 8]), ALU.is_lt, r=["gm", "top8"], w=["bstg"])
                        for j in range(4):
                            S.op("pe", lambda j=j, tpsb=tpsb: nc.tensor.transpose(tpsb[0:72, j * 128:(j + 1) * 128], bstg4[:, j, :], IDENT), r=["bstg", "cmat"], w=[tk], inc=(j == 3))
                        ACT(qb[hh][64:72, :], tpsb[64:72, 0:512], AF.Copy, r=[tk], w=[("Qb", qt % 2, hh)])
                    for hh in range(2):
                        O, ok = ringB.next()
                        nk = 4 * qt + 4
                        pend = []
                        for kt in range(nk):
                            jd = kt - 4 * qt
                            c0 = 128 * jd if jd > 0 else 0
                            sp_, sk = ringA.next()
                            MM(sp_[:, c0:512], Kaug[hh][0:72, kt * 128:(kt + 1) * 128], qb[hh][0:72, c0:512], True, jd < 0,
                               r=[("K", hh, kt // 4), ("Kind", hh), ("Qa", qt % 2, hh), ("Qb", qt % 2, hh)], w=[sk], inc=(jd < 0))
                            if jd >= 0:
                                MM(sp_[:, c0:c0 + 128], IDENT, TRI, False, True, r=["cmat"], w=[sk])
                            P, Pk = Pr.next()
                            ACT(P[:, c0:512], sp_[:, c0:512], AF.Exp, r=[sk], w=[Pk], scale=0.125)

                            def pv_(kt=kt, c0=c0, P=P, Pk=Pk, O=O, ok=ok, hh=hh, nk=nk):
                                MM(O[:, c0:512], vaug[:, kt, hh, :], P[:, c0:512], kt == 0, kt == nk - 1, r=[Pk, ("V", kt // 4)], w=[ok])
                            pend.append(pv_)
                            if len(pend) > 2:
                                pend.pop(0)()
                        for f in pend:
                            f()
                        attn_finalize_recip(O, ok, rden)
                        TTo("dve", catT[hh * 64:(hh + 1) * 64, c, sl(qt)], O[0:64, :], rden[0][0:64, :], ALU.mult, r=[ok, rden[1]], w=[("cat", c, qt)])
            if l == 0:
                dump("oa_pre", catT[:, 0, 0:512], [("cat", 0, 0)])
            for t in range(NT):
                rmsnorm_tile([catT[:, c, sl(t)] for c in range(2)], [("cat", c, t) for c in range(2)],
                             [catT[:, c, sl(t)] for c in range(2)], [("cat", c, t) for c in range(2)],
                             [vg[:, l, 4, c:c + 1] for c in range(2)], GW)
            if l == 0:
                dump("oa", catT[:, 0, 0:512], [("cat", 0, 0)])
            mstream.done()
            S.barrier()
            A.release(m0)

            rope_tables(1)
            lam_init = 0.8 - 0.6 * math.exp(-0.3 * l)
            lt = small[:, 0:8]
            prod = A.f32(64)
            TTo("dve", prod[:, 0:32], lamv[:, l, 0, :], lamv[:, l, 1, :], ALU.mult, r=["pv"], w=["prod"])
            S.op("dve", lambda: nc.vector.tensor_reduce(out=lt[:, 0:1], in_=prod[:, 0:32], axis=AX.X, op=ALU.add), r=["prod"], w=["lt"])
            TTo("dve", prod[:, 32:64], lamv[:, l, 2, :], lamv[:, l, 3, :], ALU.mult, r=["pv"], w=["prod2"])
            S.op("dve", lambda: nc.vector.tensor_reduce(out=lt[:, 1:2], in_=prod[:, 32:64], axis=AX.X, op=ALU.add), r=["prod2"], w=["lt"])
            ACT(lt[:, 2:4], lt[:, 0:2], AF.Exp, r=["lt"], w=["lt"])
            TTo("dve", lt[:, 4:5], lt[:, 3:4], lt[:, 2:3], ALU.subtract, r=["lt"], w=["lt"])
            TS("dve", lt[:, 5:6], lt[:, 4:5], -lam_init, None, ALU.add, None, r=["lt"], w=["lt"])
            TS("dve", lt[:, 6:7], gsub[:, l:l + 1], 1.0 - lam_init, None, ALU.mult, None, r=["pv", "lt"], w=["lt"])
            NEGLAM = lt[:, 5:6]
            GS = lt[:, 6:7]

            m0 = A.mark()
            Kd = A.bf16(SEQ)
            vaug = A.bf16(16 * 2 * 128).rearrange("p (s h d) -> p s h d", s=16, h=2)
            Qd = [A.bf16(512) for _ in range(2)]
            Pr = Ring([(A.bf16(512), ("P", i)) for i in range(4)])
            zbr = Ring([(A.bf16(512), ("zb", i)) for i in range(2)])
            t1 = (A.f32(512), "t1")
            t2 = (A.f32(512), "t2")
            rden = (A.f32(512), "rden")
            od = A.f32(512)
            MSET("dve", vaug[:, :, :, 64:128], 1.0, w=[("V", g) for g in range(4)])
            wq, wqk = win_block(7)
            wk_, wkk = win_block(8)
            wv_, wvk = win_block(9)
            dscale = 32.0 ** -0.5
            for c in range(2):
                cs = slice(c * 128, (c + 1) * 128)
                for t in range(NT):
                    ps, pk = ringA.next()
                    rf, kf = hrhs(t)
                    proj(ps, pk, wk_, wkk, cs, rf, kf)
                    rope_apply(ps, pk, t, R32, zbr, t1, t2)
                    TTo("dve", Kd[:, sl(t)], t1[0], t2[0], ALU.add, r=[t1[1], t2[1]], w=[("Kd", t)])
                vproj(wv_, wvk, cs, vaug)
                for qt in range(NT):
                    qd = Qd[qt % 2]
                    ps, pk = ringA.next()
                    rf, kf = hrhs(qt)
                    proj(ps, pk, wq, wqk, cs, rf, kf)
                    rope_apply(ps, pk, qt, R32, zbr, t1, t2)
                    TTo("dve", qd, t1[0], t2[0], ALU.add, r=[t1[1], t2[1]], w=[("Qd", qt % 2)])
                    for hh in range(2):
                        Os = [ringB.next(), ringB.next()]
                        nk = 4 * qt + 4
                        pend = []
                        for kt in range(nk):
                            jd = kt - 4 * qt
                            c0 = 128 * jd if jd > 0 else 0
                            cur = []
                            for m_ in range(2):
                                g = hh * 2 + m_
                                sp_, sk = ringA.next()
                                MM(sp_[:, c0:512], Kd[32 * g:32 * g + 32, kt * 128:(kt + 1) * 128], qd[32 * g:32 * g + 32, c0:512], True, jd < 0,
                                   r=[("Kd", kt // 4), ("Qd", qt % 2)], w=[sk], inc=(jd < 0), tp=(32 * g, 0))
                                if jd >= 0:
                                    MM(sp_[:, c0:c0 + 128], IDENT, TRI, False, True, r=["cmat"], w=[sk])
                                cur.append((sp_, sk))
                            for f in pend:
                                f()
                            pend = []
                            for m_ in range(2):
                                sp_, sk = cur[m_]
                                P, Pk = Pr.next()
                                ACT(P[:, c0:512], sp_[:, c0:512], AF.Exp, r=[sk], w=[Pk], scale=dscale)

                                def pv_(kt=kt, c0=c0, P=P, Pk=Pk, Oo=Os[m_], hh=hh, nk=nk):
                                    MM(Oo[0][:, c0:512], vaug[:, kt, hh, :], P[:, c0:512], kt == 0, kt == nk - 1, r=[Pk, ("V", kt // 4)], w=[Oo[1]])
                                pend.append(pv_)
                        for f in pend:
                            f()
                        hs = slice(hh * 64, (hh + 1) * 64)
                        attn_finalize_recip(Os[0][0], Os[0][1], rden)
                        TTo("dve", t1[0][0:64, :], Os[0][0][0:64, :], rden[0][0:64, :], ALU.mult, r=[Os[0][1], rden[1]], w=[t1[1]])
                        attn_finalize_recip(Os[1][0], Os[1][1], rden)
                        TTo("dve", t2[0][0:64, :], Os[1][0][0:64, :], rden[0][0:64, :], ALU.mult, r=[Os[1][1], rden[1]], w=[t2[1]])
                        STT(od[hs, :], t2[0][0:64, :], NEGLAM[0:64, :], t1[0][0:64, :], ALU.mult, ALU.add, r=[t1[1], t2[1], "lt"], w=[("od", hh)])
                    ps, pk = ringB.next()
                    q_, qk_ = sqring.next()
                    ACT(q_, od, AF.Square, r=[("od", 0), ("od", 1)], w=[qk_])
                    MM(ps, BLK2, q_, True, True, r=[qk_, "cmat"], w=[pk])
                    rstd_from(ps, pk, 1.0 / 64, 1e-5)
                    STT(catT[:, 6 + c, sl(qt)], od, GS, rstd, ALU.mult, ALU.mult, r=[("od", 0), ("od", 1), "rstd", "lt"], w=[("cat", 6 + c, qt)])
            if l == 0:
                dump("od", catT[:, 6, 0:512], [("cat", 6, 0)])
            mstream.done()
            S.barrier()
            A.release(m0)

            m0 = A.mark()
            uT = A.bf16(2 * SEQ).rearrange("p (c s) -> p c s", c=2)
            vgl = A.bf16(16 * 256).rearrange("p (n d) -> p n d", n=16)
            glnb = A.f32(512).rearrange("p (v d) -> p v d", v=2)
            gb = A.f32(1024).rearrange("p (c s) -> p c s", c=2)
            wsf = A.f32(512).rearrange("p (h i) -> p h i", h=4)
            wsb = A.bf16(512).rearrange("p (h i) -> p h i", h=4)
            stats = A.f32(16 * 6)
            mv = A.f32(16 * 2).rearrange("p (n k) -> p n k", k=2)
            rsd = A.f32(16)
            vtmp = A.f32(256)
            vln = [A.bf16(256) for _ in range(2)]
            stmp = A.f32(512)
            DMA("sp", glnb, glnb_d[l], "ld_g1", r=[], w=["glnb"])
            DMA("sp", gb, gb_d[l].rearrange("c p s -> p c s"), "ld_g2", r=[], w=["gb"])
            DMA("sp", wsf, wsT_d[l].rearrange("h j i -> j h i"), "ld_g3", r=[], w=["wsf"])
            for h in range(4):
                TTo("dve", wsb[:, h, :], wsf[:, h, :], TRIL, ALU.mult, r=["wsf", "cf"], w=["wsb"])
            wu, wuk = win_block(3)
            wvv, wvvk = win_block(4)
            for t in range(NT):
                for cc in range(2):
                    ps, pk = ringA.next()
                    rf, kf = hrhs(t)
                    proj(ps, pk, wu, wuk, slice(cc * 128, (cc + 1) * 128), rf, kf)
                    ACT(uT[:, cc, sl(t)], ps, AF.Gelu_apprx_tanh, r=[pk], w=[("uT", cc, t)])
            for n2 in range(8):
                ps, pk = ringA.next()
                for s2 in range(2):
                    n = n2 * 2 + s2
                    for kc in range(8):
                        MM(ps[:, s2 * 256:(s2 + 1) * 256], hT[:, kc, n * 128:(n + 1) * 128], wvv[:, kc, :], kc == 0, kc == 7,
                           r=[wvvk, ("hT", kc, n // 4)], w=[pk], inc=(kc == 7 and s2 == 1))
                ACT(vgl[:, n2 * 2:(n2 + 1) * 2, :], ps.rearrange("p (s d) -> p s d", s=2), AF.Gelu_apprx_tanh, r=[pk], w=[("vgl", n2)])
            for n in range(16):
                S.op("dve", lambda n=n: nc.vector.bn_stats(out=stats[:, n * 6:(n + 1) * 6], in_=vgl[:, n, :]), r=[("vgl", n // 2)], w=[("st", n)])
                S.op("dve", lambda n=n: nc.vector.bn_aggr(out=mv[:, n, :], in_=stats[:, n * 6:(n + 1) * 6]), r=[("st", n)], w=["mv"])
            ACT(rsd, mv[:, :, 1], AF.Ln, r=["mv"], w=["rsd"], bias=EPS)
            ACT(rsd, rsd, AF.Exp, r=["rsd"], w=["rsd"], scale=-0.5)
            for t in range(NT):
                pss = [ringA.next(), ringA.next()]
                for s4 in range(4):
                    n = t * 4 + s4
                    vl = vln[n % 2]
                    vk = ("vln", n % 2)
                    TS("dve", vtmp, vgl[:, n, :], mv[:, n, 0:1], rsd[:, n:n + 1], ALU.subtract, ALU.mult, r=[("vgl", n // 2), "mv", "rsd"], w=["vtmp"])
                    TTo("dve", vtmp, vtmp, glnb[:, 0, :], ALU.mult, r=["vtmp", "glnb"], w=["vtmp"])
                    TTo("dve", vl, vtmp, glnb[:, 1, :], ALU.add, r=["vtmp", "glnb"], w=[vk])
                    for cc in range(2):
                        for hh in range(2):
                            h = 2 * cc + hh
                            MM(pss[cc][0][hh * 64:(hh + 1) * 64, s4 * 128:(s4 + 1) * 128], vl[:, h * 64:(h + 1) * 64], wsb[:, h, :], True, True,
                               r=[vk, "wsb"], w=[pss[cc][1]], tp=(0, hh * 64))
                for cc in range(2):
                    TTo("dve", stmp, pss[cc][0], gb[:, cc, :], ALU.add, r=[pss[cc][1], "gb"], w=["stmp"])
                    TTo("dve", catT[:, 2 + cc, sl(t)], stmp, uT[:, cc, sl(t)], ALU.mult, r=["stmp", ("uT", cc, t)], w=[("cat", 2 + cc, t)])
            if l == 0:
                dump("ob_pre", catT[:, 2, 0:512], [("cat", 2, 0)])
            for t in range(NT):
                rmsnorm_tile([catT[:, 2 + c, sl(t)] for c in range(2)], [("cat", 2 + c, t) for c in range(2)],
                             [catT[:, 2 + c, sl(t)] for c in range(2)], [("cat", 2 + c, t) for c in range(2)],
                             [vg[:, l, 5, c:c + 1] for c in range(2)], GW)
            mstream.done()
            S.barrier()
            A.release(m0)

            m0 = A.mark()
            ybuf = A.bf16(32 + SEQ)
            diag = A.bf16(31 * 128).rearrange("p (k n) -> p k n", k=31)
            cy = A.bf16(2 * SEQ).rearrange("p (c s) -> p c s", c=2)
            sg = A.f32(512)
            wpw = A.bf16(2 * 256).rearrange("p (k n) -> p k n", k=2)
            mstat = A.f32(512)
            m2 = A.f32(512)
            yn = A.f32(512)
            sil = A.bf16(1024).rearrange("p (c s) -> p c s", c=2)
            Y0 = 2
            wa, wak = win_block(5)
            wg_, wgk = win_block(6)
            wload(wpw, w_pw_d[l].rearrange("(k p) n -> p k n", p=128), ("wpw",))
            MSET("dve", ybuf[:, 0:32], 0.0, w=["ypad"])
            for cc in range(2):
                cs = slice(cc * 128, (cc + 1) * 128)
                for k in range(31):
                    TS("dve", diag[:, k, :], IDENT, wdw[:, l, cc, k:k + 1], None, ALU.mult, None, r=["cmat", "pv"], w=[("diag", k)])
                for t in range(NT):
                    pa, pak = ringA.next()
                    rf, kf = hrhs(t)
                    proj(pa, pak, wa, wak, cs, rf, kf)
                    pg, pgk = ringA.next()
                    proj(pg, pgk, wg_, wgk, cs, rf, kf)
                    ACT(sg, pg, AF.Sigmoid, r=[pgk], w=["sg"])
                    TTo("dve", ybuf[:, 32 + t * TW:32 + (t + 1) * TW], pa, sg, ALU.mult, r=[pak, "sg"], w=[("yb", t)])
                for t in range(NT):
                    ps, pk = ringB.next()
                    for k in range(31):
                        o = Y0 + t * TW + k
                        rk = ["ypad", ("diag", k), ("yb", t)] + ([("yb", t - 1)] if t > 0 else [])
                        MM(ps, diag[:, k, :], ybuf[:, o:o + TW], k == 0, k == 30, r=rk, w=[pk], inc=(k == 30))
                    ACT(cy[:, cc, sl(t)], ps, AF.Identity, r=[pk, "pv"], w=[("cy", cc, t)], bias=vg[:, l, 0, cc:cc + 1])
            if l == 0:
                dump("cy", cy[:, 0, 0:512], [("cy", 0, 0)])
            for t in range(NT):
                ps1, pk1 = ringB.next()
                ps2, pk2 = ringB.next()
                for cc in range(2):
                    MM(ps1, ONES, cy[:, cc, sl(t)], cc == 0, cc == 1, r=[("cy", cc, t), "cmat"], w=[pk1])
                for cc in range(2):
                    q_, qk_ = sqring.next()
                    ACT(q_, cy[:, cc, sl(t)], AF.Square, r=[("cy", cc, t)], w=[qk_])
                    MM(ps2, ONES, q_, cc == 0, cc == 1, r=[qk_, "cmat"], w=[pk2])
                TS("dve", mstat, ps1, 1.0 / GW, None, ALU.mult, None, r=[pk1], w=["mstat"])
                TTo("dve", m2, mstat, mstat, ALU.mult, r=["mstat"], w=["m2"])
                STT(m2, ps2, 1.0 / GW, m2, ALU.mult, ALU.subtract, r=[pk2, "m2"], w=["m2"])
                ACT(lnv, m2, AF.Ln, r=["m2"], w=["lnv"], bias=EPS)
                ACT(rstd, lnv, AF.Exp, r=["lnv"], w=["rstd"], scale=-0.5)
                for cc in range(2):
                    TTo("dve", yn, cy[:, cc, sl(t)], mstat, ALU.subtract, r=[("cy", cc, t), "mstat"], w=["yn"])
                    TTo("dve", yn, yn, rstd, ALU.mult, r=["yn", "rstd"], w=["yn"])
                    ACT(sil[:, cc, :], yn, AF.Silu, r=["yn", "pv"], w=[("sil", cc)], scale=vg[:, l, 1, cc:cc + 1], bias=vg[:, l, 2, cc:cc + 1])
                for co in range(2):
                    ps, pk = ringA.next()
                    for ci in range(2):
                        MM(ps, wpw[:, ci, co * 128:(co + 1) * 128], sil[:, ci, :], ci == 0, ci == 1, r=[("wpw",), ("sil", ci)], w=[pk])
                    ACT(catT[:, 4 + co, sl(t)], ps, AF.Identity, r=[pk, "pv"], w=[("cat", 4 + co, t)], bias=vg[:, l, 3, co:co + 1])
            if l == 0:
                dump("oc_pre", catT[:, 4, 0:512], [("cat", 4, 0)])
            for t in range(NT):
                rmsnorm_tile([catT[:, 4 + c, sl(t)] for c in range(2)], [("cat", 4 + c, t) for c in range(2)],
                             [catT[:, 4 + c, sl(t)] for c in range(2)], [("cat", 4 + c, t) for c in range(2)],
                             [vg[:, l, 6, c:c + 1] for c in range(2)], GW)
            mstream.done()
            S.barrier()
            A.release(m0)

            def out_proj_norm_res(nkc, wsrc, rhs_fn, rkeys_fn, vidx, wr, ytile):
                for t in range(NT):
                    for db in range(4):
                        wv, wk = wr.get()
                        for dd in range(2):
                            dc = db * 2 + dd
                            ps, pk = ringA.next()
                            for kc in range(nkc):
                                MM(ps, wv[:, kc, dd * 128:(dd + 1) * 128], rhs_fn(kc, t), kc == 0, kc == nkc - 1, r=[wk] + rkeys_fn(kc, t), w=[pk], inc=(kc == nkc - 1))
                            if dc % 2 == 0:
                                ACT(ytile[:, dc, :], ps, AF.Copy, r=[pk], w=[("yt", dc)])
                            else:
                                CP("dve", ytile[:, dc, :], ps, r=[pk], w=[("yt", dc)])
                        wr.done()
                    rmsnorm_tile([ytile[:, dc, :] for dc in range(8)], [("yt", dc) for dc in range(8)],
                                 [ytile[:, dc, :] for dc in range(8)], [("yt", dc) for dc in range(8)],
                                 [vd[:, l, vidx, dc:dc + 1] for dc in range(8)], D)
                    for dc in range(8):
                        TTo("dve", xT[:, dc, sl(t)], xT[:, dc, sl(t)], ytile[:, dc, :], ALU.add, r=[("xT", dc, t), ("yt", dc)], w=[("xT", dc, t)])

            m0 = A.mark()
            ytile = A.f32(8 * 512).rearrange("p (c s) -> p c s", c=8)
            out_proj_norm_res(8, w_out_d[l].rearrange("(k p) n -> p k n", p=128), lambda kc, t: catT[:, kc, sl(t)], lambda kc, t: [("cat", kc, t)], 1, mstream, ytile)
            if l == 0:
                dump("x1", xT[:, 0, 0:512], [("xT", 0, 0)])
            S.barrier()
            A.release(MX)
            A.release(PH)

            norm_x_to_h(l, 2)
            ytile = A.f32(8 * 512).rearrange("p (c s) -> p c s", c=8)
            fT = A.bf16(NJ * 512).rearrange("p (j s) -> p j s", j=NJ)
            ub = [[A.f32(2 + 512) for _ in range(2)] for _ in range(2)]
            halo = A.f32(44 * 2).rearrange("p (j k) -> p j k", k=2)
            tas = [A.f32(512) for _ in range(2)]
            tbs = [A.f32(512) for _ in range(2)]
            cgs = [A.f32(512) for _ in range(2)]
            cvs = [A.f32(512) for _ in range(2)]
            wups = [A.bf16(8 * 2 * 128).rearrange("p (k g n) -> p k g n", k=8, g=2) for _ in range(3)]
            wdns = [A.bf16(NJ * 128).rearrange("p (j n) -> p j n", j=NJ) for _ in range(3)]
            MSET("dve", halo, 0.0, w=["halo"])
            wupv = w_up_d[l].rearrange("(k p) (g n) -> p k g n", p=128, g=2)
            wdnv = w_down_d[l].rearrange("(j p) n -> p j n", p=128)

            def ld_up(v, k, j):
                for gi_ in range(2):
                    wload(v[:, :, gi_, :], wupv[:, :, gi_, j * 128:(j + 1) * 128], k)

            wur = WStream([(wups[i], ("wu", i)) for i in range(3)], [(lambda v, k, j=j: ld_up(v, k, j)) for _t in range(NT) for j in range(NJ)])
            wdr = WStream([(wdns[i], ("wd", i)) for i in range(3)], [(lambda v, k, dc=dc: wload(v, wdnv[:, :, dc * 128:(dc + 1) * 128], k)) for _t in range(NT) for dc in range(8)])
            wur.top()
            wdr.top()
            pend_f = None
            for t in range(NT):
                for j in range(NJ):
                    wv, wk = wur.get()
                    u = ub[j % 2]
                    b2 = j % 2
                    ta, tb, cg, cv = tas[b2], tbs[b2], cgs[b2], cvs[b2]
                    for gi in range(2):
                        jj = j + gi * NJ
                        ps, pk = ringA.next()
                        for kc in range(8):
                            MM(ps, wv[:, kc, gi, :], hT[:, kc, sl(t)], kc == 0, kc == 7, r=[wk, ("hT", kc, t)], w=[pk], inc=(kc == 7))
                        uk = ("ub", b2, gi)
                        ACT(u[gi][:, 2:514], ps, AF.Copy, r=[pk], w=[uk])
                        CP("dve", u[gi][:, 0:2], halo[:, jj, :], r=["halo", ("halo", jj)], w=[uk])
                        tmp = ta if gi == 0 else tb
                        tk_ = ("ta", b2) if gi == 0 else ("tb", b2)
                        ACT(tmp, ps, AF.Identity, r=[pk, "pv"], w=[tk_], scale=fcw[:, l, jj, 2:3], bias=fcb[:, l, jj:jj + 1])
                        ACT(halo[:, jj, :], ps[:, 510:512], AF.Copy, r=[pk], w=[("halo", jj)])
                        dst = cg if gi == 0 else cv
                        dk = ("cg", b2) if gi == 0 else ("cv", b2)
                        STT(tmp, u[gi][:, 1:513], fcw[:, l, jj, 1:2], tmp, ALU.mult, ALU.add, r=[uk, tk_, "pv"], w=[tk_])
                        STT(dst, u[gi][:, 0:512], fcw[:, l, jj, 0:1], tmp, ALU.mult, ALU.add, r=[uk, tk_, "pv"], w=[dk])
                    if pend_f is not None:
                        pend_f()

                    def fin(j=j, b2=b2, cg=cg, cv=cv):
                        ACT(cg, cg, AF.Gelu_apprx_tanh, r=[("cg", b2)], w=[("cg", b2)])
                        TTo("dve", fT[:, j, :], cg, cv, ALU.mult, r=[("cg", b2), ("cv", b2)], w=[("fT", j)])
                    pend_f = fin
                pend_f()
                pend_f = None
                if l == 0 and t == 0:
                    dump("fT", fT[:, 0, :], [("fT", 0)])
                for dc in range(8):
                    wv, wk = wdr.get()
                    ps, pk = ringB.next()
                    for j in range(NJ):
                        MM(ps, wv[:, j, :], fT[:, j, :], j == 0, j == NJ - 1, r=[wk, ("fT", j)], w=[pk], inc=(j == NJ - 1))
                    if dc % 2 == 0:
                        ACT(ytile[:, dc, :], ps, AF.Copy, r=[pk], w=[("yt", dc)])
                    else:
                        CP("dve", ytile[:, dc, :], ps, r=[pk], w=[("yt", dc)])
                rmsnorm_tile([ytile[:, dc, :] for dc in range(8)], [("yt", dc) for dc in range(8)],
                             [ytile[:, dc, :] for dc in range(8)], [("yt", dc) for dc in range(8)],
                             [vd[:, l, 3, dc:dc + 1] for dc in range(8)], D)
                for dc in range(8):
                    TTo("dve", xT[:, dc, sl(t)], xT[:, dc, sl(t)], ytile[:, dc, :], ALU.add, r=[("xT", dc, t), ("yt", dc)], w=[("xT", dc, t)])
            if l == 0:
                dump("x2", xT[:, 0, 0:512], [("xT", 0, 0)])
            S.barrier()
            A.release(PH)

            norm_x_to_h(l, 4)
            pTb = A.bf16(2 * SEQ).rearrange("p (c s) -> p c s", c=2)
            wgs = [A.bf16(8 * 256).rearrange("p (k n) -> p k n", k=8) for _ in range(3)]
            wpj = A.bf16(2 * D).rearrange("p (k n) -> p k n", k=2)
            sgt = [A.f32(512) for _ in range(2)]
            pjt = [A.f32(512) for _ in range(2)]
            wload(pTb, pT_d[l].rearrange("(k p) s -> p k s", p=128), ("pTb",))
            wload(wpj, w_proj_d[l].rearrange("(k p) n -> p k n", p=128), ("wpj",))
            wgv = w_gate_d[l].rearrange("(k p) n -> p k n", p=128)
            wgr = WStream([(wgs[i], ("wg", i)) for i in range(3)], [(lambda v, k, db=db: wload(v, wgv[:, :, db * 256:(db + 1) * 256], k)) for db in range(4)])
            wgr.top()
            it = 0
            for db in range(4):
                wv, wk = wgr.get()
                for dd in range(2):
                    dc = db * 2 + dd
                    for t in range(NT):
                        b = it % 2
                        it += 1
                        ps, pk = ringA.next()
                        rf, kf = hrhs(t)
                        proj(ps, pk, wv, wk, slice(dd * 128, (dd + 1) * 128), rf, kf)
                        ACT(sgt[b], ps, AF.Sigmoid, r=[pk], w=[("sgt", b)])
                        ps2, pk2 = ringB.next()
                        for kc in range(2):
                            MM(ps2, wpj[:, kc, dc * 128:(dc + 1) * 128], pTb[:, kc, sl(t)], kc == 0, kc == 1, r=[("wpj",), ("pTb",)], w=[pk2], inc=(kc == 1))
                        TTo("dve", pjt[b], ps2, sgt[b], ALU.mult, r=[pk2, ("sgt", b)], w=[("pjt", b)])
                        TTo("dve", xT[:, dc, sl(t)], xT[:, dc, sl(t)], pjt[b], ALU.add, r=[("xT", dc, t), ("pjt", b)], w=[("xT", dc, t)])
            if l == 0:
                dump("x3", xT[:, 0, 0:512], [("xT", 0, 0)])

        for c in range(8):
            DMA("sp", out_d[c * 128:(c + 1) * 128, :], xT[:, c, :], "st_out", r=[("xT", c, t) for t in range(NT)], w=[("out", c)])
        S.barrier()
        S.emit()
        build.arena_hi = A.hi
    return nc


_CACHE = {}


def _prep_inputs(inp):
    cmat, cf, ind = _host_consts()
    pv, glnb, gb, wsT = _host_params(inp)
    x = np.asarray(inp["x"], np.float32)
    p = np.asarray(inp["p"], np.float32)
    pos = np.asarray(inp["positions"], np.int32)
    shared = dict(cmat=cmat, cf=cf, ind=ind, pv=pv, glnb=glnb, gb=gb, wsT=wsT)
    for k in ("w_in", "w_out", "w_up", "w_down", "w_pe_gate", "w_pe_proj", "conv_pw_w"):
        shared[k] = np.ascontiguousarray(np.asarray(inp[k], np.float32))
    maps = []
    for b in range(8):
        m = dict(shared)
        m["xT"] = np.ascontiguousarray(x[b].T)
        m["pT"] = np.ascontiguousarray(p[:, b].transpose(0, 2, 1))
        m["pos"] = np.ascontiguousarray(pos[b][None, :])
        maps.append(m)
    return maps


def kernel(**inputs):
    if "nc" not in _CACHE:
        _CACHE["nc"] = build()
    nc = _CACHE["nc"]
    maps = _prep_inputs(inputs)
    res = run_bass_kernel_spmd(nc, maps, core_ids=list(range(8)))
    out = np.stack([np.asarray(r["outT"], np.float32).T for r in res.results], axis=0)
    return np.ascontiguousarray(out)
```

```python
import math
import numpy as np
from contextlib import ExitStack
import concourse.bass as bass
import concourse.mybir as mybir
from concourse.bass_utils import run_bass_kernel_spmd

F32 = mybir.dt.float32
BF16 = mybir.dt.bfloat16
I32 = mybir.dt.int32
AF = mybir.ActivationFunctionType
ALU = mybir.AluOpType
AX = mybir.AxisListType

L = 2
D = 1024
SEQ = 2048
GW = 256
DFF = 2816
NJ = DFF // 128
NT = 4
TW = 512
MASK = 30000.0
BIGG = 1.0e6
EPS = 1e-6
ENGS = ["pe", "act", "dve", "pool", "sp"]


class Sched:
    def __init__(self, nc, es, same_engine_sync=True):
        self.nc = nc
        self.es = es
        self.same = same_engine_sync
        self.prog = {e: [] for e in ENGS}
        self.cnt = {}
        self.sems = {}
        self.seen = {e: {} for e in ENGS}
        self.state = {}
        for e in ENGS:
            self._sem(e)

    def _sem(self, key):
        if key not in self.sems:
            self.sems[key] = self.es.enter_context(self.nc.semaphore("s_" + str(key)))
            self.cnt[key] = 0
        return self.sems[key]

    def _engobj(self, e):
        nc = self.nc
        return dict(pe=nc.tensor, act=nc.scalar, dve=nc.vector, pool=nc.gpsimd, sp=nc.sync)[e]

    def _deps(self, e, r, w):
        toks = {}
        for k in r:
            st = self.state.get(k)
            if st and st[0] is not None:
                t = st[0]
                toks[t[0]] = max(toks.get(t[0], 0), t[1])
        for k in w:
            st = self.state.get(k)
            if st:
                if st[0] is not None:
                    t = st[0]
                    toks[t[0]] = max(toks.get(t[0], 0), t[1])
                for t in st[1]:
                    toks[t[0]] = max(toks.get(t[0], 0), t[1])
        waits = []
        for sk, v in toks.items():
            if sk == e and (not self.same or e == "pe"):
                continue
            if self.seen[e].get(sk, 0) >= v:
                continue
            self.seen[e][sk] = v
            waits.append((sk, v))
        return waits

    def _record(self, tok, r, w):
        for k in r:
            st = self.state.setdefault(k, [None, []])
            st[1].append(tok)
        for k in w:
            self.state[k] = [tok, []]

    def op(self, e, fn, r=(), w=(), inc=True):
        waits = self._deps(e, r, w)
        tok = (e, self.cnt[e] + 1)
        if inc:
            self.cnt[e] += 1
        self._record(tok, r, w)
        self.prog[e].append((waits, fn, (e, 1) if inc else None))
        return tok

    def dma(self, q, fn, semkey, r=(), w=()):
        self._sem(semkey)
        waits = self._deps(q, r, w)
        self.cnt[semkey] += 16
        tok = (semkey, self.cnt[semkey])
        self._record(tok, r, w)
        self.prog[q].append((waits, fn, (semkey, 16)))
        return tok

    def barrier(self):
        for e in ENGS:
            waits = []
            for sk, v in self.cnt.items():
                if v == 0 or self.seen[e].get(sk, 0) >= v:
                    continue
                if sk == e and e == "pe":
                    continue
                self.seen[e][sk] = v
                waits.append((sk, v))
            self.prog[e].append((waits, None, None))

    def simulate(self):
        val = {k: 0 for k in self.sems}
        pc = {e: 0 for e in ENGS}
        progress = True
        while progress:
            progress = False
            for e in ENGS:
                while pc[e] < len(self.prog[e]):
                    waits, fn, inc = self.prog[e][pc[e]]
                    if any(val[sk] < v for sk, v in waits):
                        break
                    if inc is not None:
                        val[inc[0]] += inc[1]
                    pc[e] += 1
                    progress = True
        stuck = {e: (pc[e], len(self.prog[e])) for e in ENGS if pc[e] < len(self.prog[e])}
        if stuck:
            msg = []
            for e, (i, n) in stuck.items():
                waits = self.prog[e][i][0]
                msg.append("%s@%d/%d waits %s" % (e, i, n, [(sk, v, val[sk]) for sk, v in waits if val[sk] < v]))
            raise RuntimeError("DEADLOCK in schedule: " + "; ".join(msg))
        for k in self.sems:
            assert val[k] == self.cnt[k], (k, val[k], self.cnt[k])

    def emit(self):
        self.simulate()
        nc = self.nc
        with nc.Block() as block:
            def run(e):
                eng = self._engobj(e)
                for waits, fn, inc in self.prog[e]:
                    for sk, v in waits:
                        eng.wait_ge(self.sems[sk], v)
                    if fn is None:
                        continue
                    ins = fn()
                    if inc is not None:
                        ins.then_inc(self.sems[inc[0]], inc[1])

            @block.tensor
            def _(e):
                run("pe")

            @block.scalar
            def _(e):
                run("act")

            @block.vector
            def _(e):
                run("dve")

            @block.gpsimd
            def _(e):
                run("pool")

            @block.sync
            def _(e):
                run("sp")


class Arena:
    def __init__(self, t, nbytes):
        self.t = t
        self.n = nbytes
        self.off = 0
        self.hi = 0

    def mark(self):
        return self.off

    def release(self, m):
        self.off = m

    def _take(self, nbytes):
        nbytes = (nbytes + 31) // 32 * 32
        o = self.off
        self.off += nbytes
        self.hi = max(self.hi, self.off)
        assert self.off <= self.n, ("arena overflow", self.off, self.n)
        return o

    def f32(self, n):
        o = self._take(4 * n)
        return self.t[:, o // 4: o // 4 + n]

    def bf16(self, n):
        o = self._take(2 * n)
        return self.t[:, o // 4: o // 4 + (n + 1) // 2].bitcast(BF16)

    def i32(self, n):
        o = self._take(4 * n)
        return self.t[:, o // 4: o // 4 + n].bitcast(I32)


class Ring:
    def __init__(self, items):
        self.items = items
        self.i = 0

    def next(self):
        it = self.items[self.i % len(self.items)]
        self.i += 1
        return it


class WStream:
    def __init__(self, slots, plan, auto=True):
        self.slots = slots
        self.plan = plan
        self.auto = auto
        self.issued = 0
        self.used = 0
        self.freed = 0

    def top(self):
        while self.issued < len(self.plan) and self.issued - self.freed < len(self.slots):
            i = self.issued
            v, k = self.slots[i % len(self.slots)]
            self.plan[i](v, k)
            self.issued += 1

    def done(self):
        self.freed = self.used
        self.top()

    def get(self):
        i = self.used
        if self.auto:
            self.freed = i
        self.top()
        assert self.issued > i, "weight stream: block not issued (slots exhausted)"
        self.used += 1
        return self.slots[i % len(self.slots)]


def _host_consts():
    ident = np.eye(128, dtype=np.float32)
    ones = np.ones((128, 128), np.float32)
    blk2 = np.zeros((128, 128), np.float32)
    blk2[0:64, 0:64] = 1
    blk2[64:128, 64:128] = 1

    def rotT(block):
        h = block // 2
        m = np.zeros((128, 128), np.float32)
        for b0 in range(0, 128, block):
            for i in range(h):
                m[b0 + i + h, b0 + i] = -1.0
                m[b0 + i, b0 + i + h] = 1.0
        return m

    tri = np.where(np.arange(128)[None, :] >= np.arange(128)[:, None], 0.0, -MASK).astype(np.float32)
    cmat = np.concatenate([ident, ones, blk2, rotT(64), rotT(32), tri], axis=1)
    tril = (np.arange(128)[:, None] <= np.arange(128)[None, :]).astype(np.float32)
    pm = np.zeros((16, 8), np.float32)
    for qs in range(16):
        cur = qs // 2
        for n in range(8):
            pm[qs, n] = 0.0 if n < cur else (BIGG if n == cur else -2 * BIGG)
    pmask = np.broadcast_to(pm.reshape(1, 128), (128, 128))
    p = np.arange(128)
    invf64 = (10000.0 ** (-(2.0 * (p % 32)) / 64.0)) / (2 * math.pi)
    invf32 = (10000.0 ** (-(2.0 * (p % 16)) / 32.0)) / (2 * math.pi)
    cf = np.concatenate([tril, pmask, invf64[:, None], invf32[:, None]], axis=1).astype(np.float32)
    ind = (-MASK) * (np.arange(SEQ)[None, :] // 256 == np.arange(8)[:, None]).astype(np.float32)
    return np.ascontiguousarray(cmat), np.ascontiguousarray(cf), np.ascontiguousarray(ind)


def _fm(v, c):
    return np.ascontiguousarray(np.asarray(v, np.float32).reshape(c, 128).T)


def _host_params(inp):
    vd = np.stack([np.stack([_fm(inp[k][l], 8) for k in ("pre_mix_norm", "post_mix_norm", "pre_ffn_norm", "post_ffn_norm", "pe_gate_norm")], 1) for l in range(L)], 1)
    vg = np.stack([np.stack([_fm(inp[k][l], 2) for k in ("conv_dw_b", "conv_ln_g", "conv_ln_b", "conv_pw_b", "out_norm_a", "out_norm_b", "out_norm_c")], 1) for l in range(L)], 1)
    wdw = np.stack([np.asarray(inp["conv_dw_w"][l], np.float32).reshape(31, 2, 128).transpose(2, 1, 0) for l in range(L)], 1)
    fcw = np.stack([np.asarray(inp["ffn_conv_w"][l], np.float32).reshape(3, 44, 128).transpose(2, 1, 0) for l in range(L)], 1)
    fcb = np.stack([_fm(inp["ffn_conv_b"][l], 44) for l in range(L)], 1)
    gsub = np.stack([np.asarray(inp["diff_subln_g"][l], np.float32)[np.arange(128) % 64] for l in range(L)], 1)
    lamv = np.stack([np.stack([np.asarray(inp[k][l], np.float32) for k in ("diff_lq1", "diff_lk1", "diff_lq2", "diff_lk2")], 0) for l in range(L)], 0)
    lamv = np.broadcast_to(lamv.reshape(1, L * 4 * 32), (128, L * 4 * 32))
    pv = np.concatenate([vd.reshape(128, -1), vg.reshape(128, -1), wdw.reshape(128, -1), fcw.reshape(128, -1), fcb.reshape(128, -1), gsub.reshape(128, -1), lamv], axis=1)
    glnb = np.stack([np.stack([np.broadcast_to(np.asarray(inp[k][l], np.float32)[None, :], (128, 256)) for k in ("gmlp_ln_g", "gmlp_ln_b")], 1) for l in range(L)], 0)
    bs = np.asarray(inp["gmlp_bs"], np.float32)
    gb = np.zeros((L, 2, 128, 512), np.float32)
    for l in range(L):
        for cc in range(2):
            for hh in range(2):
                gb[l, cc, hh * 64:(hh + 1) * 64, :] = np.tile(bs[l, 2 * cc + hh], 4)[None, :]
    wsT = np.ascontiguousarray(np.asarray(inp["gmlp_ws"], np.float32).transpose(0, 1, 3, 2))
    return np.ascontiguousarray(pv.astype(np.float32)), np.ascontiguousarray(glnb), gb, wsT


PV_OFF = {}


def _pv_layout():
    o = 0
    for name, n in (("vd", L * 5 * 8), ("vg", L * 7 * 2), ("wdw", L * 2 * 31), ("fcw", L * 44 * 3), ("fcb", L * 44), ("gsub", L), ("lamv", L * 4 * 32)):
        PV_OFF[name] = o
        o += n
    return o


PV_N = _pv_layout()


def build(dbg=None, nlayers=L):
    dbg = dbg or []
    nc = bass.Bass("TRN2", target_bir_lowering=False)

    def din(name, shape, dt=F32):
        return nc.dram_tensor(name, list(shape), dt, kind="ExternalInput").ap()

    xT_d = din("xT", [D, SEQ])
    pT_d = din("pT", [L, GW, SEQ])
    pos_d = din("pos", [1, SEQ], I32)
    cmat_d = din("cmat", [128, 768])
    cf_d = din("cf", [128, 258])
    ind_d = din("ind", [8, SEQ])
    pv_d = din("pv", [128, PV_N])
    glnb_d = din("glnb", [L, 128, 2, 256])
    gb_d = din("gb", [L, 2, 128, 512])
    wsT_d = din("wsT", [L, 4, 128, 128])
    w_in_d = din("w_in", [L, D, 10 * GW])
    w_out_d = din("w_out", [L, D, D])
    w_up_d = din("w_up", [L, D, 2 * DFF])
    w_down_d = din("w_down", [L, DFF, D])
    w_gate_d = din("w_pe_gate", [L, D, D])
    w_proj_d = din("w_pe_proj", [L, GW, D])
    w_pw_d = din("conv_pw_w", [L, GW, GW])
    out_d = nc.dram_tensor("outT", [D, SEQ], F32, kind="ExternalOutput").ap()
    wup_s = nc.dram_tensor("wup_s", [L, NJ, 128, 8 * 2 * 128], BF16, kind="Internal").ap()
    wdn_s = nc.dram_tensor("wdn_s", [L, 8, 128, NJ * 128], BF16, kind="Internal").ap()
    dbg_d = {n: nc.dram_tensor("dbg_" + n, [128, w], F32, kind="ExternalOutput").ap() for n, w in dbg}

    es = ExitStack()
    with es:
        S = Sched(nc, es)
        NB = 204 * 1024
        big = es.enter_context(nc.sbuf_tensor("arena", [128, NB // 4], F32))
        A = Arena(big, NB)
        psb = [es.enter_context(nc.psum_tensor("ps%d" % i, [128, 512], F32)) for i in range(8)]
        PS = [(psb[i][:, :], ("ps", i)) for i in range(8)]
        ringA = Ring(PS[0:4])
        ringB = Ring(PS[4:8])

        def MM(out, lhsT, rhs, start, stop, r, w, inc=True, tp=None):
            kw = {}
            if tp is not None:
                kw["tile_position"] = tp
            S.op("pe", lambda: nc.tensor.matmul(out, lhsT=lhsT, rhs=rhs, start=start, stop=stop, **kw), r=r, w=w, inc=inc)

        def ACT(out, in_, func, r, w, scale=None, bias=None):
            kw = {}
            if scale is not None:
                kw["scale"] = scale
            if bias is not None:
                kw["bias"] = bias
            S.op("act", lambda: nc.scalar.activation(out=out, in_=in_, func=func, **kw), r=r, w=w)

        def TTo(eng, out, in0, in1, op, r, w):
            e = nc.vector if eng == "dve" else nc.gpsimd
            S.op(eng, lambda: e.tensor_tensor(out=out, in0=in0, in1=in1, op=op), r=r, w=w)

        def TS(eng, out, in0, s1, s2, op0, op1, r, w):
            e = nc.vector if eng == "dve" else nc.gpsimd
            if op1 is None:
                S.op(eng, lambda: e.tensor_scalar(out=out, in0=in0, scalar1=s1, scalar2=None, op0=op0), r=r, w=w)
            else:
                S.op(eng, lambda: e.tensor_scalar(out=out, in0=in0, scalar1=s1, scalar2=s2, op0=op0, op1=op1), r=r, w=w)

        def STT(out, in0, scalar, in1, op0, op1, r, w):
            S.op("dve", lambda: nc.vector.scalar_tensor_tensor(out=out, in0=in0, scalar=scalar, in1=in1, op0=op0, op1=op1), r=r, w=w)

        def CP(eng, out, in_, r, w):
            e = nc.vector if eng == "dve" else nc.gpsimd
            S.op(eng, lambda: e.tensor_copy(out=out, in_=in_), r=r, w=w)

        def MSET(eng, ap, val, w):
            e = nc.vector if eng == "dve" else nc.gpsimd
            S.op(eng, lambda: e.memset(ap, val), w=w)

        def DMA(q, out, in_, semkey, r, w):
            e = dict(sp=nc.sync, pool=nc.gpsimd, act=nc.scalar)[q]
            S.dma(q, lambda: e.dma_start(out=out, in_=in_), semkey, r=r, w=w)

        def dump(name, ap, keys):
            if name in dbg_d:
                w_ = ap.shape[-1]
                stg = A.f32(w_)
                CP("dve", stg[0:ap.shape[0], :], ap, r=keys, w=[("dbgs", name)])
                DMA("sp", dbg_d[name][0:ap.shape[0], 0:w_], stg[0:ap.shape[0], :], "dbg", r=[("dbgs", name)], w=[("dbgo", name)])

        xT = A.f32(8 * SEQ).rearrange("p (c s) -> p c s", c=8)
        hT = A.bf16(8 * SEQ).rearrange("p (c s) -> p c s", c=8)
        cmat = A.bf16(768)
        IDENT, ONES, BLK2, R64, R32, TRI = [cmat[:, i * 128:(i + 1) * 128] for i in range(6)]
        cf = A.f32(258)
        TRIL = cf[:, 0:128]
        PMASK = cf[:, 128:256].rearrange("p (q n) -> p q n", n=8)
        INVF = [cf[:, 256:257], cf[:, 257:258]]
        pv = A.f32(PV_N)

        def PVv(name, n):
            return pv[:, PV_OFF[name]:PV_OFF[name] + n]

        vd = PVv("vd", L * 5 * 8).rearrange("p (l v c) -> p l v c", l=L, v=5)
        vg = PVv("vg", L * 7 * 2).rearrange("p (l v c) -> p l v c", l=L, v=7)
        wdw = PVv("wdw", L * 2 * 31).rearrange("p (l c k) -> p l c k", l=L, c=2)
        fcw = PVv("fcw", L * 44 * 3).rearrange("p (l j k) -> p l j k", l=L, j=44)
        fcb = PVv("fcb", L * 44).rearrange("p (l j) -> p l j", l=L)
        gsub = PVv("gsub", L)
        lamv = PVv("lamv", L * 4 * 32).rearrange("p (l v k) -> p l v k", l=L, v=4)
        small = A.f32(64)
        lnv = A.f32(512)
        rstd = A.f32(512)
        sq = [A.bf16(512) for _ in range(3)]
        sqring = Ring([(sq[i], ("sq", i)) for i in range(3)])
        PH = A.mark()

        DMA("pool", cmat, cmat_d[:, :], "ld_c", r=[], w=["cmat"])
        DMA("sp", cf, cf_d[:, :], "ld_c2", r=[], w=["cf"])
        DMA("sp", pv, pv_d[:, :], "ld_c3", r=[], w=["pv"])
        for c in range(8):
            DMA("sp", xT[:, c, :], xT_d[c * 128:(c + 1) * 128, :], ("ld_x", c), r=[], w=[("xT", c, t) for t in range(NT)])

        def wload(dst, src, key):
            DMA("pool", dst, src, ("wsem",) + key, r=[], w=[key])

        def precast_ffn(l):
            wupv_ = w_up_d[l].rearrange("(k p) (g n) -> p k g n", p=128, g=2)
            wdnv_ = w_down_d[l].rearrange("(j p) n -> p j n", p=128)
            sk = ("pcs", l)
            keys = []
            for j in range(NJ):
                for gi_ in range(2):
                    dst = wup_s[l, j].rearrange("p (k g n) -> p k g n", k=8, g=2)[:, :, gi_, :]
                    DMA("pool", dst, wupv_[:, :, gi_, j * 128:(j + 1) * 128], sk, r=[], w=[("pcu", l, j, gi_)])
                    keys.append(("pcu", l, j, gi_))
            for dc in range(8):
                dst = wdn_s[l, dc].rearrange("p (j n) -> p j n", j=NJ)
                DMA("pool", dst, wdnv_[:, :, dc * 128:(dc + 1) * 128], sk, r=[], w=[("pcd", l, dc)])
                keys.append(("pcd", l, dc))
            tot = S.cnt[sk]
            for k in keys:
                S.state[k] = [(sk, tot), []]

        def rstd_from(ps_ap, ps_key, inv_n, eps, extra_r=()):
            ACT(lnv, ps_ap, AF.Ln, r=[ps_key] + list(extra_r), w=["lnv"], scale=inv_n, bias=eps)
            ACT(rstd, lnv, AF.Exp, r=["lnv"], w=["rstd"], scale=-0.5)

        def rmsnorm_tile(srcs, skeys, dsts, dkeys, gains, n_feat, eps=EPS, lhs=None, lkey="cmat"):
            ps, pk = ringB.next()
            C = len(srcs)
            for c in range(C):
                q, qk = sqring.next()
                ACT(q, srcs[c], AF.Square, r=[skeys[c]], w=[qk])
                MM(ps, lhs if lhs is not None else ONES, q, c == 0, c == C - 1, r=[qk, lkey], w=[pk])
            rstd_from(ps, pk, 1.0 / n_feat, eps)
            for c in range(C):
                STT(dsts[c], srcs[c], gains[c], rstd, ALU.mult, ALU.mult, r=[skeys[c], "rstd", "pv"], w=[dkeys[c]])

        def sl(t):
            return slice(t * TW, (t + 1) * TW)

        def norm_x_to_h(l, vidx):
            for t in range(NT):
                rmsnorm_tile([xT[:, c, sl(t)] for c in range(8)], [("xT", c, t) for c in range(8)],
                             [hT[:, c, sl(t)] for c in range(8)], [("hT", c, t) for c in range(8)],
                             [vd[:, l, vidx, c:c + 1] for c in range(8)], D)

        def proj(ps, pk, wv, wkey, cs, rhs_fn, rkeys_fn, nk=8):
            for kc in range(nk):
                MM(ps, wv[:, kc, cs], rhs_fn(kc), kc == 0, kc == nk - 1, r=[wkey] + rkeys_fn(kc), w=[pk], inc=(kc == nk - 1))

        def hrhs(t):
            return (lambda kc: hT[:, kc, sl(t)]), (lambda kc: [("hT", kc, t)])

        for l in range(nlayers):
            S.barrier()
            A.release(PH)
            catT = A.bf16(8 * SEQ).rearrange("p (c s) -> p c s", c=8)
            ropeC = A.bf16(SEQ)
            ropeS = A.bf16(SEQ)
            wslots = [A.bf16(8 * 256).rearrange("p (k n) -> p k n", k=8) for _ in range(4)]
            MX = A.mark()
            winv = w_in_d[l].rearrange("(k p) n -> p k n", p=128)
            woutv = w_out_d[l].rearrange("(k p) n -> p k n", p=128)
            mplan = [(lambda v, k, bi=bi: wload(v, winv[:, :, bi * 256:(bi + 1) * 256], k)) for bi in (0, 1, 2, 7, 8, 9, 3, 4, 5, 6)]
            mplan += [(lambda v, k, db=db: wload(v, woutv[:, :, db * 256:(db + 1) * 256], k)) for _t in range(NT) for db in range(4)]
            mstream = WStream([(wslots[i], ("ws", i)) for i in range(4)], mplan, auto=False)

            def win_block(bi):
                return mstream.get()

            def rope_tables(which):
                m = A.mark()
                posi = A.i32(SEQ)
                v = A.f32(SEQ)
                ki = A.i32(SEQ)
                DMA("sp", posi, pos_d[0:1, :].broadcast_to([128, SEQ]), "ld_pos", r=[], w=["posi"])
                for tab, shift, key in ((ropeS, 0.0, "ropeS"), (ropeC, 0.25, "ropeC")):
                    TS("dve", v, posi, INVF[which], shift, ALU.mult, ALU.add, r=["posi", "cf"], w=["ropev"])
                    CP("dve", ki, v, r=["ropev"], w=["ropek"])
                    TTo("dve", v, v, ki, ALU.subtract, r=["ropev", "ropek"], w=["ropev"])
                    ACT(tab, v, AF.Sin, r=["ropev"], w=[key], scale=2 * math.pi * (1 - 1e-6))
                S.barrier()
                A.release(m)

            def rope_apply(ps, pk, t, RM, zbr, t1, t2):
                zb, zk = zbr.next()
                ACT(zb, ps, AF.Copy, r=[pk], w=[zk])
                ps2, pk2 = ringA.next()
                MM(ps2, RM, zb, True, True, r=[zk, "cmat"], w=[pk2])
                TTo("dve", t1[0], zb, ropeC[:, sl(t)], ALU.mult, r=[zk, "ropeC"], w=[t1[1]])
                TTo("dve", t2[0], ps2, ropeS[:, sl(t)], ALU.mult, r=[pk2, "ropeS"], w=[t2[1]])

            def attn_finalize_recip(O, ok, dst_rden):
                ACT(dst_rden[0][0:64, :], O[64:128, :], AF.Ln, r=[ok], w=[dst_rden[1]])
                ACT(dst_rden[0][0:64, :], dst_rden[0][0:64, :], AF.Exp, r=[dst_rden[1]], w=[dst_rden[1]], scale=-1.0)

            def vproj(wv, wk, cs, vaug):
                for g in range(4):
                    ps, pk = ringA.next()
                    for s4 in range(4):
                        st = g * 4 + s4
                        for kc in range(8):
                            MM(ps[:, s4 * 128:(s4 + 1) * 128], hT[:, kc, st * 128:(st + 1) * 128], wv[:, kc, cs], kc == 0, kc == 7,
                               r=[wk, ("hT", kc, st // 4)], w=[pk], inc=(kc == 7 and s4 == 3))
                    ACT(vaug[:, g * 4:(g + 1) * 4, :, 0:64], ps.rearrange("p (s h d) -> p s h d", s=4, h=2), AF.Copy, r=[pk], w=[("V", g)])

            norm_x_to_h(l, 0)
            if l == 0:
                dump("hT0", hT[:, 0, 0:512], [("hT", 0, 0)])

            rope_tables(0)
            m0 = A.mark()
            Kaug = [A.bf16(SEQ) for _ in range(2)]
            vaug = A.bf16(16 * 2 * 128).rearrange("p (s h d) -> p s h d", s=16, h=2)
            Qa = [[A.bf16(512) for _ in range(2)] for _ in range(2)]
            Pr = Ring([(A.bf16(512), ("P", i)) for i in range(4)])
            zbr = Ring([(A.bf16(512), ("zb", i)) for i in range(2)])
            t1 = (A.f32(512), "t1")
            t2 = (A.f32(512), "t2")
            rden = (A.f32(512), "rden")
            bstg4 = A.bf16(4 * 72).rearrange("p (j n) -> p j n", j=4)
            gm = A.f32(64)
            top8 = A.f32(32)
            kmf = A.f32(16)
            kmb = A.bf16(16)
            MSET("dve", vaug[:, :, :, 64:128], 1.0, w=[("V", g) for g in range(4)])
            MSET("dve", bstg4, 0.0, w=["bstg"])
            for hh in range(2):
                DMA("pool", Kaug[hh][64:72, :], ind_d[:, :], "ld_ind", r=[], w=[("Kind", hh)])
            wq, wqk = win_block(0)
            wk_, wkk = win_block(1)
            wv_, wvk = win_block(2)
            precast_ffn(l)
            for c in range(2):
                cs = slice(c * 128, (c + 1) * 128)
                for t in range(NT):
                    ps, pk = ringA.next()
                    rf, kf = hrhs(t)
                    proj(ps, pk, wk_, wkk, cs, rf, kf)
                    rope_apply(ps, pk, t, R64, zbr, t1, t2)
                    for hh in range(2):
                        TTo("dve", Kaug[hh][0:64, sl(t)], t1[0][hh * 64:(hh + 1) * 64, :], t2[0][hh * 64:(hh + 1) * 64, :], ALU.add,
                            r=[t1[1], t2[1]], w=[("K", hh, t)])
                for hh in range(2):
                    S.op("dve", lambda hh=hh: nc.vector.tensor_reduce(out=kmf[0:64, hh * 8:(hh + 1) * 8], in_=Kaug[hh][0:64, :].rearrange("p (n k) -> p n k", k=256), axis=AX.X, op=ALU.add),
                         r=[("K", hh, t) for t in range(NT)], w=[("kmf", hh)])
                    TS("dve", kmb[0:64, hh * 8:(hh + 1) * 8], kmf[0:64, hh * 8:(hh + 1) * 8], 1.0 / 256, None, ALU.mult, None, r=[("kmf", hh)], w=[("kmb", hh)])
                vproj(wv_, wvk, cs, vaug)
                for qt in range(NT):
                    qb = Qa[qt % 2]
                    ps, pk = ringA.next()
                    rf, kf = hrhs(qt)
                    proj(ps, pk, wq, wqk, cs, rf, kf)
                    rope_apply(ps, pk, qt, R64, zbr, t1, t2)
                    for hh in range(2):
                        TTo("dve", qb[hh][0:64, :], t1[0][hh * 64:(hh + 1) * 64, :], t2[0][hh * 64:(hh + 1) * 64, :], ALU.add,
                            r=[t1[1], t2[1]], w=[("Qa", qt % 2, hh)])
                    for hh in range(2):
                        gps, gk = ringA.next()
                        for j in range(4):
                            MM(gps[:, j * 8:(j + 1) * 8], qb[hh][0:64, j * 128:(j + 1) * 128], kmb[0:64, hh * 8:(hh + 1) * 8], True, True,
                               r=[("Qa", qt % 2, hh), ("kmb", hh)], w=[gk], inc=(j == 3))
                        gmv = gm[:, 0:32].rearrange("p (j n) -> p j n", n=8)
                        TTo("dve", gmv, gps[:, 0:32].rearrange("p (j n) -> p j n", n=8), PMASK[:, qt * 4:(qt + 1) * 4, :], ALU.add, r=[gk, "cf"], w=["gm"])
                        tps, tk = ringA.next()
                        tpsb = tps.bitcast(BF16)
                        top8v = top8.rearrange("p (j n) -> p j n", n=8)
                        for j in range(4):
                            S.op("dve", lambda j=j: nc.vector.max(out=top8[:, j * 8:(j + 1) * 8], in_=gm[:, j * 8:(j + 1) * 8]), r=["gm"], w=["top8"])
                        TS("dve", top8v[:, :, 3:4], top8v[:, :, 3:4], -BIGG, None, ALU.max, None, r=["top8"], w=["top8"])
                        TTo("dve", bstg4[:, :, 64:72], gmv, top8v[:, :, 3:4].broadcast_to([128, 4, 8]), ALU.is_lt, r=["gm", "top8"], w=["bstg"])
                        for j in range(4):
                            S.op("pe", lambda j=j, tpsb=tpsb: nc.tensor.transpose(tpsb[0:72, j * 128:(j + 1) * 128], bstg4[:, j, :], IDENT), r=["bstg", "cmat"], w=[tk], inc=(j == 3))
                        ACT(qb[hh][64:72, :], tpsb[64:72, 0:512], AF.Copy, r=[tk], w=[("Qb", qt % 2, hh)])
                    for hh in range(2):
                        O, ok = ringB.next()
                        nk = 4 * qt + 4
                        pend = []
                        for kt in range(nk):
                            jd = kt - 4 * qt
                            c0 = 128 * jd if jd > 0 else 0
                            sp_, sk = ringA.next()
                            MM(sp_[:, c0:512], Kaug[hh][0:72, kt * 128:(kt + 1) * 128], qb[hh][0:72, c0:512], True, jd < 0,
                               r=[("K", hh, kt // 4), ("Kind", hh), ("Qa", qt % 2, hh), ("Qb", qt % 2, hh)], w=[sk], inc=(jd < 0))
                            if jd >= 0:
                                MM(sp_[:, c0:c0 + 128], IDENT, TRI, False, True, r=["cmat"], w=[sk])
                            P, Pk = Pr.next()
                            ACT(P[:, c0:512], sp_[:, c0:512], AF.Exp, r=[sk], w=[Pk], scale=0.125)

                            def pv_(kt=kt, c0=c0, P=P, Pk=Pk, O=O, ok=ok, hh=hh, nk=nk):
                                MM(O[:, c0:512], vaug[:, kt, hh, :], P[:, c0:512], kt == 0, kt == nk - 1, r=[Pk, ("V", kt // 4)], w=[ok])
                            pend.append(pv_)
                            if len(pend) > 2:
                                pend.pop(0)()
                        for f in pend:
                            f()
                        attn_finalize_recip(O, ok, rden)
                        TTo("dve", catT[hh * 64:(hh + 1) * 64, c, sl(qt)], O[0:64, :], rden[0][0:64, :], ALU.mult, r=[ok, rden[1]], w=[("cat", c, qt)])
            if l == 0:
                dump("oa_pre", catT[:, 0, 0:512], [("cat", 0, 0)])
            for t in range(NT):
                rmsnorm_tile([catT[:, c, sl(t)] for c in range(2)], [("cat", c, t) for c in range(2)],
                             [catT[:, c, sl(t)] for c in range(2)], [("cat", c, t) for c in range(2)],
                             [vg[:, l, 4, c:c + 1] for c in range(2)], GW)
            if l == 0:
                dump("oa", catT[:, 0, 0:512], [("cat", 0, 0)])
            mstream.done()
            S.barrier()
            A.release(m0)

            rope_tables(1)
            lam_init = 0.8 - 0.6 * math.exp(-0.3 * l)
            lt = small[:, 0:8]
            prod = A.f32(64)
            TTo("dve", prod[:, 0:32], lamv[:, l, 0, :], lamv[:, l, 1, :], ALU.mult, r=["pv"], w=["prod"])
            S.op("dve", lambda: nc.vector.tensor_reduce(out=lt[:, 0:1], in_=prod[:, 0:32], axis=AX.X, op=ALU.add), r=["prod"], w=["lt"])
            TTo("dve", prod[:, 32:64], lamv[:, l, 2, :], lamv[:, l, 3, :], ALU.mult, r=["pv"], w=["prod2"])
            S.op("dve", lambda: nc.vector.tensor_reduce(out=lt[:, 1:2], in_=prod[:, 32:64], axis=AX.X, op=ALU.add), r=["prod2"], w=["lt"])
            ACT(lt[:, 2:4], lt[:, 0:2], AF.Exp, r=["lt"], w=["lt"])
            TTo("dve", lt[:, 4:5], lt[:, 3:4], lt[:, 2:3], ALU.subtract, r=["lt"], w=["lt"])
            TS("dve", lt[:, 5:6], lt[:, 4:5], -lam_init, None, ALU.add, None, r=["lt"], w=["lt"])
            TS("dve", lt[:, 6:7], gsub[:, l:l + 1], 1.0 - lam_init, None, ALU.mult, None, r=["pv", "lt"], w=["lt"])
            NEGLAM = lt[:, 5:6]
            GS = lt[:, 6:7]

            m0 = A.mark()
            Kd = A.bf16(SEQ)
            vaug = A.bf16(16 * 2 * 128).rearrange("p (s h d) -> p s h d", s=16, h=2)
            Qd = [A.bf16(512) for _ in range(2)]
            Pr = Ring([(A.bf16(512), ("P", i)) for i in range(4)])
            zbr = Ring([(A.bf16(512), ("zb", i)) for i in range(2)])
            t1 = (A.f32(512), "t1")
            t2 = (A.f32(512), "t2")
            rden = (A.f32(512), "rden")
            od = A.f32(512)
            MSET("dve", vaug[:, :, :, 64:128], 1.0, w=[("V", g) for g in range(4)])
            wq, wqk = win_block(7)
            wk_, wkk = win_block(8)
            wv_, wvk = win_block(9)
            dscale = 32.0 ** -0.5
            for c in range(2):
                cs = slice(c * 128, (c + 1) * 128)
                for t in range(NT):
                    ps, pk = ringA.next()
                    rf, kf = hrhs(t)
                    proj(ps, pk, wk_, wkk, cs, rf, kf)
                    rope_apply(ps, pk, t, R32, zbr, t1, t2)
                    TTo("dve", Kd[:, sl(t)], t1[0], t2[0], ALU.add, r=[t1[1], t2[1]], w=[("Kd", t)])
                vproj(wv_, wvk, cs, vaug)
                for qt in range(NT):
                    qd = Qd[qt % 2]
                    ps, pk = ringA.next()
                    rf, kf = hrhs(qt)
                    proj(ps, pk, wq, wqk, cs, rf, kf)
                    rope_apply(ps, pk, qt, R32, zbr, t1, t2)
                    TTo("dve", qd, t1[0], t2[0], ALU.add, r=[t1[1], t2[1]], w=[("Qd", qt % 2)])
                    for hh in range(2):
                        Os = [ringB.next(), ringB.next()]
                        nk = 4 * qt + 4
                        pend = []
                        for kt in range(nk):
                            jd = kt - 4 * qt
                            c0 = 128 * jd if jd > 0 else 0
                            cur = []
                            for m_ in range(2):
                                g = hh * 2 + m_
                                sp_, sk = ringA.next()
                                MM(sp_[:, c0:512], Kd[32 * g:32 * g + 32, kt * 128:(kt + 1) * 128], qd[32 * g:32 * g + 32, c0:512], True, jd < 0,
                                   r=[("Kd", kt // 4), ("Qd", qt % 2)], w=[sk], inc=(jd < 0), tp=(32 * g, 0))
                                if jd >= 0:
                                    MM(sp_[:, c0:c0 + 128], IDENT, TRI, False, True, r=["cmat"], w=[sk])
                                cur.append((sp_, sk))
                            for f in pend:
                                f()
                            pend = []
                            for m_ in range(2):
                                sp_, sk = cur[m_]
                                P, Pk = Pr.next()
                                ACT(P[:, c0:512], sp_[:, c0:512], AF.Exp, r=[sk], w=[Pk], scale=dscale)

                                def pv_(kt=kt, c0=c0, P=P, Pk=Pk, Oo=Os[m_], hh=hh, nk=nk):
                                    MM(Oo[0][:, c0:512], vaug[:, kt, hh, :], P[:, c0:512], kt == 0, kt == nk - 1, r=[Pk, ("V", kt // 4)], w=[Oo[1]])
                                pend.append(pv_)
                        for f in pend:
                            f()
                        hs = slice(hh * 64, (hh + 1) * 64)
                        attn_finalize_recip(Os[0][0], Os[0][1], rden)
                        TTo("dve", t1[0][0:64, :], Os[0][0][0:64, :], rden[0][0:64, :], ALU.mult, r=[Os[0][1], rden[1]], w=[t1[1]])
                        attn_finalize_recip(Os[1][0], Os[1][1], rden)
                        TTo("dve", t2[0][0:64, :], Os[1][0][0:64, :], rden[0][0:64, :], ALU.mult, r=[Os[1][1], rden[1]], w=[t2[1]])
                        STT(od[hs, :], t2[0][0:64, :], NEGLAM[0:64, :], t1[0][0:64, :], ALU.mult, ALU.add, r=[t1[1], t2[1], "lt"], w=[("od", hh)])
                    ps, pk = ringB.next()
                    q_, qk_ = sqring.next()
                    ACT(q_, od, AF.Square, r=[("od", 0), ("od", 1)], w=[qk_])
                    MM(ps, BLK2, q_, True, True, r=[qk_, "cmat"], w=[pk])
                    rstd_from(ps, pk, 1.0 / 64, 1e-5)
                    STT(catT[:, 6 + c, sl(qt)], od, GS, rstd, ALU.mult, ALU.mult, r=[("od", 0), ("od", 1), "rstd", "lt"], w=[("cat", 6 + c, qt)])
            if l == 0:
                dump("od", catT[:, 6, 0:512], [("cat", 6, 0)])
            mstream.done()
            S.barrier()
            A.release(m0)

            m0 = A.mark()
            uT = A.bf16(2 * SEQ).rearrange("p (c s) -> p c s", c=2)
            vgl = A.bf16(16 * 256).rearrange("p (n d) -> p n d", n=16)
            glnb = A.f32(512).rearrange("p (v d) -> p v d", v=2)
            gb = A.f32(1024).rearrange("p (c s) -> p c s", c=2)
            wsf = A.f32(512).rearrange("p (h i) -> p h i", h=4)
            wsb = A.bf16(512).rearrange("p (h i) -> p h i", h=4)
            stats = A.f32(16 * 6)
            mv = A.f32(16 * 2).rearrange("p (n k) -> p n k", k=2)
            rsd = A.f32(16)
            vtmp = A.f32(256)
            vln = [A.bf16(256) for _ in range(2)]
            stmp = A.f32(512)
            DMA("sp", glnb, glnb_d[l], "ld_g1", r=[], w=["glnb"])
            DMA("sp", gb, gb_d[l].rearrange("c p s -> p c s"), "ld_g2", r=[], w=["gb"])
            DMA("sp", wsf, wsT_d[l].rearrange("h j i -> j h i"), "ld_g3", r=[], w=["wsf"])
            for h in range(4):
                TTo("dve", wsb[:, h, :], wsf[:, h, :], TRIL, ALU.mult, r=["wsf", "cf"], w=["wsb"])
            wu, wuk = win_block(3)
            wvv, wvvk = win_block(4)
            for t in range(NT):
                for cc in range(2):
                    ps, pk = ringA.next()
                    rf, kf = hrhs(t)
                    proj(ps, pk, wu, wuk, slice(cc * 128, (cc + 1) * 128), rf, kf)
                    ACT(uT[:, cc, sl(t)], ps, AF.Gelu_apprx_tanh, r=[pk], w=[("uT", cc, t)])
            for n2 in range(8):
                ps, pk = ringA.next()
                for s2 in range(2):
                    n = n2 * 2 + s2
                    for kc in range(8):
                        MM(ps[:, s2 * 256:(s2 + 1) * 256], hT[:, kc, n * 128:(n + 1) * 128], wvv[:, kc, :], kc == 0, kc == 7,
                           r=[wvvk, ("hT", kc, n // 4)], w=[pk], inc=(kc == 7 and s2 == 1))
                ACT(vgl[:, n2 * 2:(n2 + 1) * 2, :], ps.rearrange("p (s d) -> p s d", s=2), AF.Gelu_apprx_tanh, r=[pk], w=[("vgl", n2)])
            for n in range(16):
                S.op("dve", lambda n=n: nc.vector.bn_stats(out=stats[:, n * 6:(n + 1) * 6], in_=vgl[:, n, :]), r=[("vgl", n // 2)], w=[("st", n)])
                S.op("dve", lambda n=n: nc.vector.bn_aggr(out=mv[:, n, :], in_=stats[:, n * 6:(n + 1) * 6]), r=[("st", n)], w=["mv"])
            ACT(rsd, mv[:, :, 1], AF.Ln, r=["mv"], w=["rsd"], bias=EPS)
            ACT(rsd, rsd, AF.Exp, r=["rsd"], w=["rsd"], scale=-0.5)
            for t in range(NT):
                pss = [ringA.next(), ringA.next()]
                for s4 in range(4):
                    n = t * 4 + s4
                    vl = vln[n % 2]
                    vk = ("vln", n % 2)
                    TS("dve", vtmp, vgl[:, n, :], mv[:, n, 0:1], rsd[:, n:n + 1], ALU.subtract, ALU.mult, r=[("vgl", n // 2), "mv", "rsd"], w=["vtmp"])
                    TTo("dve", vtmp, vtmp, glnb[:, 0, :], ALU.mult, r=["vtmp", "glnb"], w=["vtmp"])
                    TTo("dve", vl, vtmp, glnb[:, 1, :], ALU.add, r=["vtmp", "glnb"], w=[vk])
                    for cc in range(2):
                        for hh in range(2):
                            h = 2 * cc + hh
                            MM(pss[cc][0][hh * 64:(hh + 1) * 64, s4 * 128:(s4 + 1) * 128], vl[:, h * 64:(h + 1) * 64], wsb[:, h, :], True, True,
                               r=[vk, "wsb"], w=[pss[cc][1]], tp=(0, hh * 64))
                for cc in range(2):
                    TTo("dve", stmp, pss[cc][0], gb[:, cc, :], ALU.add, r=[pss[cc][1], "gb"], w=["stmp"])
                    TTo("dve", catT[:, 2 + cc, sl(t)], stmp, uT[:, cc, sl(t)], ALU.mult, r=["stmp", ("uT", cc, t)], w=[("cat", 2 + cc, t)])
            if l == 0:
                dump("ob_pre", catT[:, 2, 0:512], [("cat", 2, 0)])
            for t in range(NT):
                rmsnorm_tile([catT[:, 2 + c, sl(t)] for c in range(2)], [("cat", 2 + c, t) for c in range(2)],
                             [catT[:, 2 + c, sl(t)] for c in range(2)], [("cat", 2 + c, t) for c in range(2)],
                             [vg[:, l, 5, c:c + 1] for c in range(2)], GW)
            mstream.done()
            S.barrier()
            A.release(m0)

            m0 = A.mark()
            ybuf = A.bf16(32 + SEQ)
            diag = A.bf16(31 * 128).rearrange("p (k n) -> p k n", k=31)
            cy = A.bf16(2 * SEQ).rearrange("p (c s) -> p c s", c=2)
            sg = A.f32(512)
            wpw = A.bf16(2 * 256).rearrange("p (k n) -> p k n", k=2)
            mstat = A.f32(512)
            m2 = A.f32(512)
            yn = A.f32(512)
            sil = A.bf16(1024).rearrange("p (c s) -> p c s", c=2)
            Y0 = 2
            wa, wak = win_block(5)
            wg_, wgk = win_block(6)
            wload(wpw, w_pw_d[l].rearrange("(k p) n -> p k n", p=128), ("wpw",))
            MSET("dve", ybuf[:, 0:32], 0.0, w=["ypad"])
            for cc in range(2):
                cs = slice(cc * 128, (cc + 1) * 128)
                for k in range(31):
                    TS("dve", diag[:, k, :], IDENT, wdw[:, l, cc, k:k + 1], None, ALU.mult, None, r=["cmat", "pv"], w=[("diag", k)])
                for t in range(NT):
                    pa, pak = ringA.next()
                    rf, kf = hrhs(t)
                    proj(pa, pak, wa, wak, cs, rf, kf)
                    pg, pgk = ringA.next()
                    proj(pg, pgk, wg_, wgk, cs, rf, kf)
                    ACT(sg, pg, AF.Sigmoid, r=[pgk], w=["sg"])
                    TTo("dve", ybuf[:, 32 + t * TW:32 + (t + 1) * TW], pa, sg, ALU.mult, r=[pak, "sg"], w=[("yb", t)])
                for t in range(NT):
                    ps, pk = ringB.next()
                    for k in range(31):
                        o = Y0 + t * TW + k
                        rk = ["ypad", ("diag", k), ("yb", t)] + ([("yb", t - 1)] if t > 0 else [])
                        MM(ps, diag[:, k, :], ybuf[:, o:o + TW], k == 0, k == 30, r=rk, w=[pk], inc=(k == 30))
                    ACT(cy[:, cc, sl(t)], ps, AF.Identity, r=[pk, "pv"], w=[("cy", cc, t)], bias=vg[:, l, 0, cc:cc + 1])
            if l == 0:
                dump("cy", cy[:, 0, 0:512], [("cy", 0, 0)])
            for t in range(NT):
                ps1, pk1 = ringB.next()
                ps2, pk2 = ringB.next()
                for cc in range(2):
                    MM(ps1, ONES, cy[:, cc, sl(t)], cc == 0, cc == 1, r=[("cy", cc, t), "cmat"], w=[pk1])
                for cc in range(2):
                    q_, qk_ = sqring.next()
                    ACT(q_, cy[:, cc, sl(t)], AF.Square, r=[("cy", cc, t)], w=[qk_])
                    MM(ps2, ONES, q_, cc == 0, cc == 1, r=[qk_, "cmat"], w=[pk2])
                TS("dve", mstat, ps1, 1.0 / GW, None, ALU.mult, None, r=[pk1], w=["mstat"])
                TTo("dve", m2, mstat, mstat, ALU.mult, r=["mstat"], w=["m2"])
                STT(m2, ps2, 1.0 / GW, m2, ALU.mult, ALU.subtract, r=[pk2, "m2"], w=["m2"])
                ACT(lnv, m2, AF.Ln, r=["m2"], w=["lnv"], bias=EPS)
                ACT(rstd, lnv, AF.Exp, r=["lnv"], w=["rstd"], scale=-0.5)
                for cc in range(2):
                    TTo("dve", yn, cy[:, cc, sl(t)], mstat, ALU.subtract, r=[("cy", cc, t), "mstat"], w=["yn"])
                    TTo("dve", yn, yn, rstd, ALU.mult, r=["yn", "rstd"], w=["yn"])
                    ACT(sil[:, cc, :], yn, AF.Silu, r=["yn", "pv"], w=[("sil", cc)], scale=vg[:, l, 1, cc:cc + 1], bias=vg[:, l, 2, cc:cc + 1])
                for co in range(2):
                    ps, pk = ringA.next()
                    for ci in range(2):
                        MM(ps, wpw[:, ci, co * 128:(co + 1) * 128], sil[:, ci, :], ci == 0, ci == 1, r=[("wpw",), ("sil", ci)], w=[pk])
                    ACT(catT[:, 4 + co, sl(t)], ps, AF.Identity, r=[pk, "pv"], w=[("cat", 4 + co, t)], bias=vg[:, l, 3, co:co + 1])
            if l == 0:
                dump("oc_pre", catT[:, 4, 0:512], [("cat", 4, 0)])
            for t in range(NT):
                rmsnorm_tile([catT[:, 4 + c, sl(t)] for c in range(2)], [("cat", 4 + c, t) for c in range(2)],
                             [catT[:, 4 + c, sl(t)] for c in range(2)], [("cat", 4 + c, t) for c in range(2)],
                             [vg[:, l, 6, c:c + 1] for c in range(2)], GW)
            mstream.done()
            S.barrier()
            A.release(m0)

            def out_proj_norm_res(nkc, wsrc, rhs_fn, rkeys_fn, vidx, wr, ytile):
                for t in range(NT):
                    for db in range(4):
                        wv, wk = wr.get()
                        for dd in range(2):
                            dc = db * 2 + dd
                            ps, pk = ringA.next()
                            for kc in range(nkc):
                                MM(ps, wv[:, kc, dd * 128:(dd + 1) * 128], rhs_fn(kc, t), kc == 0, kc == nkc - 1, r=[wk] + rkeys_fn(kc, t), w=[pk], inc=(kc == nkc - 1))
                            if dc % 2 == 0:
                                ACT(ytile[:, dc, :], ps, AF.Copy, r=[pk], w=[("yt", dc)])
                            else:
                                CP("dve", ytile[:, dc, :], ps, r=[pk], w=[("yt", dc)])
                        wr.done()
                    rmsnorm_tile([ytile[:, dc, :] for dc in range(8)], [("yt", dc) for dc in range(8)],
                                 [ytile[:, dc, :] for dc in range(8)], [("yt", dc) for dc in range(8)],
                                 [vd[:, l, vidx, dc:dc + 1] for dc in range(8)], D)
                    for dc in range(8):
                        TTo("dve", xT[:, dc, sl(t)], xT[:, dc, sl(t)], ytile[:, dc, :], ALU.add, r=[("xT", dc, t), ("yt", dc)], w=[("xT", dc, t)])

            m0 = A.mark()
            ytile = A.f32(8 * 512).rearrange("p (c s) -> p c s", c=8)
            out_proj_norm_res(8, w_out_d[l].rearrange("(k p) n -> p k n", p=128), lambda kc, t: catT[:, kc, sl(t)], lambda kc, t: [("cat", kc, t)], 1, mstream, ytile)
            if l == 0:
                dump("x1", xT[:, 0, 0:512], [("xT", 0, 0)])
            S.barrier()
            A.release(MX)
            A.release(PH)

            norm_x_to_h(l, 2)
            ytile = A.f32(8 * 512).rearrange("p (c s) -> p c s", c=8)
            fT = A.bf16(NJ * 512).rearrange("p (j s) -> p j s", j=NJ)
            ub = [[A.f32(2 + 512) for _ in range(2)] for _ in range(2)]
            halo = A.f32(44 * 2).rearrange("p (j k) -> p j k", k=2)
            tas = [A.f32(512) for _ in range(2)]
            tbs = [A.f32(512) for _ in range(2)]
            cgs = [A.f32(512) for _ in range(2)]
            cvs = [A.f32(512) for _ in range(2)]
            wups = [A.bf16(8 * 2 * 128).rearrange("p (k g n) -> p k g n", k=8, g=2) for _ in range(3)]
            wdns = [A.bf16(NJ * 128).rearrange("p (j n) -> p j n", j=NJ) for _ in range(3)]
            MSET("dve", halo, 0.0, w=["halo"])
            wupv = w_up_d[l].rearrange("(k p) (g n) -> p k g n", p=128, g=2)
            wdnv = w_down_d[l].rearrange("(j p) n -> p j n", p=128)

            def ld_up(v, k, j):
                DMA("sp", v.rearrange("p k g n -> p (k g n)"), wup_s[l, j], ("wsem",) + k, r=[("pcu", l, j, 0), ("pcu", l, j, 1)], w=[k])

            def ld_dn(v, k, dc):
                DMA("sp", v.rearrange("p j n -> p (j n)"), wdn_s[l, dc], ("wsem",) + k, r=[("pcd", l, dc)], w=[k])

            wur = WStream([(wups[i], ("wu", i)) for i in range(3)], [(lambda v, k, j=j: ld_up(v, k, j)) for _t in range(NT) for j in range(NJ)])
            wdr = WStream([(wdns[i], ("wd", i)) for i in range(3)], [(lambda v, k, dc=dc: ld_dn(v, k, dc)) for _t in range(NT) for dc in range(8)])
            wur.top()
            wdr.top()
            pend_f = None
            for t in range(NT):
                for j in range(NJ):
                    wv, wk = wur.get()
                    u = ub[j % 2]
                    b2 = j % 2
                    ta, tb, cg, cv = tas[b2], tbs[b2], cgs[b2], cvs[b2]
                    for gi in range(2):
                        jj = j + gi * NJ
                        ps, pk = ringA.next()
                        for kc in range(8):
                            MM(ps, wv[:, kc, gi, :], hT[:, kc, sl(t)], kc == 0, kc == 7, r=[wk, ("hT", kc, t)], w=[pk], inc=(kc == 7))
                        uk = ("ub", b2, gi)
                        ACT(u[gi][:, 2:514], ps, AF.Copy, r=[pk], w=[uk])
                        CP("dve", u[gi][:, 0:2], halo[:, jj, :], r=["halo", ("halo", jj)], w=[uk])
                        tmp = ta if gi == 0 else tb
                        tk_ = ("ta", b2) if gi == 0 else ("tb", b2)
                        ACT(tmp, ps, AF.Identity, r=[pk, "pv"], w=[tk_], scale=fcw[:, l, jj, 2:3], bias=fcb[:, l, jj:jj + 1])
                        ACT(halo[:, jj, :], ps[:, 510:512], AF.Copy, r=[pk], w=[("halo", jj)])
                        dst = cg if gi == 0 else cv
                        dk = ("cg", b2) if gi == 0 else ("cv", b2)
                        STT(tmp, u[gi][:, 1:513], fcw[:, l, jj, 1:2], tmp, ALU.mult, ALU.add, r=[uk, tk_, "pv"], w=[tk_])
                        STT(dst, u[gi][:, 0:512], fcw[:, l, jj, 0:1], tmp, ALU.mult, ALU.add, r=[uk, tk_, "pv"], w=[dk])
                    if pend_f is not None:
                        pend_f()

                    def fin(j=j, b2=b2, cg=cg, cv=cv):
                        ACT(cg, cg, AF.Gelu_apprx_tanh, r=[("cg", b2)], w=[("cg", b2)])
                        TTo("dve", fT[:, j, :], cg, cv, ALU.mult, r=[("cg", b2), ("cv", b2)], w=[("fT", j)])
                    pend_f = fin
                pend_f()
                pend_f = None
                if l == 0 and t == 0:
                    dump("fT", fT[:, 0, :], [("fT", 0)])
                for dc in range(8):
                    wv, wk = wdr.get()
                    ps, pk = ringB.next()
                    for j in range(NJ):
                        MM(ps, wv[:, j, :], fT[:, j, :], j == 0, j == NJ - 1, r=[wk, ("fT", j)], w=[pk], inc=(j == NJ - 1))
                    if dc % 2 == 0:
                        ACT(ytile[:, dc, :], ps, AF.Copy, r=[pk], w=[("yt", dc)])
                    else:
                        CP("dve", ytile[:, dc, :], ps, r=[pk], w=[("yt", dc)])
                rmsnorm_tile([ytile[:, dc, :] for dc in range(8)], [("yt", dc) for dc in range(8)],
                             [ytile[:, dc, :] for dc in range(8)], [("yt", dc) for dc in range(8)],
                             [vd[:, l, 3, dc:dc + 1] for dc in range(8)], D)
                for dc in range(8):
                    TTo("dve", xT[:, dc, sl(t)], xT[:, dc, sl(t)], ytile[:, dc, :], ALU.add, r=[("xT", dc, t), ("yt", dc)], w=[("xT", dc, t)])
            if l == 0:
                dump("x2", xT[:, 0, 0:512], [("xT", 0, 0)])
            S.barrier()
            A.release(PH)

            norm_x_to_h(l, 4)
            pTb = A.bf16(2 * SEQ).rearrange("p (c s) -> p c s", c=2)
            wgs = [A.bf16(8 * 256).rearrange("p (k n) -> p k n", k=8) for _ in range(3)]
            wpj = A.bf16(2 * D).rearrange("p (k n) -> p k n", k=2)
            sgt = [A.f32(512) for _ in range(2)]
            pjt = [A.f32(512) for _ in range(2)]
            wload(pTb, pT_d[l].rearrange("(k p) s -> p k s", p=128), ("pTb",))
            wload(wpj, w_proj_d[l].rearrange("(k p) n -> p k n", p=128), ("wpj",))
            wgv = w_gate_d[l].rearrange("(k p) n -> p k n", p=128)
            wgr = WStream([(wgs[i], ("wg", i)) for i in range(3)], [(lambda v, k, db=db: wload(v, wgv[:, :, db * 256:(db + 1) * 256], k)) for db in range(4)])
            wgr.top()
            it = 0
            for db in range(4):
                wv, wk = wgr.get()
                for dd in range(2):
                    dc = db * 2 + dd
                    for t in range(NT):
                        b = it % 2
                        it += 1
                        ps, pk = ringA.next()
                        rf, kf = hrhs(t)
                        proj(ps, pk, wv, wk, slice(dd * 128, (dd + 1) * 128), rf, kf)
                        ACT(sgt[b], ps, AF.Sigmoid, r=[pk], w=[("sgt", b)])
                        ps2, pk2 = ringB.next()
                        for kc in range(2):
                            MM(ps2, wpj[:, kc, dc * 128:(dc + 1) * 128], pTb[:, kc, sl(t)], kc == 0, kc == 1, r=[("wpj",), ("pTb",)], w=[pk2], inc=(kc == 1))
                        TTo("dve", pjt[b], ps2, sgt[b], ALU.mult, r=[pk2, ("sgt", b)], w=[("pjt", b)])
                        TTo("dve", xT[:, dc, sl(t)], xT[:, dc, sl(t)], pjt[b], ALU.add, r=[("xT", dc, t), ("pjt", b)], w=[("xT", dc, t)])
            if l == 0:
                dump("x3", xT[:, 0, 0:512], [("xT", 0, 0)])

        for c in range(8):
            DMA("sp", out_d[c * 128:(c + 1) * 128, :], xT[:, c, :], "st_out", r=[("xT", c, t) for t in range(NT)], w=[("out", c)])
        S.barrier()
        S.emit()
        build.arena_hi = A.hi
    return nc


_CACHE = {}


def _prep_inputs(inp):
    cmat, cf, ind = _host_consts()
    pv, glnb, gb, wsT = _host_params(inp)
    x = np.asarray(inp["x"], np.float32)
    p = np.asarray(inp["p"], np.float32)
    pos = np.asarray(inp["positions"], np.int32)
    shared = dict(cmat=cmat, cf=cf, ind=ind, pv=pv, glnb=glnb, gb=gb, wsT=wsT)
    for k in ("w_in", "w_out", "w_up", "w_down", "w_pe_gate", "w_pe_proj", "conv_pw_w"):
        shared[k] = np.ascontiguousarray(np.asarray(inp[k], np.float32))
    maps = []
    for b in range(8):
        m = dict(shared)
        m["xT"] = np.ascontiguousarray(x[b].T)
        m["pT"] = np.ascontiguousarray(p[:, b].transpose(0, 2, 1))
        m["pos"] = np.ascontiguousarray(pos[b][None, :])
        maps.append(m)
    return maps


def kernel(**inputs):
    if "nc" not in _CACHE:
        _CACHE["nc"] = build()
    nc = _CACHE["nc"]
    maps = _prep_inputs(inputs)
    res = run_bass_kernel_spmd(nc, maps, core_ids=list(range(8)))
    out = np.stack([np.asarray(r["outT"], np.float32).T for r in res.results], axis=0)
    return np.ascontiguousarray(out)
```

```python
import math
import numpy as np
from contextlib import ExitStack
import concourse.bass as bass
import concourse.mybir as mybir
from concourse.bass_utils import run_bass_kernel_spmd

F32 = mybir.dt.float32
BF16 = mybir.dt.bfloat16
I32 = mybir.dt.int32
AF = mybir.ActivationFunctionType
ALU = mybir.AluOpType
AX = mybir.AxisListType

L = 2
D = 1024
SEQ = 2048
GW = 256
DFF = 2816
NJ = DFF // 128
NT = 4
TW = 512
MASK = 30000.0
BIGG = 1.0e6
EPS = 1e-6
ENGS = ["pe", "act", "dve", "pool", "sp"]


class Sched:
    def __init__(self, nc, es, same_engine_sync=True):
        self.nc = nc
        self.es = es
        self.same = same_engine_sync
        self.prog = {e: [] for e in ENGS}
        self.cnt = {}
        self.sems = {}
        self.seen = {e: {} for e in ENGS}
        self.state = {}
        for e in ENGS:
            self._sem(e)

    def _sem(self, key):
        if key not in self.sems:
            self.sems[key] = self.es.enter_context(self.nc.semaphore("s_" + str(key)))
            self.cnt[key] = 0
        return self.sems[key]

    def _engobj(self, e):
        nc = self.nc
        return dict(pe=nc.tensor, act=nc.scalar, dve=nc.vector, pool=nc.gpsimd, sp=nc.sync)[e]

    def _deps(self, e, r, w):
        toks = {}
        for k in r:
            st = self.state.get(k)
            if st and st[0] is not None:
                t = st[0]
                toks[t[0]] = max(toks.get(t[0], 0), t[1])
        for k in w:
            st = self.state.get(k)
            if st:
                if st[0] is not None:
                    t = st[0]
                    toks[t[0]] = max(toks.get(t[0], 0), t[1])
                for t in st[1]:
                    toks[t[0]] = max(toks.get(t[0], 0), t[1])
        waits = []
        for sk, v in toks.items():
            if sk == e and (not self.same or e == "pe"):
                continue
            if self.seen[e].get(sk, 0) >= v:
                continue
            self.seen[e][sk] = v
            waits.append((sk, v))
        return waits

    def _record(self, tok, r, w):
        for k in r:
            st = self.state.setdefault(k, [None, []])
            st[1].append(tok)
        for k in w:
            self.state[k] = [tok, []]

    def op(self, e, fn, r=(), w=(), inc=True):
        waits = self._deps(e, r, w)
        tok = (e, self.cnt[e] + 1)
        if inc:
            self.cnt[e] += 1
        self._record(tok, r, w)
        self.prog[e].append((waits, fn, (e, 1) if inc else None))
        return tok

    def dma(self, q, fn, semkey, r=(), w=()):
        self._sem(semkey)
        waits = self._deps(q, r, w)
        self.cnt[semkey] += 16
        tok = (semkey, self.cnt[semkey])
        self._record(tok, r, w)
        self.prog[q].append((waits, fn, (semkey, 16)))
        return tok

    def barrier(self):
        for e in ENGS:
            waits = []
            for sk, v in self.cnt.items():
                if v == 0 or self.seen[e].get(sk, 0) >= v:
                    continue
                if sk == e and e == "pe":
                    continue
                self.seen[e][sk] = v
                waits.append((sk, v))
            self.prog[e].append((waits, None, None))

    def simulate(self):
        val = {k: 0 for k in self.sems}
        pc = {e: 0 for e in ENGS}
        progress = True
        while progress:
            progress = False
            for e in ENGS:
                while pc[e] < len(self.prog[e]):
                    waits, fn, inc = self.prog[e][pc[e]]
                    if any(val[sk] < v for sk, v in waits):
                        break
                    if inc is not None:
                        val[inc[0]] += inc[1]
                    pc[e] += 1
                    progress = True
        stuck = {e: (pc[e], len(self.prog[e])) for e in ENGS if pc[e] < len(self.prog[e])}
        if stuck:
            msg = []
            for e, (i, n) in stuck.items():
                waits = self.prog[e][i][0]
                msg.append("%s@%d/%d waits %s" % (e, i, n, [(sk, v, val[sk]) for sk, v in waits if val[sk] < v]))
            raise RuntimeError("DEADLOCK in schedule: " + "; ".join(msg))
        for k in self.sems:
            assert val[k] == self.cnt[k], (k, val[k], self.cnt[k])

    def emit(self):
        self.simulate()
        nc = self.nc
        with nc.Block() as block:
            def run(e):
                eng = self._engobj(e)
                for waits, fn, inc in self.prog[e]:
                    for sk, v in waits:
                        eng.wait_ge(self.sems[sk], v)
                    if fn is None:
                        continue
                    ins = fn()
                    if inc is not None:
                        ins.then_inc(self.sems[inc[0]], inc[1])

            @block.tensor
            def _(e):
                run("pe")

            @block.scalar
            def _(e):
                run("act")

            @block.vector
            def _(e):
                run("dve")

            @block.gpsimd
            def _(e):
                run("pool")

            @block.sync
            def _(e):
                run("sp")


class Arena:
    def __init__(self, t, nbytes):
        self.t = t
        self.n = nbytes
        self.off = 0
        self.hi = 0

    def mark(self):
        return self.off

    def release(self, m):
        self.off = m

    def _take(self, nbytes):
        nbytes = (nbytes + 31) // 32 * 32
        o = self.off
        self.off += nbytes
        self.hi = max(self.hi, self.off)
        assert self.off <= self.n, ("arena overflow", self.off, self.n)
        return o

    def f32(self, n):
        o = self._take(4 * n)
        return self.t[:, o // 4: o // 4 + n]

    def bf16(self, n):
        o = self._take(2 * n)
        return self.t[:, o // 4: o // 4 + (n + 1) // 2].bitcast(BF16)

    def i32(self, n):
        o = self._take(4 * n)
        return self.t[:, o // 4: o // 4 + n].bitcast(I32)


class Ring:
    def __init__(self, items):
        self.items = items
        self.i = 0

    def next(self):
        it = self.items[self.i % len(self.items)]
        self.i += 1
        return it


class WStream:
    def __init__(self, slots, plan, auto=True):
        self.slots = slots
        self.plan = plan
        self.auto = auto
        self.issued = 0
        self.used = 0
        self.freed = 0

    def top(self):
        while self.issued < len(self.plan) and self.issued - self.freed < len(self.slots):
            i = self.issued
            v, k = self.slots[i % len(self.slots)]
            self.plan[i](v, k)
            self.issued += 1

    def done(self):
        self.freed = self.used
        self.top()

    def get(self):
        i = self.used
        if self.auto:
            self.freed = i
        self.top()
        assert self.issued > i, "weight stream: block not issued (slots exhausted)"
        self.used += 1
        return self.slots[i % len(self.slots)]


def _host_consts():
    ident = np.eye(128, dtype=np.float32)
    ones = np.ones((128, 128), np.float32)
    blk2 = np.zeros((128, 128), np.float32)
    blk2[0:64, 0:64] = 1
    blk2[64:128, 64:128] = 1

    def rotT(block):
        h = block // 2
        m = np.zeros((128, 128), np.float32)
        for b0 in range(0, 128, block):
            for i in range(h):
                m[b0 + i + h, b0 + i] = -1.0
                m[b0 + i, b0 + i + h] = 1.0
        return m

    tri = np.where(np.arange(128)[None, :] >= np.arange(128)[:, None], 0.0, -MASK).astype(np.float32)
    cmat = np.concatenate([ident, ones, blk2, rotT(64), rotT(32), tri], axis=1)
    tril = (np.arange(128)[:, None] <= np.arange(128)[None, :]).astype(np.float32)
    pm = np.zeros((16, 8), np.float32)
    for qs in range(16):
        cur = qs // 2
        for n in range(8):
            pm[qs, n] = 0.0 if n < cur else (BIGG if n == cur else -2 * BIGG)
    pmask = np.broadcast_to(pm.reshape(1, 128), (128, 128))
    p = np.arange(128)
    invf64 = (10000.0 ** (-(2.0 * (p % 32)) / 64.0)) / (2 * math.pi)
    invf32 = (10000.0 ** (-(2.0 * (p % 16)) / 32.0)) / (2 * math.pi)
    cf = np.concatenate([tril, pmask, invf64[:, None], invf32[:, None]], axis=1).astype(np.float32)
    ind = (-MASK) * (np.arange(SEQ)[None, :] // 256 == np.arange(8)[:, None]).astype(np.float32)
    return np.ascontiguousarray(cmat), np.ascontiguousarray(cf), np.ascontiguousarray(ind)


def _fm(v, c):
    return np.ascontiguousarray(np.asarray(v, np.float32).reshape(c, 128).T)


def _host_params(inp):
    vd = np.stack([np.stack([_fm(inp[k][l], 8) for k in ("pre_mix_norm", "post_mix_norm", "pre_ffn_norm", "post_ffn_norm", "pe_gate_norm")], 1) for l in range(L)], 1)
    vg = np.stack([np.stack([_fm(inp[k][l], 2) for k in ("conv_dw_b", "conv_ln_g", "conv_ln_b", "conv_pw_b", "out_norm_a", "out_norm_b", "out_norm_c")], 1) for l in range(L)], 1)
    wdw = np.stack([np.asarray(inp["conv_dw_w"][l], np.float32).reshape(31, 2, 128).transpose(2, 1, 0) for l in range(L)], 1)
    fcw = np.stack([np.asarray(inp["ffn_conv_w"][l], np.float32).reshape(3, 44, 128).transpose(2, 1, 0) for l in range(L)], 1)
    fcb = np.stack([_fm(inp["ffn_conv_b"][l], 44) for l in range(L)], 1)
    gsub = np.stack([np.asarray(inp["diff_subln_g"][l], np.float32)[np.arange(128) % 64] for l in range(L)], 1)
    lamv = np.stack([np.stack([np.asarray(inp[k][l], np.float32) for k in ("diff_lq1", "diff_lk1", "diff_lq2", "diff_lk2")], 0) for l in range(L)], 0)
    lamv = np.broadcast_to(lamv.reshape(1, L * 4 * 32), (128, L * 4 * 32))
    pv = np.concatenate([vd.reshape(128, -1), vg.reshape(128, -1), wdw.reshape(128, -1), fcw.reshape(128, -1), fcb.reshape(128, -1), gsub.reshape(128, -1), lamv], axis=1)
    glnb = np.stack([np.stack([np.broadcast_to(np.asarray(inp[k][l], np.float32)[None, :], (128, 256)) for k in ("gmlp_ln_g", "gmlp_ln_b")], 1) for l in range(L)], 0)
    bs = np.asarray(inp["gmlp_bs"], np.float32)
    gb = np.zeros((L, 2, 128, 512), np.float32)
    for l in range(L):
        for cc in range(2):
            for hh in range(2):
                gb[l, cc, hh * 64:(hh + 1) * 64, :] = np.tile(bs[l, 2 * cc + hh], 4)[None, :]
    wsT = np.ascontiguousarray(np.asarray(inp["gmlp_ws"], np.float32).transpose(0, 1, 3, 2))
    return np.ascontiguousarray(pv.astype(np.float32)), np.ascontiguousarray(glnb), gb, wsT


PV_OFF = {}


def _pv_layout():
    o = 0
    for name, n in (("vd", L * 5 * 8), ("vg", L * 7 * 2), ("wdw", L * 2 * 31), ("fcw", L * 44 * 3), ("fcb", L * 44), ("gsub", L), ("lamv", L * 4 * 32)):
        PV_OFF[name] = o
        o += n
    return o


PV_N = _pv_layout()


def build(dbg=None, nlayers=L):
    dbg = dbg or []
    nc = bass.Bass("TRN2", target_bir_lowering=False)

    def din(name, shape, dt=F32):
        return nc.dram_tensor(name, list(shape), dt, kind="ExternalInput").ap()

    xT_d = din("xT", [D, SEQ])
    pT_d = din("pT", [L, GW, SEQ])
    pos_d = din("pos", [1, SEQ], I32)
    cmat_d = din("cmat", [128, 768])
    cf_d = din("cf", [128, 258])
    ind_d = din("ind", [8, SEQ])
    pv_d = din("pv", [128, PV_N])
    glnb_d = din("glnb", [L, 128, 2, 256])
    gb_d = din("gb", [L, 2, 128, 512])
    wsT_d = din("wsT", [L, 4, 128, 128])
    w_in_d = din("w_in", [L, D, 10 * GW])
    w_out_d = din("w_out", [L, D, D])
    w_up_d = din("w_up", [L, D, 2 * DFF])
    w_down_d = din("w_down", [L, DFF, D])
    w_gate_d = din("w_pe_gate", [L, D, D])
    w_proj_d = din("w_pe_proj", [L, GW, D])
    w_pw_d = din("conv_pw_w", [L, GW, GW])
    out_d = nc.dram_tensor("outT", [D, SEQ], F32, kind="ExternalOutput").ap()
    wup_s = nc.dram_tensor("wup_s", [L, NJ, 128, 8 * 2 * 128], BF16, kind="Internal").ap()
    wdn_s = nc.dram_tensor("wdn_s", [L, 8, 128, NJ * 128], BF16, kind="Internal").ap()
    dbg_d = {n: nc.dram_tensor("dbg_" + n, [128, w], F32, kind="ExternalOutput").ap() for n, w in dbg}

    es = ExitStack()
    with es:
        S = Sched(nc, es)
        NB = 204 * 1024
        big = es.enter_context(nc.sbuf_tensor("arena", [128, NB // 4], F32))
        A = Arena(big, NB)
        psb = [es.enter_context(nc.psum_tensor("ps%d" % i, [128, 512], F32)) for i in range(8)]
        PS = [(psb[i][:, :], ("ps", i)) for i in range(8)]
        ringA = Ring(PS[0:4])
        ringB = Ring(PS[4:8])

        def MM(out, lhsT, rhs, start, stop, r, w, inc=True, tp=None):
            kw = {}
            if tp is not None:
                kw["tile_position"] = tp
            S.op("pe", lambda: nc.tensor.matmul(out, lhsT=lhsT, rhs=rhs, start=start, stop=stop, **kw), r=r, w=w, inc=inc)

        def ACT(out, in_, func, r, w, scale=None, bias=None):
            kw = {}
            if scale is not None:
                kw["scale"] = scale
            if bias is not None:
                kw["bias"] = bias
            S.op("act", lambda: nc.scalar.activation(out=out, in_=in_, func=func, **kw), r=r, w=w)

        def TTo(eng, out, in0, in1, op, r, w):
            e = nc.vector if eng == "dve" else nc.gpsimd
            S.op(eng, lambda: e.tensor_tensor(out=out, in0=in0, in1=in1, op=op), r=r, w=w)

        def TS(eng, out, in0, s1, s2, op0, op1, r, w):
            e = nc.vector if eng == "dve" else nc.gpsimd
            if op1 is None:
                S.op(eng, lambda: e.tensor_scalar(out=out, in0=in0, scalar1=s1, scalar2=None, op0=op0), r=r, w=w)
            else:
                S.op(eng, lambda: e.tensor_scalar(out=out, in0=in0, scalar1=s1, scalar2=s2, op0=op0, op1=op1), r=r, w=w)

        def STT(out, in0, scalar, in1, op0, op1, r, w):
            S.op("dve", lambda: nc.vector.scalar_tensor_tensor(out=out, in0=in0, scalar=scalar, in1=in1, op0=op0, op1=op1), r=r, w=w)

        def CP(eng, out, in_, r, w):
            e = nc.vector if eng == "dve" else nc.gpsimd
            S.op(eng, lambda: e.tensor_copy(out=out, in_=in_), r=r, w=w)

        def MSET(eng, ap, val, w):
            e = nc.vector if eng == "dve" else nc.gpsimd
            S.op(eng, lambda: e.memset(ap, val), w=w)

        def DMA(q, out, in_, semkey, r, w):
            e = dict(sp=nc.sync, pool=nc.gpsimd, act=nc.scalar)[q]
            S.dma(q, lambda: e.dma_start(out=out, in_=in_), semkey, r=r, w=w)

        def dump(name, ap, keys):
            if name in dbg_d:
                w_ = ap.shape[-1]
                stg = A.f32(w_)
                CP("dve", stg[0:ap.shape[0], :], ap, r=keys, w=[("dbgs", name)])
                DMA("sp", dbg_d[name][0:ap.shape[0], 0:w_], stg[0:ap.shape[0], :], "dbg", r=[("dbgs", name)], w=[("dbgo", name)])

        xT = A.f32(8 * SEQ).rearrange("p (c s) -> p c s", c=8)
        hT = A.bf16(8 * SEQ).rearrange("p (c s) -> p c s", c=8)
        cmat = A.bf16(768)
        IDENT, ONES, BLK2, R64, R32, TRI = [cmat[:, i * 128:(i + 1) * 128] for i in range(6)]
        cf = A.f32(258)
        TRIL = cf[:, 0:128]
        PMASK = cf[:, 128:256].rearrange("p (q n) -> p q n", n=8)
        INVF = [cf[:, 256:257], cf[:, 257:258]]
        pv = A.f32(PV_N)

        def PVv(name, n):
            return pv[:, PV_OFF[name]:PV_OFF[name] + n]

        vd = PVv("vd", L * 5 * 8).rearrange("p (l v c) -> p l v c", l=L, v=5)
        vg = PVv("vg", L * 7 * 2).rearrange("p (l v c) -> p l v c", l=L, v=7)
        wdw = PVv("wdw", L * 2 * 31).rearrange("p (l c k) -> p l c k", l=L, c=2)
        fcw = PVv("fcw", L * 44 * 3).rearrange("p (l j k) -> p l j k", l=L, j=44)
        fcb = PVv("fcb", L * 44).rearrange("p (l j) -> p l j", l=L)
        gsub = PVv("gsub", L)
        lamv = PVv("lamv", L * 4 * 32).rearrange("p (l v k) -> p l v k", l=L, v=4)
        small = A.f32(64)
        lnv = A.f32(512)
        rstd = A.f32(512)
        sq = [A.bf16(512) for _ in range(3)]
        sqring = Ring([(sq[i], ("sq", i)) for i in range(3)])
        PH = A.mark()

        DMA("pool", cmat, cmat_d[:, :], "ld_c", r=[], w=["cmat"])
        DMA("sp", cf, cf_d[:, :], "ld_c2", r=[], w=["cf"])
        DMA("sp", pv, pv_d[:, :], "ld_c3", r=[], w=["pv"])
        for c in range(8):
            DMA("sp", xT[:, c, :], xT_d[c * 128:(c + 1) * 128, :], ("ld_x", c), r=[], w=[("xT", c, t) for t in range(NT)])

        def wload(dst, src, key):
            DMA("pool", dst, src, ("wsem",) + key, r=[], w=[key])

        def precast_ffn(l):
            wupv_ = w_up_d[l].rearrange("(k p) (g n) -> p k g n", p=128, g=2)
            wdnv_ = w_down_d[l].rearrange("(j p) n -> p j n", p=128)
            sk = ("pcs", l)
            keys = []
            for j in range(NJ):
                for gi_ in range(2):
                    dst = wup_s[l, j].rearrange("p (k g n) -> p k g n", k=8, g=2)[:, :, gi_, :]
                    DMA("pool", dst, wupv_[:, :, gi_, j * 128:(j + 1) * 128], sk, r=[], w=[("pcu", l, j, gi_)])
                    keys.append(("pcu", l, j, gi_))
            for dc in range(8):
                dst = wdn_s[l, dc].rearrange("p (j n) -> p j n", j=NJ)
                DMA("pool", dst, wdnv_[:, :, dc * 128:(dc + 1) * 128], sk, r=[], w=[("pcd", l, dc)])
                keys.append(("pcd", l, dc))
            tot = S.cnt[sk]
            for k in keys:
                S.state[k] = [(sk, tot), []]

        def rstd_from(ps_ap, ps_key, inv_n, eps, extra_r=()):
            ACT(lnv, ps_ap, AF.Ln, r=[ps_key] + list(extra_r), w=["lnv"], scale=inv_n, bias=eps)
            ACT(rstd, lnv, AF.Exp, r=["lnv"], w=["rstd"], scale=-0.5)

        def rmsnorm_tile(srcs, skeys, dsts, dkeys, gains, n_feat, eps=EPS, lhs=None, lkey="cmat"):
            ps, pk = ringB.next()
            C = len(srcs)
            for c in range(C):
                q, qk = sqring.next()
                ACT(q, srcs[c], AF.Square, r=[skeys[c]], w=[qk])
                MM(ps, lhs if lhs is not None else ONES, q, c == 0, c == C - 1, r=[qk, lkey], w=[pk])
            rstd_from(ps, pk, 1.0 / n_feat, eps)
            for c in range(C):
                STT(dsts[c], srcs[c], gains[c], rstd, ALU.mult, ALU.mult, r=[skeys[c], "rstd", "pv"], w=[dkeys[c]])

        def sl(t):
            return slice(t * TW, (t + 1) * TW)

        def norm_x_to_h(l, vidx):
            for t in range(NT):
                rmsnorm_tile([xT[:, c, sl(t)] for c in range(8)], [("xT", c, t) for c in range(8)],
                             [hT[:, c, sl(t)] for c in range(8)], [("hT", c, t) for c in range(8)],
                             [vd[:, l, vidx, c:c + 1] for c in range(8)], D)

        def proj(ps, pk, wv, wkey, cs, rhs_fn, rkeys_fn, nk=8):
            for kc in range(nk):
                MM(ps, wv[:, kc, cs], rhs_fn(kc), kc == 0, kc == nk - 1, r=[wkey] + rkeys_fn(kc), w=[pk], inc=(kc == nk - 1))

        def hrhs(t):
            return (lambda kc: hT[:, kc, sl(t)]), (lambda kc: [("hT", kc, t)])

        for l in range(nlayers):
            S.barrier()
            A.release(PH)
            catT = A.bf16(8 * SEQ).rearrange("p (c s) -> p c s", c=8)
            ropeC = A.bf16(SEQ)
            ropeS = A.bf16(SEQ)
            wslots = [A.bf16(8 * 256).rearrange("p (k n) -> p k n", k=8) for _ in range(4)]
            MX = A.mark()
            winv = w_in_d[l].rearrange("(k p) n -> p k n", p=128)
            woutv = w_out_d[l].rearrange("(k p) n -> p k n", p=128)
            mplan = [(lambda v, k, bi=bi: wload(v, winv[:, :, bi * 256:(bi + 1) * 256], k)) for bi in (0, 1, 2, 7, 8, 9, 3, 4, 5, 6)]
            mplan += [(lambda v, k, db=db: wload(v, woutv[:, :, db * 256:(db + 1) * 256], k)) for _t in range(NT) for db in range(4)]
            mstream = WStream([(wslots[i], ("ws", i)) for i in range(4)], mplan, auto=False)

            def win_block(bi):
                return mstream.get()

            def rope_tables(which):
                m = A.mark()
                posi = A.i32(SEQ)
                v = A.f32(SEQ)
                ki = A.i32(SEQ)
                DMA("sp", posi, pos_d[0:1, :].broadcast_to([128, SEQ]), "ld_pos", r=[], w=["posi"])
                for tab, shift, key in ((ropeS, 0.0, "ropeS"), (ropeC, 0.25, "ropeC")):
                    TS("dve", v, posi, INVF[which], shift, ALU.mult, ALU.add, r=["posi", "cf"], w=["ropev"])
                    CP("dve", ki, v, r=["ropev"], w=["ropek"])
                    TTo("dve", v, v, ki, ALU.subtract, r=["ropev", "ropek"], w=["ropev"])
                    ACT(tab, v, AF.Sin, r=["ropev"], w=[key], scale=2 * math.pi * (1 - 1e-6))
                S.barrier()
                A.release(m)

            def rope_apply(ps, pk, t, RM, zbr, t1, t2):
                zb, zk = zbr.next()
                ACT(zb, ps, AF.Copy, r=[pk], w=[zk])
                ps2, pk2 = ringA.next()
                MM(ps2, RM, zb, True, True, r=[zk, "cmat"], w=[pk2])
                TTo("dve", t1[0], zb, ropeC[:, sl(t)], ALU.mult, r=[zk, "ropeC"], w=[t1[1]])
                TTo("dve", t2[0], ps2, ropeS[:, sl(t)], ALU.mult, r=[pk2, "ropeS"], w=[t2[1]])

            def attn_finalize_recip(O, ok, dst_rden):
                ACT(dst_rden[0][0:64, :], O[64:128, :], AF.Ln, r=[ok], w=[dst_rden[1]])
                ACT(dst_rden[0][0:64, :], dst_rden[0][0:64, :], AF.Exp, r=[dst_rden[1]], w=[dst_rden[1]], scale=-1.0)

            def vproj(wv, wk, cs, vaug):
                for g in range(4):
                    ps, pk = ringA.next()
                    for s4 in range(4):
                        st = g * 4 + s4
                        for kc in range(8):
                            MM(ps[:, s4 * 128:(s4 + 1) * 128], hT[:, kc, st * 128:(st + 1) * 128], wv[:, kc, cs], kc == 0, kc == 7,
                               r=[wk, ("hT", kc, st // 4)], w=[pk], inc=(kc == 7 and s4 == 3))
                    ACT(vaug[:, g * 4:(g + 1) * 4, :, 0:64], ps.rearrange("p (s h d) -> p s h d", s=4, h=2), AF.Copy, r=[pk], w=[("V", g)])

            norm_x_to_h(l, 0)
            if l == 0:
                dump("hT0", hT[:, 0, 0:512], [("hT", 0, 0)])

            rope_tables(0)
            m0 = A.mark()
            Kaug = [A.bf16(SEQ) for _ in range(2)]
            vaug = A.bf16(16 * 2 * 128).rearrange("p (s h d) -> p s h d", s=16, h=2)
            Qa = [[A.bf16(512) for _ in range(2)] for _ in range(NT)]
            Pr = Ring([(A.bf16(512), ("P", i)) for i in range(4)])
            zbr = Ring([(A.bf16(512), ("zb", i)) for i in range(2)])
            t1 = (A.f32(512), "t1")
            t2 = (A.f32(512), "t2")
            rden = (A.f32(512), "rden")
            bstg4 = A.bf16(4 * 72).rearrange("p (j n) -> p j n", j=4)
            gm = A.f32(64)
            top8 = A.f32(32)
            kmf = A.f32(16)
            kmb = A.bf16(16)
            MSET("dve", vaug[:, :, :, 64:128], 1.0, w=[("V", g) for g in range(4)])
            MSET("dve", bstg4, 0.0, w=["bstg"])
            for hh in range(2):
                DMA("pool", Kaug[hh][64:72, :], ind_d[:, :], "ld_ind", r=[], w=[("Kind", hh)])
            wq, wqk = win_block(0)
            wk_, wkk = win_block(1)
            wv_, wvk = win_block(2)
            precast_ffn(l)
            for c in range(2):
                cs = slice(c * 128, (c + 1) * 128)
                for t in range(NT):
                    ps, pk = ringA.next()
                    rf, kf = hrhs(t)
                    proj(ps, pk, wk_, wkk, cs, rf, kf)
                    rope_apply(ps, pk, t, R64, zbr, t1, t2)
                    for hh in range(2):
                        TTo("dve", Kaug[hh][0:64, sl(t)], t1[0][hh * 64:(hh + 1) * 64, :], t2[0][hh * 64:(hh + 1) * 64, :], ALU.add,
                            r=[t1[1], t2[1]], w=[("K", hh, t)])
                for hh in range(2):
                    S.op("dve", lambda hh=hh: nc.vector.tensor_reduce(out=kmf[0:64, hh * 8:(hh + 1) * 8], in_=Kaug[hh][0:64, :].rearrange("p (n k) -> p n k", k=256), axis=AX.X, op=ALU.add),
                         r=[("K", hh, t) for t in range(NT)], w=[("kmf", hh)])
                    TS("dve", kmb[0:64, hh * 8:(hh + 1) * 8], kmf[0:64, hh * 8:(hh + 1) * 8], 1.0 / 256, None, ALU.mult, None, r=[("kmf", hh)], w=[("kmb", hh)])
                vproj(wv_, wvk, cs, vaug)
                for qt in range(NT):
                    qb = Qa[qt]
                    ps, pk = ringA.next()
                    rf, kf = hrhs(qt)
                    proj(ps, pk, wq, wqk, cs, rf, kf)
                    rope_apply(ps, pk, qt, R64, zbr, t1, t2)
                    for hh in range(2):
                        TTo("dve", qb[hh][0:64, :], t1[0][hh * 64:(hh + 1) * 64, :], t2[0][hh * 64:(hh + 1) * 64, :], ALU.add,
                            r=[t1[1], t2[1]], w=[("Qa", qt, hh)])
                    for hh in range(2):
                        gps, gk = ringA.next()
                        for j in range(4):
                            MM(gps[:, j * 8:(j + 1) * 8], qb[hh][0:64, j * 128:(j + 1) * 128], kmb[0:64, hh * 8:(hh + 1) * 8], True, True,
                               r=[("Qa", qt, hh), ("kmb", hh)], w=[gk], inc=(j == 3))
                        gmv = gm[:, 0:32].rearrange("p (j n) -> p j n", n=8)
                        TTo("dve", gmv, gps[:, 0:32].rearrange("p (j n) -> p j n", n=8), PMASK[:, qt * 4:(qt + 1) * 4, :], ALU.add, r=[gk, "cf"], w=["gm"])
                        tps, tk = ringA.next()
                        tpsb = tps.bitcast(BF16)
                        top8v = top8.rearrange("p (j n) -> p j n", n=8)
                        for j in range(4):
                            S.op("dve", lambda j=j: nc.vector.max(out=top8[:, j * 8:(j + 1) * 8], in_=gm[:, j * 8:(j + 1) * 8]), r=["gm"], w=["top8"])
                        TS("dve", top8v[:, :, 3:4], top8v[:, :, 3:4], -BIGG, None, ALU.max, None, r=["top8"], w=["top8"])
                        TTo("dve", bstg4[:, :, 64:72], gmv, top8v[:, :, 3:4].broadcast_to([128, 4, 8]), ALU.is_lt, r=["gm", "top8"], w=["bstg"])
                        for j in range(4):
                            S.op("pe", lambda j=j, tpsb=tpsb: nc.tensor.transpose(tpsb[0:72, j * 128:(j + 1) * 128], bstg4[:, j, :], IDENT), r=["bstg", "cmat"], w=[tk], inc=(j == 3))
                        ACT(qb[hh][64:72, :], tpsb[64:72, 0:512], AF.Copy, r=[tk], w=[("Qb", qt, hh)])
                for qt in range(NT):
                    qb = Qa[qt]
                    for hh in range(2):
                        O, ok = ringB.next()
                        nk = 4 * qt + 4
                        pend = []
                        for kt in range(nk):
                            jd = kt - 4 * qt
                            c0 = 128 * jd if jd > 0 else 0
                            sp_, sk = ringA.next()
                            MM(sp_[:, c0:512], Kaug[hh][0:72, kt * 128:(kt + 1) * 128], qb[hh][0:72, c0:512], True, jd < 0,
                               r=[("K", hh, kt // 4), ("Kind", hh), ("Qa", qt, hh), ("Qb", qt, hh)], w=[sk], inc=(jd < 0))
                            if jd >= 0:
                                MM(sp_[:, c0:c0 + 128], IDENT, TRI, False, True, r=["cmat"], w=[sk])
                            P, Pk = Pr.next()
                            ACT(P[:, c0:512], sp_[:, c0:512], AF.Exp, r=[sk], w=[Pk], scale=0.125)

                            def pv_(kt=kt, c0=c0, P=P, Pk=Pk, O=O, ok=ok, hh=hh, nk=nk):
                                MM(O[:, c0:512], vaug[:, kt, hh, :], P[:, c0:512], kt == 0, kt == nk - 1, r=[Pk, ("V", kt // 4)], w=[ok])
                            pend.append(pv_)
                            if len(pend) > 2:
                                pend.pop(0)()
                        for f in pend:
                            f()
                        attn_finalize_recip(O, ok, rden)
                        TTo("dve", catT[hh * 64:(hh + 1) * 64, c, sl(qt)], O[0:64, :], rden[0][0:64, :], ALU.mult, r=[ok, rden[1]], w=[("cat", c, qt)])
            if l == 0:
                dump("oa_pre", catT[:, 0, 0:512], [("cat", 0, 0)])
            for t in range(NT):
                rmsnorm_tile([catT[:, c, sl(t)] for c in range(2)], [("cat", c, t) for c in range(2)],
                             [catT[:, c, sl(t)] for c in range(2)], [("cat", c, t) for c in range(2)],
                             [vg[:, l, 4, c:c + 1] for c in range(2)], GW)
            if l == 0:
                dump("oa", catT[:, 0, 0:512], [("cat", 0, 0)])
            mstream.done()
            S.barrier()
            A.release(m0)

            rope_tables(1)
            lam_init = 0.8 - 0.6 * math.exp(-0.3 * l)
            lt = small[:, 0:8]
            prod = A.f32(64)
            TTo("dve", prod[:, 0:32], lamv[:, l, 0, :], lamv[:, l, 1, :], ALU.mult, r=["pv"], w=["prod"])
            S.op("dve", lambda: nc.vector.tensor_reduce(out=lt[:, 0:1], in_=prod[:, 0:32], axis=AX.X, op=ALU.add), r=["prod"], w=["lt"])
            TTo("dve", prod[:, 32:64], lamv[:, l, 2, :], lamv[:, l, 3, :], ALU.mult, r=["pv"], w=["prod2"])
            S.op("dve", lambda: nc.vector.tensor_reduce(out=lt[:, 1:2], in_=prod[:, 32:64], axis=AX.X, op=ALU.add), r=["prod2"], w=["lt"])
            ACT(lt[:, 2:4], lt[:, 0:2], AF.Exp, r=["lt"], w=["lt"])
            TTo("dve", lt[:, 4:5], lt[:, 3:4], lt[:, 2:3], ALU.subtract, r=["lt"], w=["lt"])
            TS("dve", lt[:, 5:6], lt[:, 4:5], -lam_init, None, ALU.add, None, r=["lt"], w=["lt"])
            TS("dve", lt[:, 6:7], gsub[:, l:l + 1], 1.0 - lam_init, None, ALU.mult, None, r=["pv", "lt"], w=["lt"])
            NEGLAM = lt[:, 5:6]
            GS = lt[:, 6:7]

            m0 = A.mark()
            Kd = A.bf16(SEQ)
            vaug = A.bf16(16 * 2 * 128).rearrange("p (s h d) -> p s h d", s=16, h=2)
            Qd = [A.bf16(512) for _ in range(NT)]
            Pr = Ring([(A.bf16(512), ("P", i)) for i in range(4)])
            zbr = Ring([(A.bf16(512), ("zb", i)) for i in range(2)])
            t1 = (A.f32(512), "t1")
            t2 = (A.f32(512), "t2")
            rden = (A.f32(512), "rden")
            od = A.f32(512)
            MSET("dve", vaug[:, :, :, 64:128], 1.0, w=[("V", g) for g in range(4)])
            wq, wqk = win_block(7)
            wk_, wkk = win_block(8)
            wv_, wvk = win_block(9)
            dscale = 32.0 ** -0.5
            for c in range(2):
                cs = slice(c * 128, (c + 1) * 128)
                for t in range(NT):
                    ps, pk = ringA.next()
                    rf, kf = hrhs(t)
                    proj(ps, pk, wk_, wkk, cs, rf, kf)
                    rope_apply(ps, pk, t, R32, zbr, t1, t2)
                    TTo("dve", Kd[:, sl(t)], t1[0], t2[0], ALU.add, r=[t1[1], t2[1]], w=[("Kd", t)])
                vproj(wv_, wvk, cs, vaug)
                for qt in range(NT):
                    qd = Qd[qt]
                    ps, pk = ringA.next()
                    rf, kf = hrhs(qt)
                    proj(ps, pk, wq, wqk, cs, rf, kf)
                    rope_apply(ps, pk, qt, R32, zbr, t1, t2)
                    TTo("dve", qd, t1[0], t2[0], ALU.add, r=[t1[1], t2[1]], w=[("Qd", qt)])
                for qt in range(NT):
                    qd = Qd[qt]
                    for hh in range(2):
                        Os = [ringB.next(), ringB.next()]
                        nk = 4 * qt + 4
                        pend = []
                        for kt in range(nk):
                            jd = kt - 4 * qt
                            c0 = 128 * jd if jd > 0 else 0
                            cur = []
                            for m_ in range(2):
                                g = hh * 2 + m_
                                sp_, sk = ringA.next()
                                MM(sp_[:, c0:512], Kd[32 * g:32 * g + 32, kt * 128:(kt + 1) * 128], qd[32 * g:32 * g + 32, c0:512], True, jd < 0,
                                   r=[("Kd", kt // 4), ("Qd", qt)], w=[sk], inc=(jd < 0), tp=(32 * g, 0))
                                if jd >= 0:
                                    MM(sp_[:, c0:c0 + 128], IDENT, TRI, False, True, r=["cmat"], w=[sk])
                                cur.append((sp_, sk))
                            for f in pend:
                                f()
                            pend = []
                            for m_ in range(2):
                                sp_, sk = cur[m_]
                                P, Pk = Pr.next()
                                ACT(P[:, c0:512], sp_[:, c0:512], AF.Exp, r=[sk], w=[Pk], scale=dscale)

                                def pv_(kt=kt, c0=c0, P=P, Pk=Pk, Oo=Os[m_], hh=hh, nk=nk):
                                    MM(Oo[0][:, c0:512], vaug[:, kt, hh, :], P[:, c0:512], kt == 0, kt == nk - 1, r=[Pk, ("V", kt // 4)], w=[Oo[1]])
                                pend.append(pv_)
                        for f in pend:
                            f()
                        hs = slice(hh * 64, (hh + 1) * 64)
                        attn_finalize_recip(Os[0][0], Os[0][1], rden)
                        TTo("dve", t1[0][0:64, :], Os[0][0][0:64, :], rden[0][0:64, :], ALU.mult, r=[Os[0][1], rden[1]], w=[t1[1]])
                        attn_finalize_recip(Os[1][0], Os[1][1], rden)
                        TTo("dve", t2[0][0:64, :], Os[1][0][0:64, :], rden[0][0:64, :], ALU.mult, r=[Os[1][1], rden[1]], w=[t2[1]])
                        STT(od[hs, :], t2[0][0:64, :], NEGLAM[0:64, :], t1[0][0:64, :], ALU.mult, ALU.add, r=[t1[1], t2[1], "lt"], w=[("od", hh)])
                    ps, pk = ringB.next()
                    q_, qk_ = sqring.next()
                    ACT(q_, od, AF.Square, r=[("od", 0), ("od", 1)], w=[qk_])
                    MM(ps, BLK2, q_, True, True, r=[qk_, "cmat"], w=[pk])
                    rstd_from(ps, pk, 1.0 / 64, 1e-5)
                    STT(catT[:, 6 + c, sl(qt)], od, GS, rstd, ALU.mult, ALU.mult, r=[("od", 0), ("od", 1), "rstd", "lt"], w=[("cat", 6 + c, qt)])
            if l == 0:
                dump("od", catT[:, 6, 0:512], [("cat", 6, 0)])
            mstream.done()
            S.barrier()
            A.release(m0)

            m0 = A.mark()
            uT = A.bf16(2 * SEQ).rearrange("p (c s) -> p c s", c=2)
            vgl = A.bf16(16 * 256).rearrange("p (n d) -> p n d", n=16)
            glnb = A.f32(512).rearrange("p (v d) -> p v d", v=2)
            gb = A.f32(1024).rearrange("p (c s) -> p c s", c=2)
            wsf = A.f32(512).rearrange("p (h i) -> p h i", h=4)
            wsb = A.bf16(512).rearrange("p (h i) -> p h i", h=4)
            stats = A.f32(16 * 6)
            mv = A.f32(16 * 2).rearrange("p (n k) -> p n k", k=2)
            rsd = A.f32(16)
            vtmp = A.f32(256)
            vln = [A.bf16(256) for _ in range(2)]
            stmp = A.f32(512)
            DMA("sp", glnb, glnb_d[l], "ld_g1", r=[], w=["glnb"])
            DMA("sp", gb, gb_d[l].rearrange("c p s -> p c s"), "ld_g2", r=[], w=["gb"])
            DMA("sp", wsf, wsT_d[l].rearrange("h j i -> j h i"), "ld_g3", r=[], w=["wsf"])
            for h in range(4):
                TTo("dve", wsb[:, h, :], wsf[:, h, :], TRIL, ALU.mult, r=["wsf", "cf"], w=["wsb"])
            wu, wuk = win_block(3)
            wvv, wvvk = win_block(4)
            for t in range(NT):
                for cc in range(2):
                    ps, pk = ringA.next()
                    rf, kf = hrhs(t)
                    proj(ps, pk, wu, wuk, slice(cc * 128, (cc + 1) * 128), rf, kf)
                    ACT(uT[:, cc, sl(t)], ps, AF.Gelu_apprx_tanh, r=[pk], w=[("uT", cc, t)])
            for n2 in range(8):
                ps, pk = ringA.next()
                for s2 in range(2):
                    n = n2 * 2 + s2
                    for kc in range(8):
                        MM(ps[:, s2 * 256:(s2 + 1) * 256], hT[:, kc, n * 128:(n + 1) * 128], wvv[:, kc, :], kc == 0, kc == 7,
                           r=[wvvk, ("hT", kc, n // 4)], w=[pk], inc=(kc == 7 and s2 == 1))
                ACT(vgl[:, n2 * 2:(n2 + 1) * 2, :], ps.rearrange("p (s d) -> p s d", s=2), AF.Gelu_apprx_tanh, r=[pk], w=[("vgl", n2)])
            for n in range(16):
                S.op("dve", lambda n=n: nc.vector.bn_stats(out=stats[:, n * 6:(n + 1) * 6], in_=vgl[:, n, :]), r=[("vgl", n // 2)], w=[("st", n)])
                S.op("dve", lambda n=n: nc.vector.bn_aggr(out=mv[:, n, :], in_=stats[:, n * 6:(n + 1) * 6]), r=[("st", n)], w=["mv"])
            ACT(rsd, mv[:, :, 1], AF.Ln, r=["mv"], w=["rsd"], bias=EPS)
            ACT(rsd, rsd, AF.Exp, r=["rsd"], w=["rsd"], scale=-0.5)
            for t in range(NT):
                pss = [ringA.next(), ringA.next()]
                for s4 in range(4):
                    n = t * 4 + s4
                    vl = vln[n % 2]
                    vk = ("vln", n % 2)
                    TS("dve", vtmp, vgl[:, n, :], mv[:, n, 0:1], rsd[:, n:n + 1], ALU.subtract, ALU.mult, r=[("vgl", n // 2), "mv", "rsd"], w=["vtmp"])
                    TTo("dve", vtmp, vtmp, glnb[:, 0, :], ALU.mult, r=["vtmp", "glnb"], w=["vtmp"])
                    TTo("dve", vl, vtmp, glnb[:, 1, :], ALU.add, r=["vtmp", "glnb"], w=[vk])
                    for cc in range(2):
                        for hh in range(2):
                            h = 2 * cc + hh
                            MM(pss[cc][0][hh * 64:(hh + 1) * 64, s4 * 128:(s4 + 1) * 128], vl[:, h * 64:(h + 1) * 64], wsb[:, h, :], True, True,
                               r=[vk, "wsb"], w=[pss[cc][1]], tp=(0, hh * 64))
                for cc in range(2):
                    TTo("dve", stmp, pss[cc][0], gb[:, cc, :], ALU.add, r=[pss[cc][1], "gb"], w=["stmp"])
                    TTo("dve", catT[:, 2 + cc, sl(t)], stmp, uT[:, cc, sl(t)], ALU.mult, r=["stmp", ("uT", cc, t)], w=[("cat", 2 + cc, t)])
            if l == 0:
                dump("ob_pre", catT[:, 2, 0:512], [("cat", 2, 0)])
            for t in range(NT):
                rmsnorm_tile([catT[:, 2 + c, sl(t)] for c in range(2)], [("cat", 2 + c, t) for c in range(2)],
                             [catT[:, 2 + c, sl(t)] for c in range(2)], [("cat", 2 + c, t) for c in range(2)],
                             [vg[:, l, 5, c:c + 1] for c in range(2)], GW)
            mstream.done()
            S.barrier()
            A.release(m0)

            m0 = A.mark()
            ybuf = A.bf16(32 + SEQ)
            diag = A.bf16(31 * 128).rearrange("p (k n) -> p k n", k=31)
            cy = A.bf16(2 * SEQ).rearrange("p (c s) -> p c s", c=2)
            sg = A.f32(512)
            wpw = A.bf16(2 * 256).rearrange("p (k n) -> p k n", k=2)
            mstat = A.f32(512)
            m2 = A.f32(512)
            yn = A.f32(512)
            sil = A.bf16(1024).rearrange("p (c s) -> p c s", c=2)
            Y0 = 2
            wa, wak = win_block(5)
            wg_, wgk = win_block(6)
            wload(wpw, w_pw_d[l].rearrange("(k p) n -> p k n", p=128), ("wpw",))
            MSET("dve", ybuf[:, 0:32], 0.0, w=["ypad"])
            for cc in range(2):
                cs = slice(cc * 128, (cc + 1) * 128)
                for k in range(31):
                    TS("dve", diag[:, k, :], IDENT, wdw[:, l, cc, k:k + 1], None, ALU.mult, None, r=["cmat", "pv"], w=[("diag", k)])
                for t in range(NT):
                    pa, pak = ringA.next()
                    rf, kf = hrhs(t)
                    proj(pa, pak, wa, wak, cs, rf, kf)
                    pg, pgk = ringA.next()
                    proj(pg, pgk, wg_, wgk, cs, rf, kf)
                    ACT(sg, pg, AF.Sigmoid, r=[pgk], w=["sg"])
                    TTo("dve", ybuf[:, 32 + t * TW:32 + (t + 1) * TW], pa, sg, ALU.mult, r=[pak, "sg"], w=[("yb", t)])
                for t in range(NT):
                    ps, pk = ringB.next()
                    for k in range(31):
                        o = Y0 + t * TW + k
                        rk = ["ypad", ("diag", k), ("yb", t)] + ([("yb", t - 1)] if t > 0 else [])
                        MM(ps, diag[:, k, :], ybuf[:, o:o + TW], k == 0, k == 30, r=rk, w=[pk], inc=(k == 30))
                    ACT(cy[:, cc, sl(t)], ps, AF.Identity, r=[pk, "pv"], w=[("cy", cc, t)], bias=vg[:, l, 0, cc:cc + 1])
            if l == 0:
                dump("cy", cy[:, 0, 0:512], [("cy", 0, 0)])
            for t in range(NT):
                ps1, pk1 = ringB.next()
                ps2, pk2 = ringB.next()
                for cc in range(2):
                    MM(ps1, ONES, cy[:, cc, sl(t)], cc == 0, cc == 1, r=[("cy", cc, t), "cmat"], w=[pk1])
                for cc in range(2):
                    q_, qk_ = sqring.next()
                    ACT(q_, cy[:, cc, sl(t)], AF.Square, r=[("cy", cc, t)], w=[qk_])
                    MM(ps2, ONES, q_, cc == 0, cc == 1, r=[qk_, "cmat"], w=[pk2])
                TS("dve", mstat, ps1, 1.0 / GW, None, ALU.mult, None, r=[pk1], w=["mstat"])
                TTo("dve", m2, mstat, mstat, ALU.mult, r=["mstat"], w=["m2"])
                STT(m2, ps2, 1.0 / GW, m2, ALU.mult, ALU.subtract, r=[pk2, "m2"], w=["m2"])
                ACT(lnv, m2, AF.Ln, r=["m2"], w=["lnv"], bias=EPS)
                ACT(rstd, lnv, AF.Exp, r=["lnv"], w=["rstd"], scale=-0.5)
                for cc in range(2):
                    TTo("dve", yn, cy[:, cc, sl(t)], mstat, ALU.subtract, r=[("cy", cc, t), "mstat"], w=["yn"])
                    TTo("dve", yn, yn, rstd, ALU.mult, r=["yn", "rstd"], w=["yn"])
                    ACT(sil[:, cc, :], yn, AF.Silu, r=["yn", "pv"], w=[("sil", cc)], scale=vg[:, l, 1, cc:cc + 1], bias=vg[:, l, 2, cc:cc + 1])
                for co in range(2):
                    ps, pk = ringA.next()
                    for ci in range(2):
                        MM(ps, wpw[:, ci, co * 128:(co + 1) * 128], sil[:, ci, :], ci == 0, ci == 1, r=[("wpw",), ("sil", ci)], w=[pk])
                    ACT(catT[:, 4 + co, sl(t)], ps, AF.Identity, r=[pk, "pv"], w=[("cat", 4 + co, t)], bias=vg[:, l, 3, co:co + 1])
            if l == 0:
                dump("oc_pre", catT[:, 4, 0:512], [("cat", 4, 0)])
            for t in range(NT):
                rmsnorm_tile([catT[:, 4 + c, sl(t)] for c in range(2)], [("cat", 4 + c, t) for c in range(2)],
                             [catT[:, 4 + c, sl(t)] for c in range(2)], [("cat", 4 + c, t) for c in range(2)],
                             [vg[:, l, 6, c:c + 1] for c in range(2)], GW)
            mstream.done()
            S.barrier()
            A.release(m0)

            def out_proj_norm_res(nkc, wsrc, rhs_fn, rkeys_fn, vidx, wr, ytile):
                for t in range(NT):
                    for db in range(4):
                        wv, wk = wr.get()
                        for dd in range(2):
                            dc = db * 2 + dd
                            ps, pk = ringA.next()
                            for kc in range(nkc):
                                MM(ps, wv[:, kc, dd * 128:(dd + 1) * 128], rhs_fn(kc, t), kc == 0, kc == nkc - 1, r=[wk] + rkeys_fn(kc, t), w=[pk], inc=(kc == nkc - 1))
                            if dc % 2 == 0:
                                ACT(ytile[:, dc, :], ps, AF.Copy, r=[pk], w=[("yt", dc)])
                            else:
                                CP("dve", ytile[:, dc, :], ps, r=[pk], w=[("yt", dc)])
                        wr.done()
                    rmsnorm_tile([ytile[:, dc, :] for dc in range(8)], [("yt", dc) for dc in range(8)],
                                 [ytile[:, dc, :] for dc in range(8)], [("yt", dc) for dc in range(8)],
                                 [vd[:, l, vidx, dc:dc + 1] for dc in range(8)], D)
                    for dc in range(8):
                        TTo("dve", xT[:, dc, sl(t)], xT[:, dc, sl(t)], ytile[:, dc, :], ALU.add, r=[("xT", dc, t), ("yt", dc)], w=[("xT", dc, t)])

            m0 = A.mark()
            ytile = A.f32(8 * 512).rearrange("p (c s) -> p c s", c=8)
            out_proj_norm_res(8, w_out_d[l].rearrange("(k p) n -> p k n", p=128), lambda kc, t: catT[:, kc, sl(t)], lambda kc, t: [("cat", kc, t)], 1, mstream, ytile)
            if l == 0:
                dump("x1", xT[:, 0, 0:512], [("xT", 0, 0)])
            S.barrier()
            A.release(MX)
            A.release(PH)

            norm_x_to_h(l, 2)
            ytile = A.f32(8 * 512).rearrange("p (c s) -> p c s", c=8)
            fT = A.bf16(NJ * 512).rearrange("p (j s) -> p j s", j=NJ)
            ub = [[A.f32(2 + 512) for _ in range(2)] for _ in range(2)]
            halo = A.f32(44 * 2).rearrange("p (j k) -> p j k", k=2)
            tas = [A.f32(512) for _ in range(2)]
            tbs = [A.f32(512) for _ in range(2)]
            cgs = [A.f32(512) for _ in range(2)]
            cvs = [A.f32(512) for _ in range(2)]
            wups = [A.bf16(8 * 2 * 128).rearrange("p (k g n) -> p k g n", k=8, g=2) for _ in range(3)]
            wdns = [A.bf16(NJ * 128).rearrange("p (j n) -> p j n", j=NJ) for _ in range(3)]
            MSET("dve", halo, 0.0, w=["halo"])
            wupv = w_up_d[l].rearrange("(k p) (g n) -> p k g n", p=128, g=2)
            wdnv = w_down_d[l].rearrange("(j p) n -> p j n", p=128)

            def ld_up(v, k, j):
                DMA("sp", v.rearrange("p k g n -> p (k g n)"), wup_s[l, j], ("wsem",) + k, r=[("pcu", l, j, 0), ("pcu", l, j, 1)], w=[k])

            def ld_dn(v, k, dc):
                DMA("sp", v.rearrange("p j n -> p (j n)"), wdn_s[l, dc], ("wsem",) + k, r=[("pcd", l, dc)], w=[k])

            wur = WStream([(wups[i], ("wu", i)) for i in range(3)], [(lambda v, k, j=j: ld_up(v, k, j)) for _t in range(NT) for j in range(NJ)])
            wdr = WStream([(wdns[i], ("wd", i)) for i in range(3)], [(lambda v, k, dc=dc: ld_dn(v, k, dc)) for _t in range(NT) for dc in range(8)])
            wur.top()
            wdr.top()
            pend_f = None
            for t in range(NT):
                for j in range(NJ):
                    wv, wk = wur.get()
                    u = ub[j % 2]
                    b2 = j % 2
                    ta, tb, cg, cv = tas[b2], tbs[b2], cgs[b2], cvs[b2]
                    for gi in range(2):
                        jj = j + gi * NJ
                        ps, pk = ringA.next()
                        for kc in range(8):
                            MM(ps, wv[:, kc, gi, :], hT[:, kc, sl(t)], kc == 0, kc == 7, r=[wk, ("hT", kc, t)], w=[pk], inc=(kc == 7))
                        uk = ("ub", b2, gi)
                        ACT(u[gi][:, 2:514], ps, AF.Copy, r=[pk], w=[uk])
                        CP("dve", u[gi][:, 0:2], halo[:, jj, :], r=["halo", ("halo", jj)], w=[uk])
                        tmp = ta if gi == 0 else tb
                        tk_ = ("ta", b2) if gi == 0 else ("tb", b2)
                        ACT(tmp, ps, AF.Identity, r=[pk, "pv"], w=[tk_], scale=fcw[:, l, jj, 2:3], bias=fcb[:, l, jj:jj + 1])
                        ACT(halo[:, jj, :], ps[:, 510:512], AF.Copy, r=[pk], w=[("halo", jj)])
                        dst = cg if gi == 0 else cv
                        dk = ("cg", b2) if gi == 0 else ("cv", b2)
                        STT(tmp, u[gi][:, 1:513], fcw[:, l, jj, 1:2], tmp, ALU.mult, ALU.add, r=[uk, tk_, "pv"], w=[tk_])
                        STT(dst, u[gi][:, 0:512], fcw[:, l, jj, 0:1], tmp, ALU.mult, ALU.add, r=[uk, tk_, "pv"], w=[dk])
                    if pend_f is not None:
                        pend_f()

                    def fin(j=j, b2=b2, cg=cg, cv=cv):
                        ACT(cg, cg, AF.Gelu_apprx_tanh, r=[("cg", b2)], w=[("cg", b2)])
                        TTo("dve", fT[:, j, :], cg, cv, ALU.mult, r=[("cg", b2), ("cv", b2)], w=[("fT", j)])
                    pend_f = fin
                pend_f()
                pend_f = None
                if l == 0 and t == 0:
                    dump("fT", fT[:, 0, :], [("fT", 0)])
                for dc in range(8):
                    wv, wk = wdr.get()
                    ps, pk = ringB.next()
                    for j in range(NJ):
                        MM(ps, wv[:, j, :], fT[:, j, :], j == 0, j == NJ - 1, r=[wk, ("fT", j)], w=[pk], inc=(j == NJ - 1))
                    if dc % 2 == 0:
                        ACT(ytile[:, dc, :], ps, AF.Copy, r=[pk], w=[("yt", dc)])
                    else:
                        CP("dve", ytile[:, dc, :], ps, r=[pk], w=[("yt", dc)])
                rmsnorm_tile([ytile[:, dc, :] for dc in range(8)], [("yt", dc) for dc in range(8)],
                             [ytile[:, dc, :] for dc in range(8)], [("yt", dc) for dc in range(8)],
                             [vd[:, l, 3, dc:dc + 1] for dc in range(8)], D)
                for dc in range(8):
                    TTo("dve", xT[:, dc, sl(t)], xT[:, dc, sl(t)], ytile[:, dc, :], ALU.add, r=[("xT", dc, t), ("yt", dc)], w=[("xT", dc, t)])
            if l == 0:
                dump("x2", xT[:, 0, 0:512], [("xT", 0, 0)])
            S.barrier()
            A.release(PH)

            norm_x_to_h(l, 4)
            pTb = A.bf16(2 * SEQ).rearrange("p (c s) -> p c s", c=2)
            wgs = [A.bf16(8 * 256).rearrange("p (k n) -> p k n", k=8) for _ in range(3)]
            wpj = A.bf16(2 * D).rearrange("p (k n) -> p k n", k=2)
            sgt = [A.f32(512) for _ in range(2)]
            pjt = [A.f32(512) for _ in range(2)]
            wload(pTb, pT_d[l].rearrange("(k p) s -> p k s", p=128), ("pTb",))
            wload(wpj, w_proj_d[l].rearrange("(k p) n -> p k n", p=128), ("wpj",))
            wgv = w_gate_d[l].rearrange("(k p) n -> p k n", p=128)
            wgr = WStream([(wgs[i], ("wg", i)) for i in range(3)], [(lambda v, k, db=db: wload(v, wgv[:, :, db * 256:(db + 1) * 256], k)) for db in range(4)])
            wgr.top()
            it = 0
            for db in range(4):
                wv, wk = wgr.get()
                for dd in range(2):
                    dc = db * 2 + dd
                    for t in range(NT):
                        b = it % 2
                        it += 1
                        ps, pk = ringA.next()
                        rf, kf = hrhs(t)
                        proj(ps, pk, wv, wk, slice(dd * 128, (dd + 1) * 128), rf, kf)
                        ACT(sgt[b], ps, AF.Sigmoid, r=[pk], w=[("sgt", b)])
                        ps2, pk2 = ringB.next()
                        for kc in range(2):
                            MM(ps2, wpj[:, kc, dc * 128:(dc + 1) * 128], pTb[:, kc, sl(t)], kc == 0, kc == 1, r=[("wpj",), ("pTb",)], w=[pk2], inc=(kc == 1))
                        TTo("dve", pjt[b], ps2, sgt[b], ALU.mult, r=[pk2, ("sgt", b)], w=[("pjt", b)])
                        TTo("dve", xT[:, dc, sl(t)], xT[:, dc, sl(t)], pjt[b], ALU.add, r=[("xT", dc, t), ("pjt", b)], w=[("xT", dc, t)])
            if l == 0:
                dump("x3", xT[:, 0, 0:512], [("xT", 0, 0)])

        for c in range(8):
            DMA("sp", out_d[c * 128:(c + 1) * 128, :], xT[:, c, :], "st_out", r=[("xT", c, t) for t in range(NT)], w=[("out", c)])
        S.barrier()
        S.emit()
        build.arena_hi = A.hi
    return nc


_CACHE = {}


def _prep_inputs(inp):
    cmat, cf, ind = _host_consts()
    pv, glnb, gb, wsT = _host_params(inp)
    x = np.asarray(inp["x"], np.float32)
    p = np.asarray(inp["p"], np.float32)
    pos = np.asarray(inp["positions"], np.int32)
    shared = dict(cmat=cmat, cf=cf, ind=ind, pv=pv, glnb=glnb, gb=gb, wsT=wsT)
    for k in ("w_in", "w_out", "w_up", "w_down", "w_pe_gate", "w_pe_proj", "conv_pw_w"):
        shared[k] = np.ascontiguousarray(np.asarray(inp[k], np.float32))
    maps = []
    for b in range(8):
        m = dict(shared)
        m["xT"] = np.ascontiguousarray(x[b].T)
        m["pT"] = np.ascontiguousarray(p[:, b].transpose(0, 2, 1))
        m["pos"] = np.ascontiguousarray(pos[b][None, :])
        maps.append(m)
    return maps


def kernel(**inputs):
    if "nc" not in _CACHE:
        _CACHE["nc"] = build()
    nc = _CACHE["nc"]
    maps = _prep_inputs(inputs)
    res = run_bass_kernel_spmd(nc, maps, core_ids=list(range(8)))
    out = np.stack([np.asarray(r["outT"], np.float32).T for r in res.results], axis=0)
    return np.ascontiguousarray(out)
```

```python
import math
import numpy as np
from contextlib import ExitStack
import concourse.bass as bass
import concourse.mybir as mybir
from concourse.bass_utils import run_bass_kernel_spmd

F32 = mybir.dt.float32
BF16 = mybir.dt.bfloat16
I32 = mybir.dt.int32
AF = mybir.ActivationFunctionType
ALU = mybir.AluOpType
AX = mybir.AxisListType

L = 2
D = 1024
SEQ = 2048
GW = 256
DFF = 2816
NJ = DFF // 128
NT = 4
TW = 512
MASK = 30000.0
BIGG = 1.0e6
EPS = 1e-6
ENGS = ["pe", "act", "dve", "pool", "sp"]


class Sched:
    def __init__(self, nc, es, same_engine_sync=True):
        self.nc = nc
        self.es = es
        self.same = same_engine_sync
        self.prog = {e: [] for e in ENGS}
        self.cnt = {}
        self.sems = {}
        self.seen = {e: {} for e in ENGS}
        self.state = {}
        for e in ENGS:
            self._sem(e)

    def _sem(self, key):
        if key not in self.sems:
            self.sems[key] = self.es.enter_context(self.nc.semaphore("s_" + str(key)))
            self.cnt[key] = 0
        return self.sems[key]

    def _engobj(self, e):
        nc = self.nc
        return dict(pe=nc.tensor, act=nc.scalar, dve=nc.vector, pool=nc.gpsimd, sp=nc.sync)[e]

    def _deps(self, e, r, w):
        toks = {}
        for k in r:
            st = self.state.get(k)
            if st and st[0] is not None:
                t = st[0]
                toks[t[0]] = max(toks.get(t[0], 0), t[1])
        for k in w:
            st = self.state.get(k)
            if st:
                if st[0] is not None:
                    t = st[0]
                    toks[t[0]] = max(toks.get(t[0], 0), t[1])
                for t in st[1]:
                    toks[t[0]] = max(toks.get(t[0], 0), t[1])
        waits = []
        for sk, v in toks.items():
            if sk == e and (not self.same or e == "pe"):
                continue
            if self.seen[e].get(sk, 0) >= v:
                continue
            self.seen[e][sk] = v
            waits.append((sk, v))
        return waits

    def _record(self, tok, r, w):
        for k in r:
            st = self.state.setdefault(k, [None, []])
            st[1].append(tok)
        for k in w:
            self.state[k] = [tok, []]

    def op(self, e, fn, r=(), w=(), inc=True):
        waits = self._deps(e, r, w)
        tok = (e, self.cnt[e] + 1)
        if inc:
            self.cnt[e] += 1
        self._record(tok, r, w)
        self.prog[e].append((waits, fn, (e, 1) if inc else None))
        return tok

    def dma(self, q, fn, semkey, r=(), w=()):
        self._sem(semkey)
        waits = self._deps(q, r, w)
        self.cnt[semkey] += 16
        tok = (semkey, self.cnt[semkey])
        self._record(tok, r, w)
        self.prog[q].append((waits, fn, (semkey, 16)))
        return tok

    def barrier(self):
        for e in ENGS:
            waits = []
            for sk, v in self.cnt.items():
                if v == 0 or self.seen[e].get(sk, 0) >= v:
                    continue
                if sk == e and e == "pe":
                    continue
                self.seen[e][sk] = v
                waits.append((sk, v))
            self.prog[e].append((waits, None, None))

    def simulate(self):
        val = {k: 0 for k in self.sems}
        pc = {e: 0 for e in ENGS}
        progress = True
        while progress:
            progress = False
            for e in ENGS:
                while pc[e] < len(self.prog[e]):
                    waits, fn, inc = self.prog[e][pc[e]]
                    if any(val[sk] < v for sk, v in waits):
                        break
                    if inc is not None:
                        val[inc[0]] += inc[1]
                    pc[e] += 1
                    progress = True
        stuck = {e: (pc[e], len(self.prog[e])) for e in ENGS if pc[e] < len(self.prog[e])}
        if stuck:
            msg = []
            for e, (i, n) in stuck.items():
                waits = self.prog[e][i][0]
                msg.append("%s@%d/%d waits %s" % (e, i, n, [(sk, v, val[sk]) for sk, v in waits if val[sk] < v]))
            raise RuntimeError("DEADLOCK in schedule: " + "; ".join(msg))
        for k in self.sems:
            assert val[k] == self.cnt[k], (k, val[k], self.cnt[k])

    def emit(self):
        self.simulate()
        nc = self.nc
        with nc.Block() as block:
            def run(e):
                eng = self._engobj(e)
                for waits, fn, inc in self.prog[e]:
                    for sk, v in waits:
                        eng.wait_ge(self.sems[sk], v)
                    if fn is None:
                        continue
                    ins = fn()
                    if inc is not None:
                        ins.then_inc(self.sems[inc[0]], inc[1])

            @block.tensor
            def _(e):
                run("pe")

            @block.scalar
            def _(e):
                run("act")

            @block.vector
            def _(e):
                run("dve")

            @block.gpsimd
            def _(e):
                run("pool")

            @block.sync
            def _(e):
                run("sp")


class Arena:
    def __init__(self, t, nbytes):
        self.t = t
        self.n = nbytes
        self.off = 0
        self.hi = 0

    def mark(self):
        return self.off

    def release(self, m):
        self.off = m

    def _take(self, nbytes):
        nbytes = (nbytes + 31) // 32 * 32
        o = self.off
        self.off += nbytes
        self.hi = max(self.hi, self.off)
        assert self.off <= self.n, ("arena overflow", self.off, self.n)
        return o

    def f32(self, n):
        o = self._take(4 * n)
        return self.t[:, o // 4: o // 4 + n]

    def bf16(self, n):
        o = self._take(2 * n)
        return self.t[:, o // 4: o // 4 + (n + 1) // 2].bitcast(BF16)

    def i32(self, n):
        o = self._take(4 * n)
        return self.t[:, o // 4: o // 4 + n].bitcast(I32)


class Ring:
    def __init__(self, items):
        self.items = items
        self.i = 0

    def next(self):
        it = self.items[self.i % len(self.items)]
        self.i += 1
        return it


class WStream:
    def __init__(self, slots, plan, auto=True):
        self.slots = slots
        self.plan = plan
        self.auto = auto
        self.issued = 0
        self.used = 0
        self.freed = 0

    def top(self):
        while self.issued < len(self.plan) and self.issued - self.freed < len(self.slots):
            i = self.issued
            v, k = self.slots[i % len(self.slots)]
            self.plan[i](v, k)
            self.issued += 1

    def done(self):
        self.freed = self.used
        self.top()

    def get(self):
        i = self.used
        if self.auto:
            self.freed = i
        self.top()
        assert self.issued > i, "weight stream: block not issued (slots exhausted)"
        self.used += 1
        return self.slots[i % len(self.slots)]


def _host_consts():
    ident = np.eye(128, dtype=np.float32)
    ones = np.ones((128, 128), np.float32)
    blk2 = np.zeros((128, 128), np.float32)
    blk2[0:64, 0:64] = 1
    blk2[64:128, 64:128] = 1

    def rotT(block):
        h = block // 2
        m = np.zeros((128, 128), np.float32)
        for b0 in range(0, 128, block):
            for i in range(h):
                m[b0 + i + h, b0 + i] = -1.0
                m[b0 + i, b0 + i + h] = 1.0
        return m

    tri = np.where(np.arange(128)[None, :] >= np.arange(128)[:, None], 0.0, -MASK).astype(np.float32)
    cmat = np.concatenate([ident, ones, blk2, rotT(64), rotT(32), tri], axis=1)
    tril = (np.arange(128)[:, None] <= np.arange(128)[None, :]).astype(np.float32)
    pm = np.zeros((16, 8), np.float32)
    for qs in range(16):
        cur = qs // 2
        for n in range(8):
            pm[qs, n] = 0.0 if n < cur else (BIGG if n == cur else -2 * BIGG)
    pmask = np.broadcast_to(pm.reshape(1, 128), (128, 128))
    p = np.arange(128)
    invf64 = (10000.0 ** (-(2.0 * (p % 32)) / 64.0)) / (2 * math.pi)
    invf32 = (10000.0 ** (-(2.0 * (p % 16)) / 32.0)) / (2 * math.pi)
    cf = np.concatenate([tril, pmask, invf64[:, None], invf32[:, None]], axis=1).astype(np.float32)
    ind = (-MASK) * (np.arange(SEQ)[None, :] // 256 == np.arange(8)[:, None]).astype(np.float32)
    return np.ascontiguousarray(cmat), np.ascontiguousarray(cf), np.ascontiguousarray(ind)


def _fm(v, c):
    return np.ascontiguousarray(np.asarray(v, np.float32).reshape(c, 128).T)


def _host_params(inp):
    vd = np.stack([np.stack([_fm(inp[k][l], 8) for k in ("pre_mix_norm", "post_mix_norm", "pre_ffn_norm", "post_ffn_norm", "pe_gate_norm")], 1) for l in range(L)], 1)
    vg = np.stack([np.stack([_fm(inp[k][l], 2) for k in ("conv_dw_b", "conv_ln_g", "conv_ln_b", "conv_pw_b", "out_norm_a", "out_norm_b", "out_norm_c")], 1) for l in range(L)], 1)
    wdw = np.stack([np.asarray(inp["conv_dw_w"][l], np.float32).reshape(31, 2, 128).transpose(2, 1, 0) for l in range(L)], 1)
    fcw = np.stack([np.asarray(inp["ffn_conv_w"][l], np.float32).reshape(3, 44, 128).transpose(2, 1, 0) for l in range(L)], 1)
    fcb = np.stack([_fm(inp["ffn_conv_b"][l], 44) for l in range(L)], 1)
    gsub = np.stack([np.asarray(inp["diff_subln_g"][l], np.float32)[np.arange(128) % 64] for l in range(L)], 1)
    lamv = np.stack([np.stack([np.asarray(inp[k][l], np.float32) for k in ("diff_lq1", "diff_lk1", "diff_lq2", "diff_lk2")], 0) for l in range(L)], 0)
    lamv = np.broadcast_to(lamv.reshape(1, L * 4 * 32), (128, L * 4 * 32))
    pv = np.concatenate([vd.reshape(128, -1), vg.reshape(128, -1), wdw.reshape(128, -1), fcw.reshape(128, -1), fcb.reshape(128, -1), gsub.reshape(128, -1), lamv], axis=1)
    glnb = np.stack([np.stack([np.broadcast_to(np.asarray(inp[k][l], np.float32)[None, :], (128, 256)) for k in ("gmlp_ln_g", "gmlp_ln_b")], 1) for l in range(L)], 0)
    bs = np.asarray(inp["gmlp_bs"], np.float32)
    gb = np.zeros((L, 2, 128, 512), np.float32)
    for l in range(L):
        for cc in range(2):
            for hh in range(2):
                gb[l, cc, hh * 64:(hh + 1) * 64, :] = np.tile(bs[l, 2 * cc + hh], 4)[None, :]
    wsT = np.ascontiguousarray(np.asarray(inp["gmlp_ws"], np.float32).transpose(0, 1, 3, 2))
    return np.ascontiguousarray(pv.astype(np.float32)), np.ascontiguousarray(glnb), gb, wsT


PV_OFF = {}


def _pv_layout():
    o = 0
    for name, n in (("vd", L * 5 * 8), ("vg", L * 7 * 2), ("wdw", L * 2 * 31), ("fcw", L * 44 * 3), ("fcb", L * 44), ("gsub", L), ("lamv", L * 4 * 32)):
        PV_OFF[name] = o
        o += n
    return o


PV_N = _pv_layout()


def build(dbg=None, nlayers=L):
    dbg = dbg or []
    nc = bass.Bass("TRN2", target_bir_lowering=False)

    def din(name, shape, dt=F32):
        return nc.dram_tensor(name, list(shape), dt, kind="ExternalInput").ap()

    xT_d = din("xT", [D, SEQ])
    pT_d = din("pT", [L, GW, SEQ])
    pos_d = din("pos", [1, SEQ], I32)
    cmat_d = din("cmat", [128, 768])
    cf_d = din("cf", [128, 258])
    ind_d = din("ind", [8, SEQ])
    pv_d = din("pv", [128, PV_N])
    glnb_d = din("glnb", [L, 128, 2, 256])
    gb_d = din("gb", [L, 2, 128, 512])
    wsT_d = din("wsT", [L, 4, 128, 128])
    w_in_d = din("w_in", [L, D, 10 * GW])
    w_out_d = din("w_out", [L, D, D])
    w_up_d = din("w_up", [L, D, 2 * DFF])
    w_down_d = din("w_down", [L, DFF, D])
    w_gate_d = din("w_pe_gate", [L, D, D])
    w_proj_d = din("w_pe_proj", [L, GW, D])
    w_pw_d = din("conv_pw_w", [L, GW, GW])
    out_d = nc.dram_tensor("outT", [D, SEQ], F32, kind="ExternalOutput").ap()
    wup_s = nc.dram_tensor("wup_s", [L, NJ, 128, 8 * 2 * 128], BF16, kind="Internal").ap()
    wdn_s = nc.dram_tensor("wdn_s", [L, 8, 128, NJ * 128], BF16, kind="Internal").ap()
    dbg_d = {n: nc.dram_tensor("dbg_" + n, [128, w], F32, kind="ExternalOutput").ap() for n, w in dbg}

    es = ExitStack()
    with es:
        S = Sched(nc, es)
        NB = 204 * 1024
        big = es.enter_context(nc.sbuf_tensor("arena", [128, NB // 4], F32))
        A = Arena(big, NB)
        psb = [es.enter_context(nc.psum_tensor("ps%d" % i, [128, 512], F32)) for i in range(8)]
        PS = [(psb[i][:, :], ("ps", i)) for i in range(8)]
        ringA = Ring(PS[0:4])
        ringB = Ring(PS[4:8])

        def MM(out, lhsT, rhs, start, stop, r, w, inc=True, tp=None):
            kw = {}
            if tp is not None:
                kw["tile_position"] = tp
            S.op("pe", lambda: nc.tensor.matmul(out, lhsT=lhsT, rhs=rhs, start=start, stop=stop, **kw), r=r, w=w, inc=inc)

        def ACT(out, in_, func, r, w, scale=None, bias=None):
            kw = {}
            if scale is not None:
                kw["scale"] = scale
            if bias is not None:
                kw["bias"] = bias
            S.op("act", lambda: nc.scalar.activation(out=out, in_=in_, func=func, **kw), r=r, w=w)

        def TTo(eng, out, in0, in1, op, r, w):
            e = nc.vector if eng == "dve" else nc.gpsimd
            S.op(eng, lambda: e.tensor_tensor(out=out, in0=in0, in1=in1, op=op), r=r, w=w)

        def TS(eng, out, in0, s1, s2, op0, op1, r, w):
            e = nc.vector if eng == "dve" else nc.gpsimd
            if op1 is None:
                S.op(eng, lambda: e.tensor_scalar(out=out, in0=in0, scalar1=s1, scalar2=None, op0=op0), r=r, w=w)
            else:
                S.op(eng, lambda: e.tensor_scalar(out=out, in0=in0, scalar1=s1, scalar2=s2, op0=op0, op1=op1), r=r, w=w)

        def STT(out, in0, scalar, in1, op0, op1, r, w):
            S.op("dve", lambda: nc.vector.scalar_tensor_tensor(out=out, in0=in0, scalar=scalar, in1=in1, op0=op0, op1=op1), r=r, w=w)

        def CP(eng, out, in_, r, w):
            e = nc.vector if eng == "dve" else nc.gpsimd
            S.op(eng, lambda: e.tensor_copy(out=out, in_=in_), r=r, w=w)

        def MSET(eng, ap, val, w):
            e = nc.vector if eng == "dve" else nc.gpsimd
            S.op(eng, lambda: e.memset(ap, val), w=w)

        def DMA(q, out, in_, semkey, r, w):
            e = dict(sp=nc.sync, pool=nc.gpsimd, act=nc.scalar)[q]
            S.dma(q, lambda: e.dma_start(out=out, in_=in_), semkey, r=r, w=w)

        def dump(name, ap, keys):
            if name in dbg_d:
                w_ = ap.shape[-1]
                stg = A.f32(w_)
                CP("dve", stg[0:ap.shape[0], :], ap, r=keys, w=[("dbgs", name)])
                DMA("sp", dbg_d[name][0:ap.shape[0], 0:w_], stg[0:ap.shape[0], :], "dbg", r=[("dbgs", name)], w=[("dbgo", name)])

        xT = A.f32(8 * SEQ).rearrange("p (c s) -> p c s", c=8)
        hT = A.bf16(8 * SEQ).rearrange("p (c s) -> p c s", c=8)
        cmat = A.bf16(768)
        IDENT, ONES, BLK2, R64, R32, TRI = [cmat[:, i * 128:(i + 1) * 128] for i in range(6)]
        cf = A.f32(258)
        TRIL = cf[:, 0:128]
        PMASK = cf[:, 128:256].rearrange("p (q n) -> p q n", n=8)
        INVF = [cf[:, 256:257], cf[:, 257:258]]
        pv = A.f32(PV_N)

        def PVv(name, n):
            return pv[:, PV_OFF[name]:PV_OFF[name] + n]

        vd = PVv("vd", L * 5 * 8).rearrange("p (l v c) -> p l v c", l=L, v=5)
        vg = PVv("vg", L * 7 * 2).rearrange("p (l v c) -> p l v c", l=L, v=7)
        wdw = PVv("wdw", L * 2 * 31).rearrange("p (l c k) -> p l c k", l=L, c=2)
        fcw = PVv("fcw", L * 44 * 3).rearrange("p (l j k) -> p l j k", l=L, j=44)
        fcb = PVv("fcb", L * 44).rearrange("p (l j) -> p l j", l=L)
        gsub = PVv("gsub", L)
        lamv = PVv("lamv", L * 4 * 32).rearrange("p (l v k) -> p l v k", l=L, v=4)
        small = A.f32(64)
        lnv = A.f32(512)
        rstd = A.f32(512)
        sq = [A.bf16(512) for _ in range(3)]
        sqring = Ring([(sq[i], ("sq", i)) for i in range(3)])
        PH = A.mark()

        DMA("pool", cmat, cmat_d[:, :], "ld_c", r=[], w=["cmat"])
        DMA("sp", cf, cf_d[:, :], "ld_c2", r=[], w=["cf"])
        DMA("sp", pv, pv_d[:, :], "ld_c3", r=[], w=["pv"])
        for c in range(8):
            DMA("sp", xT[:, c, :], xT_d[c * 128:(c + 1) * 128, :], ("ld_x", c), r=[], w=[("xT", c, t) for t in range(NT)])

        def wload(dst, src, key):
            DMA("pool", dst, src, ("wsem",) + key, r=[], w=[key])

        def precast_ffn(l):
            wupv_ = w_up_d[l].rearrange("(k p) (g n) -> p k g n", p=128, g=2)
            wdnv_ = w_down_d[l].rearrange("(j p) n -> p j n", p=128)
            sk = ("pcs", l)
            keys = []
            for j in range(NJ):
                for gi_ in range(2):
                    dst = wup_s[l, j].rearrange("p (k g n) -> p k g n", k=8, g=2)[:, :, gi_, :]
                    DMA("pool", dst, wupv_[:, :, gi_, j * 128:(j + 1) * 128], sk, r=[], w=[("pcu", l, j, gi_)])
                    keys.append(("pcu", l, j, gi_))
            for dc in range(8):
                dst = wdn_s[l, dc].rearrange("p (j n) -> p j n", j=NJ)
                DMA("pool", dst, wdnv_[:, :, dc * 128:(dc + 1) * 128], sk, r=[], w=[("pcd", l, dc)])
                keys.append(("pcd", l, dc))
            tot = S.cnt[sk]
            for k in keys:
                S.state[k] = [(sk, tot), []]

        def rstd_from(ps_ap, ps_key, inv_n, eps, extra_r=()):
            ACT(lnv, ps_ap, AF.Ln, r=[ps_key] + list(extra_r), w=["lnv"], scale=inv_n, bias=eps)
            ACT(rstd, lnv, AF.Exp, r=["lnv"], w=["rstd"], scale=-0.5)

        def rmsnorm_tile(srcs, skeys, dsts, dkeys, gains, n_feat, eps=EPS, lhs=None, lkey="cmat"):
            ps, pk = ringB.next()
            C = len(srcs)
            for c in range(C):
                q, qk = sqring.next()
                ACT(q, srcs[c], AF.Square, r=[skeys[c]], w=[qk])
                MM(ps, lhs if lhs is not None else ONES, q, c == 0, c == C - 1, r=[qk, lkey], w=[pk])
            rstd_from(ps, pk, 1.0 / n_feat, eps)
            for c in range(C):
                STT(dsts[c], srcs[c], gains[c], rstd, ALU.mult, ALU.mult, r=[skeys[c], "rstd", "pv"], w=[dkeys[c]])

        def sl(t):
            return slice(t * TW, (t + 1) * TW)

        def norm_x_to_h(l, vidx):
            for t in range(NT):
                rmsnorm_tile([xT[:, c, sl(t)] for c in range(8)], [("xT", c, t) for c in range(8)],
                             [hT[:, c, sl(t)] for c in range(8)], [("hT", c, t) for c in range(8)],
                             [vd[:, l, vidx, c:c + 1] for c in range(8)], D)

        def proj(ps, pk, wv, wkey, cs, rhs_fn, rkeys_fn, nk=8):
            for kc in range(nk):
                MM(ps, wv[:, kc, cs], rhs_fn(kc), kc == 0, kc == nk - 1, r=[wkey] + rkeys_fn(kc), w=[pk], inc=(kc == nk - 1))

        def hrhs(t):
            return (lambda kc: hT[:, kc, sl(t)]), (lambda kc: [("hT", kc, t)])

        for l in range(nlayers):
            S.barrier()
            A.release(PH)
            catT = A.bf16(8 * SEQ).rearrange("p (c s) -> p c s", c=8)
            ropeC = A.bf16(SEQ)
            ropeS = A.bf16(SEQ)
            wslots = [A.bf16(8 * 256).rearrange("p (k n) -> p k n", k=8) for _ in range(4)]
            MX = A.mark()
            winv = w_in_d[l].rearrange("(k p) n -> p k n", p=128)
            woutv = w_out_d[l].rearrange("(k p) n -> p k n", p=128)
            mplan = [(lambda v, k, bi=bi: wload(v, winv[:, :, bi * 256:(bi + 1) * 256], k)) for bi in (0, 1, 2, 7, 8, 9, 3, 4, 5, 6)]
            mplan += [(lambda v, k, db=db: wload(v, woutv[:, :, db * 256:(db + 1) * 256], k)) for _t in range(NT) for db in range(4)]
            mstream = WStream([(wslots[i], ("ws", i)) for i in range(4)], mplan, auto=False)

            def win_block(bi):
                return mstream.get()

            def rope_tables(which):
                m = A.mark()
                posi = A.i32(SEQ)
                v = A.f32(SEQ)
                ki = A.i32(SEQ)
                DMA("sp", posi, pos_d[0:1, :].broadcast_to([128, SEQ]), "ld_pos", r=[], w=["posi"])
                for tab, shift, key in ((ropeS, 0.0, "ropeS"), (ropeC, 0.25, "ropeC")):
                    TS("dve", v, posi, INVF[which], shift, ALU.mult, ALU.add, r=["posi", "cf"], w=["ropev"])
                    CP("dve", ki, v, r=["ropev"], w=["ropek"])
                    TTo("dve", v, v, ki, ALU.subtract, r=["ropev", "ropek"], w=["ropev"])
                    ACT(tab, v, AF.Sin, r=["ropev"], w=[key], scale=2 * math.pi * (1 - 1e-6))
                S.barrier()
                A.release(m)

            def rope_apply(ps, pk, t, RM, zbr, t1, t2):
                zb, zk = zbr.next()
                ACT(zb, ps, AF.Copy, r=[pk], w=[zk])
                ps2, pk2 = ringA.next()
                MM(ps2, RM, zb, True, True, r=[zk, "cmat"], w=[pk2])
                TTo("dve", t1[0], zb, ropeC[:, sl(t)], ALU.mult, r=[zk, "ropeC"], w=[t1[1]])
                TTo("dve", t2[0], ps2, ropeS[:, sl(t)], ALU.mult, r=[pk2, "ropeS"], w=[t2[1]])

            def attn_finalize_recip(O, ok, dst_rden):
                ACT(dst_rden[0][0:64, :], O[64:128, :], AF.Ln, r=[ok], w=[dst_rden[1]])
                ACT(dst_rden[0][0:64, :], dst_rden[0][0:64, :], AF.Exp, r=[dst_rden[1]], w=[dst_rden[1]], scale=-1.0)

            def vproj(wv, wk, cs, vaug):
                for g in range(4):
                    ps, pk = ringA.next()
                    for s4 in range(4):
                        st = g * 4 + s4
                        for kc in range(8):
                            MM(ps[:, s4 * 128:(s4 + 1) * 128], hT[:, kc, st * 128:(st + 1) * 128], wv[:, kc, cs], kc == 0, kc == 7,
                               r=[wk, ("hT", kc, st // 4)], w=[pk], inc=(kc == 7 and s4 == 3))
                    ACT(vaug[:, g * 4:(g + 1) * 4, :, 0:64], ps.rearrange("p (s h d) -> p s h d", s=4, h=2), AF.Copy, r=[pk], w=[("V", g)])

            norm_x_to_h(l, 0)
            if l == 0:
                dump("hT0", hT[:, 0, 0:512], [("hT", 0, 0)])

            rope_tables(0)
            m0 = A.mark()
            Kaug = [A.bf16(SEQ) for _ in range(2)]
            vaug = A.bf16(16 * 2 * 128).rearrange("p (s h d) -> p s h d", s=16, h=2)
            Qa = [[A.bf16(512) for _ in range(2)] for _ in range(NT)]
            Pr = Ring([(A.bf16(512), ("P", i)) for i in range(4)])
            zbr = Ring([(A.bf16(512), ("zb", i)) for i in range(2)])
            t1 = (A.f32(512), "t1")
            t2 = (A.f32(512), "t2")
            rden = (A.f32(512), "rden")
            bstg4 = A.bf16(4 * 72).rearrange("p (j n) -> p j n", j=4)
            gm = A.f32(64)
            top8 = A.f32(32)
            kmf = A.f32(16)
            kmb = A.bf16(16)
            MSET("dve", vaug[:, :, :, 64:128], 1.0, w=[("V", g) for g in range(4)])
            MSET("dve", bstg4, 0.0, w=["bstg"])
            for hh in range(2):
                DMA("pool", Kaug[hh][64:72, :], ind_d[:, :], "ld_ind", r=[], w=[("Kind", hh)])
            wq, wqk = win_block(0)
            wk_, wkk = win_block(1)
            wv_, wvk = win_block(2)
            precast_ffn(l)
            for c in range(2):
                cs = slice(c * 128, (c + 1) * 128)
                for t in range(NT):
                    ps, pk = ringA.next()
                    rf, kf = hrhs(t)
                    proj(ps, pk, wk_, wkk, cs, rf, kf)
                    rope_apply(ps, pk, t, R64, zbr, t1, t2)
                    for hh in range(2):
                        TTo("dve", Kaug[hh][0:64, sl(t)], t1[0][hh * 64:(hh + 1) * 64, :], t2[0][hh * 64:(hh + 1) * 64, :], ALU.add,
                            r=[t1[1], t2[1]], w=[("K", hh, t)])
                for hh in range(2):
                    S.op("dve", lambda hh=hh: nc.vector.tensor_reduce(out=kmf[0:64, hh * 8:(hh + 1) * 8], in_=Kaug[hh][0:64, :].rearrange("p (n k) -> p n k", k=256), axis=AX.X, op=ALU.add),
                         r=[("K", hh, t) for t in range(NT)], w=[("kmf", hh)])
                    TS("dve", kmb[0:64, hh * 8:(hh + 1) * 8], kmf[0:64, hh * 8:(hh + 1) * 8], 1.0 / 256, None, ALU.mult, None, r=[("kmf", hh)], w=[("kmb", hh)])
                vproj(wv_, wvk, cs, vaug)
                for qt in range(NT):
                    qb = Qa[qt]
                    ps, pk = ringA.next()
                    rf, kf = hrhs(qt)
                    proj(ps, pk, wq, wqk, cs, rf, kf)
                    rope_apply(ps, pk, qt, R64, zbr, t1, t2)
                    for hh in range(2):
                        TTo("dve", qb[hh][0:64, :], t1[0][hh * 64:(hh + 1) * 64, :], t2[0][hh * 64:(hh + 1) * 64, :], ALU.add,
                            r=[t1[1], t2[1]], w=[("Qa", qt, hh)])
                    for hh in range(2):
                        gps, gk = ringA.next()
                        for j in range(4):
                            MM(gps[:, j * 8:(j + 1) * 8], qb[hh][0:64, j * 128:(j + 1) * 128], kmb[0:64, hh * 8:(hh + 1) * 8], True, True,
                               r=[("Qa", qt, hh), ("kmb", hh)], w=[gk], inc=(j == 3))
                        gmv = gm[:, 0:32].rearrange("p (j n) -> p j n", n=8)
                        TTo("dve", gmv, gps[:, 0:32].rearrange("p (j n) -> p j n", n=8), PMASK[:, qt * 4:(qt + 1) * 4, :], ALU.add, r=[gk, "cf"], w=["gm"])
                        tps, tk = ringA.next()
                        tpsb = tps.bitcast(BF16)
                        top8v = top8.rearrange("p (j n) -> p j n", n=8)
                        for j in range(4):
                            S.op("dve", lambda j=j: nc.vector.max(out=top8[:, j * 8:(j + 1) * 8], in_=gm[:, j * 8:(j + 1) * 8]), r=["gm"], w=["top8"])
                        TS("dve", top8v[:, :, 3:4], top8v[:, :, 3:4], -BIGG, None, ALU.max, None, r=["top8"], w=["top8"])
                        TTo("dve", bstg4[:, :, 64:72], gmv, top8v[:, :, 3:4].broadcast_to([128, 4, 8]), ALU.is_lt, r=["gm", "top8"], w=["bstg"])
                        for j in range(4):
                            S.op("pe", lambda j=j, tpsb=tpsb: nc.tensor.transpose(tpsb[0:72, j * 128:(j + 1) * 128], bstg4[:, j, :], IDENT), r=["bstg", "cmat"], w=[tk], inc=(j == 3))
                        ACT(qb[hh][64:72, :], tpsb[64:72, 0:512], AF.Copy, r=[tk], w=[("Qb", qt, hh)])
                for qt in range(NT):
                    qb = Qa[qt]
                    for hh in range(2):
                        O, ok = ringB.next()
                        nk = 4 * qt + 4
                        pend = []
                        for kt in range(nk):
                            jd = kt - 4 * qt
                            c0 = 128 * jd if jd > 0 else 0
                            sp_, sk = ringA.next()
                            MM(sp_[:, c0:512], Kaug[hh][0:72, kt * 128:(kt + 1) * 128], qb[hh][0:72, c0:512], True, jd < 0,
                               r=[("K", hh, kt // 4), ("Kind", hh), ("Qa", qt, hh), ("Qb", qt, hh)], w=[sk], inc=(jd < 0))
                            if jd >= 0:
                                MM(sp_[:, c0:c0 + 128], IDENT, TRI, False, True, r=["cmat"], w=[sk])
                            P, Pk = Pr.next()
                            ACT(P[:, c0:512], sp_[:, c0:512], AF.Exp, r=[sk], w=[Pk], scale=0.125)

                            def pv_(kt=kt, c0=c0, P=P, Pk=Pk, O=O, ok=ok, hh=hh, nk=nk):
                                MM(O[:, c0:512], vaug[:, kt, hh, :], P[:, c0:512], kt == 0, kt == nk - 1, r=[Pk, ("V", kt // 4)], w=[ok])
                            pend.append(pv_)
                            if len(pend) > 2:
                                pend.pop(0)()
                        for f in pend:
                            f()
                        attn_finalize_recip(O, ok, rden)
                        TTo("dve", catT[hh * 64:(hh + 1) * 64, c, sl(qt)], O[0:64, :], rden[0][0:64, :], ALU.mult, r=[ok, rden[1]], w=[("cat", c, qt)])
            if l == 0:
                dump("oa_pre", catT[:, 0, 0:512], [("cat", 0, 0)])
            for t in range(NT):
                rmsnorm_tile([catT[:, c, sl(t)] for c in range(2)], [("cat", c, t) for c in range(2)],
                             [catT[:, c, sl(t)] for c in range(2)], [("cat", c, t) for c in range(2)],
                             [vg[:, l, 4, c:c + 1] for c in range(2)], GW)
            if l == 0:
                dump("oa", catT[:, 0, 0:512], [("cat", 0, 0)])
            mstream.done()
            S.barrier()
            A.release(m0)

            rope_tables(1)
            lam_init = 0.8 - 0.6 * math.exp(-0.3 * l)
            lt = small[:, 0:8]
            prod = A.f32(64)
            TTo("dve", prod[:, 0:32], lamv[:, l, 0, :], lamv[:, l, 1, :], ALU.mult, r=["pv"], w=["prod"])
            S.op("dve", lambda: nc.vector.tensor_reduce(out=lt[:, 0:1], in_=prod[:, 0:32], axis=AX.X, op=ALU.add), r=["prod"], w=["lt"])
            TTo("dve", prod[:, 32:64], lamv[:, l, 2, :], lamv[:, l, 3, :], ALU.mult, r=["pv"], w=["prod2"])
            S.op("dve", lambda: nc.vector.tensor_reduce(out=lt[:, 1:2], in_=prod[:, 32:64], axis=AX.X, op=ALU.add), r=["prod2"], w=["lt"])
            ACT(lt[:, 2:4], lt[:, 0:2], AF.Exp, r=["lt"], w=["lt"])
            TTo("dve", lt[:, 4:5], lt[:, 3:4], lt[:, 2:3], ALU.subtract, r=["lt"], w=["lt"])
            TS("dve", lt[:, 5:6], lt[:, 4:5], -lam_init, None, ALU.add, None, r=["lt"], w=["lt"])
            TS("dve", lt[:, 6:7], gsub[:, l:l + 1], 1.0 - lam_init, None, ALU.mult, None, r=["pv", "lt"], w=["lt"])
            NEGLAM = lt[:, 5:6]
            GS = lt[:, 6:7]

            m0 = A.mark()
            Kd = A.bf16(SEQ)
            vaug = A.bf16(16 * 2 * 128).rearrange("p (s h d) -> p s h d", s=16, h=2)
            Qd = [A.bf16(512) for _ in range(NT)]
            Pr = Ring([(A.bf16(512), ("P", i)) for i in range(4)])
            zbr = Ring([(A.bf16(512), ("zb", i)) for i in range(2)])
            t1 = (A.f32(512), "t1")
            t2 = (A.f32(512), "t2")
            rden = (A.f32(512), "rden")
            ods = [A.f32(512) for _ in range(2)]
            MSET("dve", vaug[:, :, :, 64:128], 1.0, w=[("V", g) for g in range(4)])
            wq, wqk = win_block(7)
            wk_, wkk = win_block(8)
            wv_, wvk = win_block(9)
            dscale = 32.0 ** -0.5
            for c in range(2):
                cs = slice(c * 128, (c + 1) * 128)
                for t in range(NT):
                    ps, pk = ringA.next()
                    rf, kf = hrhs(t)
                    proj(ps, pk, wk_, wkk, cs, rf, kf)
                    rope_apply(ps, pk, t, R32, zbr, t1, t2)
                    TTo("dve", Kd[:, sl(t)], t1[0], t2[0], ALU.add, r=[t1[1], t2[1]], w=[("Kd", t)])
                vproj(wv_, wvk, cs, vaug)
                for qt in range(NT):
                    qd = Qd[qt]
                    ps, pk = ringA.next()
                    rf, kf = hrhs(qt)
                    proj(ps, pk, wq, wqk, cs, rf, kf)
                    rope_apply(ps, pk, qt, R32, zbr, t1, t2)
                    TTo("dve", qd, t1[0], t2[0], ALU.add, r=[t1[1], t2[1]], w=[("Qd", qt)])
                pend_n = None
                for qt in range(NT):
                    qd = Qd[qt]
                    od = ods[qt % 2]
                    for hh in range(2):
                        Os = [ringB.next(), ringB.next()]
                        nk = 4 * qt + 4
                        pend = []
                        for kt in range(nk):
                            jd = kt - 4 * qt
                            c0 = 128 * jd if jd > 0 else 0
                            cur = []
                            for m_ in range(2):
                                g = hh * 2 + m_
                                sp_, sk = ringA.next()
                                MM(sp_[:, c0:512], Kd[32 * g:32 * g + 32, kt * 128:(kt + 1) * 128], qd[32 * g:32 * g + 32, c0:512], True, jd < 0,
                                   r=[("Kd", kt // 4), ("Qd", qt)], w=[sk], inc=(jd < 0), tp=(32 * g, 0))
                                if jd >= 0:
                                    MM(sp_[:, c0:c0 + 128], IDENT, TRI, False, True, r=["cmat"], w=[sk])
                                cur.append((sp_, sk))
                            for f in pend:
                                f()
                            pend = []
                            for m_ in range(2):
                                sp_, sk = cur[m_]
                                P, Pk = Pr.next()
                                ACT(P[:, c0:512], sp_[:, c0:512], AF.Exp, r=[sk], w=[Pk], scale=dscale)

                                def pv_(kt=kt, c0=c0, P=P, Pk=Pk, Oo=Os[m_], hh=hh, nk=nk):
                                    MM(Oo[0][:, c0:512], vaug[:, kt, hh, :], P[:, c0:512], kt == 0, kt == nk - 1, r=[Pk, ("V", kt // 4)], w=[Oo[1]])
                                pend.append(pv_)
                        for f in pend:
                            f()
                        hs = slice(hh * 64, (hh + 1) * 64)
                        attn_finalize_recip(Os[0][0], Os[0][1], rden)
                        TTo("dve", t1[0][0:64, :], Os[0][0][0:64, :], rden[0][0:64, :], ALU.mult, r=[Os[0][1], rden[1]], w=[t1[1]])
                        attn_finalize_recip(Os[1][0], Os[1][1], rden)
                        TTo("dve", t2[0][0:64, :], Os[1][0][0:64, :], rden[0][0:64, :], ALU.mult, r=[Os[1][1], rden[1]], w=[t2[1]])
                        STT(od[hs, :], t2[0][0:64, :], NEGLAM[0:64, :], t1[0][0:64, :], ALU.mult, ALU.add, r=[t1[1], t2[1], "lt"], w=[("od", qt % 2, hh)])
                        if hh == 0 and pend_n is not None:
                            pend_n()
                            pend_n = None
                    def norm_(qt=qt, od=od, c=c):
                        ps, pk = ringA.next()
                        q_, qk_ = sqring.next()
                        ACT(q_, od, AF.Square, r=[("od", qt % 2, 0), ("od", qt % 2, 1)], w=[qk_])
                        MM(ps, BLK2, q_, True, True, r=[qk_, "cmat"], w=[pk])
                        rstd_from(ps, pk, 1.0 / 64, 1e-5)
                        STT(catT[:, 6 + c, sl(qt)], od, GS, rstd, ALU.mult, ALU.mult, r=[("od", qt % 2, 0), ("od", qt % 2, 1), "rstd", "lt"], w=[("cat", 6 + c, qt)])
                    pend_n = norm_
                pend_n()
                pend_n = None
            if l == 0:
                dump("od", catT[:, 6, 0:512], [("cat", 6, 0)])
            mstream.done()
            S.barrier()
            A.release(m0)

            m0 = A.mark()
            uT = A.bf16(2 * SEQ).rearrange("p (c s) -> p c s", c=2)
            vgl = A.bf16(16 * 256).rearrange("p (n d) -> p n d", n=16)
            glnb = A.f32(512).rearrange("p (v d) -> p v d", v=2)
            gb = A.f32(1024).rearrange("p (c s) -> p c s", c=2)
            wsf = A.f32(512).rearrange("p (h i) -> p h i", h=4)
            wsb = A.bf16(512).rearrange("p (h i) -> p h i", h=4)
            stats = A.f32(16 * 6)
            mv = A.f32(16 * 2).rearrange("p (n k) -> p n k", k=2)
            rsd = A.f32(16)
            vtmp = A.f32(256)
            vln = [A.bf16(256) for _ in range(2)]
            stmp = A.f32(512)
            DMA("sp", glnb, glnb_d[l], "ld_g1", r=[], w=["glnb"])
            DMA("sp", gb, gb_d[l].rearrange("c p s -> p c s"), "ld_g2", r=[], w=["gb"])
            DMA("sp", wsf, wsT_d[l].rearrange("h j i -> j h i"), "ld_g3", r=[], w=["wsf"])
            for h in range(4):
                TTo("dve", wsb[:, h, :], wsf[:, h, :], TRIL, ALU.mult, r=["wsf", "cf"], w=["wsb"])
            wu, wuk = win_block(3)
            wvv, wvvk = win_block(4)
            for t in range(NT):
                for cc in range(2):
                    ps, pk = ringA.next()
                    rf, kf = hrhs(t)
                    proj(ps, pk, wu, wuk, slice(cc * 128, (cc + 1) * 128), rf, kf)
                    ACT(uT[:, cc, sl(t)], ps, AF.Gelu_apprx_tanh, r=[pk], w=[("uT", cc, t)])
            for n2 in range(8):
                ps, pk = ringA.next()
                for s2 in range(2):
                    n = n2 * 2 + s2
                    for kc in range(8):
                        MM(ps[:, s2 * 256:(s2 + 1) * 256], hT[:, kc, n * 128:(n + 1) * 128], wvv[:, kc, :], kc == 0, kc == 7,
                           r=[wvvk, ("hT", kc, n // 4)], w=[pk], inc=(kc == 7 and s2 == 1))
                ACT(vgl[:, n2 * 2:(n2 + 1) * 2, :], ps.rearrange("p (s d) -> p s d", s=2), AF.Gelu_apprx_tanh, r=[pk], w=[("vgl", n2)])
            for n in range(16):
                S.op("dve", lambda n=n: nc.vector.bn_stats(out=stats[:, n * 6:(n + 1) * 6], in_=vgl[:, n, :]), r=[("vgl", n // 2)], w=[("st", n)])
                S.op("dve", lambda n=n: nc.vector.bn_aggr(out=mv[:, n, :], in_=stats[:, n * 6:(n + 1) * 6]), r=[("st", n)], w=["mv"])
            ACT(rsd, mv[:, :, 1], AF.Ln, r=["mv"], w=["rsd"], bias=EPS)
            ACT(rsd, rsd, AF.Exp, r=["rsd"], w=["rsd"], scale=-0.5)
            for t in range(NT):
                pss = [ringA.next(), ringA.next()]
                for s4 in range(4):
                    n = t * 4 + s4
                    vl = vln[n % 2]
                    vk = ("vln", n % 2)
                    TS("dve", vtmp, vgl[:, n, :], mv[:, n, 0:1], rsd[:, n:n + 1], ALU.subtract, ALU.mult, r=[("vgl", n // 2), "mv", "rsd"], w=["vtmp"])
                    TTo("dve", vtmp, vtmp, glnb[:, 0, :], ALU.mult, r=["vtmp", "glnb"], w=["vtmp"])
                    TTo("dve", vl, vtmp, glnb[:, 1, :], ALU.add, r=["vtmp", "glnb"], w=[vk])
                    for cc in range(2):
                        for hh in range(2):
                            h = 2 * cc + hh
                            MM(pss[cc][0][hh * 64:(hh + 1) * 64, s4 * 128:(s4 + 1) * 128], vl[:, h * 64:(h + 1) * 64], wsb[:, h, :], True, True,
                               r=[vk, "wsb"], w=[pss[cc][1]], tp=(0, hh * 64))
                for cc in range(2):
                    TTo("dve", stmp, pss[cc][0], gb[:, cc, :], ALU.add, r=[pss[cc][1], "gb"], w=["stmp"])
                    TTo("dve", catT[:, 2 + cc, sl(t)], stmp, uT[:, cc, sl(t)], ALU.mult, r=["stmp", ("uT", cc, t)], w=[("cat", 2 + cc, t)])
            if l == 0:
                dump("ob_pre", catT[:, 2, 0:512], [("cat", 2, 0)])
            for t in range(NT):
                rmsnorm_tile([catT[:, 2 + c, sl(t)] for c in range(2)], [("cat", 2 + c, t) for c in range(2)],
                             [catT[:, 2 + c, sl(t)] for c in range(2)], [("cat", 2 + c, t) for c in range(2)],
                             [vg[:, l, 5, c:c + 1] for c in range(2)], GW)
            mstream.done()
            S.barrier()
            A.release(m0)

            m0 = A.mark()
            ybuf = A.bf16(32 + SEQ)
            diag = A.bf16(31 * 128).rearrange("p (k n) -> p k n", k=31)
            cy = A.bf16(2 * SEQ).rearrange("p (c s) -> p c s", c=2)
            sg = A.f32(512)
            wpw = A.bf16(2 * 256).rearrange("p (k n) -> p k n", k=2)
            mstat = A.f32(512)
            m2 = A.f32(512)
            yn = A.f32(512)
            sil = A.bf16(1024).rearrange("p (c s) -> p c s", c=2)
            Y0 = 2
            wa, wak = win_block(5)
            wg_, wgk = win_block(6)
            wload(wpw, w_pw_d[l].rearrange("(k p) n -> p k n", p=128), ("wpw",))
            MSET("dve", ybuf[:, 0:32], 0.0, w=["ypad"])
            for cc in range(2):
                cs = slice(cc * 128, (cc + 1) * 128)
                for k in range(31):
                    TS("dve", diag[:, k, :], IDENT, wdw[:, l, cc, k:k + 1], None, ALU.mult, None, r=["cmat", "pv"], w=[("diag", k)])
                for t in range(NT):
                    pa, pak = ringA.next()
                    rf, kf = hrhs(t)
                    proj(pa, pak, wa, wak, cs, rf, kf)
                    pg, pgk = ringA.next()
                    proj(pg, pgk, wg_, wgk, cs, rf, kf)
                    ACT(sg, pg, AF.Sigmoid, r=[pgk], w=["sg"])
                    TTo("dve", ybuf[:, 32 + t * TW:32 + (t + 1) * TW], pa, sg, ALU.mult, r=[pak, "sg"], w=[("yb", t)])
                for t in range(NT):
                    ps, pk = ringB.next()
                    for k in range(31):
                        o = Y0 + t * TW + k
                        rk = ["ypad", ("diag", k), ("yb", t)] + ([("yb", t - 1)] if t > 0 else [])
                        MM(ps, diag[:, k, :], ybuf[:, o:o + TW], k == 0, k == 30, r=rk, w=[pk], inc=(k == 30))
                    ACT(cy[:, cc, sl(t)], ps, AF.Identity, r=[pk, "pv"], w=[("cy", cc, t)], bias=vg[:, l, 0, cc:cc + 1])
            if l == 0:
                dump("cy", cy[:, 0, 0:512], [("cy", 0, 0)])
            for t in range(NT):
                ps1, pk1 = ringB.next()
                ps2, pk2 = ringB.next()
                for cc in range(2):
                    MM(ps1, ONES, cy[:, cc, sl(t)], cc == 0, cc == 1, r=[("cy", cc, t), "cmat"], w=[pk1])
                for cc in range(2):
                    q_, qk_ = sqring.next()
                    ACT(q_, cy[:, cc, sl(t)], AF.Square, r=[("cy", cc, t)], w=[qk_])
                    MM(ps2, ONES, q_, cc == 0, cc == 1, r=[qk_, "cmat"], w=[pk2])
                TS("dve", mstat, ps1, 1.0 / GW, None, ALU.mult, None, r=[pk1], w=["mstat"])
                TTo("dve", m2, mstat, mstat, ALU.mult, r=["mstat"], w=["m2"])
                STT(m2, ps2, 1.0 / GW, m2, ALU.mult, ALU.subtract, r=[pk2, "m2"], w=["m2"])
                ACT(lnv, m2, AF.Ln, r=["m2"], w=["lnv"], bias=EPS)
                ACT(rstd, lnv, AF.Exp, r=["lnv"], w=["rstd"], scale=-0.5)
                for cc in range(2):
                    TTo("dve", yn, cy[:, cc, sl(t)], mstat, ALU.subtract, r=[("cy", cc, t), "mstat"], w=["yn"])
                    TTo("dve", yn, yn, rstd, ALU.mult, r=["yn", "rstd"], w=["yn"])
                    ACT(sil[:, cc, :], yn, AF.Silu, r=["yn", "pv"], w=[("sil", cc)], scale=vg[:, l, 1, cc:cc + 1], bias=vg[:, l, 2, cc:cc + 1])
                for co in range(2):
                    ps, pk = ringA.next()
                    for ci in range(2):
                        MM(ps, wpw[:, ci, co * 128:(co + 1) * 128], sil[:, ci, :], ci == 0, ci == 1, r=[("wpw",), ("sil", ci)], w=[pk])
                    ACT(catT[:, 4 + co, sl(t)], ps, AF.Identity, r=[pk, "pv"], w=[("cat", 4 + co, t)], bias=vg[:, l, 3, co:co + 1])
            if l == 0:
                dump("oc_pre", catT[:, 4, 0:512], [("cat", 4, 0)])
            for t in range(NT):
                rmsnorm_tile([catT[:, 4 + c, sl(t)] for c in range(2)], [("cat", 4 + c, t) for c in range(2)],
                             [catT[:, 4 + c, sl(t)] for c in range(2)], [("cat", 4 + c, t) for c in range(2)],
                             [vg[:, l, 6, c:c + 1] for c in range(2)], GW)
            mstream.done()
            S.barrier()
            A.release(m0)

            def out_proj_norm_res(nkc, wsrc, rhs_fn, rkeys_fn, vidx, wr, ytile):
                for t in range(NT):
                    for db in range(4):
                        wv, wk = wr.get()
                        for dd in range(2):
                            dc = db * 2 + dd
                            ps, pk = ringA.next()
                            for kc in range(nkc):
                                MM(ps, wv[:, kc, dd * 128:(dd + 1) * 128], rhs_fn(kc, t), kc == 0, kc == nkc - 1, r=[wk] + rkeys_fn(kc, t), w=[pk], inc=(kc == nkc - 1))
                            if dc % 2 == 0:
                                ACT(ytile[:, dc, :], ps, AF.Copy, r=[pk], w=[("yt", dc)])
                            else:
                                CP("dve", ytile[:, dc, :], ps, r=[pk], w=[("yt", dc)])
                        wr.done()
                    rmsnorm_tile([ytile[:, dc, :] for dc in range(8)], [("yt", dc) for dc in range(8)],
                                 [ytile[:, dc, :] for dc in range(8)], [("yt", dc) for dc in range(8)],
                                 [vd[:, l, vidx, dc:dc + 1] for dc in range(8)], D)
                    for dc in range(8):
                        TTo("dve", xT[:, dc, sl(t)], xT[:, dc, sl(t)], ytile[:, dc, :], ALU.add, r=[("xT", dc, t), ("yt", dc)], w=[("xT", dc, t)])

            m0 = A.mark()
            ytile = A.f32(8 * 512).rearrange("p (c s) -> p c s", c=8)
            out_proj_norm_res(8, w_out_d[l].rearrange("(k p) n -> p k n", p=128), lambda kc, t: catT[:, kc, sl(t)], lambda kc, t: [("cat", kc, t)], 1, mstream, ytile)
            if l == 0:
                dump("x1", xT[:, 0, 0:512], [("xT", 0, 0)])
            S.barrier()
            A.release(MX)
            A.release(PH)

            norm_x_to_h(l, 2)
            ytile = A.f32(8 * 512).rearrange("p (c s) -> p c s", c=8)
            fT = A.bf16(NJ * 512).rearrange("p (j s) -> p j s", j=NJ)
            ub = [[A.f32(2 + 512) for _ in range(2)] for _ in range(2)]
            halo = A.f32(44 * 2).rearrange("p (j k) -> p j k", k=2)
            tas = [A.f32(512) for _ in range(2)]
            tbs = [A.f32(512) for _ in range(2)]
            cgs = [A.f32(512) for _ in range(2)]
            cvs = [A.f32(512) for _ in range(2)]
            wups = [A.bf16(8 * 2 * 128).rearrange("p (k g n) -> p k g n", k=8, g=2) for _ in range(3)]
            wdns = [A.bf16(NJ * 128).rearrange("p (j n) -> p j n", j=NJ) for _ in range(3)]
            MSET("dve", halo, 0.0, w=["halo"])
            wupv = w_up_d[l].rearrange("(k p) (g n) -> p k g n", p=128, g=2)
            wdnv = w_down_d[l].rearrange("(j p) n -> p j n", p=128)

            def ld_up(v, k, j):
                DMA("sp", v.rearrange("p k g n -> p (k g n)"), wup_s[l, j], ("wsem",) + k, r=[("pcu", l, j, 0), ("pcu", l, j, 1)], w=[k])

            def ld_dn(v, k, dc):
                DMA("sp", v.rearrange("p j n -> p (j n)"), wdn_s[l, dc], ("wsem",) + k, r=[("pcd", l, dc)], w=[k])

            wur = WStream([(wups[i], ("wu", i)) for i in range(3)], [(lambda v, k, j=j: ld_up(v, k, j)) for _t in range(NT) for j in range(NJ)])
            wdr = WStream([(wdns[i], ("wd", i)) for i in range(3)], [(lambda v, k, dc=dc: ld_dn(v, k, dc)) for _t in range(NT) for dc in range(8)])
            wur.top()
            wdr.top()
            pend_f = None
            for t in range(NT):
                for j in range(NJ):
                    wv, wk = wur.get()
                    u = ub[j % 2]
                    b2 = j % 2
                    ta, tb, cg, cv = tas[b2], tbs[b2], cgs[b2], cvs[b2]
                    for gi in range(2):
                        jj = j + gi * NJ
                        ps, pk = ringA.next()
                        for kc in range(8):
                            MM(ps, wv[:, kc, gi, :], hT[:, kc, sl(t)], kc == 0, kc == 7, r=[wk, ("hT", kc, t)], w=[pk], inc=(kc == 7))
                        uk = ("ub", b2, gi)
                        ACT(u[gi][:, 2:514], ps, AF.Copy, r=[pk], w=[uk])
                        CP("dve", u[gi][:, 0:2], halo[:, jj, :], r=["halo", ("halo", jj)], w=[uk])
                        tmp = ta if gi == 0 else tb
                        tk_ = ("ta", b2) if gi == 0 else ("tb", b2)
                        ACT(tmp, ps, AF.Identity, r=[pk, "pv"], w=[tk_], scale=fcw[:, l, jj, 2:3], bias=fcb[:, l, jj:jj + 1])
                        ACT(halo[:, jj, :], ps[:, 510:512], AF.Copy, r=[pk], w=[("halo", jj)])
                        dst = cg if gi == 0 else cv
                        dk = ("cg", b2) if gi == 0 else ("cv", b2)
                        STT(tmp, u[gi][:, 1:513], fcw[:, l, jj, 1:2], tmp, ALU.mult, ALU.add, r=[uk, tk_, "pv"], w=[tk_])
                        STT(dst, u[gi][:, 0:512], fcw[:, l, jj, 0:1], tmp, ALU.mult, ALU.add, r=[uk, tk_, "pv"], w=[dk])
                    if pend_f is not None:
                        pend_f()

                    def fin(j=j, b2=b2, cg=cg, cv=cv):
                        ACT(cg, cg, AF.Gelu_apprx_tanh, r=[("cg", b2)], w=[("cg", b2)])
                        TTo("dve", fT[:, j, :], cg, cv, ALU.mult, r=[("cg", b2), ("cv", b2)], w=[("fT", j)])
                    pend_f = fin
                pend_f()
                pend_f = None
                if l == 0 and t == 0:
                    dump("fT", fT[:, 0, :], [("fT", 0)])
                for dc in range(8):
                    wv, wk = wdr.get()
                    ps, pk = ringB.next()
                    for j in range(NJ):
                        MM(ps, wv[:, j, :], fT[:, j, :], j == 0, j == NJ - 1, r=[wk, ("fT", j)], w=[pk], inc=(j == NJ - 1))
                    if dc % 2 == 0:
                        ACT(ytile[:, dc, :], ps, AF.Copy, r=[pk], w=[("yt", dc)])
                    else:
                        CP("dve", ytile[:, dc, :], ps, r=[pk], w=[("yt", dc)])
                rmsnorm_tile([ytile[:, dc, :] for dc in range(8)], [("yt", dc) for dc in range(8)],
                             [ytile[:, dc, :] for dc in range(8)], [("yt", dc) for dc in range(8)],
                             [vd[:, l, 3, dc:dc + 1] for dc in range(8)], D)
                for dc in range(8):
                    TTo("dve", xT[:, dc, sl(t)], xT[:, dc, sl(t)], ytile[:, dc, :], ALU.add, r=[("xT", dc, t), ("yt", dc)], w=[("xT", dc, t)])
            if l == 0:
                dump("x2", xT[:, 0, 0:512], [("xT", 0, 0)])
            S.barrier()
            A.release(PH)

            norm_x_to_h(l, 4)
            pTb = A.bf16(2 * SEQ).rearrange("p (c s) -> p c s", c=2)
            wgs = [A.bf16(8 * 256).rearrange("p (k n) -> p k n", k=8) for _ in range(3)]
            wpj = A.bf16(2 * D).rearrange("p (k n) -> p k n", k=2)
            sgt = [A.f32(512) for _ in range(2)]
            pjt = [A.f32(512) for _ in range(2)]
            wload(pTb, pT_d[l].rearrange("(k p) s -> p k s", p=128), ("pTb",))
            wload(wpj, w_proj_d[l].rearrange("(k p) n -> p k n", p=128), ("wpj",))
            wgv = w_gate_d[l].rearrange("(k p) n -> p k n", p=128)
            wgr = WStream([(wgs[i], ("wg", i)) for i in range(3)], [(lambda v, k, db=db: wload(v, wgv[:, :, db * 256:(db + 1) * 256], k)) for db in range(4)])
            wgr.top()
            it = 0
            for db in range(4):
                wv, wk = wgr.get()
                for dd in range(2):
                    dc = db * 2 + dd
                    for t in range(NT):
                        b = it % 2
                        it += 1
                        ps, pk = ringA.next()
                        rf, kf = hrhs(t)
                        proj(ps, pk, wv, wk, slice(dd * 128, (dd + 1) * 128), rf, kf)
                        ACT(sgt[b], ps, AF.Sigmoid, r=[pk], w=[("sgt", b)])
                        ps2, pk2 = ringB.next()
                        for kc in range(2):
                            MM(ps2, wpj[:, kc, dc * 128:(dc + 1) * 128], pTb[:, kc, sl(t)], kc == 0, kc == 1, r=[("wpj",), ("pTb",)], w=[pk2], inc=(kc == 1))
                        TTo("dve", pjt[b], ps2, sgt[b], ALU.mult, r=[pk2, ("sgt", b)], w=[("pjt", b)])
                        TTo("dve", xT[:, dc, sl(t)], xT[:, dc, sl(t)], pjt[b], ALU.add, r=[("xT", dc, t), ("pjt", b)], w=[("xT", dc, t)])
            if l == 0:
                dump("x3", xT[:, 0, 0:512], [("xT", 0, 0)])

        for c in range(8):
            DMA("sp", out_d[c * 128:(c + 1) * 128, :], xT[:, c, :], "st_out", r=[("xT", c, t) for t in range(NT)], w=[("out", c)])
        S.barrier()
        S.emit()
        build.arena_hi = A.hi
    return nc


_CACHE = {}


def _prep_inputs(inp):
    cmat, cf, ind = _host_consts()
    pv, glnb, gb, wsT = _host_params(inp)
    x = np.asarray(inp["x"], np.float32)
    p = np.asarray(inp["p"], np.float32)
    pos = np.asarray(inp["positions"], np.int32)
    shared = dict(cmat=cmat, cf=cf, ind=ind, pv=pv, glnb=glnb, gb=gb, wsT=wsT)
    for k in ("w_in", "w_out", "w_up", "w_down", "w_pe_gate", "w_pe_proj", "conv_pw_w"):
        shared[k] = np.ascontiguousarray(np.asarray(inp[k], np.float32))
    maps = []
    for b in range(8):
        m = dict(shared)
        m["xT"] = np.ascontiguousarray(x[b].T)
        m["pT"] = np.ascontiguousarray(p[:, b].transpose(0, 2, 1))
        m["pos"] = np.ascontiguousarray(pos[b][None, :])
        maps.append(m)
    return maps


def kernel(**inputs):
    if "nc" not in _CACHE:
        _CACHE["nc"] = build()
    nc = _CACHE["nc"]
    maps = _prep_inputs(inputs)
    res = run_bass_kernel_spmd(nc, maps, core_ids=list(range(8)))
    out = np.stack([np.asarray(r["outT"], np.float32).T for r in res.results], axis=0)
    return np.ascontiguousarray(out)
```

```python
import math
import numpy as np
from contextlib import ExitStack
import concourse.bass as bass
import concourse.mybir as mybir
from concourse.bass_utils import run_bass_kernel_spmd

F32 = mybir.dt.float32
BF16 = mybir.dt.bfloat16
I32 = mybir.dt.int32
AF = mybir.ActivationFunctionType
ALU = mybir.AluOpType
AX = mybir.AxisListType

L = 2
D = 1024
SEQ = 2048
GW = 256
DFF = 2816
NJ = DFF // 128
NT = 4
TW = 512
MASK = 30000.0
BIGG = 1.0e6
EPS = 1e-6
ENGS = ["pe", "act", "dve", "pool", "sp"]


class Sched:
    def __init__(self, nc, es, same_engine_sync=True):
        self.nc = nc
        self.es = es
        self.same = same_engine_sync
        self.prog = {e: [] for e in ENGS}
        self.cnt = {}
        self.sems = {}
        self.seen = {e: {} for e in ENGS}
        self.state = {}
        for e in ENGS:
            self._sem(e)

    def _sem(self, key):
        if key not in self.sems:
            self.sems[key] = self.es.enter_context(self.nc.semaphore("s_" + str(key)))
            self.cnt[key] = 0
        return self.sems[key]

    def _engobj(self, e):
        nc = self.nc
        return dict(pe=nc.tensor, act=nc.scalar, dve=nc.vector, pool=nc.gpsimd, sp=nc.sync)[e]

    def _deps(self, e, r, w):
        toks = {}
        for k in r:
            st = self.state.get(k)
            if st and st[0] is not None:
                t = st[0]
                toks[t[0]] = max(toks.get(t[0], 0), t[1])
        for k in w:
            st = self.state.get(k)
            if st:
                if st[0] is not None:
                    t = st[0]
                    toks[t[0]] = max(toks.get(t[0], 0), t[1])
                for t in st[1]:
                    toks[t[0]] = max(toks.get(t[0], 0), t[1])
        waits = []
        for sk, v in toks.items():
            if sk == e and (not self.same or e == "pe"):
                continue
            if self.seen[e].get(sk, 0) >= v:
                continue
            self.seen[e][sk] = v
            waits.append((sk, v))
        return waits

    def _record(self, tok, r, w):
        for k in r:
            st = self.state.setdefault(k, [None, []])
            st[1].append(tok)
        for k in w:
            self.state[k] = [tok, []]

    def op(self, e, fn, r=(), w=(), inc=True):
        waits = self._deps(e, r, w)
        tok = (e, self.cnt[e] + 1)
        if inc:
            self.cnt[e] += 1
        self._record(tok, r, w)
        self.prog[e].append((waits, fn, (e, 1) if inc else None))
        return tok

    def dma(self, q, fn, semkey, r=(), w=()):
        self._sem(semkey)
        waits = self._deps(q, r, w)
        self.cnt[semkey] += 16
        tok = (semkey, self.cnt[semkey])
        self._record(tok, r, w)
        self.prog[q].append((waits, fn, (semkey, 16)))
        return tok

    def barrier(self):
        for e in ENGS:
            waits = []
            for sk, v in self.cnt.items():
                if v == 0 or self.seen[e].get(sk, 0) >= v:
                    continue
                if sk == e and e == "pe":
                    continue
                self.seen[e][sk] = v
                waits.append((sk, v))
            self.prog[e].append((waits, None, None))

    def simulate(self):
        val = {k: 0 for k in self.sems}
        pc = {e: 0 for e in ENGS}
        progress = True
        while progress:
            progress = False
            for e in ENGS:
                while pc[e] < len(self.prog[e]):
                    waits, fn, inc = self.prog[e][pc[e]]
                    if any(val[sk] < v for sk, v in waits):
                        break
                    if inc is not None:
                        val[inc[0]] += inc[1]
                    pc[e] += 1
                    progress = True
        stuck = {e: (pc[e], len(self.prog[e])) for e in ENGS if pc[e] < len(self.prog[e])}
        if stuck:
            msg = []
            for e, (i, n) in stuck.items():
                waits = self.prog[e][i][0]
                msg.append("%s@%d/%d waits %s" % (e, i, n, [(sk, v, val[sk]) for sk, v in waits if val[sk] < v]))
            raise RuntimeError("DEADLOCK in schedule: " + "; ".join(msg))
        for k in self.sems:
            assert val[k] == self.cnt[k], (k, val[k], self.cnt[k])

    def emit(self):
        self.simulate()
        nc = self.nc
        with nc.Block() as block:
            def run(e):
                eng = self._engobj(e)
                for waits, fn, inc in self.prog[e]:
                    for sk, v in waits:
                        eng.wait_ge(self.sems[sk], v)
                    if fn is None:
                        continue
                    ins = fn()
                    if inc is not None:
                        ins.then_inc(self.sems[inc[0]], inc[1])

            @block.tensor
            def _(e):
                run("pe")

            @block.scalar
            def _(e):
                run("act")

            @block.vector
            def _(e):
                run("dve")

            @block.gpsimd
            def _(e):
                run("pool")

            @block.sync
            def _(e):
                run("sp")


class Arena:
    def __init__(self, t, nbytes):
        self.t = t
        self.n = nbytes
        self.off = 0
        self.hi = 0

    def mark(self):
        return self.off

    def release(self, m):
        self.off = m

    def _take(self, nbytes):
        nbytes = (nbytes + 31) // 32 * 32
        o = self.off
        self.off += nbytes
        self.hi = max(self.hi, self.off)
        assert self.off <= self.n, ("arena overflow", self.off, self.n)
        return o

    def f32(self, n):
        o = self._take(4 * n)
        return self.t[:, o // 4: o // 4 + n]

    def bf16(self, n):
        o = self._take(2 * n)
        return self.t[:, o // 4: o // 4 + (n + 1) // 2].bitcast(BF16)

    def i32(self, n):
        o = self._take(4 * n)
        return self.t[:, o // 4: o // 4 + n].bitcast(I32)


class Ring:
    def __init__(self, items):
        self.items = items
        self.i = 0

    def next(self):
        it = self.items[self.i % len(self.items)]
        self.i += 1
        return it


class WStream:
    def __init__(self, slots, plan, auto=True):
        self.slots = slots
        self.plan = plan
        self.auto = auto
        self.issued = 0
        self.used = 0
        self.freed = 0

    def top(self):
        while self.issued < len(self.plan) and self.issued - self.freed < len(self.slots):
            i = self.issued
            v, k = self.slots[i % len(self.slots)]
            self.plan[i](v, k)
            self.issued += 1

    def done(self):
        self.freed = self.used
        self.top()

    def get(self):
        i = self.used
        if self.auto:
            self.freed = i
        self.top()
        assert self.issued > i, "weight stream: block not issued (slots exhausted)"
        self.used += 1
        return self.slots[i % len(self.slots)]


def _host_consts():
    ident = np.eye(128, dtype=np.float32)
    ones = np.ones((128, 128), np.float32)
    blk2 = np.zeros((128, 128), np.float32)
    blk2[0:64, 0:64] = 1
    blk2[64:128, 64:128] = 1

    def rotT(block):
        h = block // 2
        m = np.zeros((128, 128), np.float32)
        for b0 in range(0, 128, block):
            for i in range(h):
                m[b0 + i + h, b0 + i] = -1.0
                m[b0 + i, b0 + i + h] = 1.0
        return m

    tri = np.where(np.arange(128)[None, :] >= np.arange(128)[:, None], 0.0, -MASK).astype(np.float32)
    cmat = np.concatenate([ident, ones, blk2, rotT(64), rotT(32), tri], axis=1)
    tril = (np.arange(128)[:, None] <= np.arange(128)[None, :]).astype(np.float32)
    pm = np.zeros((16, 8), np.float32)
    for qs in range(16):
        cur = qs // 2
        for n in range(8):
            pm[qs, n] = 0.0 if n < cur else (BIGG if n == cur else -2 * BIGG)
    pmask = np.broadcast_to(pm.reshape(1, 128), (128, 128))
    p = np.arange(128)
    invf64 = (10000.0 ** (-(2.0 * (p % 32)) / 64.0)) / (2 * math.pi)
    invf32 = (10000.0 ** (-(2.0 * (p % 16)) / 32.0)) / (2 * math.pi)
    gmask = (np.arange(128)[:, None] // 32 == np.arange(4)[None, :]).astype(np.float32)
    cf = np.concatenate([tril, pmask, invf64[:, None], invf32[:, None], gmask], axis=1).astype(np.float32)
    ind = (-MASK) * (np.arange(SEQ)[None, :] // 256 == np.arange(8)[:, None]).astype(np.float32)
    return np.ascontiguousarray(cmat), np.ascontiguousarray(cf), np.ascontiguousarray(ind)


def _fm(v, c):
    return np.ascontiguousarray(np.asarray(v, np.float32).reshape(c, 128).T)


def _host_params(inp):
    vd = np.stack([np.stack([_fm(inp[k][l], 8) for k in ("pre_mix_norm", "post_mix_norm", "pre_ffn_norm", "post_ffn_norm", "pe_gate_norm")], 1) for l in range(L)], 1)
    vg = np.stack([np.stack([_fm(inp[k][l], 2) for k in ("conv_dw_b", "conv_ln_g", "conv_ln_b", "conv_pw_b", "out_norm_a", "out_norm_b", "out_norm_c")], 1) for l in range(L)], 1)
    wdw = np.stack([np.asarray(inp["conv_dw_w"][l], np.float32).reshape(31, 2, 128).transpose(2, 1, 0) for l in range(L)], 1)
    fcw = np.stack([np.asarray(inp["ffn_conv_w"][l], np.float32).reshape(3, 44, 128).transpose(2, 1, 0) for l in range(L)], 1)
    fcb = np.stack([_fm(inp["ffn_conv_b"][l], 44) for l in range(L)], 1)
    gsub = np.stack([np.asarray(inp["diff_subln_g"][l], np.float32)[np.arange(128) % 64] for l in range(L)], 1)
    lamv = np.stack([np.stack([np.asarray(inp[k][l], np.float32) for k in ("diff_lq1", "diff_lk1", "diff_lq2", "diff_lk2")], 0) for l in range(L)], 0)
    lamv = np.broadcast_to(lamv.reshape(1, L * 4 * 32), (128, L * 4 * 32))
    pv = np.concatenate([vd.reshape(128, -1), vg.reshape(128, -1), wdw.reshape(128, -1), fcw.reshape(128, -1), fcb.reshape(128, -1), gsub.reshape(128, -1), lamv], axis=1)
    glnb = np.stack([np.stack([np.broadcast_to(np.asarray(inp[k][l], np.float32)[None, :], (128, 256)) for k in ("gmlp_ln_g", "gmlp_ln_b")], 1) for l in range(L)], 0)
    bs = np.asarray(inp["gmlp_bs"], np.float32)
    gb = np.zeros((L, 2, 128, 512), np.float32)
    for l in range(L):
        for cc in range(2):
            for hh in range(2):
                gb[l, cc, hh * 64:(hh + 1) * 64, :] = np.tile(bs[l, 2 * cc + hh], 4)[None, :]
    wsT = np.ascontiguousarray(np.asarray(inp["gmlp_ws"], np.float32).transpose(0, 1, 3, 2))
    return np.ascontiguousarray(pv.astype(np.float32)), np.ascontiguousarray(glnb), gb, wsT


PV_OFF = {}


def _pv_layout():
    o = 0
    for name, n in (("vd", L * 5 * 8), ("vg", L * 7 * 2), ("wdw", L * 2 * 31), ("fcw", L * 44 * 3), ("fcb", L * 44), ("gsub", L), ("lamv", L * 4 * 32)):
        PV_OFF[name] = o
        o += n
    return o


PV_N = _pv_layout()


def build(dbg=None, nlayers=L):
    dbg = dbg or []
    nc = bass.Bass("TRN2", target_bir_lowering=False)

    def din(name, shape, dt=F32):
        return nc.dram_tensor(name, list(shape), dt, kind="ExternalInput").ap()

    xT_d = din("xT", [D, SEQ])
    pT_d = din("pT", [L, GW, SEQ])
    pos_d = din("pos", [1, SEQ], I32)
    cmat_d = din("cmat", [128, 768])
    cf_d = din("cf", [128, 262])
    ind_d = din("ind", [8, SEQ])
    pv_d = din("pv", [128, PV_N])
    glnb_d = din("glnb", [L, 128, 2, 256])
    gb_d = din("gb", [L, 2, 128, 512])
    wsT_d = din("wsT", [L, 4, 128, 128])
    w_in_d = din("w_in", [L, D, 10 * GW])
    w_out_d = din("w_out", [L, D, D])
    w_up_d = din("w_up", [L, D, 2 * DFF])
    w_down_d = din("w_down", [L, DFF, D])
    w_gate_d = din("w_pe_gate", [L, D, D])
    w_proj_d = din("w_pe_proj", [L, GW, D])
    w_pw_d = din("conv_pw_w", [L, GW, GW])
    out_d = nc.dram_tensor("outT", [D, SEQ], F32, kind="ExternalOutput").ap()
    wup_s = nc.dram_tensor("wup_s", [L, NJ, 128, 8 * 2 * 128], BF16, kind="Internal").ap()
    wdn_s = nc.dram_tensor("wdn_s", [L, 8, 128, NJ * 128], BF16, kind="Internal").ap()
    dbg_d = {n: nc.dram_tensor("dbg_" + n, [128, w], F32, kind="ExternalOutput").ap() for n, w in dbg}

    es = ExitStack()
    with es:
        S = Sched(nc, es)
        NB = 206 * 1024
        big = es.enter_context(nc.sbuf_tensor("arena", [128, NB // 4], F32))
        A = Arena(big, NB)
        psb = [es.enter_context(nc.psum_tensor("ps%d" % i, [128, 512], F32)) for i in range(8)]
        PS = [(psb[i][:, :], ("ps", i)) for i in range(8)]
        ringA = Ring(PS[0:4])
        ringB = Ring(PS[4:8])

        def MM(out, lhsT, rhs, start, stop, r, w, inc=True, tp=None):
            kw = {}
            if tp is not None:
                kw["tile_position"] = tp
            S.op("pe", lambda: nc.tensor.matmul(out, lhsT=lhsT, rhs=rhs, start=start, stop=stop, **kw), r=r, w=w, inc=inc)

        def ACT(out, in_, func, r, w, scale=None, bias=None):
            kw = {}
            if scale is not None:
                kw["scale"] = scale
            if bias is not None:
                kw["bias"] = bias
            S.op("act", lambda: nc.scalar.activation(out=out, in_=in_, func=func, **kw), r=r, w=w)

        def TTo(eng, out, in0, in1, op, r, w):
            e = nc.vector if eng == "dve" else nc.gpsimd
            S.op(eng, lambda: e.tensor_tensor(out=out, in0=in0, in1=in1, op=op), r=r, w=w)

        def TS(eng, out, in0, s1, s2, op0, op1, r, w):
            e = nc.vector if eng == "dve" else nc.gpsimd
            if op1 is None:
                S.op(eng, lambda: e.tensor_scalar(out=out, in0=in0, scalar1=s1, scalar2=None, op0=op0), r=r, w=w)
            else:
                S.op(eng, lambda: e.tensor_scalar(out=out, in0=in0, scalar1=s1, scalar2=s2, op0=op0, op1=op1), r=r, w=w)

        def STT(out, in0, scalar, in1, op0, op1, r, w):
            S.op("dve", lambda: nc.vector.scalar_tensor_tensor(out=out, in0=in0, scalar=scalar, in1=in1, op0=op0, op1=op1), r=r, w=w)

        def CP(eng, out, in_, r, w):
            e = nc.vector if eng == "dve" else nc.gpsimd
            S.op(eng, lambda: e.tensor_copy(out=out, in_=in_), r=r, w=w)

        def MSET(eng, ap, val, w):
            e = nc.vector if eng == "dve" else nc.gpsimd
            S.op(eng, lambda: e.memset(ap, val), w=w)

        def DMA(q, out, in_, semkey, r, w):
            e = dict(sp=nc.sync, pool=nc.gpsimd, act=nc.scalar)[q]
            S.dma(q, lambda: e.dma_start(out=out, in_=in_), semkey, r=r, w=w)

        def dump(name, ap, keys):
            if name in dbg_d:
                w_ = ap.shape[-1]
                stg = A.f32(w_)
                CP("dve", stg[0:ap.shape[0], :], ap, r=keys, w=[("dbgs", name)])
                DMA("sp", dbg_d[name][0:ap.shape[0], 0:w_], stg[0:ap.shape[0], :], "dbg", r=[("dbgs", name)], w=[("dbgo", name)])

        xT = A.f32(8 * SEQ).rearrange("p (c s) -> p c s", c=8)
        hT = A.bf16(8 * SEQ).rearrange("p (c s) -> p c s", c=8)
        cmat = A.bf16(768)
        IDENT, ONES, BLK2, R64, R32, TRI = [cmat[:, i * 128:(i + 1) * 128] for i in range(6)]
        cf = A.f32(262)
        TRIL = cf[:, 0:128]
        PMASK = cf[:, 128:256].rearrange("p (q n) -> p q n", n=8)
        INVF = [cf[:, 256:257], cf[:, 257:258]]
        GMASK = cf[:, 258:262]
        pv = A.f32(PV_N)

        def PVv(name, n):
            return pv[:, PV_OFF[name]:PV_OFF[name] + n]

        vd = PVv("vd", L * 5 * 8).rearrange("p (l v c) -> p l v c", l=L, v=5)
        vg = PVv("vg", L * 7 * 2).rearrange("p (l v c) -> p l v c", l=L, v=7)
        wdw = PVv("wdw", L * 2 * 31).rearrange("p (l c k) -> p l c k", l=L, c=2)
        fcw = PVv("fcw", L * 44 * 3).rearrange("p (l j k) -> p l j k", l=L, j=44)
        fcb = PVv("fcb", L * 44).rearrange("p (l j) -> p l j", l=L)
        gsub = PVv("gsub", L)
        lamv = PVv("lamv", L * 4 * 32).rearrange("p (l v k) -> p l v k", l=L, v=4)
        small = A.f32(64)
        lnv = A.f32(512)
        rstd = A.f32(512)
        sq = [A.bf16(512) for _ in range(3)]
        sqring = Ring([(sq[i], ("sq", i)) for i in range(3)])
        PH = A.mark()

        DMA("pool", cmat, cmat_d[:, :], "ld_c", r=[], w=["cmat"])
        DMA("sp", cf, cf_d[:, :], "ld_c2", r=[], w=["cf"])
        DMA("sp", pv, pv_d[:, :], "ld_c3", r=[], w=["pv"])
        for c in range(8):
            DMA("sp", xT[:, c, :], xT_d[c * 128:(c + 1) * 128, :], ("ld_x", c), r=[], w=[("xT", c, t) for t in range(NT)])

        def wload(dst, src, key):
            DMA("pool", dst, src, ("wsem",) + key, r=[], w=[key])

        def precast_ffn(l):
            wupv_ = w_up_d[l].rearrange("(k p) (g n) -> p k g n", p=128, g=2)
            wdnv_ = w_down_d[l].rearrange("(j p) n -> p j n", p=128)
            sk = ("pcs", l)
            keys = []
            for j in range(NJ):
                for gi_ in range(2):
                    dst = wup_s[l, j].rearrange("p (k g n) -> p k g n", k=8, g=2)[:, :, gi_, :]
                    DMA("pool", dst, wupv_[:, :, gi_, j * 128:(j + 1) * 128], sk, r=[], w=[("pcu", l, j, gi_)])
                    keys.append(("pcu", l, j, gi_))
            for dc in range(8):
                dst = wdn_s[l, dc].rearrange("p (j n) -> p j n", j=NJ)
                DMA("pool", dst, wdnv_[:, :, dc * 128:(dc + 1) * 128], sk, r=[], w=[("pcd", l, dc)])
                keys.append(("pcd", l, dc))
            tot = S.cnt[sk]
            for k in keys:
                S.state[k] = [(sk, tot), []]

        def rstd_from(ps_ap, ps_key, inv_n, eps, extra_r=()):
            ACT(lnv, ps_ap, AF.Ln, r=[ps_key] + list(extra_r), w=["lnv"], scale=inv_n, bias=eps)
            ACT(rstd, lnv, AF.Exp, r=["lnv"], w=["rstd"], scale=-0.5)

        def rmsnorm_tile(srcs, skeys, dsts, dkeys, gains, n_feat, eps=EPS, lhs=None, lkey="cmat"):
            ps, pk = ringB.next()
            C = len(srcs)
            for c in range(C):
                q, qk = sqring.next()
                ACT(q, srcs[c], AF.Square, r=[skeys[c]], w=[qk])
                MM(ps, lhs if lhs is not None else ONES, q, c == 0, c == C - 1, r=[qk, lkey], w=[pk])
            rstd_from(ps, pk, 1.0 / n_feat, eps)
            for c in range(C):
                STT(dsts[c], srcs[c], gains[c], rstd, ALU.mult, ALU.mult, r=[skeys[c], "rstd", "pv"], w=[dkeys[c]])

        def sl(t):
            return slice(t * TW, (t + 1) * TW)

        def norm_x_to_h(l, vidx):
            for t in range(NT):
                rmsnorm_tile([xT[:, c, sl(t)] for c in range(8)], [("xT", c, t) for c in range(8)],
                             [hT[:, c, sl(t)] for c in range(8)], [("hT", c, t) for c in range(8)],
                             [vd[:, l, vidx, c:c + 1] for c in range(8)], D)

        def proj(ps, pk, wv, wkey, cs, rhs_fn, rkeys_fn, nk=8):
            for kc in range(nk):
                MM(ps, wv[:, kc, cs], rhs_fn(kc), kc == 0, kc == nk - 1, r=[wkey] + rkeys_fn(kc), w=[pk], inc=(kc == nk - 1))

        def hrhs(t):
            return (lambda kc: hT[:, kc, sl(t)]), (lambda kc: [("hT", kc, t)])

        for l in range(nlayers):
            S.barrier()
            A.release(PH)
            catT = A.bf16(8 * SEQ).rearrange("p (c s) -> p c s", c=8)
            ropeC = A.bf16(SEQ)
            ropeS = A.bf16(SEQ)
            wslots = [A.bf16(8 * 256).rearrange("p (k n) -> p k n", k=8) for _ in range(4)]
            MX = A.mark()
            winv = w_in_d[l].rearrange("(k p) n -> p k n", p=128)
            woutv = w_out_d[l].rearrange("(k p) n -> p k n", p=128)
            mplan = [(lambda v, k, bi=bi: wload(v, winv[:, :, bi * 256:(bi + 1) * 256], k)) for bi in (0, 1, 2, 7, 8, 9, 3, 4, 5, 6)]
            mplan += [(lambda v, k, db=db: wload(v, woutv[:, :, db * 256:(db + 1) * 256], k)) for _t in range(NT) for db in range(4)]
            mstream = WStream([(wslots[i], ("ws", i)) for i in range(4)], mplan, auto=False)

            def win_block(bi):
                return mstream.get()

            def rope_tables(which):
                m = A.mark()
                posi = A.i32(SEQ)
                v = A.f32(SEQ)
                ki = A.i32(SEQ)
                DMA("sp", posi, pos_d[0:1, :].broadcast_to([128, SEQ]), "ld_pos", r=[], w=["posi"])
                for tab, shift, key in ((ropeS, 0.0, "ropeS"), (ropeC, 0.25, "ropeC")):
                    TS("dve", v, posi, INVF[which], shift, ALU.mult, ALU.add, r=["posi", "cf"], w=["ropev"])
                    CP("dve", ki, v, r=["ropev"], w=["ropek"])
                    TTo("dve", v, v, ki, ALU.subtract, r=["ropev", "ropek"], w=["ropev"])
                    ACT(tab, v, AF.Sin, r=["ropev"], w=[key], scale=2 * math.pi * (1 - 1e-6))
                S.barrier()
                A.release(m)

            def rope_apply(ps, pk, t, RM, zbr, t1, t2):
                zb, zk = zbr.next()
                ACT(zb, ps, AF.Copy, r=[pk], w=[zk])
                ps2, pk2 = ringA.next()
                MM(ps2, RM, zb, True, True, r=[zk, "cmat"], w=[pk2])
                TTo("dve", t1[0], zb, ropeC[:, sl(t)], ALU.mult, r=[zk, "ropeC"], w=[t1[1]])
                TTo("dve", t2[0], ps2, ropeS[:, sl(t)], ALU.mult, r=[pk2, "ropeS"], w=[t2[1]])

            def attn_finalize_recip(O, ok, dst_rden):
                ACT(dst_rden[0][0:64, :], O[64:128, :], AF.Ln, r=[ok], w=[dst_rden[1]])
                ACT(dst_rden[0][0:64, :], dst_rden[0][0:64, :], AF.Exp, r=[dst_rden[1]], w=[dst_rden[1]], scale=-1.0)

            def vproj(wv, wk, cs, vaug):
                for g in range(4):
                    ps, pk = ringA.next()
                    for s4 in range(4):
                        st = g * 4 + s4
                        for kc in range(8):
                            MM(ps[:, s4 * 128:(s4 + 1) * 128], hT[:, kc, st * 128:(st + 1) * 128], wv[:, kc, cs], kc == 0, kc == 7,
                               r=[wk, ("hT", kc, st // 4)], w=[pk], inc=(kc == 7 and s4 == 3))
                    ACT(vaug[:, g * 4:(g + 1) * 4, :, 0:64], ps.rearrange("p (s h d) -> p s h d", s=4, h=2), AF.Copy, r=[pk], w=[("V", g)])

            norm_x_to_h(l, 0)
            if l == 0:
                dump("hT0", hT[:, 0, 0:512], [("hT", 0, 0)])

            rope_tables(0)
            m0 = A.mark()
            Kaug = [A.bf16(SEQ) for _ in range(2)]
            vaug = A.bf16(16 * 2 * 128).rearrange("p (s h d) -> p s h d", s=16, h=2)
            Qa = [[A.bf16(512) for _ in range(2)] for _ in range(NT)]
            Pr = Ring([(A.bf16(512), ("P", i)) for i in range(4)])
            zbr = Ring([(A.bf16(512), ("zb", i)) for i in range(2)])
            t1 = (A.f32(512), "t1")
            t2 = (A.f32(512), "t2")
            rden = (A.f32(512), "rden")
            bstg4 = A.bf16(4 * 72).rearrange("p (j n) -> p j n", j=4)
            gm = A.f32(64)
            top8 = A.f32(32)
            kmf = A.f32(16)
            kmb = A.bf16(16)
            MSET("dve", vaug[:, :, :, 64:128], 1.0, w=[("V", g) for g in range(4)])
            MSET("dve", bstg4, 0.0, w=["bstg"])
            for hh in range(2):
                DMA("pool", Kaug[hh][64:72, :], ind_d[:, :], "ld_ind", r=[], w=[("Kind", hh)])
            wq, wqk = win_block(0)
            wk_, wkk = win_block(1)
            wv_, wvk = win_block(2)
            precast_ffn(l)
            for c in range(2):
                cs = slice(c * 128, (c + 1) * 128)
                for t in range(NT):
                    ps, pk = ringA.next()
                    rf, kf = hrhs(t)
                    proj(ps, pk, wk_, wkk, cs, rf, kf)
                    rope_apply(ps, pk, t, R64, zbr, t1, t2)
                    for hh in range(2):
                        TTo("dve", Kaug[hh][0:64, sl(t)], t1[0][hh * 64:(hh + 1) * 64, :], t2[0][hh * 64:(hh + 1) * 64, :], ALU.add,
                            r=[t1[1], t2[1]], w=[("K", hh, t)])
                for hh in range(2):
                    S.op("dve", lambda hh=hh: nc.vector.tensor_reduce(out=kmf[0:64, hh * 8:(hh + 1) * 8], in_=Kaug[hh][0:64, :].rearrange("p (n k) -> p n k", k=256), axis=AX.X, op=ALU.add),
                         r=[("K", hh, t) for t in range(NT)], w=[("kmf", hh)])
                    TS("dve", kmb[0:64, hh * 8:(hh + 1) * 8], kmf[0:64, hh * 8:(hh + 1) * 8], 1.0 / 256, None, ALU.mult, None, r=[("kmf", hh)], w=[("kmb", hh)])
                vproj(wv_, wvk, cs, vaug)
                for qt in range(NT):
                    qb = Qa[qt]
                    ps, pk = ringA.next()
                    rf, kf = hrhs(qt)
                    proj(ps, pk, wq, wqk, cs, rf, kf)
                    rope_apply(ps, pk, qt, R64, zbr, t1, t2)
                    for hh in range(2):
                        TTo("dve", qb[hh][0:64, :], t1[0][hh * 64:(hh + 1) * 64, :], t2[0][hh * 64:(hh + 1) * 64, :], ALU.add,
                            r=[t1[1], t2[1]], w=[("Qa", qt, hh)])
                    for hh in range(2):
                        gps, gk = ringA.next()
                        for j in range(4):
                            MM(gps[:, j * 8:(j + 1) * 8], qb[hh][0:64, j * 128:(j + 1) * 128], kmb[0:64, hh * 8:(hh + 1) * 8], True, True,
                               r=[("Qa", qt, hh), ("kmb", hh)], w=[gk], inc=(j == 3))
                        gmv = gm[:, 0:32].rearrange("p (j n) -> p j n", n=8)
                        TTo("dve", gmv, gps[:, 0:32].rearrange("p (j n) -> p j n", n=8), PMASK[:, qt * 4:(qt + 1) * 4, :], ALU.add, r=[gk, "cf"], w=["gm"])
                        tps, tk = ringA.next()
                        tpsb = tps.bitcast(BF16)
                        top8v = top8.rearrange("p (j n) -> p j n", n=8)
                        for j in range(4):
                            S.op("dve", lambda j=j: nc.vector.max(out=top8[:, j * 8:(j + 1) * 8], in_=gm[:, j * 8:(j + 1) * 8]), r=["gm"], w=["top8"])
                        TS("dve", top8v[:, :, 3:4], top8v[:, :, 3:4], -BIGG, None, ALU.max, None, r=["top8"], w=["top8"])
                        TTo("dve", bstg4[:, :, 64:72], gmv, top8v[:, :, 3:4].broadcast_to([128, 4, 8]), ALU.is_lt, r=["gm", "top8"], w=["bstg"])
                        for j in range(4):
                            S.op("pe", lambda j=j, tpsb=tpsb: nc.tensor.transpose(tpsb[0:72, j * 128:(j + 1) * 128], bstg4[:, j, :], IDENT), r=["bstg", "cmat"], w=[tk], inc=(j == 3))
                        ACT(qb[hh][64:72, :], tpsb[64:72, 0:512], AF.Copy, r=[tk], w=[("Qb", qt, hh)])
                for qt in range(NT):
                    qb = Qa[qt]
                    for hh in range(2):
                        O, ok = ringB.next()
                        nk = 4 * qt + 4
                        pend = []
                        for kt in range(nk):
                            jd = kt - 4 * qt
                            c0 = 128 * jd if jd > 0 else 0
                            sp_, sk = ringA.next()
                            MM(sp_[:, c0:512], Kaug[hh][0:72, kt * 128:(kt + 1) * 128], qb[hh][0:72, c0:512], True, jd < 0,
                               r=[("K", hh, kt // 4), ("Kind", hh), ("Qa", qt, hh), ("Qb", qt, hh)], w=[sk], inc=(jd < 0))
                            if jd >= 0:
                                MM(sp_[:, c0:c0 + 128], IDENT, TRI, False, True, r=["cmat"], w=[sk])
                            P, Pk = Pr.next()
                            ACT(P[:, c0:512], sp_[:, c0:512], AF.Exp, r=[sk], w=[Pk], scale=0.125)

                            def pv_(kt=kt, c0=c0, P=P, Pk=Pk, O=O, ok=ok, hh=hh, nk=nk):
                                MM(O[:, c0:512], vaug[:, kt, hh, :], P[:, c0:512], kt == 0, kt == nk - 1, r=[Pk, ("V", kt // 4)], w=[ok])
                            pend.append(pv_)
                            if len(pend) > 2:
                                pend.pop(0)()
                        for f in pend:
                            f()
                        attn_finalize_recip(O, ok, rden)
                        TTo("dve", catT[hh * 64:(hh + 1) * 64, c, sl(qt)], O[0:64, :], rden[0][0:64, :], ALU.mult, r=[ok, rden[1]], w=[("cat", c, qt)])
            if l == 0:
                dump("oa_pre", catT[:, 0, 0:512], [("cat", 0, 0)])
            for t in range(NT):
                rmsnorm_tile([catT[:, c, sl(t)] for c in range(2)], [("cat", c, t) for c in range(2)],
                             [catT[:, c, sl(t)] for c in range(2)], [("cat", c, t) for c in range(2)],
                             [vg[:, l, 4, c:c + 1] for c in range(2)], GW)
            if l == 0:
                dump("oa", catT[:, 0, 0:512], [("cat", 0, 0)])
            mstream.done()
            S.barrier()
            A.release(m0)

            rope_tables(1)
            lam_init = 0.8 - 0.6 * math.exp(-0.3 * l)
            lt = small[:, 0:8]
            prod = A.f32(64)
            TTo("dve", prod[:, 0:32], lamv[:, l, 0, :], lamv[:, l, 1, :], ALU.mult, r=["pv"], w=["prod"])
            S.op("dve", lambda: nc.vector.tensor_reduce(out=lt[:, 0:1], in_=prod[:, 0:32], axis=AX.X, op=ALU.add), r=["prod"], w=["lt"])
            TTo("dve", prod[:, 32:64], lamv[:, l, 2, :], lamv[:, l, 3, :], ALU.mult, r=["pv"], w=["prod2"])
            S.op("dve", lambda: nc.vector.tensor_reduce(out=lt[:, 1:2], in_=prod[:, 32:64], axis=AX.X, op=ALU.add), r=["prod2"], w=["lt"])
            ACT(lt[:, 2:4], lt[:, 0:2], AF.Exp, r=["lt"], w=["lt"])
            TTo("dve", lt[:, 4:5], lt[:, 3:4], lt[:, 2:3], ALU.subtract, r=["lt"], w=["lt"])
            TS("dve", lt[:, 5:6], lt[:, 4:5], -lam_init, None, ALU.add, None, r=["lt"], w=["lt"])
            TS("dve", lt[:, 6:7], gsub[:, l:l + 1], 1.0 - lam_init, None, ALU.mult, None, r=["pv", "lt"], w=["lt"])
            NEGLAM = lt[:, 5:6]
            GS = lt[:, 6:7]

            m0 = A.mark()
            Kd = A.bf16(SEQ)
            vaug = A.bf16(16 * 2 * 128).rearrange("p (s h d) -> p s h d", s=16, h=2)
            Qd = [A.bf16(512) for _ in range(NT)]
            Pr = Ring([(A.bf16(512), ("P", i)) for i in range(4)])
            zbr = Ring([(A.bf16(512), ("zb", i)) for i in range(2)])
            t1 = (A.f32(512), "t1")
            t2 = (A.f32(512), "t2")
            rden = (A.f32(512), "rden")
            ods = [A.f32(512) for _ in range(2)]
            Qm = [[A.bf16(512) for _ in range(4)] for _ in range(2)]
            MSET("dve", vaug[:, :, :, 64:128], 1.0, w=[("V", g) for g in range(4)])
            wq, wqk = win_block(7)
            wk_, wkk = win_block(8)
            wv_, wvk = win_block(9)
            dscale = 32.0 ** -0.5
            for c in range(2):
                cs = slice(c * 128, (c + 1) * 128)
                for t in range(NT):
                    ps, pk = ringA.next()
                    rf, kf = hrhs(t)
                    proj(ps, pk, wk_, wkk, cs, rf, kf)
                    rope_apply(ps, pk, t, R32, zbr, t1, t2)
                    TTo("dve", Kd[:, sl(t)], t1[0], t2[0], ALU.add, r=[t1[1], t2[1]], w=[("Kd", t)])
                vproj(wv_, wvk, cs, vaug)
                for qt in range(NT):
                    qd = Qd[qt]
                    ps, pk = ringA.next()
                    rf, kf = hrhs(qt)
                    proj(ps, pk, wq, wqk, cs, rf, kf)
                    rope_apply(ps, pk, qt, R32, zbr, t1, t2)
                    TTo("dve", qd, t1[0], t2[0], ALU.add, r=[t1[1], t2[1]], w=[("Qd", qt)])
                pend_n = None

                def qmask(qt):
                    for g in range(4):
                        TS("dve", Qm[qt % 2][g], Qd[qt], GMASK[:, g:g + 1], None, ALU.mult, None, r=[("Qd", qt), "cf"], w=[("Qm", qt % 2, g)])
                qmask(0)
                for qt in range(NT):
                    qd = Qd[qt]
                    od = ods[qt % 2]
                    for hh in range(2):
                        Os = [ringB.next(), ringB.next()]
                        nk = 4 * qt + 4
                        pend = []
                        for kt in range(nk):
                            jd = kt - 4 * qt
                            c0 = 128 * jd if jd > 0 else 0
                            cur = []
                            for m_ in range(2):
                                g = hh * 2 + m_
                                sp_, sk = ringA.next()
                                MM(sp_[:, c0:512], Kd[:, kt * 128:(kt + 1) * 128], Qm[qt % 2][g][:, c0:512], True, jd < 0,
                                   r=[("Kd", kt // 4), ("Qm", qt % 2, g)], w=[sk], inc=(jd < 0))
                                if jd >= 0:
                                    MM(sp_[:, c0:c0 + 128], IDENT, TRI, False, True, r=["cmat"], w=[sk])
                                cur.append((sp_, sk))
                            for f in pend:
                                f()
                            pend = []
                            for m_ in range(2):
                                sp_, sk = cur[m_]
                                P, Pk = Pr.next()
                                ACT(P[:, c0:512], sp_[:, c0:512], AF.Exp, r=[sk], w=[Pk], scale=dscale)

                                def pv_(kt=kt, c0=c0, P=P, Pk=Pk, Oo=Os[m_], hh=hh, nk=nk):
                                    MM(Oo[0][:, c0:512], vaug[:, kt, hh, :], P[:, c0:512], kt == 0, kt == nk - 1, r=[Pk, ("V", kt // 4)], w=[Oo[1]])
                                pend.append(pv_)
                        for f in pend:
                            f()
                        hs = slice(hh * 64, (hh + 1) * 64)
                        attn_finalize_recip(Os[0][0], Os[0][1], rden)
                        TTo("dve", t1[0][0:64, :], Os[0][0][0:64, :], rden[0][0:64, :], ALU.mult, r=[Os[0][1], rden[1]], w=[t1[1]])
                        attn_finalize_recip(Os[1][0], Os[1][1], rden)
                        TTo("dve", t2[0][0:64, :], Os[1][0][0:64, :], rden[0][0:64, :], ALU.mult, r=[Os[1][1], rden[1]], w=[t2[1]])
                        STT(od[hs, :], t2[0][0:64, :], NEGLAM[0:64, :], t1[0][0:64, :], ALU.mult, ALU.add, r=[t1[1], t2[1], "lt"], w=[("od", qt % 2, hh)])
                        if hh == 0 and pend_n is not None:
                            pend_n()
                            pend_n = None
                        if hh == 0 and qt + 1 < NT:
                            qmask(qt + 1)
                    def norm_(qt=qt, od=od, c=c):
                        ps, pk = ringA.next()
                        q_, qk_ = sqring.next()
                        ACT(q_, od, AF.Square, r=[("od", qt % 2, 0), ("od", qt % 2, 1)], w=[qk_])
                        MM(ps, BLK2, q_, True, True, r=[qk_, "cmat"], w=[pk])
                        rstd_from(ps, pk, 1.0 / 64, 1e-5)
                        STT(catT[:, 6 + c, sl(qt)], od, GS, rstd, ALU.mult, ALU.mult, r=[("od", qt % 2, 0), ("od", qt % 2, 1), "rstd", "lt"], w=[("cat", 6 + c, qt)])
                    pend_n = norm_
                pend_n()
                pend_n = None
            if l == 0:
                dump("od", catT[:, 6, 0:512], [("cat", 6, 0)])
            mstream.done()
            S.barrier()
            A.release(m0)

            m0 = A.mark()
            uT = A.bf16(2 * SEQ).rearrange("p (c s) -> p c s", c=2)
            vgl = A.bf16(16 * 256).rearrange("p (n d) -> p n d", n=16)
            glnb = A.f32(512).rearrange("p (v d) -> p v d", v=2)
            gb = A.f32(1024).rearrange("p (c s) -> p c s", c=2)
            wsf = A.f32(512).rearrange("p (h i) -> p h i", h=4)
            wsb = A.bf16(512).rearrange("p (h i) -> p h i", h=4)
            stats = A.f32(16 * 6)
            mv = A.f32(16 * 2).rearrange("p (n k) -> p n k", k=2)
            rsd = A.f32(16)
            vtmp = A.f32(256)
            vln = [A.bf16(256) for _ in range(2)]
            stmp = A.f32(512)
            DMA("sp", glnb, glnb_d[l], "ld_g1", r=[], w=["glnb"])
            DMA("sp", gb, gb_d[l].rearrange("c p s -> p c s"), "ld_g2", r=[], w=["gb"])
            DMA("sp", wsf, wsT_d[l].rearrange("h j i -> j h i"), "ld_g3", r=[], w=["wsf"])
            for h in range(4):
                TTo("dve", wsb[:, h, :], wsf[:, h, :], TRIL, ALU.mult, r=["wsf", "cf"], w=["wsb"])
            wu, wuk = win_block(3)
            wvv, wvvk = win_block(4)
            for t in range(NT):
                for cc in range(2):
                    ps, pk = ringA.next()
                    rf, kf = hrhs(t)
                    proj(ps, pk, wu, wuk, slice(cc * 128, (cc + 1) * 128), rf, kf)
                    ACT(uT[:, cc, sl(t)], ps, AF.Gelu_apprx_tanh, r=[pk], w=[("uT", cc, t)])
            for n2 in range(8):
                ps, pk = ringA.next()
                for s2 in range(2):
                    n = n2 * 2 + s2
                    for kc in range(8):
                        MM(ps[:, s2 * 256:(s2 + 1) * 256], hT[:, kc, n * 128:(n + 1) * 128], wvv[:, kc, :], kc == 0, kc == 7,
                           r=[wvvk, ("hT", kc, n // 4)], w=[pk], inc=(kc == 7 and s2 == 1))
                ACT(vgl[:, n2 * 2:(n2 + 1) * 2, :], ps.rearrange("p (s d) -> p s d", s=2), AF.Gelu_apprx_tanh, r=[pk], w=[("vgl", n2)])
            for n in range(16):
                S.op("dve", lambda n=n: nc.vector.bn_stats(out=stats[:, n * 6:(n + 1) * 6], in_=vgl[:, n, :]), r=[("vgl", n // 2)], w=[("st", n)])
                S.op("dve", lambda n=n: nc.vector.bn_aggr(out=mv[:, n, :], in_=stats[:, n * 6:(n + 1) * 6]), r=[("st", n)], w=["mv"])
            ACT(rsd, mv[:, :, 1], AF.Ln, r=["mv"], w=["rsd"], bias=EPS)
            ACT(rsd, rsd, AF.Exp, r=["rsd"], w=["rsd"], scale=-0.5)
            for t in range(NT):
                pss = [ringA.next(), ringA.next()]
                for s4 in range(4):
                    n = t * 4 + s4
                    vl = vln[n % 2]
                    vk = ("vln", n % 2)
                    TS("dve", vtmp, vgl[:, n, :], mv[:, n, 0:1], rsd[:, n:n + 1], ALU.subtract, ALU.mult, r=[("vgl", n // 2), "mv", "rsd"], w=["vtmp"])
                    TTo("dve", vtmp, vtmp, glnb[:, 0, :], ALU.mult, r=["vtmp", "glnb"], w=["vtmp"])
                    TTo("dve", vl, vtmp, glnb[:, 1, :], ALU.add, r=["vtmp", "glnb"], w=[vk])
                    for cc in range(2):
                        for hh in range(2):
                            h = 2 * cc + hh
                            MM(pss[cc][0][hh * 64:(hh + 1) * 64, s4 * 128:(s4 + 1) * 128], vl[:, h * 64:(h + 1) * 64], wsb[:, h, :], True, True,
                               r=[vk, "wsb"], w=[pss[cc][1]], tp=(0, hh * 64))
                for cc in range(2):
                    TTo("dve", stmp, pss[cc][0], gb[:, cc, :], ALU.add, r=[pss[cc][1], "gb"], w=["stmp"])
                    TTo("dve", catT[:, 2 + cc, sl(t)], stmp, uT[:, cc, sl(t)], ALU.mult, r=["stmp", ("uT", cc, t)], w=[("cat", 2 + cc, t)])
            if l == 0:
                dump("ob_pre", catT[:, 2, 0:512], [("cat", 2, 0)])
            for t in range(NT):
                rmsnorm_tile([catT[:, 2 + c, sl(t)] for c in range(2)], [("cat", 2 + c, t) for c in range(2)],
                             [catT[:, 2 + c, sl(t)] for c in range(2)], [("cat", 2 + c, t) for c in range(2)],
                             [vg[:, l, 5, c:c + 1] for c in range(2)], GW)
            mstream.done()
            S.barrier()
            A.release(m0)

            m0 = A.mark()
            ybuf = A.bf16(32 + SEQ)
            diag = A.bf16(31 * 128).rearrange("p (k n) -> p k n", k=31)
            cy = A.bf16(2 * SEQ).rearrange("p (c s) -> p c s", c=2)
            sg = A.f32(512)
            wpw = A.bf16(2 * 256).rearrange("p (k n) -> p k n", k=2)
            mstat = A.f32(512)
            m2 = A.f32(512)
            yn = A.f32(512)
            sil = A.bf16(1024).rearrange("p (c s) -> p c s", c=2)
            Y0 = 2
            wa, wak = win_block(5)
            wg_, wgk = win_block(6)
            wload(wpw, w_pw_d[l].rearrange("(k p) n -> p k n", p=128), ("wpw",))
            MSET("dve", ybuf[:, 0:32], 0.0, w=["ypad"])
            for cc in range(2):
                cs = slice(cc * 128, (cc + 1) * 128)
                for k in range(31):
                    TS("dve", diag[:, k, :], IDENT, wdw[:, l, cc, k:k + 1], None, ALU.mult, None, r=["cmat", "pv"], w=[("diag", k)])
                for t in range(NT):
                    pa, pak = ringA.next()
                    rf, kf = hrhs(t)
                    proj(pa, pak, wa, wak, cs, rf, kf)
                    pg, pgk = ringA.next()
                    proj(pg, pgk, wg_, wgk, cs, rf, kf)
                    ACT(sg, pg, AF.Sigmoid, r=[pgk], w=["sg"])
                    TTo("dve", ybuf[:, 32 + t * TW:32 + (t + 1) * TW], pa, sg, ALU.mult, r=[pak, "sg"], w=[("yb", t)])
                for t in range(NT):
                    ps, pk = ringB.next()
                    for k in range(31):
                        o = Y0 + t * TW + k
                        rk = ["ypad", ("diag", k), ("yb", t)] + ([("yb", t - 1)] if t > 0 else [])
                        MM(ps, diag[:, k, :], ybuf[:, o:o + TW], k == 0, k == 30, r=rk, w=[pk], inc=(k == 30))
                    ACT(cy[:, cc, sl(t)], ps, AF.Identity, r=[pk, "pv"], w=[("cy", cc, t)], bias=vg[:, l, 0, cc:cc + 1])
            if l == 0:
                dump("cy", cy[:, 0, 0:512], [("cy", 0, 0)])
            for t in range(NT):
                ps1, pk1 = ringB.next()
                ps2, pk2 = ringB.next()
                for cc in range(2):
                    MM(ps1, ONES, cy[:, cc, sl(t)], cc == 0, cc == 1, r=[("cy", cc, t), "cmat"], w=[pk1])
                for cc in range(2):
                    q_, qk_ = sqring.next()
                    ACT(q_, cy[:, cc, sl(t)], AF.Square, r=[("cy", cc, t)], w=[qk_])
                    MM(ps2, ONES, q_, cc == 0, cc == 1, r=[qk_, "cmat"], w=[pk2])
                TS("dve", mstat, ps1, 1.0 / GW, None, ALU.mult, None, r=[pk1], w=["mstat"])
                TTo("dve", m2, mstat, mstat, ALU.mult, r=["mstat"], w=["m2"])
                STT(m2, ps2, 1.0 / GW, m2, ALU.mult, ALU.subtract, r=[pk2, "m2"], w=["m2"])
                ACT(lnv, m2, AF.Ln, r=["m2"], w=["lnv"], bias=EPS)
                ACT(rstd, lnv, AF.Exp, r=["lnv"], w=["rstd"], scale=-0.5)
                for cc in range(2):
                    TTo("dve", yn, cy[:, cc, sl(t)], mstat, ALU.subtract, r=[("cy", cc, t), "mstat"], w=["yn"])
                    TTo("dve", yn, yn, rstd, ALU.mult, r=["yn", "rstd"], w=["yn"])
                    ACT(sil[:, cc, :], yn, AF.Silu, r=["yn", "pv"], w=[("sil", cc)], scale=vg[:, l, 1, cc:cc + 1], bias=vg[:, l, 2, cc:cc + 1])
                for co in range(2):
                    ps, pk = ringA.next()
                    for ci in range(2):
                        MM(ps, wpw[:, ci, co * 128:(co + 1) * 128], sil[:, ci, :], ci == 0, ci == 1, r=[("wpw",), ("sil", ci)], w=[pk])
                    ACT(catT[:, 4 + co, sl(t)], ps, AF.Identity, r=[pk, "pv"], w=[("cat", 4 + co, t)], bias=vg[:, l, 3, co:co + 1])
            if l == 0:
                dump("oc_pre", catT[:, 4, 0:512], [("cat", 4, 0)])
            for t in range(NT):
                rmsnorm_tile([catT[:, 4 + c, sl(t)] for c in range(2)], [("cat", 4 + c, t) for c in range(2)],
                             [catT[:, 4 + c, sl(t)] for c in range(2)], [("cat", 4 + c, t) for c in range(2)],
                             [vg[:, l, 6, c:c + 1] for c in range(2)], GW)
            mstream.done()
            S.barrier()
            A.release(m0)

            def out_proj_norm_res(nkc, wsrc, rhs_fn, rkeys_fn, vidx, wr, ytile):
                for t in range(NT):
                    for db in range(4):
                        wv, wk = wr.get()
                        for dd in range(2):
                            dc = db * 2 + dd
                            ps, pk = ringA.next()
                            for kc in range(nkc):
                                MM(ps, wv[:, kc, dd * 128:(dd + 1) * 128], rhs_fn(kc, t), kc == 0, kc == nkc - 1, r=[wk] + rkeys_fn(kc, t), w=[pk], inc=(kc == nkc - 1))
                            if dc % 2 == 0:
                                ACT(ytile[:, dc, :], ps, AF.Copy, r=[pk], w=[("yt", dc)])
                            else:
                                CP("dve", ytile[:, dc, :], ps, r=[pk], w=[("yt", dc)])
                        wr.done()
                    rmsnorm_tile([ytile[:, dc, :] for dc in range(8)], [("yt", dc) for dc in range(8)],
                                 [ytile[:, dc, :] for dc in range(8)], [("yt", dc) for dc in range(8)],
                                 [vd[:, l, vidx, dc:dc + 1] for dc in range(8)], D)
                    for dc in range(8):
                        TTo("dve", xT[:, dc, sl(t)], xT[:, dc, sl(t)], ytile[:, dc, :], ALU.add, r=[("xT", dc, t), ("yt", dc)], w=[("xT", dc, t)])

            m0 = A.mark()
            ytile = A.f32(8 * 512).rearrange("p (c s) -> p c s", c=8)
            out_proj_norm_res(8, w_out_d[l].rearrange("(k p) n -> p k n", p=128), lambda kc, t: catT[:, kc, sl(t)], lambda kc, t: [("cat", kc, t)], 1, mstream, ytile)
            if l == 0:
                dump("x1", xT[:, 0, 0:512], [("xT", 0, 0)])
            S.barrier()
            A.release(MX)
            A.release(PH)

            norm_x_to_h(l, 2)
            ytile = A.f32(8 * 512).rearrange("p (c s) -> p c s", c=8)
            fT = A.bf16(NJ * 512).rearrange("p (j s) -> p j s", j=NJ)
            ub = [[A.f32(2 + 512) for _ in range(2)] for _ in range(2)]
            halo = A.f32(44 * 2).rearrange("p (j k) -> p j k", k=2)
            tas = [A.f32(512) for _ in range(2)]
            tbs = [A.f32(512) for _ in range(2)]
            cgs = [A.f32(512) for _ in range(2)]
            cvs = [A.f32(512) for _ in range(2)]
            wups = [A.bf16(8 * 2 * 128).rearrange("p (k g n) -> p k g n", k=8, g=2) for _ in range(3)]
            wdns = [A.bf16(NJ * 128).rearrange("p (j n) -> p j n", j=NJ) for _ in range(3)]
            MSET("dve", halo, 0.0, w=["halo"])
            wupv = w_up_d[l].rearrange("(k p) (g n) -> p k g n", p=128, g=2)
            wdnv = w_down_d[l].rearrange("(j p) n -> p j n", p=128)

            def ld_up(v, k, j):
                DMA("sp", v.rearrange("p k g n -> p (k g n)"), wup_s[l, j], ("wsem",) + k, r=[("pcu", l, j, 0), ("pcu", l, j, 1)], w=[k])

            def ld_dn(v, k, dc):
                DMA("sp", v.rearrange("p j n -> p (j n)"), wdn_s[l, dc], ("wsem",) + k, r=[("pcd", l, dc)], w=[k])

            wur = WStream([(wups[i], ("wu", i)) for i in range(3)], [(lambda v, k, j=j: ld_up(v, k, j)) for _t in range(NT) for j in range(NJ)])
            wdr = WStream([(wdns[i], ("wd", i)) for i in range(3)], [(lambda v, k, dc=dc: ld_dn(v, k, dc)) for _t in range(NT) for dc in range(8)])
            wur.top()
            wdr.top()
            pend_f = None
            for t in range(NT):
                for j in range(NJ):
                    wv, wk = wur.get()
                    u = ub[j % 2]
                    b2 = j % 2
                    ta, tb, cg, cv = tas[b2], tbs[b2], cgs[b2], cvs[b2]
                    for gi in range(2):
                        jj = j + gi * NJ
                        ps, pk = ringA.next()
                        for kc in range(8):
                            MM(ps, wv[:, kc, gi, :], hT[:, kc, sl(t)], kc == 0, kc == 7, r=[wk, ("hT", kc, t)], w=[pk], inc=(kc == 7))
                        uk = ("ub", b2, gi)
                        ACT(u[gi][:, 2:514], ps, AF.Copy, r=[pk], w=[uk])
                        CP("dve", u[gi][:, 0:2], halo[:, jj, :], r=["halo", ("halo", jj)], w=[uk])
                        tmp = ta if gi == 0 else tb
                        tk_ = ("ta", b2) if gi == 0 else ("tb", b2)
                        ACT(tmp, ps, AF.Identity, r=[pk, "pv"], w=[tk_], scale=fcw[:, l, jj, 2:3], bias=fcb[:, l, jj:jj + 1])
                        ACT(halo[:, jj, :], ps[:, 510:512], AF.Copy, r=[pk], w=[("halo", jj)])
                        dst = cg if gi == 0 else cv
                        dk = ("cg", b2) if gi == 0 else ("cv", b2)
                        STT(tmp, u[gi][:, 1:513], fcw[:, l, jj, 1:2], tmp, ALU.mult, ALU.add, r=[uk, tk_, "pv"], w=[tk_])
                        STT(dst, u[gi][:, 0:512], fcw[:, l, jj, 0:1], tmp, ALU.mult, ALU.add, r=[uk, tk_, "pv"], w=[dk])
                    if pend_f is not None:
                        pend_f()

                    def fin(j=j, b2=b2, cg=cg, cv=cv):
                        ACT(cg, cg, AF.Gelu_apprx_tanh, r=[("cg", b2)], w=[("cg", b2)])
                        TTo("dve", fT[:, j, :], cg, cv, ALU.mult, r=[("cg", b2), ("cv", b2)], w=[("fT", j)])
                    pend_f = fin
                pend_f()
                pend_f = None
                if l == 0 and t == 0:
                    dump("fT", fT[:, 0, :], [("fT", 0)])
                for dc in range(8):
                    wv, wk = wdr.get()
                    ps, pk = ringB.next()
                    for j in range(NJ):
                        MM(ps, wv[:, j, :], fT[:, j, :], j == 0, j == NJ - 1, r=[wk, ("fT", j)], w=[pk], inc=(j == NJ - 1))
                    if dc % 2 == 0:
                        ACT(ytile[:, dc, :], ps, AF.Copy, r=[pk], w=[("yt", dc)])
                    else:
                        CP("dve", ytile[:, dc, :], ps, r=[pk], w=[("yt", dc)])
                rmsnorm_tile([ytile[:, dc, :] for dc in range(8)], [("yt", dc) for dc in range(8)],
                             [ytile[:, dc, :] for dc in range(8)], [("yt", dc) for dc in range(8)],
                             [vd[:, l, 3, dc:dc + 1] for dc in range(8)], D)
                for dc in range(8):
                    TTo("dve", xT[:, dc, sl(t)], xT[:, dc, sl(t)], ytile[:, dc, :], ALU.add, r=[("xT", dc, t), ("yt", dc)], w=[("xT", dc, t)])
            if l == 0:
                dump("x2", xT[:, 0, 0:512], [("xT", 0, 0)])
            S.barrier()
            A.release(PH)

            norm_x_to_h(l, 4)
            pTb = A.bf16(2 * SEQ).rearrange("p (c s) -> p c s", c=2)
            wgs = [A.bf16(8 * 256).rearrange("p (k n) -> p k n", k=8) for _ in range(3)]
            wpj = A.bf16(2 * D).rearrange("p (k n) -> p k n", k=2)
            sgt = [A.f32(512) for _ in range(2)]
            pjt = [A.f32(512) for _ in range(2)]
            wload(pTb, pT_d[l].rearrange("(k p) s -> p k s", p=128), ("pTb",))
            wload(wpj, w_proj_d[l].rearrange("(k p) n -> p k n", p=128), ("wpj",))
            wgv = w_gate_d[l].rearrange("(k p) n -> p k n", p=128)
            wgr = WStream([(wgs[i], ("wg", i)) for i in range(3)], [(lambda v, k, db=db: wload(v, wgv[:, :, db * 256:(db + 1) * 256], k)) for db in range(4)])
            wgr.top()
            it = 0
            for db in range(4):
                wv, wk = wgr.get()
                for dd in range(2):
                    dc = db * 2 + dd
                    for t in range(NT):
                        b = it % 2
                        it += 1
                        ps, pk = ringA.next()
                        rf, kf = hrhs(t)
                        proj(ps, pk, wv, wk, slice(dd * 128, (dd + 1) * 128), rf, kf)
                        ACT(sgt[b], ps, AF.Sigmoid, r=[pk], w=[("sgt", b)])
                        ps2, pk2 = ringB.next()
                        for kc in range(2):
                            MM(ps2, wpj[:, kc, dc * 128:(dc + 1) * 128], pTb[:, kc, sl(t)], kc == 0, kc == 1, r=[("wpj",), ("pTb",)], w=[pk2], inc=(kc == 1))
                        TTo("dve", pjt[b], ps2, sgt[b], ALU.mult, r=[pk2, ("sgt", b)], w=[("pjt", b)])
                        TTo("dve", xT[:, dc, sl(t)], xT[:, dc, sl(t)], pjt[b], ALU.add, r=[("xT", dc, t), ("pjt", b)], w=[("xT", dc, t)])
            if l == 0:
                dump("x3", xT[:, 0, 0:512], [("xT", 0, 0)])

        for c in range(8):
            DMA("sp", out_d[c * 128:(c + 1) * 128, :], xT[:, c, :], "st_out", r=[("xT", c, t) for t in range(NT)], w=[("out", c)])
        S.barrier()
        S.emit()
        build.arena_hi = A.hi
    return nc


_CACHE = {}


def _prep_inputs(inp):
    cmat, cf, ind = _host_consts()
    pv, glnb, gb, wsT = _host_params(inp)
    x = np.asarray(inp["x"], np.float32)
    p = np.asarray(inp["p"], np.float32)
    pos = np.asarray(inp["positions"], np.int32)
    shared = dict(cmat=cmat, cf=cf, ind=ind, pv=pv, glnb=glnb, gb=gb, wsT=wsT)
    for k in ("w_in", "w_out", "w_up", "w_down", "w_pe_gate", "w_pe_proj", "conv_pw_w"):
        shared[k] = np.ascontiguousarray(np.asarray(inp[k], np.float32))
    maps = []
    for b in range(8):
        m = dict(shared)
        m["xT"] = np.ascontiguousarray(x[b].T)
        m["pT"] = np.ascontiguousarray(p[:, b].transpose(0, 2, 1))
        m["pos"] = np.ascontiguousarray(pos[b][None, :])
        maps.append(m)
    return maps


def kernel(**inputs):
    if "nc" not in _CACHE:
        _CACHE["nc"] = build()
    nc = _CACHE["nc"]
    maps = _prep_inputs(inputs)
    res = run_bass_kernel_spmd(nc, maps, core_ids=list(range(8)))
    out = np.stack([np.asarray(r["outT"], np.float32).T for r in res.results], axis=0)
    return np.ascontiguousarray(out)
```

```python
import math
import numpy as np
from contextlib import ExitStack
import concourse.bass as bass
import concourse.mybir as mybir
from concourse.bass_utils import run_bass_kernel_spmd

F32 = mybir.dt.float32
BF16 = mybir.dt.bfloat16
I32 = mybir.dt.int32
AF = mybir.ActivationFunctionType
ALU = mybir.AluOpType
AX = mybir.AxisListType

L = 2
D = 1024
SEQ = 2048
GW = 256
DFF = 2816
NJ = DFF // 128
NT = 4
TW = 512
MASK = 30000.0
BIGG = 1.0e6
EPS = 1e-6
ENGS = ["pe", "act", "dve", "pool", "sp"]


class Sched:
    def __init__(self, nc, es, same_engine_sync=True):
        self.nc = nc
        self.es = es
        self.same = same_engine_sync
        self.prog = {e: [] for e in ENGS}
        self.cnt = {}
        self.sems = {}
        self.seen = {e: {} for e in ENGS}
        self.state = {}
        for e in ENGS:
            self._sem(e)

    def _sem(self, key):
        if key not in self.sems:
            self.sems[key] = self.es.enter_context(self.nc.semaphore("s_" + str(key)))
            self.cnt[key] = 0
        return self.sems[key]

    def _engobj(self, e):
        nc = self.nc
        return dict(pe=nc.tensor, act=nc.scalar, dve=nc.vector, pool=nc.gpsimd, sp=nc.sync)[e]

    def _deps(self, e, r, w):
        toks = {}
        for k in r:
            st = self.state.get(k)
            if st and st[0] is not None:
                t = st[0]
                toks[t[0]] = max(toks.get(t[0], 0), t[1])
        for k in w:
            st = self.state.get(k)
            if st:
                if st[0] is not None:
                    t = st[0]
                    toks[t[0]] = max(toks.get(t[0], 0), t[1])
                for t in st[1]:
                    toks[t[0]] = max(toks.get(t[0], 0), t[1])
        waits = []
        for sk, v in toks.items():
            if sk == e and (not self.same or e == "pe"):
                continue
            if self.seen[e].get(sk, 0) >= v:
                continue
            self.seen[e][sk] = v
            waits.append((sk, v))
        return waits

    def _record(self, tok, r, w):
        for k in r:
            st = self.state.setdefault(k, [None, []])
            st[1].append(tok)
        for k in w:
            self.state[k] = [tok, []]

    def op(self, e, fn, r=(), w=(), inc=True):
        waits = self._deps(e, r, w)
        tok = (e, self.cnt[e] + 1)
        if inc:
            self.cnt[e] += 1
        self._record(tok, r, w)
        self.prog[e].append((waits, fn, (e, 1) if inc else None))
        return tok

    def dma(self, q, fn, semkey, r=(), w=()):
        self._sem(semkey)
        waits = self._deps(q, r, w)
        self.cnt[semkey] += 16
        tok = (semkey, self.cnt[semkey])
        self._record(tok, r, w)
        self.prog[q].append((waits, fn, (semkey, 16)))
        return tok

    def barrier(self):
        for e in ENGS:
            waits = []
            for sk, v in self.cnt.items():
                if v == 0 or self.seen[e].get(sk, 0) >= v:
                    continue
                if sk == e and e == "pe":
                    continue
                self.seen[e][sk] = v
                waits.append((sk, v))
            self.prog[e].append((waits, None, None))

    def simulate(self):
        val = {k: 0 for k in self.sems}
        pc = {e: 0 for e in ENGS}
        progress = True
        while progress:
            progress = False
            for e in ENGS:
                while pc[e] < len(self.prog[e]):
                    waits, fn, inc = self.prog[e][pc[e]]
                    if any(val[sk] < v for sk, v in waits):
                        break
                    if inc is not None:
                        val[inc[0]] += inc[1]
                    pc[e] += 1
                    progress = True
        stuck = {e: (pc[e], len(self.prog[e])) for e in ENGS if pc[e] < len(self.prog[e])}
        if stuck:
            msg = []
            for e, (i, n) in stuck.items():
                waits = self.prog[e][i][0]
                msg.append("%s@%d/%d waits %s" % (e, i, n, [(sk, v, val[sk]) for sk, v in waits if val[sk] < v]))
            raise RuntimeError("DEADLOCK in schedule: " + "; ".join(msg))
        for k in self.sems:
            assert val[k] == self.cnt[k], (k, val[k], self.cnt[k])

    def emit(self):
        self.simulate()
        nc = self.nc
        with nc.Block() as block:
            def run(e):
                eng = self._engobj(e)
                for waits, fn, inc in self.prog[e]:
                    for sk, v in waits:
                        eng.wait_ge(self.sems[sk], v)
                    if fn is None:
                        continue
                    ins = fn()
                    if inc is not None:
                        ins.then_inc(self.sems[inc[0]], inc[1])

            @block.tensor
            def _(e):
                run("pe")

            @block.scalar
            def _(e):
                run("act")

            @block.vector
            def _(e):
                run("dve")

            @block.gpsimd
            def _(e):
                run("pool")

            @block.sync
            def _(e):
                run("sp")


class Arena:
    def __init__(self, t, nbytes):
        self.t = t
        self.n = nbytes
        self.off = 0
        self.hi = 0

    def mark(self):
        return self.off

    def release(self, m):
        self.off = m

    def _take(self, nbytes):
        nbytes = (nbytes + 31) // 32 * 32
        o = self.off
        self.off += nbytes
        self.hi = max(self.hi, self.off)
        assert self.off <= self.n, ("arena overflow", self.off, self.n)
        return o

    def f32(self, n):
        o = self._take(4 * n)
        return self.t[:, o // 4: o // 4 + n]

    def bf16(self, n):
        o = self._take(2 * n)
        return self.t[:, o // 4: o // 4 + (n + 1) // 2].bitcast(BF16)

    def i32(self, n):
        o = self._take(4 * n)
        return self.t[:, o // 4: o // 4 + n].bitcast(I32)


class Ring:
    def __init__(self, items):
        self.items = items
        self.i = 0

    def next(self):
        it = self.items[self.i % len(self.items)]
        self.i += 1
        return it


class WStream:
    def __init__(self, slots, plan, auto=True):
        self.slots = slots
        self.plan = plan
        self.auto = auto
        self.issued = 0
        self.used = 0
        self.freed = 0

    def top(self):
        while self.issued < len(self.plan) and self.issued - self.freed < len(self.slots):
            i = self.issued
            v, k = self.slots[i % len(self.slots)]
            self.plan[i](v, k)
            self.issued += 1

    def done(self):
        self.freed = self.used
        self.top()

    def get(self):
        i = self.used
        if self.auto:
            self.freed = i
        self.top()
        assert self.issued > i, "weight stream: block not issued (slots exhausted)"
        self.used += 1
        return self.slots[i % len(self.slots)]


def _host_consts():
    ident = np.eye(128, dtype=np.float32)
    ones = np.ones((128, 128), np.float32)
    blk2 = np.zeros((128, 128), np.float32)
    blk2[0:64, 0:64] = 1
    blk2[64:128, 64:128] = 1

    def rotT(block):
        h = block // 2
        m = np.zeros((128, 128), np.float32)
        for b0 in range(0, 128, block):
            for i in range(h):
                m[b0 + i + h, b0 + i] = -1.0
                m[b0 + i, b0 + i + h] = 1.0
        return m

    tri = np.where(np.arange(128)[None, :] >= np.arange(128)[:, None], 0.0, -MASK).astype(np.float32)
    cmat = np.concatenate([ident, ones, blk2, rotT(64), rotT(32), tri], axis=1)
    tril = (np.arange(128)[:, None] <= np.arange(128)[None, :]).astype(np.float32)
    pm = np.zeros((16, 8), np.float32)
    for qs in range(16):
        cur = qs // 2
        for n in range(8):
            pm[qs, n] = 0.0 if n < cur else (BIGG if n == cur else -2 * BIGG)
    pmask = np.broadcast_to(pm.reshape(1, 128), (128, 128))
    p = np.arange(128)
    invf64 = (10000.0 ** (-(2.0 * (p % 32)) / 64.0)) / (2 * math.pi)
    invf32 = (10000.0 ** (-(2.0 * (p % 16)) / 32.0)) / (2 * math.pi)
    gmask = (np.arange(128)[:, None] // 32 == np.arange(4)[None, :]).astype(np.float32)
    cf = np.concatenate([tril, pmask, invf64[:, None], invf32[:, None], gmask], axis=1).astype(np.float32)
    ind = (-MASK) * (np.arange(SEQ)[None, :] // 256 == np.arange(8)[:, None]).astype(np.float32)
    return np.ascontiguousarray(cmat), np.ascontiguousarray(cf), np.ascontiguousarray(ind)


def _fm(v, c):
    return np.ascontiguousarray(np.asarray(v, np.float32).reshape(c, 128).T)


def _host_params(inp):
    vd = np.stack([np.stack([_fm(inp[k][l], 8) for k in ("pre_mix_norm", "post_mix_norm", "pre_ffn_norm", "post_ffn_norm", "pe_gate_norm")], 1) for l in range(L)], 1)
    vg = np.stack([np.stack([_fm(inp[k][l], 2) for k in ("conv_dw_b", "conv_ln_g", "conv_ln_b", "conv_pw_b", "out_norm_a", "out_norm_b", "out_norm_c")], 1) for l in range(L)], 1)
    wdw = np.stack([np.asarray(inp["conv_dw_w"][l], np.float32).reshape(31, 2, 128).transpose(2, 1, 0) for l in range(L)], 1)
    fcw = np.stack([np.asarray(inp["ffn_conv_w"][l], np.float32).reshape(3, 44, 128).transpose(2, 1, 0) for l in range(L)], 1)
    fcb = np.stack([_fm(inp["ffn_conv_b"][l], 44) for l in range(L)], 1)
    gsub = np.stack([np.asarray(inp["diff_subln_g"][l], np.float32)[np.arange(128) % 64] for l in range(L)], 1)
    lamv = np.stack([np.stack([np.asarray(inp[k][l], np.float32) for k in ("diff_lq1", "diff_lk1", "diff_lq2", "diff_lk2")], 0) for l in range(L)], 0)
    lamv = np.broadcast_to(lamv.reshape(1, L * 4 * 32), (128, L * 4 * 32))
    pv = np.concatenate([vd.reshape(128, -1), vg.reshape(128, -1), wdw.reshape(128, -1), fcw.reshape(128, -1), fcb.reshape(128, -1), gsub.reshape(128, -1), lamv], axis=1)
    glnb = np.stack([np.stack([np.broadcast_to(np.asarray(inp[k][l], np.float32)[None, :], (128, 256)) for k in ("gmlp_ln_g", "gmlp_ln_b")], 1) for l in range(L)], 0)
    bs = np.asarray(inp["gmlp_bs"], np.float32)
    gb = np.zeros((L, 2, 128, 512), np.float32)
    for l in range(L):
        for cc in range(2):
            for hh in range(2):
                gb[l, cc, hh * 64:(hh + 1) * 64, :] = np.tile(bs[l, 2 * cc + hh], 4)[None, :]
    wsT = np.ascontiguousarray(np.asarray(inp["gmlp_ws"], np.float32).transpose(0, 1, 3, 2))
    return np.ascontiguousarray(pv.astype(np.float32)), np.ascontiguousarray(glnb), gb, wsT


PV_OFF = {}


def _pv_layout():
    o = 0
    for name, n in (("vd", L * 5 * 8), ("vg", L * 7 * 2), ("wdw", L * 2 * 31), ("fcw", L * 44 * 3), ("fcb", L * 44), ("gsub", L), ("lamv", L * 4 * 32)):
        PV_OFF[name] = o
        o += n
    return o


PV_N = _pv_layout()


def build(dbg=None, nlayers=L):
    dbg = dbg or []
    nc = bass.Bass("TRN2", target_bir_lowering=False)

    def din(name, shape, dt=F32):
        return nc.dram_tensor(name, list(shape), dt, kind="ExternalInput").ap()

    xT_d = din("xT", [D, SEQ])
    pT_d = din("pT", [L, GW, SEQ])
    pos_d = din("pos", [1, SEQ], I32)
    cmat_d = din("cmat", [128, 768])
    cf_d = din("cf", [128, 262])
    ind_d = din("ind", [8, SEQ])
    pv_d = din("pv", [128, PV_N])
    glnb_d = din("glnb", [L, 128, 2, 256])
    gb_d = din("gb", [L, 2, 128, 512])
    wsT_d = din("wsT", [L, 4, 128, 128])
    w_in_d = din("w_in", [L, D, 10 * GW])
    w_out_d = din("w_out", [L, D, D])
    w_up_d = din("w_up", [L, D, 2 * DFF])
    w_down_d = din("w_down", [L, DFF, D])
    w_gate_d = din("w_pe_gate", [L, D, D])
    w_proj_d = din("w_pe_proj", [L, GW, D])
    w_pw_d = din("conv_pw_w", [L, GW, GW])
    out_d = nc.dram_tensor("outT", [D, SEQ], F32, kind="ExternalOutput").ap()
    wup_s = nc.dram_tensor("wup_s", [L, NJ, 128, 8 * 2 * 128], BF16, kind="Internal").ap()
    wdn_s = nc.dram_tensor("wdn_s", [L, 8, 128, NJ * 128], BF16, kind="Internal").ap()
    dbg_d = {n: nc.dram_tensor("dbg_" + n, [128, w], F32, kind="ExternalOutput").ap() for n, w in dbg}

    es = ExitStack()
    with es:
        S = Sched(nc, es)
        NB = 206 * 1024
        big = es.enter_context(nc.sbuf_tensor("arena", [128, NB // 4], F32))
        A = Arena(big, NB)
        psb = [es.enter_context(nc.psum_tensor("ps%d" % i, [128, 512], F32)) for i in range(8)]
        PS = [(psb[i][:, :], ("ps", i)) for i in range(8)]
        ringA = Ring(PS[0:4])
        ringB = Ring(PS[4:8])

        def MM(out, lhsT, rhs, start, stop, r, w, inc=True, tp=None):
            kw = {}
            if tp is not None:
                kw["tile_position"] = tp
            S.op("pe", lambda: nc.tensor.matmul(out, lhsT=lhsT, rhs=rhs, start=start, stop=stop, **kw), r=r, w=w, inc=inc)

        def ACT(out, in_, func, r, w, scale=None, bias=None):
            kw = {}
            if scale is not None:
                kw["scale"] = scale
            if bias is not None:
                kw["bias"] = bias
            S.op("act", lambda: nc.scalar.activation(out=out, in_=in_, func=func, **kw), r=r, w=w)

        def TTo(eng, out, in0, in1, op, r, w):
            e = nc.vector if eng == "dve" else nc.gpsimd
            S.op(eng, lambda: e.tensor_tensor(out=out, in0=in0, in1=in1, op=op), r=r, w=w)

        def TS(eng, out, in0, s1, s2, op0, op1, r, w):
            e = nc.vector if eng == "dve" else nc.gpsimd
            if op1 is None:
                S.op(eng, lambda: e.tensor_scalar(out=out, in0=in0, scalar1=s1, scalar2=None, op0=op0), r=r, w=w)
            else:
                S.op(eng, lambda: e.tensor_scalar(out=out, in0=in0, scalar1=s1, scalar2=s2, op0=op0, op1=op1), r=r, w=w)

        def STT(out, in0, scalar, in1, op0, op1, r, w):
            S.op("dve", lambda: nc.vector.scalar_tensor_tensor(out=out, in0=in0, scalar=scalar, in1=in1, op0=op0, op1=op1), r=r, w=w)

        def CP(eng, out, in_, r, w):
            e = nc.vector if eng == "dve" else nc.gpsimd
            S.op(eng, lambda: e.tensor_copy(out=out, in_=in_), r=r, w=w)

        def MSET(eng, ap, val, w):
            e = nc.vector if eng == "dve" else nc.gpsimd
            S.op(eng, lambda: e.memset(ap, val), w=w)

        def DMA(q, out, in_, semkey, r, w):
            e = dict(sp=nc.sync, pool=nc.gpsimd, act=nc.scalar)[q]
            S.dma(q, lambda: e.dma_start(out=out, in_=in_), semkey, r=r, w=w)

        def dump(name, ap, keys):
            if name in dbg_d:
                w_ = ap.shape[-1]
                stg = A.f32(w_)
                CP("dve", stg[0:ap.shape[0], :], ap, r=keys, w=[("dbgs", name)])
                DMA("sp", dbg_d[name][0:ap.shape[0], 0:w_], stg[0:ap.shape[0], :], "dbg", r=[("dbgs", name)], w=[("dbgo", name)])

        xT = A.f32(8 * SEQ).rearrange("p (c s) -> p c s", c=8)
        hT = A.bf16(8 * SEQ).rearrange("p (c s) -> p c s", c=8)
        cmat = A.bf16(768)
        IDENT, ONES, BLK2, R64, R32, TRI = [cmat[:, i * 128:(i + 1) * 128] for i in range(6)]
        cf = A.f32(262)
        TRIL = cf[:, 0:128]
        PMASK = cf[:, 128:256].rearrange("p (q n) -> p q n", n=8)
        INVF = [cf[:, 256:257], cf[:, 257:258]]
        GMASK = cf[:, 258:262]
        pv = A.f32(PV_N)

        def PVv(name, n):
            return pv[:, PV_OFF[name]:PV_OFF[name] + n]

        vd = PVv("vd", L * 5 * 8).rearrange("p (l v c) -> p l v c", l=L, v=5)
        vg = PVv("vg", L * 7 * 2).rearrange("p (l v c) -> p l v c", l=L, v=7)
        wdw = PVv("wdw", L * 2 * 31).rearrange("p (l c k) -> p l c k", l=L, c=2)
        fcw = PVv("fcw", L * 44 * 3).rearrange("p (l j k) -> p l j k", l=L, j=44)
        fcb = PVv("fcb", L * 44).rearrange("p (l j) -> p l j", l=L)
        gsub = PVv("gsub", L)
        lamv = PVv("lamv", L * 4 * 32).rearrange("p (l v k) -> p l v k", l=L, v=4)
        small = A.f32(64)
        lnv = A.f32(512)
        rstd = A.f32(512)
        sq = [A.bf16(512) for _ in range(3)]
        sqring = Ring([(sq[i], ("sq", i)) for i in range(3)])
        PH = A.mark()

        DMA("pool", cmat, cmat_d[:, :], "ld_c", r=[], w=["cmat"])
        DMA("sp", cf, cf_d[:, :], "ld_c2", r=[], w=["cf"])
        DMA("sp", pv, pv_d[:, :], "ld_c3", r=[], w=["pv"])
        for c in range(8):
            DMA("sp", xT[:, c, :], xT_d[c * 128:(c + 1) * 128, :], ("ld_x", c), r=[], w=[("xT", c, t) for t in range(NT)])

        def wload(dst, src, key):
            DMA("pool", dst, src, ("wsem",) + key, r=[], w=[key])

        def precast_ffn(l):
            wupv_ = w_up_d[l].rearrange("(k p) (g n) -> p k g n", p=128, g=2)
            wdnv_ = w_down_d[l].rearrange("(j p) n -> p j n", p=128)
            sk = ("pcs", l)
            keys = []
            for j in range(NJ):
                for gi_ in range(2):
                    dst = wup_s[l, j].rearrange("p (k g n) -> p k g n", k=8, g=2)[:, :, gi_, :]
                    DMA("pool", dst, wupv_[:, :, gi_, j * 128:(j + 1) * 128], sk, r=[], w=[("pcu", l, j, gi_)])
                    keys.append(("pcu", l, j, gi_))
            for dc in range(8):
                dst = wdn_s[l, dc].rearrange("p (j n) -> p j n", j=NJ)
                DMA("pool", dst, wdnv_[:, :, dc * 128:(dc + 1) * 128], sk, r=[], w=[("pcd", l, dc)])
                keys.append(("pcd", l, dc))
            tot = S.cnt[sk]
            for k in keys:
                S.state[k] = [(sk, tot), []]

        def rstd_from(ps_ap, ps_key, inv_n, eps, extra_r=()):
            ACT(lnv, ps_ap, AF.Ln, r=[ps_key] + list(extra_r), w=["lnv"], scale=inv_n, bias=eps)
            ACT(rstd, lnv, AF.Exp, r=["lnv"], w=["rstd"], scale=-0.5)

        def rmsnorm_tile(srcs, skeys, dsts, dkeys, gains, n_feat, eps=EPS, lhs=None, lkey="cmat"):
            ps, pk = ringB.next()
            C = len(srcs)
            for c in range(C):
                q, qk = sqring.next()
                ACT(q, srcs[c], AF.Square, r=[skeys[c]], w=[qk])
                MM(ps, lhs if lhs is not None else ONES, q, c == 0, c == C - 1, r=[qk, lkey], w=[pk])
            rstd_from(ps, pk, 1.0 / n_feat, eps)
            for c in range(C):
                STT(dsts[c], srcs[c], gains[c], rstd, ALU.mult, ALU.mult, r=[skeys[c], "rstd", "pv"], w=[dkeys[c]])

        def sl(t):
            return slice(t * TW, (t + 1) * TW)

        def norm_x_to_h(l, vidx):
            for t in range(NT):
                rmsnorm_tile([xT[:, c, sl(t)] for c in range(8)], [("xT", c, t) for c in range(8)],
                             [hT[:, c, sl(t)] for c in range(8)], [("hT", c, t) for c in range(8)],
                             [vd[:, l, vidx, c:c + 1] for c in range(8)], D)

        def proj(ps, pk, wv, wkey, cs, rhs_fn, rkeys_fn, nk=8):
            for kc in range(nk):
                MM(ps, wv[:, kc, cs], rhs_fn(kc), kc == 0, kc == nk - 1, r=[wkey] + rkeys_fn(kc), w=[pk], inc=(kc == nk - 1))

        def hrhs(t):
            return (lambda kc: hT[:, kc, sl(t)]), (lambda kc: [("hT", kc, t)])

        for l in range(nlayers):
            S.barrier()
            A.release(PH)
            catT = A.bf16(8 * SEQ).rearrange("p (c s) -> p c s", c=8)
            ropeC = A.bf16(SEQ)
            ropeS = A.bf16(SEQ)
            wslots = [A.bf16(8 * 256).rearrange("p (k n) -> p k n", k=8) for _ in range(4)]
            MX = A.mark()
            winv = w_in_d[l].rearrange("(k p) n -> p k n", p=128)
            woutv = w_out_d[l].rearrange("(k p) n -> p k n", p=128)
            mplan = [(lambda v, k, bi=bi: wload(v, winv[:, :, bi * 256:(bi + 1) * 256], k)) for bi in (0, 1, 2, 7, 8, 9, 3, 4, 5, 6)]
            mplan += [(lambda v, k, db=db: wload(v, woutv[:, :, db * 256:(db + 1) * 256], k)) for _t in range(NT) for db in range(4)]
            mstream = WStream([(wslots[i], ("ws", i)) for i in range(4)], mplan, auto=False)

            def win_block(bi):
                return mstream.get()

            def rope_tables(which):
                m = A.mark()
                posi = A.i32(SEQ)
                v = A.f32(SEQ)
                ki = A.i32(SEQ)
                DMA("sp", posi, pos_d[0:1, :].broadcast_to([128, SEQ]), "ld_pos", r=[], w=["posi"])
                for tab, shift, key in ((ropeS, 0.0, "ropeS"), (ropeC, 0.25, "ropeC")):
                    TS("dve", v, posi, INVF[which], shift, ALU.mult, ALU.add, r=["posi", "cf"], w=["ropev"])
                    CP("dve", ki, v, r=["ropev"], w=["ropek"])
                    TTo("dve", v, v, ki, ALU.subtract, r=["ropev", "ropek"], w=["ropev"])
                    ACT(tab, v, AF.Sin, r=["ropev"], w=[key], scale=2 * math.pi * (1 - 1e-6))
                S.barrier()
                A.release(m)

            def rope_apply(ps, pk, t, RM, zbr, t1, t2):
                zb, zk = zbr.next()
                ACT(zb, ps, AF.Copy, r=[pk], w=[zk])
                ps2, pk2 = ringA.next()
                MM(ps2, RM, zb, True, True, r=[zk, "cmat"], w=[pk2])
                TTo("dve", t1[0], zb, ropeC[:, sl(t)], ALU.mult, r=[zk, "ropeC"], w=[t1[1]])
                TTo("dve", t2[0], ps2, ropeS[:, sl(t)], ALU.mult, r=[pk2, "ropeS"], w=[t2[1]])

            def attn_finalize_recip(O, ok, dst_rden):
                ACT(dst_rden[0][0:64, :], O[64:128, :], AF.Ln, r=[ok], w=[dst_rden[1]])
                ACT(dst_rden[0][0:64, :], dst_rden[0][0:64, :], AF.Exp, r=[dst_rden[1]], w=[dst_rden[1]], scale=-1.0)

            def vproj(wv, wk, cs, vaug):
                for g in range(4):
                    ps, pk = ringA.next()
                    for s4 in range(4):
                        st = g * 4 + s4
                        for kc in range(8):
                            MM(ps[:, s4 * 128:(s4 + 1) * 128], hT[:, kc, st * 128:(st + 1) * 128], wv[:, kc, cs], kc == 0, kc == 7,
                               r=[wk, ("hT", kc, st // 4)], w=[pk], inc=(kc == 7 and s4 == 3))
                    ACT(vaug[:, g * 4:(g + 1) * 4, :, 0:64], ps.rearrange("p (s h d) -> p s h d", s=4, h=2), AF.Copy, r=[pk], w=[("V", g)])

            norm_x_to_h(l, 0)
            if l == 0:
                dump("hT0", hT[:, 0, 0:512], [("hT", 0, 0)])

            rope_tables(0)
            m0 = A.mark()
            Kaug = [A.bf16(SEQ) for _ in range(2)]
            vaug = A.bf16(16 * 2 * 128).rearrange("p (s h d) -> p s h d", s=16, h=2)
            Qa = [[A.bf16(512) for _ in range(2)] for _ in range(NT)]
            Pr = Ring([(A.bf16(512), ("P", i)) for i in range(4)])
            zbr = Ring([(A.bf16(512), ("zb", i)) for i in range(2)])
            t1 = (A.f32(512), "t1")
            t2 = (A.f32(512), "t2")
            rden = (A.f32(512), "rden")
            bstg4 = A.bf16(4 * 72).rearrange("p (j n) -> p j n", j=4)
            gm = A.f32(64)
            top8 = A.f32(32)
            kmf = A.f32(16)
            kmb = A.bf16(16)
            MSET("dve", vaug[:, :, :, 64:128], 1.0, w=[("V", g) for g in range(4)])
            MSET("dve", bstg4, 0.0, w=["bstg"])
            for hh in range(2):
                DMA("pool", Kaug[hh][64:72, :], ind_d[:, :], "ld_ind", r=[], w=[("Kind", hh)])
            wq, wqk = win_block(0)
            wk_, wkk = win_block(1)
            wv_, wvk = win_block(2)
            precast_ffn(l)
            for c in range(2):
                cs = slice(c * 128, (c + 1) * 128)
                for t in range(NT):
                    ps, pk = ringA.next()
                    rf, kf = hrhs(t)
                    proj(ps, pk, wk_, wkk, cs, rf, kf)
                    rope_apply(ps, pk, t, R64, zbr, t1, t2)
                    for hh in range(2):
                        TTo("dve", Kaug[hh][0:64, sl(t)], t1[0][hh * 64:(hh + 1) * 64, :], t2[0][hh * 64:(hh + 1) * 64, :], ALU.add,
                            r=[t1[1], t2[1]], w=[("K", hh, t)])
                for hh in range(2):
                    S.op("dve", lambda hh=hh: nc.vector.tensor_reduce(out=kmf[0:64, hh * 8:(hh + 1) * 8], in_=Kaug[hh][0:64, :].rearrange("p (n k) -> p n k", k=256), axis=AX.X, op=ALU.add),
                         r=[("K", hh, t) for t in range(NT)], w=[("kmf", hh)])
                    TS("dve", kmb[0:64, hh * 8:(hh + 1) * 8], kmf[0:64, hh * 8:(hh + 1) * 8], 1.0 / 256, None, ALU.mult, None, r=[("kmf", hh)], w=[("kmb", hh)])
                vproj(wv_, wvk, cs, vaug)
                for qt in range(NT):
                    qb = Qa[qt]
                    ps, pk = ringA.next()
                    rf, kf = hrhs(qt)
                    proj(ps, pk, wq, wqk, cs, rf, kf)
                    rope_apply(ps, pk, qt, R64, zbr, t1, t2)
                    for hh in range(2):
                        TTo("dve", qb[hh][0:64, :], t1[0][hh * 64:(hh + 1) * 64, :], t2[0][hh * 64:(hh + 1) * 64, :], ALU.add,
                            r=[t1[1], t2[1]], w=[("Qa", qt, hh)])
                    for hh in range(2):
                        gps, gk = ringA.next()
                        for j in range(4):
                            MM(gps[:, j * 8:(j + 1) * 8], qb[hh][0:64, j * 128:(j + 1) * 128], kmb[0:64, hh * 8:(hh + 1) * 8], True, True,
                               r=[("Qa", qt, hh), ("kmb", hh)], w=[gk], inc=(j == 3))
                        gmv = gm[:, 0:32].rearrange("p (j n) -> p j n", n=8)
                        TTo("dve", gmv, gps[:, 0:32].rearrange("p (j n) -> p j n", n=8), PMASK[:, qt * 4:(qt + 1) * 4, :], ALU.add, r=[gk, "cf"], w=["gm"])
                        tps, tk = ringA.next()
                        tpsb = tps.bitcast(BF16)
                        top8v = top8.rearrange("p (j n) -> p j n", n=8)
                        for j in range(4):
                            S.op("dve", lambda j=j: nc.vector.max(out=top8[:, j * 8:(j + 1) * 8], in_=gm[:, j * 8:(j + 1) * 8]), r=["gm"], w=["top8"])
                        TS("dve", top8v[:, :, 3:4], top8v[:, :, 3:4], -BIGG, None, ALU.max, None, r=["top8"], w=["top8"])
                        TTo("dve", bstg4[:, :, 64:72], gmv, top8v[:, :, 3:4].broadcast_to([128, 4, 8]), ALU.is_lt, r=["gm", "top8"], w=["bstg"])
                        for j in range(4):
                            S.op("pe", lambda j=j, tpsb=tpsb: nc.tensor.transpose(tpsb[0:72, j * 128:(j + 1) * 128], bstg4[:, j, :], IDENT), r=["bstg", "cmat"], w=[tk], inc=(j == 3))
                        ACT(qb[hh][64:72, :], tpsb[64:72, 0:512], AF.Copy, r=[tk], w=[("Qb", qt, hh)])
                defer = []
                for qt in range(NT):
                    qb = Qa[qt]
                    for hh in range(2):
                        O, ok = ringB.next()
                        nk = 4 * qt + 4
                        pend = []
                        for kt in range(nk):
                            jd = kt - 4 * qt
                            c0 = 128 * jd if jd > 0 else 0
                            sp_, sk = ringA.next()
                            MM(sp_[:, c0:512], Kaug[hh][0:72, kt * 128:(kt + 1) * 128], qb[hh][0:72, c0:512], True, jd < 0,
                               r=[("K", hh, kt // 4), ("Kind", hh), ("Qa", qt, hh), ("Qb", qt, hh)], w=[sk], inc=(jd < 0))
                            if jd >= 0:
                                MM(sp_[:, c0:c0 + 128], IDENT, TRI, False, True, r=["cmat"], w=[sk])
                            P, Pk = Pr.next()
                            ACT(P[:, c0:512], sp_[:, c0:512], AF.Exp, r=[sk], w=[Pk], scale=0.125)

                            def pv_(kt=kt, c0=c0, P=P, Pk=Pk, O=O, ok=ok, hh=hh, nk=nk):
                                MM(O[:, c0:512], vaug[:, kt, hh, :], P[:, c0:512], kt == 0, kt == nk - 1, r=[Pk, ("V", kt // 4)], w=[ok])
                            pend.append(pv_)
                            if len(pend) > 2:
                                pend.pop(0)()
                            if kt == 1:
                                for f in defer:
                                    f()
                                defer = []
                        for f in pend:
                            f()
                        def fin_(O=O, ok=ok, hh=hh, c=c, qt=qt):
                            attn_finalize_recip(O, ok, rden)
                            TTo("dve", catT[hh * 64:(hh + 1) * 64, c, sl(qt)], O[0:64, :], rden[0][0:64, :], ALU.mult, r=[ok, rden[1]], w=[("cat", c, qt)])
                        defer.append(fin_)
                for f in defer:
                    f()
                defer = []
            if l == 0:
                dump("oa_pre", catT[:, 0, 0:512], [("cat", 0, 0)])
            for t in range(NT):
                rmsnorm_tile([catT[:, c, sl(t)] for c in range(2)], [("cat", c, t) for c in range(2)],
                             [catT[:, c, sl(t)] for c in range(2)], [("cat", c, t) for c in range(2)],
                             [vg[:, l, 4, c:c + 1] for c in range(2)], GW)
            if l == 0:
                dump("oa", catT[:, 0, 0:512], [("cat", 0, 0)])
            mstream.done()
            S.barrier()
            A.release(m0)

            rope_tables(1)
            lam_init = 0.8 - 0.6 * math.exp(-0.3 * l)
            lt = small[:, 0:8]
            prod = A.f32(64)
            TTo("dve", prod[:, 0:32], lamv[:, l, 0, :], lamv[:, l, 1, :], ALU.mult, r=["pv"], w=["prod"])
            S.op("dve", lambda: nc.vector.tensor_reduce(out=lt[:, 0:1], in_=prod[:, 0:32], axis=AX.X, op=ALU.add), r=["prod"], w=["lt"])
            TTo("dve", prod[:, 32:64], lamv[:, l, 2, :], lamv[:, l, 3, :], ALU.mult, r=["pv"], w=["prod2"])
            S.op("dve", lambda: nc.vector.tensor_reduce(out=lt[:, 1:2], in_=prod[:, 32:64], axis=AX.X, op=ALU.add), r=["prod2"], w=["lt"])
            ACT(lt[:, 2:4], lt[:, 0:2], AF.Exp, r=["lt"], w=["lt"])
            TTo("dve", lt[:, 4:5], lt[:, 3:4], lt[:, 2:3], ALU.subtract, r=["lt"], w=["lt"])
            TS("dve", lt[:, 5:6], lt[:, 4:5], -lam_init, None, ALU.add, None, r=["lt"], w=["lt"])
            TS("dve", lt[:, 6:7], gsub[:, l:l + 1], 1.0 - lam_init, None, ALU.mult, None, r=["pv", "lt"], w=["lt"])
            NEGLAM = lt[:, 5:6]
            GS = lt[:, 6:7]

            m0 = A.mark()
            Kd = A.bf16(SEQ)
            vaug = A.bf16(16 * 2 * 128).rearrange("p (s h d) -> p s h d", s=16, h=2)
            Qd = [A.bf16(512) for _ in range(NT)]
            Pr = Ring([(A.bf16(512), ("P", i)) for i in range(4)])
            zbr = Ring([(A.bf16(512), ("zb", i)) for i in range(2)])
            t1 = (A.f32(512), "t1")
            t2 = (A.f32(512), "t2")
            rden = (A.f32(512), "rden")
            ods = [A.f32(512) for _ in range(2)]
            Qm = [[A.bf16(512) for _ in range(4)] for _ in range(2)]
            MSET("dve", vaug[:, :, :, 64:128], 1.0, w=[("V", g) for g in range(4)])
            wq, wqk = win_block(7)
            wk_, wkk = win_block(8)
            wv_, wvk = win_block(9)
            dscale = 32.0 ** -0.5
            for c in range(2):
                cs = slice(c * 128, (c + 1) * 128)
                for t in range(NT):
                    ps, pk = ringA.next()
                    rf, kf = hrhs(t)
                    proj(ps, pk, wk_, wkk, cs, rf, kf)
                    rope_apply(ps, pk, t, R32, zbr, t1, t2)
                    TTo("dve", Kd[:, sl(t)], t1[0], t2[0], ALU.add, r=[t1[1], t2[1]], w=[("Kd", t)])
                vproj(wv_, wvk, cs, vaug)
                for qt in range(NT):
                    qd = Qd[qt]
                    ps, pk = ringA.next()
                    rf, kf = hrhs(qt)
                    proj(ps, pk, wq, wqk, cs, rf, kf)
                    rope_apply(ps, pk, qt, R32, zbr, t1, t2)
                    TTo("dve", qd, t1[0], t2[0], ALU.add, r=[t1[1], t2[1]], w=[("Qd", qt)])
                defer = []
                defer2 = []

                def qmask(qt):
                    for g in range(4):
                        TS("dve", Qm[qt % 2][g], Qd[qt], GMASK[:, g:g + 1], None, ALU.mult, None, r=[("Qd", qt), "cf"], w=[("Qm", qt % 2, g)])
                qmask(0)
                for qt in range(NT):
                    qd = Qd[qt]
                    od = ods[qt % 2]
                    for hh in range(2):
                        Os = [ringB.next(), ringB.next()]
                        nk = 4 * qt + 4
                        pend = []
                        for kt in range(nk):
                            jd = kt - 4 * qt
                            c0 = 128 * jd if jd > 0 else 0
                            cur = []
                            for m_ in range(2):
                                g = hh * 2 + m_
                                sp_, sk = ringA.next()
                                MM(sp_[:, c0:512], Kd[:, kt * 128:(kt + 1) * 128], Qm[qt % 2][g][:, c0:512], True, jd < 0,
                                   r=[("Kd", kt // 4), ("Qm", qt % 2, g)], w=[sk], inc=(jd < 0))
                                if jd >= 0:
                                    MM(sp_[:, c0:c0 + 128], IDENT, TRI, False, True, r=["cmat"], w=[sk])
                                cur.append((sp_, sk))
                            for f in pend:
                                f()
                            pend = []
                            if kt == 1:
                                for f in defer:
                                    f()
                                defer = defer2
                                defer2 = []
                            for m_ in range(2):
                                sp_, sk = cur[m_]
                                P, Pk = Pr.next()
                                ACT(P[:, c0:512], sp_[:, c0:512], AF.Exp, r=[sk], w=[Pk], scale=dscale)

                                def pv_(kt=kt, c0=c0, P=P, Pk=Pk, Oo=Os[m_], hh=hh, nk=nk):
                                    MM(Oo[0][:, c0:512], vaug[:, kt, hh, :], P[:, c0:512], kt == 0, kt == nk - 1, r=[Pk, ("V", kt // 4)], w=[Oo[1]])
                                pend.append(pv_)
                        for f in pend:
                            f()
                        def fin_(Os=Os, hh=hh, qt=qt, od=od):
                            hs = slice(hh * 64, (hh + 1) * 64)
                            attn_finalize_recip(Os[0][0], Os[0][1], rden)
                            TTo("dve", t1[0][0:64, :], Os[0][0][0:64, :], rden[0][0:64, :], ALU.mult, r=[Os[0][1], rden[1]], w=[t1[1]])
                            attn_finalize_recip(Os[1][0], Os[1][1], (lnv, "lnv"))
                            TTo("dve", t2[0][0:64, :], Os[1][0][0:64, :], lnv[0:64, :], ALU.mult, r=[Os[1][1], "lnv"], w=[t2[1]])
                            STT(od[hs, :], t2[0][0:64, :], NEGLAM[0:64, :], t1[0][0:64, :], ALU.mult, ALU.add, r=[t1[1], t2[1], "lt"], w=[("od", qt % 2, hh)])
                        defer.append(fin_)
                        if hh == 0 and qt + 1 < NT:
                            qmask(qt + 1)
                    def norm_(qt=qt, od=od, c=c):
                        ps, pk = ringA.next()
                        q_, qk_ = sqring.next()
                        ACT(q_, od, AF.Square, r=[("od", qt % 2, 0), ("od", qt % 2, 1)], w=[qk_])
                        MM(ps, BLK2, q_, True, True, r=[qk_, "cmat"], w=[pk])
                        rstd_from(ps, pk, 1.0 / 64, 1e-5)
                        STT(catT[:, 6 + c, sl(qt)], od, GS, rstd, ALU.mult, ALU.mult, r=[("od", qt % 2, 0), ("od", qt % 2, 1), "rstd", "lt"], w=[("cat", 6 + c, qt)])
                    defer2.append(norm_)
                for f in defer + defer2:
                    f()
                defer = []
                defer2 = []
            if l == 0:
                dump("od", catT[:, 6, 0:512], [("cat", 6, 0)])
            mstream.done()
            S.barrier()
            A.release(m0)

            m0 = A.mark()
            uT = A.bf16(2 * SEQ).rearrange("p (c s) -> p c s", c=2)
            vgl = A.bf16(16 * 256).rearrange("p (n d) -> p n d", n=16)
            glnb = A.f32(512).rearrange("p (v d) -> p v d", v=2)
            gb = A.f32(1024).rearrange("p (c s) -> p c s", c=2)
            wsf = A.f32(512).rearrange("p (h i) -> p h i", h=4)
            wsb = A.bf16(512).rearrange("p (h i) -> p h i", h=4)
            stats = A.f32(16 * 6)
            mv = A.f32(16 * 2).rearrange("p (n k) -> p n k", k=2)
            rsd = A.f32(16)
            vtmp = A.f32(256)
            vln = [A.bf16(256) for _ in range(2)]
            stmp = A.f32(512)
            DMA("sp", glnb, glnb_d[l], "ld_g1", r=[], w=["glnb"])
            DMA("sp", gb, gb_d[l].rearrange("c p s -> p c s"), "ld_g2", r=[], w=["gb"])
            DMA("sp", wsf, wsT_d[l].rearrange("h j i -> j h i"), "ld_g3", r=[], w=["wsf"])
            for h in range(4):
                TTo("dve", wsb[:, h, :], wsf[:, h, :], TRIL, ALU.mult, r=["wsf", "cf"], w=["wsb"])
            wu, wuk = win_block(3)
            wvv, wvvk = win_block(4)
            for t in range(NT):
                for cc in range(2):
                    ps, pk = ringA.next()
                    rf, kf = hrhs(t)
                    proj(ps, pk, wu, wuk, slice(cc * 128, (cc + 1) * 128), rf, kf)
                    ACT(uT[:, cc, sl(t)], ps, AF.Gelu_apprx_tanh, r=[pk], w=[("uT", cc, t)])
            for n2 in range(8):
                ps, pk = ringA.next()
                for s2 in range(2):
                    n = n2 * 2 + s2
                    for kc in range(8):
                        MM(ps[:, s2 * 256:(s2 + 1) * 256], hT[:, kc, n * 128:(n + 1) * 128], wvv[:, kc, :], kc == 0, kc == 7,
                           r=[wvvk, ("hT", kc, n // 4)], w=[pk], inc=(kc == 7 and s2 == 1))
                ACT(vgl[:, n2 * 2:(n2 + 1) * 2, :], ps.rearrange("p (s d) -> p s d", s=2), AF.Gelu_apprx_tanh, r=[pk], w=[("vgl", n2)])
            for n in range(16):
                S.op("dve", lambda n=n: nc.vector.bn_stats(out=stats[:, n * 6:(n + 1) * 6], in_=vgl[:, n, :]), r=[("vgl", n // 2)], w=[("st", n)])
                S.op("dve", lambda n=n: nc.vector.bn_aggr(out=mv[:, n, :], in_=stats[:, n * 6:(n + 1) * 6]), r=[("st", n)], w=["mv"])
            ACT(rsd, mv[:, :, 1], AF.Ln, r=["mv"], w=["rsd"], bias=EPS)
            ACT(rsd, rsd, AF.Exp, r=["rsd"], w=["rsd"], scale=-0.5)
            for t in range(NT):
                pss = [ringA.next(), ringA.next()]
                for s4 in range(4):
                    n = t * 4 + s4
                    vl = vln[n % 2]
                    vk = ("vln", n % 2)
                    TS("dve", vtmp, vgl[:, n, :], mv[:, n, 0:1], rsd[:, n:n + 1], ALU.subtract, ALU.mult, r=[("vgl", n // 2), "mv", "rsd"], w=["vtmp"])
                    TTo("dve", vtmp, vtmp, glnb[:, 0, :], ALU.mult, r=["vtmp", "glnb"], w=["vtmp"])
                    TTo("dve", vl, vtmp, glnb[:, 1, :], ALU.add, r=["vtmp", "glnb"], w=[vk])
                    for cc in range(2):
                        for hh in range(2):
                            h = 2 * cc + hh
                            MM(pss[cc][0][hh * 64:(hh + 1) * 64, s4 * 128:(s4 + 1) * 128], vl[:, h * 64:(h + 1) * 64], wsb[:, h, :], True, True,
                               r=[vk, "wsb"], w=[pss[cc][1]], tp=(0, hh * 64))
                for cc in range(2):
                    TTo("dve", stmp, pss[cc][0], gb[:, cc, :], ALU.add, r=[pss[cc][1], "gb"], w=["stmp"])
                    TTo("dve", catT[:, 2 + cc, sl(t)], stmp, uT[:, cc, sl(t)], ALU.mult, r=["stmp", ("uT", cc, t)], w=[("cat", 2 + cc, t)])
            if l == 0:
                dump("ob_pre", catT[:, 2, 0:512], [("cat", 2, 0)])
            for t in range(NT):
                rmsnorm_tile([catT[:, 2 + c, sl(t)] for c in range(2)], [("cat", 2 + c, t) for c in range(2)],
                             [catT[:, 2 + c, sl(t)] for c in range(2)], [("cat", 2 + c, t) for c in range(2)],
                             [vg[:, l, 5, c:c + 1] for c in range(2)], GW)
            mstream.done()
            S.barrier()
            A.release(m0)

            m0 = A.mark()
            ybuf = A.bf16(32 + SEQ)
            diag = A.bf16(31 * 128).rearrange("p (k n) -> p k n", k=31)
            cy = A.bf16(2 * SEQ).rearrange("p (c s) -> p c s", c=2)
            sg = A.f32(512)
            wpw = A.bf16(2 * 256).rearrange("p (k n) -> p k n", k=2)
            mstat = A.f32(512)
            m2 = A.f32(512)
            yn = A.f32(512)
            sil = A.bf16(1024).rearrange("p (c s) -> p c s", c=2)
            Y0 = 2
            wa, wak = win_block(5)
            wg_, wgk = win_block(6)
            wload(wpw, w_pw_d[l].rearrange("(k p) n -> p k n", p=128), ("wpw",))
            MSET("dve", ybuf[:, 0:32], 0.0, w=["ypad"])
            for cc in range(2):
                cs = slice(cc * 128, (cc + 1) * 128)
                for k in range(31):
                    TS("dve", diag[:, k, :], IDENT, wdw[:, l, cc, k:k + 1], None, ALU.mult, None, r=["cmat", "pv"], w=[("diag", k)])
                for t in range(NT):
                    pa, pak = ringA.next()
                    rf, kf = hrhs(t)
                    proj(pa, pak, wa, wak, cs, rf, kf)
                    pg, pgk = ringA.next()
                    proj(pg, pgk, wg_, wgk, cs, rf, kf)
                    ACT(sg, pg, AF.Sigmoid, r=[pgk], w=["sg"])
                    TTo("dve", ybuf[:, 32 + t * TW:32 + (t + 1) * TW], pa, sg, ALU.mult, r=[pak, "sg"], w=[("yb", t)])
                for t in range(NT):
                    ps, pk = ringB.next()
                    for k in range(31):
                        o = Y0 + t * TW + k
                        rk = ["ypad", ("diag", k), ("yb", t)] + ([("yb", t - 1)] if t > 0 else [])
                        MM(ps, diag[:, k, :], ybuf[:, o:o + TW], k == 0, k == 30, r=rk, w=[pk], inc=(k == 30))
                    ACT(cy[:, cc, sl(t)], ps, AF.Identity, r=[pk, "pv"], w=[("cy", cc, t)], bias=vg[:, l, 0, cc:cc + 1])
            if l == 0:
                dump("cy", cy[:, 0, 0:512], [("cy", 0, 0)])
            for t in range(NT):
                ps1, pk1 = ringB.next()
                ps2, pk2 = ringB.next()
                for cc in range(2):
                    MM(ps1, ONES, cy[:, cc, sl(t)], cc == 0, cc == 1, r=[("cy", cc, t), "cmat"], w=[pk1])
                for cc in range(2):
                    q_, qk_ = sqring.next()
                    ACT(q_, cy[:, cc, sl(t)], AF.Square, r=[("cy", cc, t)], w=[qk_])
                    MM(ps2, ONES, q_, cc == 0, cc == 1, r=[qk_, "cmat"], w=[pk2])
                TS("dve", mstat, ps1, 1.0 / GW, None, ALU.mult, None, r=[pk1], w=["mstat"])
                TTo("dve", m2, mstat, mstat, ALU.mult, r=["mstat"], w=["m2"])
                STT(m2, ps2, 1.0 / GW, m2, ALU.mult, ALU.subtract, r=[pk2, "m2"], w=["m2"])
                ACT(lnv, m2, AF.Ln, r=["m2"], w=["lnv"], bias=EPS)
                ACT(rstd, lnv, AF.Exp, r=["lnv"], w=["rstd"], scale=-0.5)
                for cc in range(2):
                    TTo("dve", yn, cy[:, cc, sl(t)], mstat, ALU.subtract, r=[("cy", cc, t), "mstat"], w=["yn"])
                    TTo("dve", yn, yn, rstd, ALU.mult, r=["yn", "rstd"], w=["yn"])
                    ACT(sil[:, cc, :], yn, AF.Silu, r=["yn", "pv"], w=[("sil", cc)], scale=vg[:, l, 1, cc:cc + 1], bias=vg[:, l, 2, cc:cc + 1])
                for co in range(2):
                    ps, pk = ringA.next()
                    for ci in range(2):
                        MM(ps, wpw[:, ci, co * 128:(co + 1) * 128], sil[:, ci, :], ci == 0, ci == 1, r=[("wpw",), ("sil", ci)], w=[pk])
                    ACT(catT[:, 4 + co, sl(t)], ps, AF.Identity, r=[pk, "pv"], w=[("cat", 4 + co, t)], bias=vg[:, l, 3, co:co + 1])
            if l == 0:
                dump("oc_pre", catT[:, 4, 0:512], [("cat", 4, 0)])
            for t in range(NT):
                rmsnorm_tile([catT[:, 4 + c, sl(t)] for c in range(2)], [("cat", 4 + c, t) for c in range(2)],
                             [catT[:, 4 + c, sl(t)] for c in range(2)], [("cat", 4 + c, t) for c in range(2)],
                             [vg[:, l, 6, c:c + 1] for c in range(2)], GW)
            mstream.done()
            S.barrier()
            A.release(m0)

            def out_proj_norm_res(nkc, wsrc, rhs_fn, rkeys_fn, vidx, wr, ytile):
                for t in range(NT):
                    for db in range(4):
                        wv, wk = wr.get()
                        for dd in range(2):
                            dc = db * 2 + dd
                            ps, pk = ringA.next()
                            for kc in range(nkc):
                                MM(ps, wv[:, kc, dd * 128:(dd + 1) * 128], rhs_fn(kc, t), kc == 0, kc == nkc - 1, r=[wk] + rkeys_fn(kc, t), w=[pk], inc=(kc == nkc - 1))
                            if dc % 2 == 0:
                                ACT(ytile[:, dc, :], ps, AF.Copy, r=[pk], w=[("yt", dc)])
                            else:
                                CP("dve", ytile[:, dc, :], ps, r=[pk], w=[("yt", dc)])
                        wr.done()
                    rmsnorm_tile([ytile[:, dc, :] for dc in range(8)], [("yt", dc) for dc in range(8)],
                                 [ytile[:, dc, :] for dc in range(8)], [("yt", dc) for dc in range(8)],
                                 [vd[:, l, vidx, dc:dc + 1] for dc in range(8)], D)
                    for dc in range(8):
                        TTo("dve", xT[:, dc, sl(t)], xT[:, dc, sl(t)], ytile[:, dc, :], ALU.add, r=[("xT", dc, t), ("yt", dc)], w=[("xT", dc, t)])

            m0 = A.mark()
            ytile = A.f32(8 * 512).rearrange("p (c s) -> p c s", c=8)
            out_proj_norm_res(8, w_out_d[l].rearrange("(k p) n -> p k n", p=128), lambda kc, t: catT[:, kc, sl(t)], lambda kc, t: [("cat", kc, t)], 1, mstream, ytile)
            if l == 0:
                dump("x1", xT[:, 0, 0:512], [("xT", 0, 0)])
            S.barrier()
            A.release(MX)
            A.release(PH)

            norm_x_to_h(l, 2)
            ytile = A.f32(8 * 512).rearrange("p (c s) -> p c s", c=8)
            fT = A.bf16(NJ * 512).rearrange("p (j s) -> p j s", j=NJ)
            ub = [[A.f32(2 + 512) for _ in range(2)] for _ in range(2)]
            halo = A.f32(44 * 2).rearrange("p (j k) -> p j k", k=2)
            tas = [A.f32(512) for _ in range(2)]
            tbs = [A.f32(512) for _ in range(2)]
            cgs = [A.f32(512) for _ in range(2)]
            cvs = [A.f32(512) for _ in range(2)]
            wups = [A.bf16(8 * 2 * 128).rearrange("p (k g n) -> p k g n", k=8, g=2) for _ in range(3)]
            wdns = [A.bf16(NJ * 128).rearrange("p (j n) -> p j n", j=NJ) for _ in range(3)]
            MSET("dve", halo, 0.0, w=["halo"])
            wupv = w_up_d[l].rearrange("(k p) (g n) -> p k g n", p=128, g=2)
            wdnv = w_down_d[l].rearrange("(j p) n -> p j n", p=128)

            def ld_up(v, k, j):
                DMA("sp", v.rearrange("p k g n -> p (k g n)"), wup_s[l, j], ("wsem",) + k, r=[("pcu", l, j, 0), ("pcu", l, j, 1)], w=[k])

            def ld_dn(v, k, dc):
                DMA("sp", v.rearrange("p j n -> p (j n)"), wdn_s[l, dc], ("wsem",) + k, r=[("pcd", l, dc)], w=[k])

            wur = WStream([(wups[i], ("wu", i)) for i in range(3)], [(lambda v, k, j=j: ld_up(v, k, j)) for _t in range(NT) for j in range(NJ)])
            wdr = WStream([(wdns[i], ("wd", i)) for i in range(3)], [(lambda v, k, dc=dc: ld_dn(v, k, dc)) for _t in range(NT) for dc in range(8)])
            wur.top()
            wdr.top()
            pend_f = None
            for t in range(NT):
                for j in range(NJ):
                    wv, wk = wur.get()
                    u = ub[j % 2]
                    b2 = j % 2
                    ta, tb, cg, cv = tas[b2], tbs[b2], cgs[b2], cvs[b2]
                    for gi in range(2):
                        jj = j + gi * NJ
                        ps, pk = ringA.next()
                        for kc in range(8):
                            MM(ps, wv[:, kc, gi, :], hT[:, kc, sl(t)], kc == 0, kc == 7, r=[wk, ("hT", kc, t)], w=[pk], inc=(kc == 7))
                        uk = ("ub", b2, gi)
                        ACT(u[gi][:, 2:514], ps, AF.Copy, r=[pk], w=[uk])
                        CP("dve", u[gi][:, 0:2], halo[:, jj, :], r=["halo", ("halo", jj)], w=[uk])
                        tmp = ta if gi == 0 else tb
                        tk_ = ("ta", b2) if gi == 0 else ("tb", b2)
                        ACT(tmp, ps, AF.Identity, r=[pk, "pv"], w=[tk_], scale=fcw[:, l, jj, 2:3], bias=fcb[:, l, jj:jj + 1])
                        ACT(halo[:, jj, :], ps[:, 510:512], AF.Copy, r=[pk], w=[("halo", jj)])
                        dst = cg if gi == 0 else cv
                        dk = ("cg", b2) if gi == 0 else ("cv", b2)
                        STT(tmp, u[gi][:, 1:513], fcw[:, l, jj, 1:2], tmp, ALU.mult, ALU.add, r=[uk, tk_, "pv"], w=[tk_])
                        STT(dst, u[gi][:, 0:512], fcw[:, l, jj, 0:1], tmp, ALU.mult, ALU.add, r=[uk, tk_, "pv"], w=[dk])
                    if pend_f is not None:
                        pend_f()

                    def fin(j=j, b2=b2, cg=cg, cv=cv):
                        ACT(cg, cg, AF.Gelu_apprx_tanh, r=[("cg", b2)], w=[("cg", b2)])
                        TTo("dve", fT[:, j, :], cg, cv, ALU.mult, r=[("cg", b2), ("cv", b2)], w=[("fT", j)])
                    pend_f = fin
                pend_f()
                pend_f = None
                if l == 0 and t == 0:
                    dump("fT", fT[:, 0, :], [("fT", 0)])
                for dc in range(8):
                    wv, wk = wdr.get()
                    ps, pk = ringB.next()
                    for j in range(NJ):
                        MM(ps, wv[:, j, :], fT[:, j, :], j == 0, j == NJ - 1, r=[wk, ("fT", j)], w=[pk], inc=(j == NJ - 1))
                    if dc % 2 == 0:
                        ACT(ytile[:, dc, :], ps, AF.Copy, r=[pk], w=[("yt", dc)])
                    else:
                        CP("dve", ytile[:, dc, :], ps, r=[pk], w=[("yt", dc)])
                rmsnorm_tile([ytile[:, dc, :] for dc in range(8)], [("yt", dc) for dc in range(8)],
                             [ytile[:, dc, :] for dc in range(8)], [("yt", dc) for dc in range(8)],
                             [vd[:, l, 3, dc:dc + 1] for dc in range(8)], D)
                for dc in range(8):
                    TTo("dve", xT[:, dc, sl(t)], xT[:, dc, sl(t)], ytile[:, dc, :], ALU.add, r=[("xT", dc, t), ("yt", dc)], w=[("xT", dc, t)])
            if l == 0:
                dump("x2", xT[:, 0, 0:512], [("xT", 0, 0)])
            S.barrier()
            A.release(PH)

            norm_x_to_h(l, 4)
            pTb = A.bf16(2 * SEQ).rearrange("p (c s) -> p c s", c=2)
            wgs = [A.bf16(8 * 256).rearrange("p (k n) -> p k n", k=8) for _ in range(3)]
            wpj = A.bf16(2 * D).rearrange("p (k n) -> p k n", k=2)
            sgt = [A.f32(512) for _ in range(2)]
            pjt = [A.f32(512) for _ in range(2)]
            wload(pTb, pT_d[l].rearrange("(k p) s -> p k s", p=128), ("pTb",))
            wload(wpj, w_proj_d[l].rearrange("(k p) n -> p k n", p=128), ("wpj",))
            wgv = w_gate_d[l].rearrange("(k p) n -> p k n", p=128)
            wgr = WStream([(wgs[i], ("wg", i)) for i in range(3)], [(lambda v, k, db=db: wload(v, wgv[:, :, db * 256:(db + 1) * 256], k)) for db in range(4)])
            wgr.top()
            it = 0
            for db in range(4):
                wv, wk = wgr.get()
                for dd in range(2):
                    dc = db * 2 + dd
                    for t in range(NT):
                        b = it % 2
                        it += 1
                        ps, pk = ringA.next()
                        rf, kf = hrhs(t)
                        proj(ps, pk, wv, wk, slice(dd * 128, (dd + 1) * 128), rf, kf)
                        ACT(sgt[b], ps, AF.Sigmoid, r=[pk], w=[("sgt", b)])
                        ps2, pk2 = ringB.next()
                        for kc in range(2):
                            MM(ps2, wpj[:, kc, dc * 128:(dc + 1) * 128], pTb[:, kc, sl(t)], kc == 0, kc == 1, r=[("wpj",), ("pTb",)], w=[pk2], inc=(kc == 1))
                        TTo("dve", pjt[b], ps2, sgt[b], ALU.mult, r=[pk2, ("sgt", b)], w=[("pjt", b)])
                        TTo("dve", xT[:, dc, sl(t)], xT[:, dc, sl(t)], pjt[b], ALU.add, r=[("xT", dc, t), ("pjt", b)], w=[("xT", dc, t)])
            if l == 0:
                dump("x3", xT[:, 0, 0:512], [("xT", 0, 0)])

        for c in range(8):
            DMA("sp", out_d[c * 128:(c + 1) * 128, :], xT[:, c, :], "st_out", r=[("xT", c, t) for t in range(NT)], w=[("out", c)])
        S.barrier()
        S.emit()
        build.arena_hi = A.hi
    return nc


_CACHE = {}


def _prep_inputs(inp):
    cmat, cf, ind = _host_consts()
    pv, glnb, gb, wsT = _host_params(inp)
    x = np.asarray(inp["x"], np.float32)
    p = np.asarray(inp["p"], np.float32)
    pos = np.asarray(inp["positions"], np.int32)
    shared = dict(cmat=cmat, cf=cf, ind=ind, pv=pv, glnb=glnb, gb=gb, wsT=wsT)
    for k in ("w_in", "w_out", "w_up", "w_down", "w_pe_gate", "w_pe_proj", "conv_pw_w"):
        shared[k] = np.ascontiguousarray(np.asarray(inp[k], np.float32))
    maps = []
    for b in range(8):
        m = dict(shared)
        m["xT"] = np.ascontiguousarray(x[b].T)
        m["pT"] = np.ascontiguousarray(p[:, b].transpose(0, 2, 1))
        m["pos"] = np.ascontiguousarray(pos[b][None, :])
        maps.append(m)
    return maps


def kernel(**inputs):
    if "nc" not in _CACHE:
        _CACHE["nc"] = build()
    nc = _CACHE["nc"]
    maps = _prep_inputs(inputs)
    res = run_bass_kernel_spmd(nc, maps, core_ids=list(range(8)))
    out = np.stack([np.asarray(r["outT"], np.float32).T for r in res.results], axis=0)
    return np.ascontiguousarray(out)
```

```python
import math
import numpy as np
from contextlib import ExitStack
import concourse.bass as bass
import concourse.mybir as mybir
from concourse.bass_utils import run_bass_kernel_spmd

F32 = mybir.dt.float32
BF16 = mybir.dt.bfloat16
I32 = mybir.dt.int32
AF = mybir.ActivationFunctionType
ALU = mybir.AluOpType
AX = mybir.AxisListType

L = 2
D = 1024
SEQ = 2048
GW = 256
DFF = 2816
NJ = DFF // 128
NT = 4
TW = 512
MASK = 30000.0
BIGG = 1.0e6
EPS = 1e-6
ENGS = ["pe", "act", "dve", "pool", "sp"]


class Sched:
    def __init__(self, nc, es, same_engine_sync=True):
        self.nc = nc
        self.es = es
        self.same = same_engine_sync
        self.prog = {e: [] for e in ENGS}
        self.cnt = {}
        self.sems = {}
        self.seen = {e: {} for e in ENGS}
        self.state = {}
        for e in ENGS:
            self._sem(e)

    def _sem(self, key):
        if key not in self.sems:
            self.sems[key] = self.es.enter_context(self.nc.semaphore("s_" + str(key)))
            self.cnt[key] = 0
        return self.sems[key]

    def _engobj(self, e):
        nc = self.nc
        return dict(pe=nc.tensor, act=nc.scalar, dve=nc.vector, pool=nc.gpsimd, sp=nc.sync)[e]

    def _deps(self, e, r, w):
        toks = {}
        for k in r:
            st = self.state.get(k)
            if st and st[0] is not None:
                t = st[0]
                toks[t[0]] = max(toks.get(t[0], 0), t[1])
        for k in w:
            st = self.state.get(k)
            if st:
                if st[0] is not None:
                    t = st[0]
                    toks[t[0]] = max(toks.get(t[0], 0), t[1])
                for t in st[1]:
                    toks[t[0]] = max(toks.get(t[0], 0), t[1])
        waits = []
        for sk, v in toks.items():
            if sk == e and (not self.same or e == "pe"):
                continue
            if self.seen[e].get(sk, 0) >= v:
                continue
            self.seen[e][sk] = v
            waits.append((sk, v))
        return waits

    def _record(self, tok, r, w):
        for k in r:
            st = self.state.setdefault(k, [None, []])
            st[1].append(tok)
        for k in w:
            self.state[k] = [tok, []]

    def op(self, e, fn, r=(), w=(), inc=True):
        waits = self._deps(e, r, w)
        tok = (e, self.cnt[e] + 1)
        if inc:
            self.cnt[e] += 1
        self._record(tok, r, w)
        self.prog[e].append((waits, fn, (e, 1) if inc else None))
        return tok

    def dma(self, q, fn, semkey, r=(), w=()):
        self._sem(semkey)
        waits = self._deps(q, r, w)
        self.cnt[semkey] += 16
        tok = (semkey, self.cnt[semkey])
        self._record(tok, r, w)
        self.prog[q].append((waits, fn, (semkey, 16)))
        return tok

    def barrier(self):
        for e in ENGS:
            waits = []
            for sk, v in self.cnt.items():
                if v == 0 or self.seen[e].get(sk, 0) >= v:
                    continue
                if sk == e and e == "pe":
                    continue
                self.seen[e][sk] = v
                waits.append((sk, v))
            self.prog[e].append((waits, None, None))

    def simulate(self):
        val = {k: 0 for k in self.sems}
        pc = {e: 0 for e in ENGS}
        progress = True
        while progress:
            progress = False
            for e in ENGS:
                while pc[e] < len(self.prog[e]):
                    waits, fn, inc = self.prog[e][pc[e]]
                    if any(val[sk] < v for sk, v in waits):
                        break
                    if inc is not None:
                        val[inc[0]] += inc[1]
                    pc[e] += 1
                    progress = True
        stuck = {e: (pc[e], len(self.prog[e])) for e in ENGS if pc[e] < len(self.prog[e])}
        if stuck:
            msg = []
            for e, (i, n) in stuck.items():
                waits = self.prog[e][i][0]
                msg.append("%s@%d/%d waits %s" % (e, i, n, [(sk, v, val[sk]) for sk, v in waits if val[sk] < v]))
            raise RuntimeError("DEADLOCK in schedule: " + "; ".join(msg))
        for k in self.sems:
            assert val[k] == self.cnt[k], (k, val[k], self.cnt[k])

    def emit(self):
        self.simulate()
        nc = self.nc
        with nc.Block() as block:
            def run(e):
                eng = self._engobj(e)
                for waits, fn, inc in self.prog[e]:
                    for sk, v in waits:
                        eng.wait_ge(self.sems[sk], v)
                    if fn is None:
                        continue
                    ins = fn()
                    if inc is not None:
                        ins.then_inc(self.sems[inc[0]], inc[1])

            @block.tensor
            def _(e):
                run("pe")

            @block.scalar
            def _(e):
                run("act")

            @block.vector
            def _(e):
                run("dve")

            @block.gpsimd
            def _(e):
                run("pool")

            @block.sync
            def _(e):
                run("sp")


class Arena:
    def __init__(self, t, nbytes):
        self.t = t
        self.n = nbytes
        self.off = 0
        self.hi = 0

    def mark(self):
        return self.off

    def release(self, m):
        self.off = m

    def _take(self, nbytes):
        nbytes = (nbytes + 31) // 32 * 32
        o = self.off
        self.off += nbytes
        self.hi = max(self.hi, self.off)
        assert self.off <= self.n, ("arena overflow", self.off, self.n)
        return o

    def f32(self, n):
        o = self._take(4 * n)
        return self.t[:, o // 4: o // 4 + n]

    def bf16(self, n):
        o = self._take(2 * n)
        return self.t[:, o // 4: o // 4 + (n + 1) // 2].bitcast(BF16)

    def i32(self, n):
        o = self._take(4 * n)
        return self.t[:, o // 4: o // 4 + n].bitcast(I32)


class Ring:
    def __init__(self, items):
        self.items = items
        self.i = 0

    def next(self):
        it = self.items[self.i % len(self.items)]
        self.i += 1
        return it


class WStream:
    def __init__(self, slots, plan, auto=True):
        self.slots = slots
        self.plan = plan
        self.auto = auto
        self.issued = 0
        self.used = 0
        self.freed = 0

    def top(self):
        while self.issued < len(self.plan) and self.issued - self.freed < len(self.slots):
            i = self.issued
            v, k = self.slots[i % len(self.slots)]
            self.plan[i](v, k)
            self.issued += 1

    def done(self):
        self.freed = self.used
        self.top()

    def get(self):
        i = self.used
        if self.auto:
            self.freed = i
        self.top()
        assert self.issued > i, "weight stream: block not issued (slots exhausted)"
        self.used += 1
        return self.slots[i % len(self.slots)]


def _host_consts():
    ident = np.eye(128, dtype=np.float32)
    ones = np.ones((128, 128), np.float32)
    blk2 = np.zeros((128, 128), np.float32)
    blk2[0:64, 0:64] = 1
    blk2[64:128, 64:128] = 1

    def rotT(block):
        h = block // 2
        m = np.zeros((128, 128), np.float32)
        for b0 in range(0, 128, block):
            for i in range(h):
                m[b0 + i + h, b0 + i] = -1.0
                m[b0 + i, b0 + i + h] = 1.0
        return m

    tri = np.where(np.arange(128)[None, :] >= np.arange(128)[:, None], 0.0, -MASK).astype(np.float32)
    cmat = np.concatenate([ident, ones, blk2, rotT(64), rotT(32), tri], axis=1)
    tril = (np.arange(128)[:, None] <= np.arange(128)[None, :]).astype(np.float32)
    pm = np.zeros((16, 8), np.float32)
    for qs in range(16):
        cur = qs // 2
        for n in range(8):
            pm[qs, n] = 0.0 if n < cur else (BIGG if n == cur else -2 * BIGG)
    pmask = np.broadcast_to(pm.reshape(1, 128), (128, 128))
    p = np.arange(128)
    invf64 = (10000.0 ** (-(2.0 * (p % 32)) / 64.0)) / (2 * math.pi)
    invf32 = (10000.0 ** (-(2.0 * (p % 16)) / 32.0)) / (2 * math.pi)
    gmask = (np.arange(128)[:, None] // 32 == np.arange(4)[None, :]).astype(np.float32)
    cf = np.concatenate([tril, pmask, invf64[:, None], invf32[:, None], gmask], axis=1).astype(np.float32)
    ind = (-MASK) * (np.arange(SEQ)[None, :] // 256 == np.arange(8)[:, None]).astype(np.float32)
    return np.ascontiguousarray(cmat), np.ascontiguousarray(cf), np.ascontiguousarray(ind)


def _fm(v, c):
    return np.ascontiguousarray(np.asarray(v, np.float32).reshape(c, 128).T)


def _host_params(inp):
    vd = np.stack([np.stack([_fm(inp[k][l], 8) for k in ("pre_mix_norm", "post_mix_norm", "pre_ffn_norm", "post_ffn_norm", "pe_gate_norm")], 1) for l in range(L)], 1)
    vg = np.stack([np.stack([_fm(inp[k][l], 2) for k in ("conv_dw_b", "conv_ln_g", "conv_ln_b", "conv_pw_b", "out_norm_a", "out_norm_b", "out_norm_c")], 1) for l in range(L)], 1)
    wdw = np.stack([np.asarray(inp["conv_dw_w"][l], np.float32).reshape(31, 2, 128).transpose(2, 1, 0) for l in range(L)], 1)
    fcw = np.stack([np.asarray(inp["ffn_conv_w"][l], np.float32).reshape(3, 44, 128).transpose(2, 1, 0) for l in range(L)], 1)
    fcb = np.stack([_fm(inp["ffn_conv_b"][l], 44) for l in range(L)], 1)
    gsub = np.stack([np.asarray(inp["diff_subln_g"][l], np.float32)[np.arange(128) % 64] for l in range(L)], 1)
    lamv = np.stack([np.stack([np.asarray(inp[k][l], np.float32) for k in ("diff_lq1", "diff_lk1", "diff_lq2", "diff_lk2")], 0) for l in range(L)], 0)
    lamv = np.broadcast_to(lamv.reshape(1, L * 4 * 32), (128, L * 4 * 32))
    pv = np.concatenate([vd.reshape(128, -1), vg.reshape(128, -1), wdw.reshape(128, -1), fcw.reshape(128, -1), fcb.reshape(128, -1), gsub.reshape(128, -1), lamv], axis=1)
    glnb = np.stack([np.stack([np.broadcast_to(np.asarray(inp[k][l], np.float32)[None, :], (128, 256)) for k in ("gmlp_ln_g", "gmlp_ln_b")], 1) for l in range(L)], 0)
    bs = np.asarray(inp["gmlp_bs"], np.float32)
    gb = np.zeros((L, 2, 128, 512), np.float32)
    for l in range(L):
        for cc in range(2):
            for hh in range(2):
                gb[l, cc, hh * 64:(hh + 1) * 64, :] = np.tile(bs[l, 2 * cc + hh], 4)[None, :]
    wsT = np.ascontiguousarray(np.asarray(inp["gmlp_ws"], np.float32).transpose(0, 1, 3, 2))
    return np.ascontiguousarray(pv.astype(np.float32)), np.ascontiguousarray(glnb), gb, wsT


PV_OFF = {}


def _pv_layout():
    o = 0
    for name, n in (("vd", L * 5 * 8), ("vg", L * 7 * 2), ("wdw", L * 2 * 31), ("fcw", L * 44 * 3), ("fcb", L * 44), ("gsub", L), ("lamv", L * 4 * 32)):
        PV_OFF[name] = o
        o += n
    return o


PV_N = _pv_layout()


def build(dbg=None, nlayers=L):
    dbg = dbg or []
    nc = bass.Bass("TRN2", target_bir_lowering=False)

    def din(name, shape, dt=F32):
        return nc.dram_tensor(name, list(shape), dt, kind="ExternalInput").ap()

    xT_d = din("xT", [D, SEQ])
    pT_d = din("pT", [L, GW, SEQ])
    pos_d = din("pos", [1, SEQ], I32)
    cmat_d = din("cmat", [128, 768])
    cf_d = din("cf", [128, 262])
    ind_d = din("ind", [8, SEQ])
    pv_d = din("pv", [128, PV_N])
    glnb_d = din("glnb", [L, 128, 2, 256])
    gb_d = din("gb", [L, 2, 128, 512])
    wsT_d = din("wsT", [L, 4, 128, 128])
    w_in_d = din("w_in", [L, D, 10 * GW])
    w_out_d = din("w_out", [L, D, D])
    w_up_d = din("w_up", [L, D, 2 * DFF])
    w_down_d = din("w_down", [L, DFF, D])
    w_gate_d = din("w_pe_gate", [L, D, D])
    w_proj_d = din("w_pe_proj", [L, GW, D])
    w_pw_d = din("conv_pw_w", [L, GW, GW])
    out_d = nc.dram_tensor("outT", [D, SEQ], F32, kind="ExternalOutput").ap()
    wup_s = nc.dram_tensor("wup_s", [L, NJ, 128, 8 * 2 * 128], BF16, kind="Internal").ap()
    wdn_s = nc.dram_tensor("wdn_s", [L, 8, 128, NJ * 128], BF16, kind="Internal").ap()
    dbg_d = {n: nc.dram_tensor("dbg_" + n, [128, w], F32, kind="ExternalOutput").ap() for n, w in dbg}

    es = ExitStack()
    with es:
        S = Sched(nc, es)
        NB = 206 * 1024
        big = es.enter_context(nc.sbuf_tensor("arena", [128, NB // 4], F32))
        A = Arena(big, NB)
        psb = [es.enter_context(nc.psum_tensor("ps%d" % i, [128, 512], F32)) for i in range(8)]
        PS = [(psb[i][:, :], ("ps", i)) for i in range(8)]
        ringA = Ring(PS[0:4])
        ringB = Ring(PS[4:8])

        def MM(out, lhsT, rhs, start, stop, r, w, inc=True, tp=None):
            kw = {}
            if tp is not None:
                kw["tile_position"] = tp
            S.op("pe", lambda: nc.tensor.matmul(out, lhsT=lhsT, rhs=rhs, start=start, stop=stop, **kw), r=r, w=w, inc=inc)

        def ACT(out, in_, func, r, w, scale=None, bias=None):
            kw = {}
            if scale is not None:
                kw["scale"] = scale
            if bias is not None:
                kw["bias"] = bias
            S.op("act", lambda: nc.scalar.activation(out=out, in_=in_, func=func, **kw), r=r, w=w)

        def TTo(eng, out, in0, in1, op, r, w):
            e = nc.vector if eng == "dve" else nc.gpsimd
            S.op(eng, lambda: e.tensor_tensor(out=out, in0=in0, in1=in1, op=op), r=r, w=w)

        def TS(eng, out, in0, s1, s2, op0, op1, r, w):
            e = nc.vector if eng == "dve" else nc.gpsimd
            if op1 is None:
                S.op(eng, lambda: e.tensor_scalar(out=out, in0=in0, scalar1=s1, scalar2=None, op0=op0), r=r, w=w)
            else:
                S.op(eng, lambda: e.tensor_scalar(out=out, in0=in0, scalar1=s1, scalar2=s2, op0=op0, op1=op1), r=r, w=w)

        def STT(out, in0, scalar, in1, op0, op1, r, w):
            S.op("dve", lambda: nc.vector.scalar_tensor_tensor(out=out, in0=in0, scalar=scalar, in1=in1, op0=op0, op1=op1), r=r, w=w)

        def CP(eng, out, in_, r, w):
            e = nc.vector if eng == "dve" else nc.gpsimd
            S.op(eng, lambda: e.tensor_copy(out=out, in_=in_), r=r, w=w)

        def MSET(eng, ap, val, w):
            e = nc.vector if eng == "dve" else nc.gpsimd
            S.op(eng, lambda: e.memset(ap, val), w=w)

        def DMA(q, out, in_, semkey, r, w):
            e = dict(sp=nc.sync, pool=nc.gpsimd, act=nc.scalar)[q]
            S.dma(q, lambda: e.dma_start(out=out, in_=in_), semkey, r=r, w=w)

        def dump(name, ap, keys):
            if name in dbg_d:
                w_ = ap.shape[-1]
                stg = A.f32(w_)
                CP("dve", stg[0:ap.shape[0], :], ap, r=keys, w=[("dbgs", name)])
                DMA("sp", dbg_d[name][0:ap.shape[0], 0:w_], stg[0:ap.shape[0], :], "dbg", r=[("dbgs", name)], w=[("dbgo", name)])

        xT = A.f32(8 * SEQ).rearrange("p (c s) -> p c s", c=8)
        hT = A.bf16(8 * SEQ).rearrange("p (c s) -> p c s", c=8)
        cmat = A.bf16(768)
        IDENT, ONES, BLK2, R64, R32, TRI = [cmat[:, i * 128:(i + 1) * 128] for i in range(6)]
        cf = A.f32(262)
        TRIL = cf[:, 0:128]
        PMASK = cf[:, 128:256].rearrange("p (q n) -> p q n", n=8)
        INVF = [cf[:, 256:257], cf[:, 257:258]]
        GMASK = cf[:, 258:262]
        pv = A.f32(PV_N)

        def PVv(name, n):
            return pv[:, PV_OFF[name]:PV_OFF[name] + n]

        vd = PVv("vd", L * 5 * 8).rearrange("p (l v c) -> p l v c", l=L, v=5)
        vg = PVv("vg", L * 7 * 2).rearrange("p (l v c) -> p l v c", l=L, v=7)
        wdw = PVv("wdw", L * 2 * 31).rearrange("p (l c k) -> p l c k", l=L, c=2)
        fcw = PVv("fcw", L * 44 * 3).rearrange("p (l j k) -> p l j k", l=L, j=44)
        fcb = PVv("fcb", L * 44).rearrange("p (l j) -> p l j", l=L)
        gsub = PVv("gsub", L)
        lamv = PVv("lamv", L * 4 * 32).rearrange("p (l v k) -> p l v k", l=L, v=4)
        small = A.f32(64)
        lnv = A.f32(512)
        rstd = A.f32(512)
        sq = [A.bf16(512) for _ in range(3)]
        sqring = Ring([(sq[i], ("sq", i)) for i in range(3)])
        PH = A.mark()

        DMA("pool", cmat, cmat_d[:, :], "ld_c", r=[], w=["cmat"])
        DMA("sp", cf, cf_d[:, :], "ld_c2", r=[], w=["cf"])
        DMA("sp", pv, pv_d[:, :], "ld_c3", r=[], w=["pv"])
        for t in range(NT):
            DMA("sp", xT[:, :, t * TW:(t + 1) * TW], xT_d.rearrange("(c p) s -> p c s", p=128)[:, :, t * TW:(t + 1) * TW], ("ld_x", t), r=[], w=[("xT", c, t) for c in range(8)])

        def wload(dst, src, key):
            DMA("pool", dst, src, ("wsem",) + key, r=[], w=[key])

        def precast_ffn(l):
            wupv_ = w_up_d[l].rearrange("(k p) (g n) -> p k g n", p=128, g=2)
            wdnv_ = w_down_d[l].rearrange("(j p) n -> p j n", p=128)
            sk = ("pcs", l)
            keys = []
            for j in range(NJ):
                for gi_ in range(2):
                    dst = wup_s[l, j].rearrange("p (k g n) -> p k g n", k=8, g=2)[:, :, gi_, :]
                    DMA("pool", dst, wupv_[:, :, gi_, j * 128:(j + 1) * 128], sk, r=[], w=[("pcu", l, j, gi_)])
                    keys.append(("pcu", l, j, gi_))
            for dc in range(8):
                dst = wdn_s[l, dc].rearrange("p (j n) -> p j n", j=NJ)
                DMA("pool", dst, wdnv_[:, :, dc * 128:(dc + 1) * 128], sk, r=[], w=[("pcd", l, dc)])
                keys.append(("pcd", l, dc))
            tot = S.cnt[sk]
            for k in keys:
                S.state[k] = [(sk, tot), []]

        def rstd_from(ps_ap, ps_key, inv_n, eps, extra_r=()):
            ACT(lnv, ps_ap, AF.Ln, r=[ps_key] + list(extra_r), w=["lnv"], scale=inv_n, bias=eps)
            ACT(rstd, lnv, AF.Exp, r=["lnv"], w=["rstd"], scale=-0.5)

        def rmsnorm_tile(srcs, skeys, dsts, dkeys, gains, n_feat, eps=EPS, lhs=None, lkey="cmat"):
            ps, pk = ringB.next()
            C = len(srcs)
            for c in range(C):
                q, qk = sqring.next()
                ACT(q, srcs[c], AF.Square, r=[skeys[c]], w=[qk])
                MM(ps, lhs if lhs is not None else ONES, q, c == 0, c == C - 1, r=[qk, lkey], w=[pk])
            rstd_from(ps, pk, 1.0 / n_feat, eps)
            for c in range(C):
                STT(dsts[c], srcs[c], gains[c], rstd, ALU.mult, ALU.mult, r=[skeys[c], "rstd", "pv"], w=[dkeys[c]])

        def sl(t):
            return slice(t * TW, (t + 1) * TW)

        def norm_x_to_h(l, vidx):
            for t in range(NT):
                rmsnorm_tile([xT[:, c, sl(t)] for c in range(8)], [("xT", c, t) for c in range(8)],
                             [hT[:, c, sl(t)] for c in range(8)], [("hT", c, t) for c in range(8)],
                             [vd[:, l, vidx, c:c + 1] for c in range(8)], D)

        def proj(ps, pk, wv, wkey, cs, rhs_fn, rkeys_fn, nk=8):
            for kc in range(nk):
                MM(ps, wv[:, kc, cs], rhs_fn(kc), kc == 0, kc == nk - 1, r=[wkey] + rkeys_fn(kc), w=[pk], inc=(kc == nk - 1))

        def hrhs(t):
            return (lambda kc: hT[:, kc, sl(t)]), (lambda kc: [("hT", kc, t)])

        for l in range(nlayers):
            S.barrier()
            A.release(PH)
            catF = A.bf16(8 * SEQ)
            catT = catF.rearrange("p (c s) -> p c s", c=8)
            ropeC = A.bf16(SEQ)
            ropeS = A.bf16(SEQ)
            wslots = [A.bf16(8 * 256).rearrange("p (k n) -> p k n", k=8) for _ in range(4)]
            MX = A.mark()
            winv = w_in_d[l].rearrange("(k p) n -> p k n", p=128)
            woutv = w_out_d[l].rearrange("(k p) n -> p k n", p=128)
            mplan = [(lambda v, k, bi=bi: wload(v, winv[:, :, bi * 256:(bi + 1) * 256], k)) for bi in (0, 1, 2, 7, 8, 9, 3, 4, 5, 6)]
            mplan += [(lambda v, k, db=db: wload(v, woutv[:, :, db * 256:(db + 1) * 256], k)) for _t in range(NT) for db in range(4)]
            mstream = WStream([(wslots[i], ("ws", i)) for i in range(4)], mplan, auto=False)

            def win_block(bi):
                return mstream.get()

            def rope_tables(which):
                posi = catF[:, 2 * SEQ:4 * SEQ].bitcast(I32)
                v = catF[:, 4 * SEQ:6 * SEQ].bitcast(F32)
                ki = catF[:, 6 * SEQ:8 * SEQ].bitcast(I32)
                DMA("sp", posi, pos_d[0:1, :].broadcast_to([128, SEQ]), "ld_pos", r=[], w=["posi"])
                for tab, shift, key in ((ropeS, 0.0, "ropeS"), (ropeC, 0.25, "ropeC")):
                    TS("dve", v, posi, INVF[which], shift, ALU.mult, ALU.add, r=["posi", "cf"], w=["ropev"])
                    CP("dve", ki, v, r=["ropev"], w=["ropek"])
                    TTo("dve", v, v, ki, ALU.subtract, r=["ropev", "ropek"], w=["ropev"])
                    ACT(tab, v, AF.Sin, r=["ropev"], w=[key], scale=2 * math.pi * (1 - 1e-6))

            def rope_apply(ps, pk, t, RM, zbr, t1, t2):
                zb, zk = zbr.next()
                ACT(zb, ps, AF.Copy, r=[pk], w=[zk])
                ps2, pk2 = ringA.next()
                MM(ps2, RM, zb, True, True, r=[zk, "cmat"], w=[pk2])
                TTo("dve", t1[0], zb, ropeC[:, sl(t)], ALU.mult, r=[zk, "ropeC"], w=[t1[1]])
                TTo("dve", t2[0], ps2, ropeS[:, sl(t)], ALU.mult, r=[pk2, "ropeS"], w=[t2[1]])

            def attn_finalize_recip(O, ok, dst_rden):
                ACT(dst_rden[0][0:64, :], O[64:128, :], AF.Ln, r=[ok], w=[dst_rden[1]])
                ACT(dst_rden[0][0:64, :], dst_rden[0][0:64, :], AF.Exp, r=[dst_rden[1]], w=[dst_rden[1]], scale=-1.0)

            def vproj(wv, wk, cs, vaug):
                for g in range(4):
                    ps, pk = ringA.next()
                    for s4 in range(4):
                        st = g * 4 + s4
                        for kc in range(8):
                            MM(ps[:, s4 * 128:(s4 + 1) * 128], hT[:, kc, st * 128:(st + 1) * 128], wv[:, kc, cs], kc == 0, kc == 7,
                               r=[wk, ("hT", kc, st // 4)], w=[pk], inc=(kc == 7 and s4 == 3))
                    ACT(vaug[:, g * 4:(g + 1) * 4, :, 0:64], ps.rearrange("p (s h d) -> p s h d", s=4, h=2), AF.Copy, r=[pk], w=[("V", g)])

            rope_tables(0)
            norm_x_to_h(l, 0)
            if l == 0:
                dump("hT0", hT[:, 0, 0:512], [("hT", 0, 0)])

            m0 = A.mark()
            Kaug = [A.bf16(SEQ) for _ in range(2)]
            vaug = A.bf16(16 * 2 * 128).rearrange("p (s h d) -> p s h d", s=16, h=2)
            Qa = [[A.bf16(512) for _ in range(2)] for _ in range(NT)]
            Pr = Ring([(A.bf16(512), ("P", i)) for i in range(4)])
            zbr = Ring([(A.bf16(512), ("zb", i)) for i in range(2)])
            t1 = (A.f32(512), "t1")
            t2 = (A.f32(512), "t2")
            rden = (A.f32(512), "rden")
            bstg4 = A.bf16(4 * 72).rearrange("p (j n) -> p j n", j=4)
            gm = A.f32(64)
            top8 = A.f32(32)
            kmf = A.f32(16)
            kmb = A.bf16(16)
            MSET("dve", vaug[:, :, :, 64:128], 1.0, w=[("V", g) for g in range(4)])
            MSET("dve", bstg4, 0.0, w=["bstg"])
            for hh in range(2):
                DMA("pool", Kaug[hh][64:72, :], ind_d[:, :], "ld_ind", r=[], w=[("Kind", hh)])
            wq, wqk = win_block(0)
            wk_, wkk = win_block(1)
            wv_, wvk = win_block(2)
            precast_ffn(l)
            for c in range(2):
                cs = slice(c * 128, (c + 1) * 128)
                for t in range(NT):
                    ps, pk = ringA.next()
                    rf, kf = hrhs(t)
                    proj(ps, pk, wk_, wkk, cs, rf, kf)
                    rope_apply(ps, pk, t, R64, zbr, t1, t2)
                    for hh in range(2):
                        TTo("dve", Kaug[hh][0:64, sl(t)], t1[0][hh * 64:(hh + 1) * 64, :], t2[0][hh * 64:(hh + 1) * 64, :], ALU.add,
                            r=[t1[1], t2[1]], w=[("K", hh, t)])
                for hh in range(2):
                    S.op("dve", lambda hh=hh: nc.vector.tensor_reduce(out=kmf[0:64, hh * 8:(hh + 1) * 8], in_=Kaug[hh][0:64, :].rearrange("p (n k) -> p n k", k=256), axis=AX.X, op=ALU.add),
                         r=[("K", hh, t) for t in range(NT)], w=[("kmf", hh)])
                    TS("dve", kmb[0:64, hh * 8:(hh + 1) * 8], kmf[0:64, hh * 8:(hh + 1) * 8], 1.0 / 256, None, ALU.mult, None, r=[("kmf", hh)], w=[("kmb", hh)])
                vproj(wv_, wvk, cs, vaug)
                for qt in range(NT):
                    qb = Qa[qt]
                    ps, pk = ringA.next()
                    rf, kf = hrhs(qt)
                    proj(ps, pk, wq, wqk, cs, rf, kf)
                    rope_apply(ps, pk, qt, R64, zbr, t1, t2)
                    for hh in range(2):
                        TTo("dve", qb[hh][0:64, :], t1[0][hh * 64:(hh + 1) * 64, :], t2[0][hh * 64:(hh + 1) * 64, :], ALU.add,
                            r=[t1[1], t2[1]], w=[("Qa", qt, hh)])
                    for hh in range(2):
                        gps, gk = ringA.next()
                        for j in range(4):
                            MM(gps[:, j * 8:(j + 1) * 8], qb[hh][0:64, j * 128:(j + 1) * 128], kmb[0:64, hh * 8:(hh + 1) * 8], True, True,
                               r=[("Qa", qt, hh), ("kmb", hh)], w=[gk], inc=(j == 3))
                        gmv = gm[:, 0:32].rearrange("p (j n) -> p j n", n=8)
                        TTo("dve", gmv, gps[:, 0:32].rearrange("p (j n) -> p j n", n=8), PMASK[:, qt * 4:(qt + 1) * 4, :], ALU.add, r=[gk, "cf"], w=["gm"])
                        tps, tk = ringA.next()
                        tpsb = tps.bitcast(BF16)
                        top8v = top8.rearrange("p (j n) -> p j n", n=8)
                        for j in range(4):
                            S.op("dve", lambda j=j: nc.vector.max(out=top8[:, j * 8:(j + 1) * 8], in_=gm[:, j * 8:(j + 1) * 8]), r=["gm"], w=["top8"])
                        TS("dve", top8v[:, :, 3:4], top8v[:, :, 3:4], -BIGG, None, ALU.max, None, r=["top8"], w=["top8"])
                        TTo("dve", bstg4[:, :, 64:72], gmv, top8v[:, :, 3:4].broadcast_to([128, 4, 8]), ALU.is_lt, r=["gm", "top8"], w=["bstg"])
                        for j in range(4):
                            S.op("pe", lambda j=j, tpsb=tpsb: nc.tensor.transpose(tpsb[0:72, j * 128:(j + 1) * 128], bstg4[:, j, :], IDENT), r=["bstg", "cmat"], w=[tk], inc=(j == 3))
                        ACT(qb[hh][64:72, :], tpsb[64:72, 0:512], AF.Copy, r=[tk], w=[("Qb", qt, hh)])
                if c == 1:
                    rope_tables(1)
                defer = []
                for qt in range(NT):
                    qb = Qa[qt]
                    for hh in range(2):
                        O, ok = ringB.next()
                        nk = 4 * qt + 4
                        pend = []
                        for kt in range(nk):
                            jd = kt - 4 * qt
                            c0 = 128 * jd if jd > 0 else 0
                            sp_, sk = ringA.next()
                            MM(sp_[:, c0:512], Kaug[hh][0:72, kt * 128:(kt + 1) * 128], qb[hh][0:72, c0:512], True, jd < 0,
                               r=[("K", hh, kt // 4), ("Kind", hh), ("Qa", qt, hh), ("Qb", qt, hh)], w=[sk], inc=(jd < 0))
                            if jd >= 0:
                                MM(sp_[:, c0:c0 + 128], IDENT, TRI, False, True, r=["cmat"], w=[sk])
                            P, Pk = Pr.next()
                            ACT(P[:, c0:512], sp_[:, c0:512], AF.Exp, r=[sk], w=[Pk], scale=0.125)

                            def pv_(kt=kt, c0=c0, P=P, Pk=Pk, O=O, ok=ok, hh=hh, nk=nk):
                                MM(O[:, c0:512], vaug[:, kt, hh, :], P[:, c0:512], kt == 0, kt == nk - 1, r=[Pk, ("V", kt // 4)], w=[ok])
                            pend.append(pv_)
                            if len(pend) > 2:
                                pend.pop(0)()
                            if kt == 1:
                                for f in defer:
                                    f()
                                defer = []
                        for f in pend:
                            f()
                        def fin_(O=O, ok=ok, hh=hh, c=c, qt=qt):
                            attn_finalize_recip(O, ok, rden)
                            TTo("dve", catT[hh * 64:(hh + 1) * 64, c, sl(qt)], O[0:64, :], rden[0][0:64, :], ALU.mult, r=[ok, rden[1]], w=[("cat", c, qt)])
                        defer.append(fin_)
                for f in defer:
                    f()
                defer = []
            if l == 0:
                dump("oa_pre", catT[:, 0, 0:512], [("cat", 0, 0)])
            for t in range(NT):
                rmsnorm_tile([catT[:, c, sl(t)] for c in range(2)], [("cat", c, t) for c in range(2)],
                             [catT[:, c, sl(t)] for c in range(2)], [("cat", c, t) for c in range(2)],
                             [vg[:, l, 4, c:c + 1] for c in range(2)], GW)
            if l == 0:
                dump("oa", catT[:, 0, 0:512], [("cat", 0, 0)])
            mstream.done()
            S.barrier()
            A.release(m0)

            lam_init = 0.8 - 0.6 * math.exp(-0.3 * l)
            lt = small[:, 0:8]
            prod = A.f32(64)
            TTo("dve", prod[:, 0:32], lamv[:, l, 0, :], lamv[:, l, 1, :], ALU.mult, r=["pv"], w=["prod"])
            S.op("dve", lambda: nc.vector.tensor_reduce(out=lt[:, 0:1], in_=prod[:, 0:32], axis=AX.X, op=ALU.add), r=["prod"], w=["lt"])
            TTo("dve", prod[:, 32:64], lamv[:, l, 2, :], lamv[:, l, 3, :], ALU.mult, r=["pv"], w=["prod2"])
            S.op("dve", lambda: nc.vector.tensor_reduce(out=lt[:, 1:2], in_=prod[:, 32:64], axis=AX.X, op=ALU.add), r=["prod2"], w=["lt"])
            ACT(lt[:, 2:4], lt[:, 0:2], AF.Exp, r=["lt"], w=["lt"])
            TTo("dve", lt[:, 4:5], lt[:, 3:4], lt[:, 2:3], ALU.subtract, r=["lt"], w=["lt"])
            TS("dve", lt[:, 5:6], lt[:, 4:5], -lam_init, None, ALU.add, None, r=["lt"], w=["lt"])
            TS("dve", lt[:, 6:7], gsub[:, l:l + 1], 1.0 - lam_init, None, ALU.mult, None, r=["pv", "lt"], w=["lt"])
            NEGLAM = lt[:, 5:6]
            GS = lt[:, 6:7]

            m0 = A.mark()
            Kd = A.bf16(SEQ)
            vaug = A.bf16(16 * 2 * 128).rearrange("p (s h d) -> p s h d", s=16, h=2)
            Qd = [A.bf16(512) for _ in range(NT)]
            Pr = Ring([(A.bf16(512), ("P", i)) for i in range(4)])
            zbr = Ring([(A.bf16(512), ("zb", i)) for i in range(2)])
            t1 = (A.f32(512), "t1")
            t2 = (A.f32(512), "t2")
            rden = (A.f32(512), "rden")
            ods = [A.f32(512) for _ in range(2)]
            Qm = [[A.bf16(512) for _ in range(4)] for _ in range(2)]
            MSET("dve", vaug[:, :, :, 64:128], 1.0, w=[("V", g) for g in range(4)])
            wq, wqk = win_block(7)
            wk_, wkk = win_block(8)
            wv_, wvk = win_block(9)
            dscale = 32.0 ** -0.5
            for c in range(2):
                cs = slice(c * 128, (c + 1) * 128)
                for t in range(NT):
                    ps, pk = ringA.next()
                    rf, kf = hrhs(t)
                    proj(ps, pk, wk_, wkk, cs, rf, kf)
                    rope_apply(ps, pk, t, R32, zbr, t1, t2)
                    TTo("dve", Kd[:, sl(t)], t1[0], t2[0], ALU.add, r=[t1[1], t2[1]], w=[("Kd", t)])
                vproj(wv_, wvk, cs, vaug)
                for qt in range(NT):
                    qd = Qd[qt]
                    ps, pk = ringA.next()
                    rf, kf = hrhs(qt)
                    proj(ps, pk, wq, wqk, cs, rf, kf)
                    rope_apply(ps, pk, qt, R32, zbr, t1, t2)
                    TTo("dve", qd, t1[0], t2[0], ALU.add, r=[t1[1], t2[1]], w=[("Qd", qt)])
                defer = []
                defer2 = []

                def qmask(qt):
                    for g in range(4):
                        TS("dve", Qm[qt % 2][g], Qd[qt], GMASK[:, g:g + 1], None, ALU.mult, None, r=[("Qd", qt), "cf"], w=[("Qm", qt % 2, g)])
                qmask(0)
                for qt in range(NT):
                    qd = Qd[qt]
                    od = ods[qt % 2]
                    for hh in range(2):
                        Os = [ringB.next(), ringB.next()]
                        nk = 4 * qt + 4
                        pend = []
                        for kt in range(nk):
                            jd = kt - 4 * qt
                            c0 = 128 * jd if jd > 0 else 0
                            cur = []
                            for m_ in range(2):
                                g = hh * 2 + m_
                                sp_, sk = ringA.next()
                                MM(sp_[:, c0:512], Kd[:, kt * 128:(kt + 1) * 128], Qm[qt % 2][g][:, c0:512], True, jd < 0,
                                   r=[("Kd", kt // 4), ("Qm", qt % 2, g)], w=[sk], inc=(jd < 0))
                                if jd >= 0:
                                    MM(sp_[:, c0:c0 + 128], IDENT, TRI, False, True, r=["cmat"], w=[sk])
                                cur.append((sp_, sk))
                            for f in pend:
                                f()
                            pend = []
                            if kt == 1:
                                for f in defer:
                                    f()
                                defer = defer2
                                defer2 = []
                            for m_ in range(2):
                                sp_, sk = cur[m_]
                                P, Pk = Pr.next()
                                ACT(P[:, c0:512], sp_[:, c0:512], AF.Exp, r=[sk], w=[Pk], scale=dscale)

                                def pv_(kt=kt, c0=c0, P=P, Pk=Pk, Oo=Os[m_], hh=hh, nk=nk):
                                    MM(Oo[0][:, c0:512], vaug[:, kt, hh, :], P[:, c0:512], kt == 0, kt == nk - 1, r=[Pk, ("V", kt // 4)], w=[Oo[1]])
                                pend.append(pv_)
                        for f in pend:
                            f()
                        def fin_(Os=Os, hh=hh, qt=qt, od=od):
                            hs = slice(hh * 64, (hh + 1) * 64)
                            attn_finalize_recip(Os[0][0], Os[0][1], rden)
                            TTo("dve", t1[0][0:64, :], Os[0][0][0:64, :], rden[0][0:64, :], ALU.mult, r=[Os[0][1], rden[1]], w=[t1[1]])
                            attn_finalize_recip(Os[1][0], Os[1][1], (lnv, "lnv"))
                            TTo("dve", t2[0][0:64, :], Os[1][0][0:64, :], lnv[0:64, :], ALU.mult, r=[Os[1][1], "lnv"], w=[t2[1]])
                            STT(od[hs, :], t2[0][0:64, :], NEGLAM[0:64, :], t1[0][0:64, :], ALU.mult, ALU.add, r=[t1[1], t2[1], "lt"], w=[("od", qt % 2, hh)])
                        defer.append(fin_)
                        if hh == 0 and qt + 1 < NT:
                            qmask(qt + 1)
                    def norm_(qt=qt, od=od, c=c):
                        ps, pk = ringA.next()
                        q_, qk_ = sqring.next()
                        ACT(q_, od, AF.Square, r=[("od", qt % 2, 0), ("od", qt % 2, 1)], w=[qk_])
                        MM(ps, BLK2, q_, True, True, r=[qk_, "cmat"], w=[pk])
                        rstd_from(ps, pk, 1.0 / 64, 1e-5)
                        STT(catT[:, 6 + c, sl(qt)], od, GS, rstd, ALU.mult, ALU.mult, r=[("od", qt % 2, 0), ("od", qt % 2, 1), "rstd", "lt"], w=[("cat", 6 + c, qt)])
                    defer2.append(norm_)
                for f in defer + defer2:
                    f()
                defer = []
                defer2 = []
            if l == 0:
                dump("od", catT[:, 6, 0:512], [("cat", 6, 0)])
            mstream.done()
            S.barrier()
            A.release(m0)

            m0 = A.mark()
            uT = A.bf16(2 * SEQ).rearrange("p (c s) -> p c s", c=2)
            vgl = A.bf16(16 * 256).rearrange("p (n d) -> p n d", n=16)
            glnb = A.f32(512).rearrange("p (v d) -> p v d", v=2)
            gb = A.f32(1024).rearrange("p (c s) -> p c s", c=2)
            wsf = A.f32(512).rearrange("p (h i) -> p h i", h=4)
            wsb = A.bf16(512).rearrange("p (h i) -> p h i", h=4)
            stats = A.f32(16 * 6)
            mv = A.f32(16 * 2).rearrange("p (n k) -> p n k", k=2)
            rsd = A.f32(16)
            vtmp = A.f32(256)
            vln = [A.bf16(256) for _ in range(2)]
            stmp = A.f32(512)
            DMA("sp", glnb, glnb_d[l], "ld_g1", r=[], w=["glnb"])
            DMA("sp", gb, gb_d[l].rearrange("c p s -> p c s"), "ld_g2", r=[], w=["gb"])
            DMA("sp", wsf, wsT_d[l].rearrange("h j i -> j h i"), "ld_g3", r=[], w=["wsf"])
            for h in range(4):
                TTo("dve", wsb[:, h, :], wsf[:, h, :], TRIL, ALU.mult, r=["wsf", "cf"], w=["wsb"])
            wu, wuk = win_block(3)
            wvv, wvvk = win_block(4)
            for t in range(NT):
                for cc in range(2):
                    ps, pk = ringA.next()
                    rf, kf = hrhs(t)
                    proj(ps, pk, wu, wuk, slice(cc * 128, (cc + 1) * 128), rf, kf)
                    ACT(uT[:, cc, sl(t)], ps, AF.Gelu_apprx_tanh, r=[pk], w=[("uT", cc, t)])
            for n2 in range(8):
                ps, pk = ringA.next()
                for s2 in range(2):
                    n = n2 * 2 + s2
                    for kc in range(8):
                        MM(ps[:, s2 * 256:(s2 + 1) * 256], hT[:, kc, n * 128:(n + 1) * 128], wvv[:, kc, :], kc == 0, kc == 7,
                           r=[wvvk, ("hT", kc, n // 4)], w=[pk], inc=(kc == 7 and s2 == 1))
                ACT(vgl[:, n2 * 2:(n2 + 1) * 2, :], ps.rearrange("p (s d) -> p s d", s=2), AF.Gelu_apprx_tanh, r=[pk], w=[("vgl", n2)])
            for n in range(16):
                S.op("dve", lambda n=n: nc.vector.bn_stats(out=stats[:, n * 6:(n + 1) * 6], in_=vgl[:, n, :]), r=[("vgl", n // 2)], w=[("st", n)])
                S.op("dve", lambda n=n: nc.vector.bn_aggr(out=mv[:, n, :], in_=stats[:, n * 6:(n + 1) * 6]), r=[("st", n)], w=["mv"])
            ACT(rsd, mv[:, :, 1], AF.Ln, r=["mv"], w=["rsd"], bias=EPS)
            ACT(rsd, rsd, AF.Exp, r=["rsd"], w=["rsd"], scale=-0.5)
            for t in range(NT):
                pss = [ringA.next(), ringA.next()]
                for s4 in range(4):
                    n = t * 4 + s4
                    vl = vln[n % 2]
                    vk = ("vln", n % 2)
                    TS("dve", vtmp, vgl[:, n, :], mv[:, n, 0:1], rsd[:, n:n + 1], ALU.subtract, ALU.mult, r=[("vgl", n // 2), "mv", "rsd"], w=["vtmp"])
                    TTo("dve", vtmp, vtmp, glnb[:, 0, :], ALU.mult, r=["vtmp", "glnb"], w=["vtmp"])
                    TTo("dve", vl, vtmp, glnb[:, 1, :], ALU.add, r=["vtmp", "glnb"], w=[vk])
                    for cc in range(2):
                        for hh in range(2):
                            h = 2 * cc + hh
                            MM(pss[cc][0][hh * 64:(hh + 1) * 64, s4 * 128:(s4 + 1) * 128], vl[:, h * 64:(h + 1) * 64], wsb[:, h, :], True, True,
                               r=[vk, "wsb"], w=[pss[cc][1]], tp=(0, hh * 64))
                for cc in range(2):
                    TTo("dve", stmp, pss[cc][0], gb[:, cc, :], ALU.add, r=[pss[cc][1], "gb"], w=["stmp"])
                    TTo("dve", catT[:, 2 + cc, sl(t)], stmp, uT[:, cc, sl(t)], ALU.mult, r=["stmp", ("uT", cc, t)], w=[("cat", 2 + cc, t)])
            if l == 0:
                dump("ob_pre", catT[:, 2, 0:512], [("cat", 2, 0)])
            for t in range(NT):
                rmsnorm_tile([catT[:, 2 + c, sl(t)] for c in range(2)], [("cat", 2 + c, t) for c in range(2)],
                             [catT[:, 2 + c, sl(t)] for c in range(2)], [("cat", 2 + c, t) for c in range(2)],
                             [vg[:, l, 5, c:c + 1] for c in range(2)], GW)
            mstream.done()
            S.barrier()
            A.release(m0)

            m0 = A.mark()
            ybuf = A.bf16(32 + SEQ)
            diag = A.bf16(31 * 128).rearrange("p (k n) -> p k n", k=31)
            cy = A.bf16(2 * SEQ).rearrange("p (c s) -> p c s", c=2)
            sg = A.f32(512)
            wpw = A.bf16(2 * 256).rearrange("p (k n) -> p k n", k=2)
            mstat = A.f32(512)
            m2 = A.f32(512)
            yn = A.f32(512)
            sil = A.bf16(1024).rearrange("p (c s) -> p c s", c=2)
            Y0 = 2
            wa, wak = win_block(5)
            wg_, wgk = win_block(6)
            wload(wpw, w_pw_d[l].rearrange("(k p) n -> p k n", p=128), ("wpw",))
            MSET("dve", ybuf[:, 0:32], 0.0, w=["ypad"])
            for cc in range(2):
                cs = slice(cc * 128, (cc + 1) * 128)
                for k in range(31):
                    TS("dve", diag[:, k, :], IDENT, wdw[:, l, cc, k:k + 1], None, ALU.mult, None, r=["cmat", "pv"], w=[("diag", k)])
                for t in range(NT):
                    pa, pak = ringA.next()
                    rf, kf = hrhs(t)
                    proj(pa, pak, wa, wak, cs, rf, kf)
                    pg, pgk = ringA.next()
                    proj(pg, pgk, wg_, wgk, cs, rf, kf)
                    ACT(sg, pg, AF.Sigmoid, r=[pgk], w=["sg"])
                    TTo("dve", ybuf[:, 32 + t * TW:32 + (t + 1) * TW], pa, sg, ALU.mult, r=[pak, "sg"], w=[("yb", t)])
                for t in range(NT):
                    ps, pk = ringB.next()
                    for k in range(31):
                        o = Y0 + t * TW + k
                        rk = ["ypad", ("diag", k), ("yb", t)] + ([("yb", t - 1)] if t > 0 else [])
                        MM(ps, diag[:, k, :], ybuf[:, o:o + TW], k == 0, k == 30, r=rk, w=[pk], inc=(k == 30))
                    ACT(cy[:, cc, sl(t)], ps, AF.Identity, r=[pk, "pv"], w=[("cy", cc, t)], bias=vg[:, l, 0, cc:cc + 1])
            if l == 0:
                dump("cy", cy[:, 0, 0:512], [("cy", 0, 0)])
            for t in range(NT):
                ps1, pk1 = ringB.next()
                ps2, pk2 = ringB.next()
                for cc in range(2):
                    MM(ps1, ONES, cy[:, cc, sl(t)], cc == 0, cc == 1, r=[("cy", cc, t), "cmat"], w=[pk1])
                for cc in range(2):
                    q_, qk_ = sqring.next()
                    ACT(q_, cy[:, cc, sl(t)], AF.Square, r=[("cy", cc, t)], w=[qk_])
                    MM(ps2, ONES, q_, cc == 0, cc == 1, r=[qk_, "cmat"], w=[pk2])
                TS("dve", mstat, ps1, 1.0 / GW, None, ALU.mult, None, r=[pk1], w=["mstat"])
                TTo("dve", m2, mstat, mstat, ALU.mult, r=["mstat"], w=["m2"])
                STT(m2, ps2, 1.0 / GW, m2, ALU.mult, ALU.subtract, r=[pk2, "m2"], w=["m2"])
                ACT(lnv, m2, AF.Ln, r=["m2"], w=["lnv"], bias=EPS)
                ACT(rstd, lnv, AF.Exp, r=["lnv"], w=["rstd"], scale=-0.5)
                for cc in range(2):
                    TTo("dve", yn, cy[:, cc, sl(t)], mstat, ALU.subtract, r=[("cy", cc, t), "mstat"], w=["yn"])
                    TTo("dve", yn, yn, rstd, ALU.mult, r=["yn", "rstd"], w=["yn"])
                    ACT(sil[:, cc, :], yn, AF.Silu, r=["yn", "pv"], w=[("sil", cc)], scale=vg[:, l, 1, cc:cc + 1], bias=vg[:, l, 2, cc:cc + 1])
                for co in range(2):
                    ps, pk = ringA.next()
                    for ci in range(2):
                        MM(ps, wpw[:, ci, co * 128:(co + 1) * 128], sil[:, ci, :], ci == 0, ci == 1, r=[("wpw",), ("sil", ci)], w=[pk])
                    ACT(catT[:, 4 + co, sl(t)], ps, AF.Identity, r=[pk, "pv"], w=[("cat", 4 + co, t)], bias=vg[:, l, 3, co:co + 1])
            if l == 0:
                dump("oc_pre", catT[:, 4, 0:512], [("cat", 4, 0)])
            for t in range(NT):
                rmsnorm_tile([catT[:, 4 + c, sl(t)] for c in range(2)], [("cat", 4 + c, t) for c in range(2)],
                             [catT[:, 4 + c, sl(t)] for c in range(2)], [("cat", 4 + c, t) for c in range(2)],
                             [vg[:, l, 6, c:c + 1] for c in range(2)], GW)
            mstream.done()
            S.barrier()
            A.release(m0)

            def out_proj_norm_res(nkc, wsrc, rhs_fn, rkeys_fn, vidx, wr, ytile):
                for t in range(NT):
                    for db in range(4):
                        wv, wk = wr.get()
                        for dd in range(2):
                            dc = db * 2 + dd
                            ps, pk = ringA.next()
                            for kc in range(nkc):
                                MM(ps, wv[:, kc, dd * 128:(dd + 1) * 128], rhs_fn(kc, t), kc == 0, kc == nkc - 1, r=[wk] + rkeys_fn(kc, t), w=[pk], inc=(kc == nkc - 1))
                            if dc % 2 == 0:
                                ACT(ytile[:, dc, :], ps, AF.Copy, r=[pk], w=[("yt", dc)])
                            else:
                                CP("dve", ytile[:, dc, :], ps, r=[pk], w=[("yt", dc)])
                        wr.done()
                    rmsnorm_tile([ytile[:, dc, :] for dc in range(8)], [("yt", dc) for dc in range(8)],
                                 [ytile[:, dc, :] for dc in range(8)], [("yt", dc) for dc in range(8)],
                                 [vd[:, l, vidx, dc:dc + 1] for dc in range(8)], D)
                    for dc in range(8):
                        TTo("dve", xT[:, dc, sl(t)], xT[:, dc, sl(t)], ytile[:, dc, :], ALU.add, r=[("xT", dc, t), ("yt", dc)], w=[("xT", dc, t)])

            m0 = A.mark()
            ytile = A.f32(8 * 512).rearrange("p (c s) -> p c s", c=8)
            out_proj_norm_res(8, w_out_d[l].rearrange("(k p) n -> p k n", p=128), lambda kc, t: catT[:, kc, sl(t)], lambda kc, t: [("cat", kc, t)], 1, mstream, ytile)
            if l == 0:
                dump("x1", xT[:, 0, 0:512], [("xT", 0, 0)])
            S.barrier()
            A.release(MX)
            A.release(PH)

            norm_x_to_h(l, 2)
            ytile = A.f32(8 * 512).rearrange("p (c s) -> p c s", c=8)
            fT = A.bf16(NJ * 512).rearrange("p (j s) -> p j s", j=NJ)
            ub = [[A.f32(2 + 512) for _ in range(2)] for _ in range(2)]
            halo = A.f32(44 * 2).rearrange("p (j k) -> p j k", k=2)
            tas = [A.f32(512) for _ in range(2)]
            tbs = [A.f32(512) for _ in range(2)]
            cgs = [A.f32(512) for _ in range(2)]
            cvs = [A.f32(512) for _ in range(2)]
            wups = [A.bf16(8 * 2 * 128).rearrange("p (k g n) -> p k g n", k=8, g=2) for _ in range(3)]
            wdns = [A.bf16(NJ * 128).rearrange("p (j n) -> p j n", j=NJ) for _ in range(3)]
            MSET("dve", halo, 0.0, w=["halo"])
            wupv = w_up_d[l].rearrange("(k p) (g n) -> p k g n", p=128, g=2)
            wdnv = w_down_d[l].rearrange("(j p) n -> p j n", p=128)

            def ld_up(v, k, j):
                DMA("sp", v.rearrange("p k g n -> p (k g n)"), wup_s[l, j], ("wsem",) + k, r=[("pcu", l, j, 0), ("pcu", l, j, 1)], w=[k])

            def ld_dn(v, k, dc):
                DMA("sp", v.rearrange("p j n -> p (j n)"), wdn_s[l, dc], ("wsem",) + k, r=[("pcd", l, dc)], w=[k])

            wur = WStream([(wups[i], ("wu", i)) for i in range(3)], [(lambda v, k, j=j: ld_up(v, k, j)) for _t in range(NT) for j in range(NJ)])
            wdr = WStream([(wdns[i], ("wd", i)) for i in range(3)], [(lambda v, k, dc=dc: ld_dn(v, k, dc)) for _t in range(NT) for dc in range(8)])
            wur.top()
            wdr.top()
            pend_f = None
            for t in range(NT):
                for j in range(NJ):
                    wv, wk = wur.get()
                    u = ub[j % 2]
                    b2 = j % 2
                    ta, tb, cg, cv = tas[b2], tbs[b2], cgs[b2], cvs[b2]
                    for gi in range(2):
                        jj = j + gi * NJ
                        ps, pk = ringA.next()
                        for kc in range(8):
                            MM(ps, wv[:, kc, gi, :], hT[:, kc, sl(t)], kc == 0, kc == 7, r=[wk, ("hT", kc, t)], w=[pk], inc=(kc == 7))
                        uk = ("ub", b2, gi)
                        ACT(u[gi][:, 2:514], ps, AF.Copy, r=[pk], w=[uk])
                        CP("dve", u[gi][:, 0:2], halo[:, jj, :], r=["halo", ("halo", jj)], w=[uk])
                        tmp = ta if gi == 0 else tb
                        tk_ = ("ta", b2) if gi == 0 else ("tb", b2)
                        ACT(tmp, ps, AF.Identity, r=[pk, "pv"], w=[tk_], scale=fcw[:, l, jj, 2:3], bias=fcb[:, l, jj:jj + 1])
                        ACT(halo[:, jj, :], ps[:, 510:512], AF.Copy, r=[pk], w=[("halo", jj)])
                        dst = cg if gi == 0 else cv
                        dk = ("cg", b2) if gi == 0 else ("cv", b2)
                        STT(tmp, u[gi][:, 1:513], fcw[:, l, jj, 1:2], tmp, ALU.mult, ALU.add, r=[uk, tk_, "pv"], w=[tk_])
                        STT(dst, u[gi][:, 0:512], fcw[:, l, jj, 0:1], tmp, ALU.mult, ALU.add, r=[uk, tk_, "pv"], w=[dk])
                    if pend_f is not None:
                        pend_f()

                    def fin(j=j, b2=b2, cg=cg, cv=cv):
                        ACT(cg, cg, AF.Gelu_apprx_tanh, r=[("cg", b2)], w=[("cg", b2)])
                        TTo("dve", fT[:, j, :], cg, cv, ALU.mult, r=[("cg", b2), ("cv", b2)], w=[("fT", j)])
                    pend_f = fin
                pend_f()
                pend_f = None
                if l == 0 and t == 0:
                    dump("fT", fT[:, 0, :], [("fT", 0)])
                for dc in range(8):
                    wv, wk = wdr.get()
                    ps, pk = ringB.next()
                    for j in range(NJ):
                        MM(ps, wv[:, j, :], fT[:, j, :], j == 0, j == NJ - 1, r=[wk, ("fT", j)], w=[pk], inc=(j == NJ - 1))
                    if dc % 2 == 0:
                        ACT(ytile[:, dc, :], ps, AF.Copy, r=[pk], w=[("yt", dc)])
                    else:
                        CP("dve", ytile[:, dc, :], ps, r=[pk], w=[("yt", dc)])
                rmsnorm_tile([ytile[:, dc, :] for dc in range(8)], [("yt", dc) for dc in range(8)],
                             [ytile[:, dc, :] for dc in range(8)], [("yt", dc) for dc in range(8)],
                             [vd[:, l, 3, dc:dc + 1] for dc in range(8)], D)
                for dc in range(8):
                    TTo("dve", xT[:, dc, sl(t)], xT[:, dc, sl(t)], ytile[:, dc, :], ALU.add, r=[("xT", dc, t), ("yt", dc)], w=[("xT", dc, t)])
            if l == 0:
                dump("x2", xT[:, 0, 0:512], [("xT", 0, 0)])
            S.barrier()
            A.release(PH)

            norm_x_to_h(l, 4)
            pTb = A.bf16(2 * SEQ).rearrange("p (c s) -> p c s", c=2)
            wgs = [A.bf16(8 * 256).rearrange("p (k n) -> p k n", k=8) for _ in range(3)]
            wpj = A.bf16(2 * D).rearrange("p (k n) -> p k n", k=2)
            sgt = [A.f32(512) for _ in range(2)]
            pjt = [A.f32(512) for _ in range(2)]
            wload(pTb, pT_d[l].rearrange("(k p) s -> p k s", p=128), ("pTb",))
            wload(wpj, w_proj_d[l].rearrange("(k p) n -> p k n", p=128), ("wpj",))
            wgv = w_gate_d[l].rearrange("(k p) n -> p k n", p=128)
            wgr = WStream([(wgs[i], ("wg", i)) for i in range(3)], [(lambda v, k, db=db: wload(v, wgv[:, :, db * 256:(db + 1) * 256], k)) for db in range(4)])
            wgr.top()
            it = 0
            for db in range(4):
                wv, wk = wgr.get()
                for dd in range(2):
                    dc = db * 2 + dd
                    for t in range(NT):
                        b = it % 2
                        it += 1
                        ps, pk = ringA.next()
                        rf, kf = hrhs(t)
                        proj(ps, pk, wv, wk, slice(dd * 128, (dd + 1) * 128), rf, kf)
                        ACT(sgt[b], ps, AF.Sigmoid, r=[pk], w=[("sgt", b)])
                        ps2, pk2 = ringB.next()
                        for kc in range(2):
                            MM(ps2, wpj[:, kc, dc * 128:(dc + 1) * 128], pTb[:, kc, sl(t)], kc == 0, kc == 1, r=[("wpj",), ("pTb",)], w=[pk2], inc=(kc == 1))
                        TTo("dve", pjt[b], ps2, sgt[b], ALU.mult, r=[pk2, ("sgt", b)], w=[("pjt", b)])
                        TTo("dve", xT[:, dc, sl(t)], xT[:, dc, sl(t)], pjt[b], ALU.add, r=[("xT", dc, t), ("pjt", b)], w=[("xT", dc, t)])
            if l == 0:
                dump("x3", xT[:, 0, 0:512], [("xT", 0, 0)])

        for c in range(8):
            DMA("sp", out_d[c * 128:(c + 1) * 128, :], xT[:, c, :], "st_out", r=[("xT", c, t) for t in range(NT)], w=[("out", c)])
        S.barrier()
        S.emit()
        build.arena_hi = A.hi
    return nc


_CACHE = {}


def _prep_inputs(inp):
    cmat, cf, ind = _host_consts()
    pv, glnb, gb, wsT = _host_params(inp)
    x = np.asarray(inp["x"], np.float32)
    p = np.asarray(inp["p"], np.float32)
    pos = np.asarray(inp["positions"], np.int32)
    shared = dict(cmat=cmat, cf=cf, ind=ind, pv=pv, glnb=glnb, gb=gb, wsT=wsT)
    for k in ("w_in", "w_out", "w_up", "w_down", "w_pe_gate", "w_pe_proj", "conv_pw_w"):
        shared[k] = np.ascontiguousarray(np.asarray(inp[k], np.float32))
    maps = []
    for b in range(8):
        m = dict(shared)
        m["xT"] = np.ascontiguousarray(x[b].T)
        m["pT"] = np.ascontiguousarray(p[:, b].transpose(0, 2, 1))
        m["pos"] = np.ascontiguousarray(pos[b][None, :])
        maps.append(m)
    return maps


def kernel(**inputs):
    if "nc" not in _CACHE:
        _CACHE["nc"] = build()
    nc = _CACHE["nc"]
    maps = _prep_inputs(inputs)
    res = run_bass_kernel_spmd(nc, maps, core_ids=list(range(8)))
    out = np.stack([np.asarray(r["outT"], np.float32).T for r in res.results], axis=0)
    return np.ascontiguousarray(out)
```

```python
import math
import numpy as np
from contextlib import ExitStack
import concourse.bass as bass
import concourse.mybir as mybir
from concourse.bass_utils import run_bass_kernel_spmd

F32 = mybir.dt.float32
BF16 = mybir.dt.bfloat16
I32 = mybir.dt.int32
AF = mybir.ActivationFunctionType
ALU = mybir.AluOpType
AX = mybir.AxisListType

L = 2
D = 1024
SEQ = 2048
GW = 256
DFF = 2816
NJ = DFF // 128
NT = 4
TW = 512
MASK = 30000.0
BIGG = 1.0e6
EPS = 1e-6
ENGS = ["pe", "act", "dve", "pool", "sp"]


class Sched:
    def __init__(self, nc, es, same_engine_sync=True):
        self.nc = nc
        self.es = es
        self.same = same_engine_sync
        self.prog = {e: [] for e in ENGS}
        self.cnt = {}
        self.sems = {}
        self.seen = {e: {} for e in ENGS}
        self.state = {}
        for e in ENGS:
            self._sem(e)

    def _sem(self, key):
        if key not in self.sems:
            self.sems[key] = self.es.enter_context(self.nc.semaphore("s_" + str(key)))
            self.cnt[key] = 0
        return self.sems[key]

    def _engobj(self, e):
        nc = self.nc
        return dict(pe=nc.tensor, act=nc.scalar, dve=nc.vector, pool=nc.gpsimd, sp=nc.sync)[e]

    def _deps(self, e, r, w):
        toks = {}
        for k in r:
            st = self.state.get(k)
            if st and st[0] is not None:
                t = st[0]
                toks[t[0]] = max(toks.get(t[0], 0), t[1])
        for k in w:
            st = self.state.get(k)
            if st:
                if st[0] is not None:
                    t = st[0]
                    toks[t[0]] = max(toks.get(t[0], 0), t[1])
                for t in st[1]:
                    toks[t[0]] = max(toks.get(t[0], 0), t[1])
        waits = []
        for sk, v in toks.items():
            if sk == e and (not self.same or e == "pe"):
                continue
            if self.seen[e].get(sk, 0) >= v:
                continue
            self.seen[e][sk] = v
            waits.append((sk, v))
        return waits

    def _record(self, tok, r, w):
        for k in r:
            st = self.state.setdefault(k, [None, []])
            st[1].append(tok)
        for k in w:
            self.state[k] = [tok, []]

    def op(self, e, fn, r=(), w=(), inc=True):
        waits = self._deps(e, r, w)
        tok = (e, self.cnt[e] + 1)
        if inc:
            self.cnt[e] += 1
        self._record(tok, r, w)
        self.prog[e].append((waits, fn, (e, 1) if inc else None))
        return tok

    def dma(self, q, fn, semkey, r=(), w=()):
        self._sem(semkey)
        waits = self._deps(q, r, w)
        self.cnt[semkey] += 16
        tok = (semkey, self.cnt[semkey])
        self._record(tok, r, w)
        self.prog[q].append((waits, fn, (semkey, 16)))
        return tok

    def barrier(self):
        for e in ENGS:
            waits = []
            for sk, v in self.cnt.items():
                if v == 0 or self.seen[e].get(sk, 0) >= v:
                    continue
                if sk == e and e == "pe":
                    continue
                self.seen[e][sk] = v
                waits.append((sk, v))
            self.prog[e].append((waits, None, None))

    def simulate(self):
        val = {k: 0 for k in self.sems}
        pc = {e: 0 for e in ENGS}
        progress = True
        while progress:
            progress = False
            for e in ENGS:
                while pc[e] < len(self.prog[e]):
                    waits, fn, inc = self.prog[e][pc[e]]
                    if any(val[sk] < v for sk, v in waits):
                        break
                    if inc is not None:
                        val[inc[0]] += inc[1]
                    pc[e] += 1
                    progress = True
        stuck = {e: (pc[e], len(self.prog[e])) for e in ENGS if pc[e] < len(self.prog[e])}
        if stuck:
            msg = []
            for e, (i, n) in stuck.items():
                waits = self.prog[e][i][0]
                msg.append("%s@%d/%d waits %s" % (e, i, n, [(sk, v, val[sk]) for sk, v in waits if val[sk] < v]))
            raise RuntimeError("DEADLOCK in schedule: " + "; ".join(msg))
        for k in self.sems:
            assert val[k] == self.cnt[k], (k, val[k], self.cnt[k])

    def emit(self):
        self.simulate()
        nc = self.nc
        with nc.Block() as block:
            def run(e):
                eng = self._engobj(e)
                for waits, fn, inc in self.prog[e]:
                    for sk, v in waits:
                        eng.wait_ge(self.sems[sk], v)
                    if fn is None:
                        continue
                    ins = fn()
                    if inc is not None:
                        ins.then_inc(self.sems[inc[0]], inc[1])

            @block.tensor
            def _(e):
                run("pe")

            @block.scalar
            def _(e):
                run("act")

            @block.vector
            def _(e):
                run("dve")

            @block.gpsimd
            def _(e):
                run("pool")

            @block.sync
            def _(e):
                run("sp")


class Arena:
    def __init__(self, t, nbytes):
        self.t = t
        self.n = nbytes
        self.off = 0
        self.hi = 0

    def mark(self):
        return self.off

    def release(self, m):
        self.off = m

    def _take(self, nbytes):
        nbytes = (nbytes + 31) // 32 * 32
        o = self.off
        self.off += nbytes
        self.hi = max(self.hi, self.off)
        assert self.off <= self.n, ("arena overflow", self.off, self.n)
        return o

    def f32(self, n):
        o = self._take(4 * n)
        return self.t[:, o // 4: o // 4 + n]

    def bf16(self, n):
        o = self._take(2 * n)
        return self.t[:, o // 4: o // 4 + (n + 1) // 2].bitcast(BF16)

    def i32(self, n):
        o = self._take(4 * n)
        return self.t[:, o // 4: o // 4 + n].bitcast(I32)


class Ring:
    def __init__(self, items):
        self.items = items
        self.i = 0

    def next(self):
        it = self.items[self.i % len(self.items)]
        self.i += 1
        return it


class WStream:
    def __init__(self, slots, plan, auto=True):
        self.slots = slots
        self.plan = plan
        self.auto = auto
        self.issued = 0
        self.used = 0
        self.freed = 0

    def top(self):
        while self.issued < len(self.plan) and self.issued - self.freed < len(self.slots):
            i = self.issued
            v, k = self.slots[i % len(self.slots)]
            self.plan[i](v, k)
            self.issued += 1

    def done(self):
        self.freed = self.used
        self.top()

    def get(self):
        i = self.used
        if self.auto:
            self.freed = i
        self.top()
        assert self.issued > i, "weight stream: block not issued (slots exhausted)"
        self.used += 1
        return self.slots[i % len(self.slots)]


def _host_consts():
    ident = np.eye(128, dtype=np.float32)
    ones = np.ones((128, 128), np.float32)
    blk2 = np.zeros((128, 128), np.float32)
    blk2[0:64, 0:64] = 1
    blk2[64:128, 64:128] = 1

    def rotT(block):
        h = block // 2
        m = np.zeros((128, 128), np.float32)
        for b0 in range(0, 128, block):
            for i in range(h):
                m[b0 + i + h, b0 + i] = -1.0
                m[b0 + i, b0 + i + h] = 1.0
        return m

    tri = np.where(np.arange(128)[None, :] >= np.arange(128)[:, None], 0.0, -MASK).astype(np.float32)
    cmat = np.concatenate([ident, ones, blk2, rotT(64), rotT(32), tri], axis=1)
    tril = (np.arange(128)[:, None] <= np.arange(128)[None, :]).astype(np.float32)
    pm = np.zeros((16, 8), np.float32)
    for qs in range(16):
        cur = qs // 2
        for n in range(8):
            pm[qs, n] = 0.0 if n < cur else (BIGG if n == cur else -2 * BIGG)
    pmask = np.broadcast_to(pm.reshape(1, 128), (128, 128))
    p = np.arange(128)
    invf64 = (10000.0 ** (-(2.0 * (p % 32)) / 64.0)) / (2 * math.pi)
    invf32 = (10000.0 ** (-(2.0 * (p % 16)) / 32.0)) / (2 * math.pi)
    gmask = (np.arange(128)[:, None] // 32 == np.arange(4)[None, :]).astype(np.float32)
    cf = np.concatenate([tril, pmask, invf64[:, None], invf32[:, None], gmask], axis=1).astype(np.float32)
    ind = (-MASK) * (np.arange(SEQ)[None, :] // 256 == np.arange(8)[:, None]).astype(np.float32)
    return np.ascontiguousarray(cmat), np.ascontiguousarray(cf), np.ascontiguousarray(ind)


def _fm(v, c):
    return np.ascontiguousarray(np.asarray(v, np.float32).reshape(c, 128).T)


def _host_params(inp):
    vd = np.stack([np.stack([_fm(inp[k][l], 8) for k in ("pre_mix_norm", "post_mix_norm", "pre_ffn_norm", "post_ffn_norm", "pe_gate_norm")], 1) for l in range(L)], 1)
    vg = np.stack([np.stack([_fm(inp[k][l], 2) for k in ("conv_dw_b", "conv_ln_g", "conv_ln_b", "conv_pw_b", "out_norm_a", "out_norm_b", "out_norm_c")], 1) for l in range(L)], 1)
    wdw = np.stack([np.asarray(inp["conv_dw_w"][l], np.float32).reshape(31, 2, 128).transpose(2, 1, 0) for l in range(L)], 1)
    fcw = np.stack([np.asarray(inp["ffn_conv_w"][l], np.float32).reshape(3, 44, 128).transpose(2, 1, 0) for l in range(L)], 1)
    fcb = np.stack([_fm(inp["ffn_conv_b"][l], 44) for l in range(L)], 1)
    gsub = np.stack([np.asarray(inp["diff_subln_g"][l], np.float32)[np.arange(128) % 64] for l in range(L)], 1)
    lamv = np.stack([np.stack([np.asarray(inp[k][l], np.float32) for k in ("diff_lq1", "diff_lk1", "diff_lq2", "diff_lk2")], 0) for l in range(L)], 0)
    lamv = np.broadcast_to(lamv.reshape(1, L * 4 * 32), (128, L * 4 * 32))
    pv = np.concatenate([vd.reshape(128, -1), vg.reshape(128, -1), wdw.reshape(128, -1), fcw.reshape(128, -1), fcb.reshape(128, -1), gsub.reshape(128, -1), lamv], axis=1)
    glnb = np.stack([np.stack([np.broadcast_to(np.asarray(inp[k][l], np.float32)[None, :], (128, 256)) for k in ("gmlp_ln_g", "gmlp_ln_b")], 1) for l in range(L)], 0)
    bs = np.asarray(inp["gmlp_bs"], np.float32)
    gb = np.zeros((L, 2, 128, 512), np.float32)
    for l in range(L):
        for cc in range(2):
            for hh in range(2):
                gb[l, cc, hh * 64:(hh + 1) * 64, :] = np.tile(bs[l, 2 * cc + hh], 4)[None, :]
    wsT = np.ascontiguousarray(np.asarray(inp["gmlp_ws"], np.float32).transpose(0, 1, 3, 2))
    return np.ascontiguousarray(pv.astype(np.float32)), np.ascontiguousarray(glnb), gb, wsT


PV_OFF = {}


def _pv_layout():
    o = 0
    for name, n in (("vd", L * 5 * 8), ("vg", L * 7 * 2), ("wdw", L * 2 * 31), ("fcw", L * 44 * 3), ("fcb", L * 44), ("gsub", L), ("lamv", L * 4 * 32)):
        PV_OFF[name] = o
        o += n
    return o


PV_N = _pv_layout()


def build(dbg=None, nlayers=L):
    dbg = dbg or []
    nc = bass.Bass("TRN2", target_bir_lowering=False)

    def din(name, shape, dt=F32):
        return nc.dram_tensor(name, list(shape), dt, kind="ExternalInput").ap()

    xT_d = din("xT", [D, SEQ])
    pT_d = din("pT", [L, GW, SEQ])
    pos_d = din("pos", [1, SEQ], I32)
    cmat_d = din("cmat", [128, 768])
    cf_d = din("cf", [128, 262])
    ind_d = din("ind", [8, SEQ])
    pv_d = din("pv", [128, PV_N])
    glnb_d = din("glnb", [L, 128, 2, 256])
    gb_d = din("gb", [L, 2, 128, 512])
    wsT_d = din("wsT", [L, 4, 128, 128])
    w_in_d = din("w_in", [L, D, 10 * GW])
    w_out_d = din("w_out", [L, D, D])
    w_up_d = din("w_up", [L, D, 2 * DFF])
    w_down_d = din("w_down", [L, DFF, D])
    w_gate_d = din("w_pe_gate", [L, D, D])
    w_proj_d = din("w_pe_proj", [L, GW, D])
    w_pw_d = din("conv_pw_w", [L, GW, GW])
    out_d = nc.dram_tensor("outT", [D, SEQ], F32, kind="ExternalOutput").ap()
    wup_s = nc.dram_tensor("wup_s", [L, NJ, 128, 8 * 2 * 128], BF16, kind="Internal").ap()
    wdn_s = nc.dram_tensor("wdn_s", [L, 8, 128, NJ * 128], BF16, kind="Internal").ap()
    dbg_d = {n: nc.dram_tensor("dbg_" + n, [128, w], F32, kind="ExternalOutput").ap() for n, w in dbg}

    es = ExitStack()
    with es:
        S = Sched(nc, es)
        NB = 206 * 1024
        big = es.enter_context(nc.sbuf_tensor("arena", [128, NB // 4], F32))
        A = Arena(big, NB)
        psb = [es.enter_context(nc.psum_tensor("ps%d" % i, [128, 512], F32)) for i in range(8)]
        PS = [(psb[i][:, :], ("ps", i)) for i in range(8)]
        ringA = Ring(PS[0:4])
        ringB = Ring(PS[4:8])

        def MM(out, lhsT, rhs, start, stop, r, w, inc=True, tp=None):
            kw = {}
            if tp is not None:
                kw["tile_position"] = tp
            S.op("pe", lambda: nc.tensor.matmul(out, lhsT=lhsT, rhs=rhs, start=start, stop=stop, **kw), r=r, w=w, inc=inc)

        def ACT(out, in_, func, r, w, scale=None, bias=None):
            kw = {}
            if scale is not None:
                kw["scale"] = scale
            if bias is not None:
                kw["bias"] = bias
            S.op("act", lambda: nc.scalar.activation(out=out, in_=in_, func=func, **kw), r=r, w=w)

        def TTo(eng, out, in0, in1, op, r, w):
            e = nc.vector if eng == "dve" else nc.gpsimd
            S.op(eng, lambda: e.tensor_tensor(out=out, in0=in0, in1=in1, op=op), r=r, w=w)

        def TS(eng, out, in0, s1, s2, op0, op1, r, w):
            e = nc.vector if eng == "dve" else nc.gpsimd
            if op1 is None:
                S.op(eng, lambda: e.tensor_scalar(out=out, in0=in0, scalar1=s1, scalar2=None, op0=op0), r=r, w=w)
            else:
                S.op(eng, lambda: e.tensor_scalar(out=out, in0=in0, scalar1=s1, scalar2=s2, op0=op0, op1=op1), r=r, w=w)

        def STT(out, in0, scalar, in1, op0, op1, r, w):
            S.op("dve", lambda: nc.vector.scalar_tensor_tensor(out=out, in0=in0, scalar=scalar, in1=in1, op0=op0, op1=op1), r=r, w=w)

        def CP(eng, out, in_, r, w):
            e = nc.vector if eng == "dve" else nc.gpsimd
            S.op(eng, lambda: e.tensor_copy(out=out, in_=in_), r=r, w=w)

        def MSET(eng, ap, val, w):
            e = nc.vector if eng == "dve" else nc.gpsimd
            S.op(eng, lambda: e.memset(ap, val), w=w)

        def DMA(q, out, in_, semkey, r, w):
            e = dict(sp=nc.sync, pool=nc.gpsimd, act=nc.scalar)[q]
            S.dma(q, lambda: e.dma_start(out=out, in_=in_), semkey, r=r, w=w)

        def dump(name, ap, keys):
            if name in dbg_d:
                w_ = ap.shape[-1]
                stg = A.f32(w_)
                CP("dve", stg[0:ap.shape[0], :], ap, r=keys, w=[("dbgs", name)])
                DMA("sp", dbg_d[name][0:ap.shape[0], 0:w_], stg[0:ap.shape[0], :], "dbg", r=[("dbgs", name)], w=[("dbgo", name)])

        xT = A.f32(8 * SEQ).rearrange("p (c s) -> p c s", c=8)
        hT = A.bf16(8 * SEQ).rearrange("p (c s) -> p c s", c=8)
        cmat = A.bf16(768)
        IDENT, ONES, BLK2, R64, R32, TRI = [cmat[:, i * 128:(i + 1) * 128] for i in range(6)]
        cf = A.f32(262)
        TRIL = cf[:, 0:128]
        PMASK = cf[:, 128:256].rearrange("p (q n) -> p q n", n=8)
        INVF = [cf[:, 256:257], cf[:, 257:258]]
        GMASK = cf[:, 258:262]
        pv = A.f32(PV_N)

        def PVv(name, n):
            return pv[:, PV_OFF[name]:PV_OFF[name] + n]

        vd = PVv("vd", L * 5 * 8).rearrange("p (l v c) -> p l v c", l=L, v=5)
        vg = PVv("vg", L * 7 * 2).rearrange("p (l v c) -> p l v c", l=L, v=7)
        wdw = PVv("wdw", L * 2 * 31).rearrange("p (l c k) -> p l c k", l=L, c=2)
        fcw = PVv("fcw", L * 44 * 3).rearrange("p (l j k) -> p l j k", l=L, j=44)
        fcb = PVv("fcb", L * 44).rearrange("p (l j) -> p l j", l=L)
        gsub = PVv("gsub", L)
        lamv = PVv("lamv", L * 4 * 32).rearrange("p (l v k) -> p l v k", l=L, v=4)
        small = A.f32(64)
        lnv = A.f32(512)
        rstd = A.f32(512)
        sq = [A.bf16(512) for _ in range(3)]
        sqring = Ring([(sq[i], ("sq", i)) for i in range(3)])
        PH = A.mark()

        DMA("pool", cmat, cmat_d[:, :], "ld_c", r=[], w=["cmat"])
        DMA("sp", cf, cf_d[:, :], "ld_c2", r=[], w=["cf"])
        DMA("sp", pv, pv_d[:, :], "ld_c3", r=[], w=["pv"])
        for t in range(NT):
            DMA("sp", xT[:, :, t * TW:(t + 1) * TW], xT_d.rearrange("(c p) s -> p c s", p=128)[:, :, t * TW:(t + 1) * TW], ("ld_x", t), r=[], w=[("xT", c, t) for c in range(8)])

        def wload(dst, src, key):
            DMA("pool", dst, src, ("wsem",) + key, r=[], w=[key])

        def precast_ffn(l):
            wupv_ = w_up_d[l].rearrange("(k p) (g n) -> p k g n", p=128, g=2)
            wdnv_ = w_down_d[l].rearrange("(j p) n -> p j n", p=128)
            sk = ("pcs", l)
            keys = []
            for j in range(NJ):
                for gi_ in range(2):
                    dst = wup_s[l, j].rearrange("p (k g n) -> p k g n", k=8, g=2)[:, :, gi_, :]
                    DMA("pool", dst, wupv_[:, :, gi_, j * 128:(j + 1) * 128], sk, r=[], w=[("pcu", l, j, gi_)])
                    keys.append(("pcu", l, j, gi_))
            for dc in range(8):
                dst = wdn_s[l, dc].rearrange("p (j n) -> p j n", j=NJ)
                DMA("pool", dst, wdnv_[:, :, dc * 128:(dc + 1) * 128], sk, r=[], w=[("pcd", l, dc)])
                keys.append(("pcd", l, dc))
            tot = S.cnt[sk]
            for k in keys:
                S.state[k] = [(sk, tot), []]

        def rstd_from(ps_ap, ps_key, inv_n, eps, extra_r=()):
            ACT(lnv, ps_ap, AF.Ln, r=[ps_key] + list(extra_r), w=["lnv"], scale=inv_n, bias=eps)
            ACT(rstd, lnv, AF.Exp, r=["lnv"], w=["rstd"], scale=-0.5)

        def rmsnorm_tile(srcs, skeys, dsts, dkeys, gains, n_feat, eps=EPS, lhs=None, lkey="cmat"):
            ps, pk = ringB.next()
            C = len(srcs)
            for c in range(C):
                q, qk = sqring.next()
                ACT(q, srcs[c], AF.Square, r=[skeys[c]], w=[qk])
                MM(ps, lhs if lhs is not None else ONES, q, c == 0, c == C - 1, r=[qk, lkey], w=[pk])
            rstd_from(ps, pk, 1.0 / n_feat, eps)
            for c in range(C):
                STT(dsts[c], srcs[c], gains[c], rstd, ALU.mult, ALU.mult, r=[skeys[c], "rstd", "pv"], w=[dkeys[c]])

        def sl(t):
            return slice(t * TW, (t + 1) * TW)

        def norm_x_to_h(l, vidx):
            for t in range(NT):
                rmsnorm_tile([xT[:, c, sl(t)] for c in range(8)], [("xT", c, t) for c in range(8)],
                             [hT[:, c, sl(t)] for c in range(8)], [("hT", c, t) for c in range(8)],
                             [vd[:, l, vidx, c:c + 1] for c in range(8)], D)

        def proj(ps, pk, wv, wkey, cs, rhs_fn, rkeys_fn, nk=8):
            for kc in range(nk):
                MM(ps, wv[:, kc, cs], rhs_fn(kc), kc == 0, kc == nk - 1, r=[wkey] + rkeys_fn(kc), w=[pk], inc=(kc == nk - 1))

        def hrhs(t):
            return (lambda kc: hT[:, kc, sl(t)]), (lambda kc: [("hT", kc, t)])

        for l in range(nlayers):
            S.barrier()
            A.release(PH)
            catF = A.bf16(8 * SEQ)
            catT = catF.rearrange("p (c s) -> p c s", c=8)
            ropeC = A.bf16(SEQ)
            ropeS = A.bf16(SEQ)
            wslots = [A.bf16(8 * 256).rearrange("p (k n) -> p k n", k=8) for _ in range(4)]
            MX = A.mark()
            winv = w_in_d[l].rearrange("(k p) n -> p k n", p=128)
            woutv = w_out_d[l].rearrange("(k p) n -> p k n", p=128)
            mplan = [(lambda v, k, bi=bi: wload(v, winv[:, :, bi * 256:(bi + 1) * 256], k)) for bi in (0, 1, 2, 7, 8, 9, 3, 4, 5, 6)]
            mplan += [(lambda v, k, db=db: wload(v, woutv[:, :, db * 256:(db + 1) * 256], k)) for _t in range(NT) for db in range(4)]
            mstream = WStream([(wslots[i], ("ws", i)) for i in range(4)], mplan, auto=False)

            def win_block(bi):
                return mstream.get()

            def rope_tables(which):
                posi = catF[:, 2 * SEQ:4 * SEQ].bitcast(I32)
                v = catF[:, 4 * SEQ:6 * SEQ].bitcast(F32)
                ki = catF[:, 6 * SEQ:8 * SEQ].bitcast(I32)
                DMA("sp", posi, pos_d[0:1, :].broadcast_to([128, SEQ]), "ld_pos", r=[], w=["posi"])
                for tab, shift, key in ((ropeS, 0.0, "ropeS"), (ropeC, 0.25, "ropeC")):
                    TS("dve", v, posi, INVF[which], shift, ALU.mult, ALU.add, r=["posi", "cf"], w=["ropev"])
                    CP("dve", ki, v, r=["ropev"], w=["ropek"])
                    TTo("dve", v, v, ki, ALU.subtract, r=["ropev", "ropek"], w=["ropev"])
                    ACT(tab, v, AF.Sin, r=["ropev"], w=[key], scale=2 * math.pi * (1 - 1e-6))

            def rope_apply(ps, pk, t, RM, zbr, t1, t2):
                zb, zk = zbr.next()
                ACT(zb, ps, AF.Copy, r=[pk], w=[zk])
                ps2, pk2 = ringA.next()
                MM(ps2, RM, zb, True, True, r=[zk, "cmat"], w=[pk2])
                TTo("dve", t1[0], zb, ropeC[:, sl(t)], ALU.mult, r=[zk, "ropeC"], w=[t1[1]])
                TTo("dve", t2[0], ps2, ropeS[:, sl(t)], ALU.mult, r=[pk2, "ropeS"], w=[t2[1]])

            def attn_finalize_recip(O, ok, dst_rden):
                S.op("dve", lambda: nc.vector.reciprocal(out=dst_rden[0][0:64, :], in_=O[64:128, :]), r=[ok], w=[dst_rden[1]])

            def vproj(wv, wk, cs, vaug):
                for g in range(4):
                    ps, pk = ringA.next()
                    for s4 in range(4):
                        st = g * 4 + s4
                        for kc in range(8):
                            MM(ps[:, s4 * 128:(s4 + 1) * 128], hT[:, kc, st * 128:(st + 1) * 128], wv[:, kc, cs], kc == 0, kc == 7,
                               r=[wk, ("hT", kc, st // 4)], w=[pk], inc=(kc == 7 and s4 == 3))
                    ACT(vaug[:, g * 4:(g + 1) * 4, :, 0:64], ps.rearrange("p (s h d) -> p s h d", s=4, h=2), AF.Copy, r=[pk], w=[("V", g)])

            rope_tables(0)
            norm_x_to_h(l, 0)
            if l == 0:
                dump("hT0", hT[:, 0, 0:512], [("hT", 0, 0)])

            m0 = A.mark()
            Kaug = [A.bf16(SEQ) for _ in range(2)]
            vaug = A.bf16(16 * 2 * 128).rearrange("p (s h d) -> p s h d", s=16, h=2)
            Qa = [[A.bf16(512) for _ in range(2)] for _ in range(NT)]
            Pr = Ring([(A.bf16(512), ("P", i)) for i in range(4)])
            zbr = Ring([(A.bf16(512), ("zb", i)) for i in range(2)])
            t1 = (A.f32(512), "t1")
            t2 = (A.f32(512), "t2")
            rden = (A.f32(512), "rden")
            bstg4 = A.bf16(4 * 72).rearrange("p (j n) -> p j n", j=4)
            gm = A.f32(64)
            top8 = A.f32(32)
            kmf = A.f32(16)
            kmb = A.bf16(16)
            MSET("dve", vaug[:, :, :, 64:128], 1.0, w=[("V", g) for g in range(4)])
            MSET("dve", bstg4, 0.0, w=["bstg"])
            for hh in range(2):
                DMA("pool", Kaug[hh][64:72, :], ind_d[:, :], "ld_ind", r=[], w=[("Kind", hh)])
            wq, wqk = win_block(0)
            wk_, wkk = win_block(1)
            wv_, wvk = win_block(2)
            precast_ffn(l)
            for c in range(2):
                cs = slice(c * 128, (c + 1) * 128)
                for t in range(NT):
                    ps, pk = ringA.next()
                    rf, kf = hrhs(t)
                    proj(ps, pk, wk_, wkk, cs, rf, kf)
                    rope_apply(ps, pk, t, R64, zbr, t1, t2)
                    for hh in range(2):
                        TTo("dve", Kaug[hh][0:64, sl(t)], t1[0][hh * 64:(hh + 1) * 64, :], t2[0][hh * 64:(hh + 1) * 64, :], ALU.add,
                            r=[t1[1], t2[1]], w=[("K", hh, t)])
                for hh in range(2):
                    S.op("dve", lambda hh=hh: nc.vector.tensor_reduce(out=kmf[0:64, hh * 8:(hh + 1) * 8], in_=Kaug[hh][0:64, :].rearrange("p (n k) -> p n k", k=256), axis=AX.X, op=ALU.add),
                         r=[("K", hh, t) for t in range(NT)], w=[("kmf", hh)])
                    TS("dve", kmb[0:64, hh * 8:(hh + 1) * 8], kmf[0:64, hh * 8:(hh + 1) * 8], 1.0 / 256, None, ALU.mult, None, r=[("kmf", hh)], w=[("kmb", hh)])
                vproj(wv_, wvk, cs, vaug)
                for qt in range(NT):
                    qb = Qa[qt]
                    ps, pk = ringA.next()
                    rf, kf = hrhs(qt)
                    proj(ps, pk, wq, wqk, cs, rf, kf)
                    rope_apply(ps, pk, qt, R64, zbr, t1, t2)
                    for hh in range(2):
                        TTo("dve", qb[hh][0:64, :], t1[0][hh * 64:(hh + 1) * 64, :], t2[0][hh * 64:(hh + 1) * 64, :], ALU.add,
                            r=[t1[1], t2[1]], w=[("Qa", qt, hh)])
                    for hh in range(2):
                        gps, gk = ringA.next()
                        for j in range(4):
                            MM(gps[:, j * 8:(j + 1) * 8], qb[hh][0:64, j * 128:(j + 1) * 128], kmb[0:64, hh * 8:(hh + 1) * 8], True, True,
                               r=[("Qa", qt, hh), ("kmb", hh)], w=[gk], inc=(j == 3))
                        gmv = gm[:, 0:32].rearrange("p (j n) -> p j n", n=8)
                        TTo("dve", gmv, gps[:, 0:32].rearrange("p (j n) -> p j n", n=8), PMASK[:, qt * 4:(qt + 1) * 4, :], ALU.add, r=[gk, "cf"], w=["gm"])
                        tps, tk = ringA.next()
                        tpsb = tps.bitcast(BF16)
                        top8v = top8.rearrange("p (j n) -> p j n", n=8)
                        for j in range(4):
                            S.op("dve", lambda j=j: nc.vector.max(out=top8[:, j * 8:(j + 1) * 8], in_=gm[:, j * 8:(j + 1) * 8]), r=["gm"], w=["top8"])
                        TS("dve", top8v[:, :, 3:4], top8v[:, :, 3:4], -BIGG, None, ALU.max, None, r=["top8"], w=["top8"])
                        TTo("dve", bstg4[:, :, 64:72], gmv, top8v[:, :, 3:4].broadcast_to([128, 4, 8]), ALU.is_lt, r=["gm", "top8"], w=["bstg"])
                        for j in range(4):
                            S.op("pe", lambda j=j, tpsb=tpsb: nc.tensor.transpose(tpsb[0:72, j * 128:(j + 1) * 128], bstg4[:, j, :], IDENT), r=["bstg", "cmat"], w=[tk], inc=(j == 3))
                        ACT(qb[hh][64:72, :], tpsb[64:72, 0:512], AF.Copy, r=[tk], w=[("Qb", qt, hh)])
                if c == 1:
                    rope_tables(1)
                defer = []
                for qt in range(NT):
                    qb = Qa[qt]
                    for hh in range(2):
                        O, ok = ringB.next()
                        nk = 4 * qt + 4
                        pend = []
                        for kt in range(nk):
                            jd = kt - 4 * qt
                            c0 = 128 * jd if jd > 0 else 0
                            sp_, sk = ringA.next()
                            MM(sp_[:, c0:512], Kaug[hh][0:72, kt * 128:(kt + 1) * 128], qb[hh][0:72, c0:512], True, jd < 0,
                               r=[("K", hh, kt // 4), ("Kind", hh), ("Qa", qt, hh), ("Qb", qt, hh)], w=[sk], inc=(jd < 0))
                            if jd >= 0:
                                MM(sp_[:, c0:c0 + 128], IDENT, TRI, False, True, r=["cmat"], w=[sk])
                            P, Pk = Pr.next()
                            ACT(P[:, c0:512], sp_[:, c0:512], AF.Exp, r=[sk], w=[Pk], scale=0.125)

                            def pv_(kt=kt, c0=c0, P=P, Pk=Pk, O=O, ok=ok, hh=hh, nk=nk):
                                MM(O[:, c0:512], vaug[:, kt, hh, :], P[:, c0:512], kt == 0, kt == nk - 1, r=[Pk, ("V", kt // 4)], w=[ok])
                            pend.append(pv_)
                            if len(pend) > 2:
                                pend.pop(0)()
                            if kt == 1:
                                for f in defer:
                                    f()
                                defer = []
                        for f in pend:
                            f()
                        def fin_(O=O, ok=ok, hh=hh, c=c, qt=qt):
                            attn_finalize_recip(O, ok, rden)
                            TTo("dve", catT[hh * 64:(hh + 1) * 64, c, sl(qt)], O[0:64, :], rden[0][0:64, :], ALU.mult, r=[ok, rden[1]], w=[("cat", c, qt)])
                        defer.append(fin_)
                for f in defer:
                    f()
                defer = []
            if l == 0:
                dump("oa_pre", catT[:, 0, 0:512], [("cat", 0, 0)])
            for t in range(NT):
                rmsnorm_tile([catT[:, c, sl(t)] for c in range(2)], [("cat", c, t) for c in range(2)],
                             [catT[:, c, sl(t)] for c in range(2)], [("cat", c, t) for c in range(2)],
                             [vg[:, l, 4, c:c + 1] for c in range(2)], GW)
            if l == 0:
                dump("oa", catT[:, 0, 0:512], [("cat", 0, 0)])
            mstream.done()
            S.barrier()
            A.release(m0)

            lam_init = 0.8 - 0.6 * math.exp(-0.3 * l)
            lt = small[:, 0:8]
            prod = A.f32(64)
            TTo("dve", prod[:, 0:32], lamv[:, l, 0, :], lamv[:, l, 1, :], ALU.mult, r=["pv"], w=["prod"])
            S.op("dve", lambda: nc.vector.tensor_reduce(out=lt[:, 0:1], in_=prod[:, 0:32], axis=AX.X, op=ALU.add), r=["prod"], w=["lt"])
            TTo("dve", prod[:, 32:64], lamv[:, l, 2, :], lamv[:, l, 3, :], ALU.mult, r=["pv"], w=["prod2"])
            S.op("dve", lambda: nc.vector.tensor_reduce(out=lt[:, 1:2], in_=prod[:, 32:64], axis=AX.X, op=ALU.add), r=["prod2"], w=["lt"])
            ACT(lt[:, 2:4], lt[:, 0:2], AF.Exp, r=["lt"], w=["lt"])
            TTo("dve", lt[:, 4:5], lt[:, 3:4], lt[:, 2:3], ALU.subtract, r=["lt"], w=["lt"])
            TS("dve", lt[:, 5:6], lt[:, 4:5], -lam_init, None, ALU.add, None, r=["lt"], w=["lt"])
            TS("dve", lt[:, 6:7], gsub[:, l:l + 1], 1.0 - lam_init, None, ALU.mult, None, r=["pv", "lt"], w=["lt"])
            NEGLAM = lt[:, 5:6]
            GS = lt[:, 6:7]

            m0 = A.mark()
            Kd = A.bf16(SEQ)
            vaug = A.bf16(16 * 2 * 128).rearrange("p (s h d) -> p s h d", s=16, h=2)
            Qd = [A.bf16(512) for _ in range(NT)]
            Pr = Ring([(A.bf16(512), ("P", i)) for i in range(4)])
            zbr = Ring([(A.bf16(512), ("zb", i)) for i in range(2)])
            t1 = (A.f32(512), "t1")
            t2 = (A.f32(512), "t2")
            rden = (A.f32(512), "rden")
            ods = [A.f32(512) for _ in range(2)]
            Qm = [[A.bf16(512) for _ in range(4)] for _ in range(2)]
            MSET("dve", vaug[:, :, :, 64:128], 1.0, w=[("V", g) for g in range(4)])
            wq, wqk = win_block(7)
            wk_, wkk = win_block(8)
            wv_, wvk = win_block(9)
            dscale = 32.0 ** -0.5
            for c in range(2):
                cs = slice(c * 128, (c + 1) * 128)
                for t in range(NT):
                    ps, pk = ringA.next()
                    rf, kf = hrhs(t)
                    proj(ps, pk, wk_, wkk, cs, rf, kf)
                    rope_apply(ps, pk, t, R32, zbr, t1, t2)
                    TTo("dve", Kd[:, sl(t)], t1[0], t2[0], ALU.add, r=[t1[1], t2[1]], w=[("Kd", t)])
                vproj(wv_, wvk, cs, vaug)
                for qt in range(NT):
                    qd = Qd[qt]
                    ps, pk = ringA.next()
                    rf, kf = hrhs(qt)
                    proj(ps, pk, wq, wqk, cs, rf, kf)
                    rope_apply(ps, pk, qt, R32, zbr, t1, t2)
                    TTo("dve", qd, t1[0], t2[0], ALU.add, r=[t1[1], t2[1]], w=[("Qd", qt)])
                defer = []
                defer2 = []

                def qmask(qt):
                    for g in range(4):
                        TS("dve", Qm[qt % 2][g], Qd[qt], GMASK[:, g:g + 1], None, ALU.mult, None, r=[("Qd", qt), "cf"], w=[("Qm", qt % 2, g)])
                qmask(0)
                for qt in range(NT):
                    qd = Qd[qt]
                    od = ods[qt % 2]
                    for hh in range(2):
                        Os = [ringB.next(), ringB.next()]
                        nk = 4 * qt + 4
                        pend = []
                        for kt in range(nk):
                            jd = kt - 4 * qt
                            c0 = 128 * jd if jd > 0 else 0
                            cur = []
                            for m_ in range(2):
                                g = hh * 2 + m_
                                sp_, sk = ringA.next()
                                MM(sp_[:, c0:512], Kd[:, kt * 128:(kt + 1) * 128], Qm[qt % 2][g][:, c0:512], True, jd < 0,
                                   r=[("Kd", kt // 4), ("Qm", qt % 2, g)], w=[sk], inc=(jd < 0))
                                if jd >= 0:
                                    MM(sp_[:, c0:c0 + 128], IDENT, TRI, False, True, r=["cmat"], w=[sk])
                                cur.append((sp_, sk))
                            for f in pend:
                                f()
                            pend = []
                            if kt == 1:
                                for f in defer:
                                    f()
                                defer = defer2
                                defer2 = []
                            for m_ in range(2):
                                sp_, sk = cur[m_]
                                P, Pk = Pr.next()
                                ACT(P[:, c0:512], sp_[:, c0:512], AF.Exp, r=[sk], w=[Pk], scale=dscale)

                                def pv_(kt=kt, c0=c0, P=P, Pk=Pk, Oo=Os[m_], hh=hh, nk=nk):
                                    MM(Oo[0][:, c0:512], vaug[:, kt, hh, :], P[:, c0:512], kt == 0, kt == nk - 1, r=[Pk, ("V", kt // 4)], w=[Oo[1]])
                                pend.append(pv_)
                        for f in pend:
                            f()
                        def fin_(Os=Os, hh=hh, qt=qt, od=od):
                            hs = slice(hh * 64, (hh + 1) * 64)
                            attn_finalize_recip(Os[0][0], Os[0][1], rden)
                            TTo("dve", t1[0][0:64, :], Os[0][0][0:64, :], rden[0][0:64, :], ALU.mult, r=[Os[0][1], rden[1]], w=[t1[1]])
                            attn_finalize_recip(Os[1][0], Os[1][1], (lnv, "lnv"))
                            TTo("dve", t2[0][0:64, :], Os[1][0][0:64, :], lnv[0:64, :], ALU.mult, r=[Os[1][1], "lnv"], w=[t2[1]])
                            STT(od[hs, :], t2[0][0:64, :], NEGLAM[0:64, :], t1[0][0:64, :], ALU.mult, ALU.add, r=[t1[1], t2[1], "lt"], w=[("od", qt % 2, hh)])
                        defer.append(fin_)
                        if hh == 0 and qt + 1 < NT:
                            qmask(qt + 1)
                    def norm_(qt=qt, od=od, c=c):
                        ps, pk = ringA.next()
                        q_, qk_ = sqring.next()
                        ACT(q_, od, AF.Square, r=[("od", qt % 2, 0), ("od", qt % 2, 1)], w=[qk_])
                        MM(ps, BLK2, q_, True, True, r=[qk_, "cmat"], w=[pk])
                        rstd_from(ps, pk, 1.0 / 64, 1e-5)
                        STT(catT[:, 6 + c, sl(qt)], od, GS, rstd, ALU.mult, ALU.mult, r=[("od", qt % 2, 0), ("od", qt % 2, 1), "rstd", "lt"], w=[("cat", 6 + c, qt)])
                    defer2.append(norm_)
                for f in defer + defer2:
                    f()
                defer = []
                defer2 = []
            if l == 0:
                dump("od", catT[:, 6, 0:512], [("cat", 6, 0)])
            mstream.done()
            S.barrier()
            A.release(m0)

            m0 = A.mark()
            uT = A.bf16(2 * SEQ).rearrange("p (c s) -> p c s", c=2)
            vgl = A.bf16(16 * 256).rearrange("p (n d) -> p n d", n=16)
            glnb = A.f32(512).rearrange("p (v d) -> p v d", v=2)
            gb = A.f32(1024).rearrange("p (c s) -> p c s", c=2)
            wsf = A.f32(512).rearrange("p (h i) -> p h i", h=4)
            wsb = A.bf16(512).rearrange("p (h i) -> p h i", h=4)
            stats = A.f32(16 * 6)
            mv = A.f32(16 * 2).rearrange("p (n k) -> p n k", k=2)
            rsd = A.f32(16)
            vtmp = A.f32(256)
            vln = [A.bf16(256) for _ in range(2)]
            stmp = A.f32(512)
            DMA("sp", glnb, glnb_d[l], "ld_g1", r=[], w=["glnb"])
            DMA("sp", gb, gb_d[l].rearrange("c p s -> p c s"), "ld_g2", r=[], w=["gb"])
            DMA("sp", wsf, wsT_d[l].rearrange("h j i -> j h i"), "ld_g3", r=[], w=["wsf"])
            for h in range(4):
                TTo("dve", wsb[:, h, :], wsf[:, h, :], TRIL, ALU.mult, r=["wsf", "cf"], w=["wsb"])
            wu, wuk = win_block(3)
            wvv, wvvk = win_block(4)
            for t in range(NT):
                for cc in range(2):
                    ps, pk = ringA.next()
                    rf, kf = hrhs(t)
                    proj(ps, pk, wu, wuk, slice(cc * 128, (cc + 1) * 128), rf, kf)
                    ACT(uT[:, cc, sl(t)], ps, AF.Gelu_apprx_tanh, r=[pk], w=[("uT", cc, t)])
            for n2 in range(8):
                ps, pk = ringA.next()
                for s2 in range(2):
                    n = n2 * 2 + s2
                    for kc in range(8):
                        MM(ps[:, s2 * 256:(s2 + 1) * 256], hT[:, kc, n * 128:(n + 1) * 128], wvv[:, kc, :], kc == 0, kc == 7,
                           r=[wvvk, ("hT", kc, n // 4)], w=[pk], inc=(kc == 7 and s2 == 1))
                ACT(vgl[:, n2 * 2:(n2 + 1) * 2, :], ps.rearrange("p (s d) -> p s d", s=2), AF.Gelu_apprx_tanh, r=[pk], w=[("vgl", n2)])
            for n in range(16):
                S.op("dve", lambda n=n: nc.vector.bn_stats(out=stats[:, n * 6:(n + 1) * 6], in_=vgl[:, n, :]), r=[("vgl", n // 2)], w=[("st", n)])
                S.op("dve", lambda n=n: nc.vector.bn_aggr(out=mv[:, n, :], in_=stats[:, n * 6:(n + 1) * 6]), r=[("st", n)], w=["mv"])
            ACT(rsd, mv[:, :, 1], AF.Ln, r=["mv"], w=["rsd"], bias=EPS)
            ACT(rsd, rsd, AF.Exp, r=["rsd"], w=["rsd"], scale=-0.5)
            for t in range(NT):
                pss = [ringA.next(), ringA.next()]
                for s4 in range(4):
                    n = t * 4 + s4
                    vl = vln[n % 2]
                    vk = ("vln", n % 2)
                    TS("dve", vtmp, vgl[:, n, :], mv[:, n, 0:1], rsd[:, n:n + 1], ALU.subtract, ALU.mult, r=[("vgl", n // 2), "mv", "rsd"], w=["vtmp"])
                    TTo("dve", vtmp, vtmp, glnb[:, 0, :], ALU.mult, r=["vtmp", "glnb"], w=["vtmp"])
                    TTo("dve", vl, vtmp, glnb[:, 1, :], ALU.add, r=["vtmp", "glnb"], w=[vk])
                    for cc in range(2):
                        for hh in range(2):
                            h = 2 * cc + hh
                            MM(pss[cc][0][hh * 64:(hh + 1) * 64, s4 * 128:(s4 + 1) * 128], vl[:, h * 64:(h + 1) * 64], wsb[:, h, :], True, True,
                               r=[vk, "wsb"], w=[pss[cc][1]], tp=(0, hh * 64))
                for cc in range(2):
                    TTo("dve", stmp, pss[cc][0], gb[:, cc, :], ALU.add, r=[pss[cc][1], "gb"], w=["stmp"])
                    TTo("dve", catT[:, 2 + cc, sl(t)], stmp, uT[:, cc, sl(t)], ALU.mult, r=["stmp", ("uT", cc, t)], w=[("cat", 2 + cc, t)])
            if l == 0:
                dump("ob_pre", catT[:, 2, 0:512], [("cat", 2, 0)])
            for t in range(NT):
                rmsnorm_tile([catT[:, 2 + c, sl(t)] for c in range(2)], [("cat", 2 + c, t) for c in range(2)],
                             [catT[:, 2 + c, sl(t)] for c in range(2)], [("cat", 2 + c, t) for c in range(2)],
                             [vg[:, l, 5, c:c + 1] for c in range(2)], GW)
            mstream.done()
            S.barrier()
            A.release(m0)

            m0 = A.mark()
            ybuf = A.bf16(32 + SEQ)
            diag = A.bf16(31 * 128).rearrange("p (k n) -> p k n", k=31)
            cy = A.bf16(2 * SEQ).rearrange("p (c s) -> p c s", c=2)
            sg = A.f32(512)
            wpw = A.bf16(2 * 256).rearrange("p (k n) -> p k n", k=2)
            mstat = A.f32(512)
            m2 = A.f32(512)
            yn = A.f32(512)
            sil = A.bf16(1024).rearrange("p (c s) -> p c s", c=2)
            Y0 = 2
            wa, wak = win_block(5)
            wg_, wgk = win_block(6)
            wload(wpw, w_pw_d[l].rearrange("(k p) n -> p k n", p=128), ("wpw",))
            MSET("dve", ybuf[:, 0:32], 0.0, w=["ypad"])
            for cc in range(2):
                cs = slice(cc * 128, (cc + 1) * 128)
                for k in range(31):
                    TS("dve", diag[:, k, :], IDENT, wdw[:, l, cc, k:k + 1], None, ALU.mult, None, r=["cmat", "pv"], w=[("diag", k)])
                for t in range(NT):
                    pa, pak = ringA.next()
                    rf, kf = hrhs(t)
                    proj(pa, pak, wa, wak, cs, rf, kf)
                    pg, pgk = ringA.next()
                    proj(pg, pgk, wg_, wgk, cs, rf, kf)
                    ACT(sg, pg, AF.Sigmoid, r=[pgk], w=["sg"])
                    TTo("dve", ybuf[:, 32 + t * TW:32 + (t + 1) * TW], pa, sg, ALU.mult, r=[pak, "sg"], w=[("yb", t)])
                for t in range(NT):
                    ps, pk = ringB.next()
                    for k in range(31):
                        o = Y0 + t * TW + k
                        rk = ["ypad", ("diag", k), ("yb", t)] + ([("yb", t - 1)] if t > 0 else [])
                        MM(ps, diag[:, k, :], ybuf[:, o:o + TW], k == 0, k == 30, r=rk, w=[pk], inc=(k == 30))
                    ACT(cy[:, cc, sl(t)], ps, AF.Identity, r=[pk, "pv"], w=[("cy", cc, t)], bias=vg[:, l, 0, cc:cc + 1])
            if l == 0:
                dump("cy", cy[:, 0, 0:512], [("cy", 0, 0)])
            for t in range(NT):
                ps1, pk1 = ringB.next()
                ps2, pk2 = ringB.next()
                for cc in range(2):
                    MM(ps1, ONES, cy[:, cc, sl(t)], cc == 0, cc == 1, r=[("cy", cc, t), "cmat"], w=[pk1])
                for cc in range(2):
                    q_, qk_ = sqring.next()
                    ACT(q_, cy[:, cc, sl(t)], AF.Square, r=[("cy", cc, t)], w=[qk_])
                    MM(ps2, ONES, q_, cc == 0, cc == 1, r=[qk_, "cmat"], w=[pk2])
                TS("dve", mstat, ps1, 1.0 / GW, None, ALU.mult, None, r=[pk1], w=["mstat"])
                TTo("dve", m2, mstat, mstat, ALU.mult, r=["mstat"], w=["m2"])
                STT(m2, ps2, 1.0 / GW, m2, ALU.mult, ALU.subtract, r=[pk2, "m2"], w=["m2"])
                ACT(lnv, m2, AF.Ln, r=["m2"], w=["lnv"], bias=EPS)
                ACT(rstd, lnv, AF.Exp, r=["lnv"], w=["rstd"], scale=-0.5)
                for cc in range(2):
                    TTo("dve", yn, cy[:, cc, sl(t)], mstat, ALU.subtract, r=[("cy", cc, t), "mstat"], w=["yn"])
                    TTo("dve", yn, yn, rstd, ALU.mult, r=["yn", "rstd"], w=["yn"])
                    ACT(sil[:, cc, :], yn, AF.Silu, r=["yn", "pv"], w=[("sil", cc)], scale=vg[:, l, 1, cc:cc + 1], bias=vg[:, l, 2, cc:cc + 1])
                for co in range(2):
                    ps, pk = ringA.next()
                    for ci in range(2):
                        MM(ps, wpw[:, ci, co * 128:(co + 1) * 128], sil[:, ci, :], ci == 0, ci == 1, r=[("wpw",), ("sil", ci)], w=[pk])
                    ACT(catT[:, 4 + co, sl(t)], ps, AF.Identity, r=[pk, "pv"], w=[("cat", 4 + co, t)], bias=vg[:, l, 3, co:co + 1])
            if l == 0:
                dump("oc_pre", catT[:, 4, 0:512], [("cat", 4, 0)])
            for t in range(NT):
                rmsnorm_tile([catT[:, 4 + c, sl(t)] for c in range(2)], [("cat", 4 + c, t) for c in range(2)],
                             [catT[:, 4 + c, sl(t)] for c in range(2)], [("cat", 4 + c, t) for c in range(2)],
                             [vg[:, l, 6, c:c + 1] for c in range(2)], GW)
            mstream.done()
            S.barrier()
            A.release(m0)

            def out_proj_norm_res(nkc, wsrc, rhs_fn, rkeys_fn, vidx, wr, ytile):
                for t in range(NT):
                    for db in range(4):
                        wv, wk = wr.get()
                        for dd in range(2):
                            dc = db * 2 + dd
                            ps, pk = ringA.next()
                            for kc in range(nkc):
                                MM(ps, wv[:, kc, dd * 128:(dd + 1) * 128], rhs_fn(kc, t), kc == 0, kc == nkc - 1, r=[wk] + rkeys_fn(kc, t), w=[pk], inc=(kc == nkc - 1))
                            if dc % 2 == 0:
                                ACT(ytile[:, dc, :], ps, AF.Copy, r=[pk], w=[("yt", dc)])
                            else:
                                CP("dve", ytile[:, dc, :], ps, r=[pk], w=[("yt", dc)])
                        wr.done()
                    rmsnorm_tile([ytile[:, dc, :] for dc in range(8)], [("yt", dc) for dc in range(8)],
                                 [ytile[:, dc, :] for dc in range(8)], [("yt", dc) for dc in range(8)],
                                 [vd[:, l, vidx, dc:dc + 1] for dc in range(8)], D)
                    for dc in range(8):
                        TTo("dve", xT[:, dc, sl(t)], xT[:, dc, sl(t)], ytile[:, dc, :], ALU.add, r=[("xT", dc, t), ("yt", dc)], w=[("xT", dc, t)])

            m0 = A.mark()
            ytile = A.f32(8 * 512).rearrange("p (c s) -> p c s", c=8)
            out_proj_norm_res(8, w_out_d[l].rearrange("(k p) n -> p k n", p=128), lambda kc, t: catT[:, kc, sl(t)], lambda kc, t: [("cat", kc, t)], 1, mstream, ytile)
            if l == 0:
                dump("x1", xT[:, 0, 0:512], [("xT", 0, 0)])
            S.barrier()
            A.release(MX)
            A.release(PH)

            norm_x_to_h(l, 2)
            ytile = A.f32(8 * 512).rearrange("p (c s) -> p c s", c=8)
            fT = A.bf16(NJ * 512).rearrange("p (j s) -> p j s", j=NJ)
            ub = [[A.f32(2 + 512) for _ in range(2)] for _ in range(2)]
            halo = A.f32(44 * 2).rearrange("p (j k) -> p j k", k=2)
            tas = [A.f32(512) for _ in range(2)]
            tbs = [A.f32(512) for _ in range(2)]
            cgs = [A.f32(512) for _ in range(2)]
            cvs = [A.f32(512) for _ in range(2)]
            wups = [A.bf16(8 * 2 * 128).rearrange("p (k g n) -> p k g n", k=8, g=2) for _ in range(3)]
            wdns = [A.bf16(NJ * 128).rearrange("p (j n) -> p j n", j=NJ) for _ in range(3)]
            MSET("dve", halo, 0.0, w=["halo"])
            wupv = w_up_d[l].rearrange("(k p) (g n) -> p k g n", p=128, g=2)
            wdnv = w_down_d[l].rearrange("(j p) n -> p j n", p=128)

            def ld_up(v, k, j):
                DMA("sp", v.rearrange("p k g n -> p (k g n)"), wup_s[l, j], ("wsem",) + k, r=[("pcu", l, j, 0), ("pcu", l, j, 1)], w=[k])

            def ld_dn(v, k, dc):
                DMA("sp", v.rearrange("p j n -> p (j n)"), wdn_s[l, dc], ("wsem",) + k, r=[("pcd", l, dc)], w=[k])

            wur = WStream([(wups[i], ("wu", i)) for i in range(3)], [(lambda v, k, j=j: ld_up(v, k, j)) for _t in range(NT) for j in range(NJ)])
            wdr = WStream([(wdns[i], ("wd", i)) for i in range(3)], [(lambda v, k, dc=dc: ld_dn(v, k, dc)) for _t in range(NT) for dc in range(8)])
            wur.top()
            wdr.top()
            pend_f = None
            for t in range(NT):
                for j in range(NJ):
                    wv, wk = wur.get()
                    u = ub[j % 2]
                    b2 = j % 2
                    ta, tb, cg, cv = tas[b2], tbs[b2], cgs[b2], cvs[b2]
                    for gi in range(2):
                        jj = j + gi * NJ
                        ps, pk = ringA.next()
                        for kc in range(8):
                            MM(ps, wv[:, kc, gi, :], hT[:, kc, sl(t)], kc == 0, kc == 7, r=[wk, ("hT", kc, t)], w=[pk], inc=(kc == 7))
                        uk = ("ub", b2, gi)
                        ACT(u[gi][:, 2:514], ps, AF.Copy, r=[pk], w=[uk])
                        CP("dve", u[gi][:, 0:2], halo[:, jj, :], r=["halo", ("halo", jj)], w=[uk])
                        tmp = ta if gi == 0 else tb
                        tk_ = ("ta", b2) if gi == 0 else ("tb", b2)
                        ACT(tmp, ps, AF.Identity, r=[pk, "pv"], w=[tk_], scale=fcw[:, l, jj, 2:3], bias=fcb[:, l, jj:jj + 1])
                        ACT(halo[:, jj, :], ps[:, 510:512], AF.Copy, r=[pk], w=[("halo", jj)])
                        dst = cg if gi == 0 else cv
                        dk = ("cg", b2) if gi == 0 else ("cv", b2)
                        STT(tmp, u[gi][:, 1:513], fcw[:, l, jj, 1:2], tmp, ALU.mult, ALU.add, r=[uk, tk_, "pv"], w=[tk_])
                        STT(dst, u[gi][:, 0:512], fcw[:, l, jj, 0:1], tmp, ALU.mult, ALU.add, r=[uk, tk_, "pv"], w=[dk])
                    if pend_f is not None:
                        pend_f()

                    def fin(j=j, b2=b2, cg=cg, cv=cv):
                        ACT(cg, cg, AF.Gelu_apprx_tanh, r=[("cg", b2)], w=[("cg", b2)])
                        TTo("dve", fT[:, j, :], cg, cv, ALU.mult, r=[("cg", b2), ("cv", b2)], w=[("fT", j)])
                    pend_f = fin
                pend_f()
                pend_f = None
                if l == 0 and t == 0:
                    dump("fT", fT[:, 0, :], [("fT", 0)])
                for dc in range(8):
                    wv, wk = wdr.get()
                    ps, pk = ringB.next()
                    for j in range(NJ):
                        MM(ps, wv[:, j, :], fT[:, j, :], j == 0, j == NJ - 1, r=[wk, ("fT", j)], w=[pk], inc=(j == NJ - 1))
                    if dc % 2 == 0:
                        ACT(ytile[:, dc, :], ps, AF.Copy, r=[pk], w=[("yt", dc)])
                    else:
                        CP("dve", ytile[:, dc, :], ps, r=[pk], w=[("yt", dc)])
                rmsnorm_tile([ytile[:, dc, :] for dc in range(8)], [("yt", dc) for dc in range(8)],
                             [ytile[:, dc, :] for dc in range(8)], [("yt", dc) for dc in range(8)],
                             [vd[:, l, 3, dc:dc + 1] for dc in range(8)], D)
                for dc in range(8):
                    TTo("dve", xT[:, dc, sl(t)], xT[:, dc, sl(t)], ytile[:, dc, :], ALU.add, r=[("xT", dc, t), ("yt", dc)], w=[("xT", dc, t)])
            if l == 0:
                dump("x2", xT[:, 0, 0:512], [("xT", 0, 0)])
            S.barrier()
            A.release(PH)

            norm_x_to_h(l, 4)
            pTb = A.bf16(2 * SEQ).rearrange("p (c s) -> p c s", c=2)
            wgs = [A.bf16(8 * 256).rearrange("p (k n) -> p k n", k=8) for _ in range(3)]
            wpj = A.bf16(2 * D).rearrange("p (k n) -> p k n", k=2)
            sgt = [A.f32(512) for _ in range(2)]
            pjt = [A.f32(512) for _ in range(2)]
            wload(pTb, pT_d[l].rearrange("(k p) s -> p k s", p=128), ("pTb",))
            wload(wpj, w_proj_d[l].rearrange("(k p) n -> p k n", p=128), ("wpj",))
            wgv = w_gate_d[l].rearrange("(k p) n -> p k n", p=128)
            wgr = WStream([(wgs[i], ("wg", i)) for i in range(3)], [(lambda v, k, db=db: wload(v, wgv[:, :, db * 256:(db + 1) * 256], k)) for db in range(4)])
            wgr.top()
            it = 0
            for db in range(4):
                wv, wk = wgr.get()
                for dd in range(2):
                    dc = db * 2 + dd
                    for t in range(NT):
                        b = it % 2
                        it += 1
                        ps, pk = ringA.next()
                        rf, kf = hrhs(t)
                        proj(ps, pk, wv, wk, slice(dd * 128, (dd + 1) * 128), rf, kf)
                        ACT(sgt[b], ps, AF.Sigmoid, r=[pk], w=[("sgt", b)])
                        ps2, pk2 = ringB.next()
                        for kc in range(2):
                            MM(ps2, wpj[:, kc, dc * 128:(dc + 1) * 128], pTb[:, kc, sl(t)], kc == 0, kc == 1, r=[("wpj",), ("pTb",)], w=[pk2], inc=(kc == 1))
                        TTo("dve", pjt[b], ps2, sgt[b], ALU.mult, r=[pk2, ("sgt", b)], w=[("pjt", b)])
                        TTo("dve", xT[:, dc, sl(t)], xT[:, dc, sl(t)], pjt[b], ALU.add, r=[("xT", dc, t), ("pjt", b)], w=[("xT", dc, t)])
            if l == 0:
                dump("x3", xT[:, 0, 0:512], [("xT", 0, 0)])

        for c in range(8):
            DMA("sp", out_d[c * 128:(c + 1) * 128, :], xT[:, c, :], "st_out", r=[("xT", c, t) for t in range(NT)], w=[("out", c)])
        S.barrier()
        S.emit()
        build.arena_hi = A.hi
    return nc


_CACHE = {}


def _prep_inputs(inp):
    cmat, cf, ind = _host_consts()
    pv, glnb, gb, wsT = _host_params(inp)
    x = np.asarray(inp["x"], np.float32)
    p = np.asarray(inp["p"], np.float32)
    pos = np.asarray(inp["positions"], np.int32)
    shared = dict(cmat=cmat, cf=cf, ind=ind, pv=pv, glnb=glnb, gb=gb, wsT=wsT)
    for k in ("w_in", "w_out", "w_up", "w_down", "w_pe_gate", "w_pe_proj", "conv_pw_w"):
        shared[k] = np.ascontiguousarray(np.asarray(inp[k], np.float32))
    maps = []
    for b in range(8):
        m = dict(shared)
        m["xT"] = np.ascontiguousarray(x[b].T)
        m["pT"] = np.ascontiguousarray(p[:, b].transpose(0, 2, 1))
        m["pos"] = np.ascontiguousarray(pos[b][None, :])
        maps.append(m)
    return maps


def kernel(**inputs):
    if "nc" not in _CACHE:
        _CACHE["nc"] = build()
    nc = _CACHE["nc"]
    maps = _prep_inputs(inputs)
    res = run_bass_kernel_spmd(nc, maps, core_ids=list(range(8)))
    out = np.stack([np.asarray(r["outT"], np.float32).T for r in res.results], axis=0)
    return np.ascontiguousarray(out)
```

```python
import math
import numpy as np
from contextlib import ExitStack
import concourse.bass as bass
import concourse.mybir as mybir
from concourse.bass_utils import run_bass_kernel_spmd

F32 = mybir.dt.float32
BF16 = mybir.dt.bfloat16
I32 = mybir.dt.int32
AF = mybir.ActivationFunctionType
ALU = mybir.AluOpType
AX = mybir.AxisListType

L = 2
D = 1024
SEQ = 2048
GW = 256
DFF = 2816
NJ = DFF // 128
NT = 4
TW = 512
MASK = 30000.0
BIGG = 1.0e6
EPS = 1e-6
ENGS = ["pe", "act", "dve", "pool", "sp"]


class Sched:
    def __init__(self, nc, es, same_engine_sync=True):
        self.nc = nc
        self.es = es
        self.same = same_engine_sync
        self.prog = {e: [] for e in ENGS}
        self.cnt = {}
        self.sems = {}
        self.seen = {e: {} for e in ENGS}
        self.state = {}
        for e in ENGS:
            self._sem(e)

    def _sem(self, key):
        if key not in self.sems:
            self.sems[key] = self.es.enter_context(self.nc.semaphore("s_" + str(key)))
            self.cnt[key] = 0
        return self.sems[key]

    def _engobj(self, e):
        nc = self.nc
        return dict(pe=nc.tensor, act=nc.scalar, dve=nc.vector, pool=nc.gpsimd, sp=nc.sync)[e]

    def _deps(self, e, r, w):
        toks = {}
        for k in r:
            st = self.state.get(k)
            if st and st[0] is not None:
                t = st[0]
                toks[t[0]] = max(toks.get(t[0], 0), t[1])
        for k in w:
            st = self.state.get(k)
            if st:
                if st[0] is not None:
                    t = st[0]
                    toks[t[0]] = max(toks.get(t[0], 0), t[1])
                for t in st[1]:
                    toks[t[0]] = max(toks.get(t[0], 0), t[1])
        waits = []
        for sk, v in toks.items():
            if sk == e and (not self.same or e == "pe"):
                continue
            if self.seen[e].get(sk, 0) >= v:
                continue
            self.seen[e][sk] = v
            waits.append((sk, v))
        return waits

    def _record(self, tok, r, w):
        for k in r:
            st = self.state.setdefault(k, [None, []])
            st[1].append(tok)
        for k in w:
            self.state[k] = [tok, []]

    def op(self, e, fn, r=(), w=(), inc=True):
        waits = self._deps(e, r, w)
        tok = (e, self.cnt[e] + 1)
        if inc:
            self.cnt[e] += 1
        self._record(tok, r, w)
        self.prog[e].append((waits, fn, (e, 1) if inc else None))
        return tok

    def dma(self, q, fn, semkey, r=(), w=()):
        self._sem(semkey)
        waits = self._deps(q, r, w)
        self.cnt[semkey] += 16
        tok = (semkey, self.cnt[semkey])
        self._record(tok, r, w)
        self.prog[q].append((waits, fn, (semkey, 16)))
        return tok

    def barrier(self):
        for e in ENGS:
            waits = []
            for sk, v in self.cnt.items():
                if v == 0 or self.seen[e].get(sk, 0) >= v:
                    continue
                if sk == e and e == "pe":
                    continue
                self.seen[e][sk] = v
                waits.append((sk, v))
            self.prog[e].append((waits, None, None))

    def simulate(self):
        val = {k: 0 for k in self.sems}
        pc = {e: 0 for e in ENGS}
        progress = True
        while progress:
            progress = False
            for e in ENGS:
                while pc[e] < len(self.prog[e]):
                    waits, fn, inc = self.prog[e][pc[e]]
                    if any(val[sk] < v for sk, v in waits):
                        break
                    if inc is not None:
                        val[inc[0]] += inc[1]
                    pc[e] += 1
                    progress = True
        stuck = {e: (pc[e], len(self.prog[e])) for e in ENGS if pc[e] < len(self.prog[e])}
        if stuck:
            msg = []
            for e, (i, n) in stuck.items():
                waits = self.prog[e][i][0]
                msg.append("%s@%d/%d waits %s" % (e, i, n, [(sk, v, val[sk]) for sk, v in waits if val[sk] < v]))
            raise RuntimeError("DEADLOCK in schedule: " + "; ".join(msg))
        for k in self.sems:
            assert val[k] == self.cnt[k], (k, val[k], self.cnt[k])

    def emit(self):
        self.simulate()
        nc = self.nc
        with nc.Block() as block:
            def run(e):
                eng = self._engobj(e)
                for waits, fn, inc in self.prog[e]:
                    for sk, v in waits:
                        eng.wait_ge(self.sems[sk], v)
                    if fn is None:
                        continue
                    ins = fn()
                    if inc is not None:
                        ins.then_inc(self.sems[inc[0]], inc[1])

            @block.tensor
            def _(e):
                run("pe")

            @block.scalar
            def _(e):
                run("act")

            @block.vector
            def _(e):
                run("dve")

            @block.gpsimd
            def _(e):
                run("pool")

            @block.sync
            def _(e):
                run("sp")


class Arena:
    def __init__(self, t, nbytes):
        self.t = t
        self.n = nbytes
        self.off = 0
        self.hi = 0

    def mark(self):
        return self.off

    def release(self, m):
        self.off = m

    def _take(self, nbytes):
        nbytes = (nbytes + 31) // 32 * 32
        o = self.off
        self.off += nbytes
        self.hi = max(self.hi, self.off)
        assert self.off <= self.n, ("arena overflow", self.off, self.n)
        return o

    def f32(self, n):
        o = self._take(4 * n)
        return self.t[:, o // 4: o // 4 + n]

    def bf16(self, n):
        o = self._take(2 * n)
        return self.t[:, o // 4: o // 4 + (n + 1) // 2].bitcast(BF16)

    def i32(self, n):
        o = self._take(4 * n)
        return self.t[:, o // 4: o // 4 + n].bitcast(I32)


class Ring:
    def __init__(self, items):
        self.items = items
        self.i = 0

    def next(self):
        it = self.items[self.i % len(self.items)]
        self.i += 1
        return it


class WStream:
    def __init__(self, slots, plan, auto=True):
        self.slots = slots
        self.plan = plan
        self.auto = auto
        self.issued = 0
        self.used = 0
        self.freed = 0

    def top(self):
        while self.issued < len(self.plan) and self.issued - self.freed < len(self.slots):
            i = self.issued
            v, k = self.slots[i % len(self.slots)]
            self.plan[i](v, k)
            self.issued += 1

    def done(self):
        self.freed = self.used
        self.top()

    def get(self):
        i = self.used
        if self.auto:
            self.freed = i
        self.top()
        assert self.issued > i, "weight stream: block not issued (slots exhausted)"
        self.used += 1
        return self.slots[i % len(self.slots)]


def _host_consts():
    ident = np.eye(128, dtype=np.float32)
    ones = np.ones((128, 128), np.float32)
    blk2 = np.zeros((128, 128), np.float32)
    blk2[0:64, 0:64] = 1
    blk2[64:128, 64:128] = 1

    def rotT(block):
        h = block // 2
        m = np.zeros((128, 128), np.float32)
        for b0 in range(0, 128, block):
            for i in range(h):
                m[b0 + i + h, b0 + i] = -1.0
                m[b0 + i, b0 + i + h] = 1.0
        return m

    tri = np.where(np.arange(128)[None, :] >= np.arange(128)[:, None], 0.0, -MASK).astype(np.float32)
    cmat = np.concatenate([ident, ones, blk2, rotT(64), rotT(32), tri], axis=1)
    tril = (np.arange(128)[:, None] <= np.arange(128)[None, :]).astype(np.float32)
    pm = np.zeros((16, 8), np.float32)
    for qs in range(16):
        cur = qs // 2
        for n in range(8):
            pm[qs, n] = 0.0 if n < cur else (BIGG if n == cur else -2 * BIGG)
    pmask = np.broadcast_to(pm.reshape(1, 128), (128, 128))
    p = np.arange(128)
    invf64 = (10000.0 ** (-(2.0 * (p % 32)) / 64.0)) / (2 * math.pi)
    invf32 = (10000.0 ** (-(2.0 * (p % 16)) / 32.0)) / (2 * math.pi)
    gmask = (np.arange(128)[:, None] // 32 == np.arange(4)[None, :]).astype(np.float32)
    cf = np.concatenate([tril, pmask, invf64[:, None], invf32[:, None], gmask], axis=1).astype(np.float32)
    ind = (-MASK) * (np.arange(SEQ)[None, :] // 256 == np.arange(8)[:, None]).astype(np.float32)
    return np.ascontiguousarray(cmat), np.ascontiguousarray(cf), np.ascontiguousarray(ind)


def _fm(v, c):
    return np.ascontiguousarray(np.asarray(v, np.float32).reshape(c, 128).T)


def _host_params(inp):
    vd = np.stack([np.stack([_fm(inp[k][l], 8) for k in ("pre_mix_norm", "post_mix_norm", "pre_ffn_norm", "post_ffn_norm", "pe_gate_norm")], 1) for l in range(L)], 1)
    vg = np.stack([np.stack([_fm(inp[k][l], 2) for k in ("conv_dw_b", "conv_ln_g", "conv_ln_b", "conv_pw_b", "out_norm_a", "out_norm_b", "out_norm_c")], 1) for l in range(L)], 1)
    wdw = np.stack([np.asarray(inp["conv_dw_w"][l], np.float32).reshape(31, 2, 128).transpose(2, 1, 0) for l in range(L)], 1)
    fcw = np.stack([np.asarray(inp["ffn_conv_w"][l], np.float32).reshape(3, 44, 128).transpose(2, 1, 0) for l in range(L)], 1)
    fcb = np.stack([_fm(inp["ffn_conv_b"][l], 44) for l in range(L)], 1)
    gsub = np.stack([np.asarray(inp["diff_subln_g"][l], np.float32)[np.arange(128) % 64] for l in range(L)], 1)
    lamv = np.stack([np.stack([np.asarray(inp[k][l], np.float32) for k in ("diff_lq1", "diff_lk1", "diff_lq2", "diff_lk2")], 0) for l in range(L)], 0)
    lamv = np.broadcast_to(lamv.reshape(1, L * 4 * 32), (128, L * 4 * 32))
    pv = np.concatenate([vd.reshape(128, -1), vg.reshape(128, -1), wdw.reshape(128, -1), fcw.reshape(128, -1), fcb.reshape(128, -1), gsub.reshape(128, -1), lamv], axis=1)
    glnb = np.stack([np.stack([np.broadcast_to(np.asarray(inp[k][l], np.float32)[None, :], (128, 256)) for k in ("gmlp_ln_g", "gmlp_ln_b")], 1) for l in range(L)], 0)
    bs = np.asarray(inp["gmlp_bs"], np.float32)
    gb = np.zeros((L, 2, 128, 512), np.float32)
    for l in range(L):
        for cc in range(2):
            for hh in range(2):
                gb[l, cc, hh * 64:(hh + 1) * 64, :] = np.tile(bs[l, 2 * cc + hh], 4)[None, :]
    wsT = np.ascontiguousarray(np.asarray(inp["gmlp_ws"], np.float32).transpose(0, 1, 3, 2))
    return np.ascontiguousarray(pv.astype(np.float32)), np.ascontiguousarray(glnb), gb, wsT


PV_OFF = {}


def _pv_layout():
    o = 0
    for name, n in (("vd", L * 5 * 8), ("vg", L * 7 * 2), ("wdw", L * 2 * 31), ("fcw", L * 44 * 3), ("fcb", L * 44), ("gsub", L), ("lamv", L * 4 * 32)):
        PV_OFF[name] = o
        o += n
    return o


PV_N = _pv_layout()


def build(dbg=None, nlayers=L):
    dbg = dbg or []
    nc = bass.Bass("TRN2", target_bir_lowering=False)

    def din(name, shape, dt=F32):
        return nc.dram_tensor(name, list(shape), dt, kind="ExternalInput").ap()

    xT_d = din("xT", [D, SEQ])
    pT_d = din("pT", [L, GW, SEQ])
    pos_d = din("pos", [1, SEQ], I32)
    cmat_d = din("cmat", [128, 768])
    cf_d = din("cf", [128, 262])
    ind_d = din("ind", [8, SEQ])
    pv_d = din("pv", [128, PV_N])
    glnb_d = din("glnb", [L, 128, 2, 256])
    gb_d = din("gb", [L, 2, 128, 512])
    wsT_d = din("wsT", [L, 4, 128, 128])
    w_in_d = din("w_in", [L, D, 10 * GW])
    w_out_d = din("w_out", [L, D, D])
    w_up_d = din("w_up", [L, D, 2 * DFF])
    w_down_d = din("w_down", [L, DFF, D])
    w_gate_d = din("w_pe_gate", [L, D, D])
    w_proj_d = din("w_pe_proj", [L, GW, D])
    w_pw_d = din("conv_pw_w", [L, GW, GW])
    out_d = nc.dram_tensor("outT", [D, SEQ], F32, kind="ExternalOutput").ap()
    wup_s = nc.dram_tensor("wup_s", [L, NJ, 128, 8 * 2 * 128], BF16, kind="Internal").ap()
    wdn_s = nc.dram_tensor("wdn_s", [L, 8, 128, NJ * 128], BF16, kind="Internal").ap()
    dbg_d = {n: nc.dram_tensor("dbg_" + n, [128, w], F32, kind="ExternalOutput").ap() for n, w in dbg}

    es = ExitStack()
    with es:
        S = Sched(nc, es)
        NB = 206 * 1024
        big = es.enter_context(nc.sbuf_tensor("arena", [128, NB // 4], F32))
        A = Arena(big, NB)
        psb = [es.enter_context(nc.psum_tensor("ps%d" % i, [128, 512], F32)) for i in range(8)]
        PS = [(psb[i][:, :], ("ps", i)) for i in range(8)]
        ringA = Ring(PS[0:4])
        ringB = Ring(PS[4:8])

        def MM(out, lhsT, rhs, start, stop, r, w, inc=True, tp=None):
            kw = {}
            if tp is not None:
                kw["tile_position"] = tp
            S.op("pe", lambda: nc.tensor.matmul(out, lhsT=lhsT, rhs=rhs, start=start, stop=stop, **kw), r=r, w=w, inc=inc)

        def ACT(out, in_, func, r, w, scale=None, bias=None):
            kw = {}
            if scale is not None:
                kw["scale"] = scale
            if bias is not None:
                kw["bias"] = bias
            S.op("act", lambda: nc.scalar.activation(out=out, in_=in_, func=func, **kw), r=r, w=w)

        def TTo(eng, out, in0, in1, op, r, w):
            e = nc.vector if eng == "dve" else nc.gpsimd
            S.op(eng, lambda: e.tensor_tensor(out=out, in0=in0, in1=in1, op=op), r=r, w=w)

        def TS(eng, out, in0, s1, s2, op0, op1, r, w):
            e = nc.vector if eng == "dve" else nc.gpsimd
            if op1 is None:
                S.op(eng, lambda: e.tensor_scalar(out=out, in0=in0, scalar1=s1, scalar2=None, op0=op0), r=r, w=w)
            else:
                S.op(eng, lambda: e.tensor_scalar(out=out, in0=in0, scalar1=s1, scalar2=s2, op0=op0, op1=op1), r=r, w=w)

        def STT(out, in0, scalar, in1, op0, op1, r, w):
            S.op("dve", lambda: nc.vector.scalar_tensor_tensor(out=out, in0=in0, scalar=scalar, in1=in1, op0=op0, op1=op1), r=r, w=w)

        def CP(eng, out, in_, r, w):
            e = nc.vector if eng == "dve" else nc.gpsimd
            S.op(eng, lambda: e.tensor_copy(out=out, in_=in_), r=r, w=w)

        def MSET(eng, ap, val, w):
            e = nc.vector if eng == "dve" else nc.gpsimd
            S.op(eng, lambda: e.memset(ap, val), w=w)

        def DMA(q, out, in_, semkey, r, w):
            e = dict(sp=nc.sync, pool=nc.gpsimd, act=nc.scalar)[q]
            S.dma(q, lambda: e.dma_start(out=out, in_=in_), semkey, r=r, w=w)

        def dump(name, ap, keys):
            if name in dbg_d:
                w_ = ap.shape[-1]
                stg = A.f32(w_)
                CP("dve", stg[0:ap.shape[0], :], ap, r=keys, w=[("dbgs", name)])
                DMA("sp", dbg_d[name][0:ap.shape[0], 0:w_], stg[0:ap.shape[0], :], "dbg", r=[("dbgs", name)], w=[("dbgo", name)])

        xT = A.f32(8 * SEQ).rearrange("p (c s) -> p c s", c=8)
        hT = A.bf16(8 * SEQ).rearrange("p (c s) -> p c s", c=8)
        cmat = A.bf16(768)
        IDENT, ONES, BLK2, R64, R32, TRI = [cmat[:, i * 128:(i + 1) * 128] for i in range(6)]
        cf = A.f32(262)
        TRIL = cf[:, 0:128]
        PMASK = cf[:, 128:256].rearrange("p (q n) -> p q n", n=8)
        INVF = [cf[:, 256:257], cf[:, 257:258]]
        GMASK = cf[:, 258:262]
        pv = A.f32(PV_N)

        def PVv(name, n):
            return pv[:, PV_OFF[name]:PV_OFF[name] + n]

        vd = PVv("vd", L * 5 * 8).rearrange("p (l v c) -> p l v c", l=L, v=5)
        vg = PVv("vg", L * 7 * 2).rearrange("p (l v c) -> p l v c", l=L, v=7)
        wdw = PVv("wdw", L * 2 * 31).rearrange("p (l c k) -> p l c k", l=L, c=2)
        fcw = PVv("fcw", L * 44 * 3).rearrange("p (l j k) -> p l j k", l=L, j=44)
        fcb = PVv("fcb", L * 44).rearrange("p (l j) -> p l j", l=L)
        gsub = PVv("gsub", L)
        lamv = PVv("lamv", L * 4 * 32).rearrange("p (l v k) -> p l v k", l=L, v=4)
        small = A.f32(64)
        lnv = A.f32(512)
        rstd = A.f32(512)
        sq = [A.bf16(512) for _ in range(3)]
        sqring = Ring([(sq[i], ("sq", i)) for i in range(3)])
        PH = A.mark()

        DMA("pool", cmat, cmat_d[:, :], "ld_c", r=[], w=["cmat"])
        DMA("sp", cf, cf_d[:, :], "ld_c2", r=[], w=["cf"])
        DMA("sp", pv, pv_d[:, :], "ld_c3", r=[], w=["pv"])
        for t in range(NT):
            DMA("sp", xT[:, :, t * TW:(t + 1) * TW], xT_d.rearrange("(c p) s -> p c s", p=128)[:, :, t * TW:(t + 1) * TW], ("ld_x", t), r=[], w=[("xT", c, t) for c in range(8)])

        def wload(dst, src, key):
            DMA("pool", dst, src, ("wsem",) + key, r=[], w=[key])

        def precast_ffn(l):
            wupv_ = w_up_d[l].rearrange("(k p) (g n) -> p k g n", p=128, g=2)
            wdnv_ = w_down_d[l].rearrange("(j p) n -> p j n", p=128)
            sk = ("pcs", l)
            keys = []
            for j in range(NJ):
                for gi_ in range(2):
                    dst = wup_s[l, j].rearrange("p (k g n) -> p k g n", k=8, g=2)[:, :, gi_, :]
                    DMA("pool", dst, wupv_[:, :, gi_, j * 128:(j + 1) * 128], sk, r=[], w=[("pcu", l, j, gi_)])
                    keys.append(("pcu", l, j, gi_))
            for dc in range(8):
                dst = wdn_s[l, dc].rearrange("p (j n) -> p j n", j=NJ)
                DMA("pool", dst, wdnv_[:, :, dc * 128:(dc + 1) * 128], sk, r=[], w=[("pcd", l, dc)])
                keys.append(("pcd", l, dc))
            tot = S.cnt[sk]
            for k in keys:
                S.state[k] = [(sk, tot), []]

        def rstd_from(ps_ap, ps_key, inv_n, eps, extra_r=()):
            ACT(lnv, ps_ap, AF.Ln, r=[ps_key] + list(extra_r), w=["lnv"], scale=inv_n, bias=eps)
            ACT(rstd, lnv, AF.Exp, r=["lnv"], w=["rstd"], scale=-0.5)

        def rmsnorm_tile(srcs, skeys, dsts, dkeys, gains, n_feat, eps=EPS, lhs=None, lkey="cmat"):
            ps, pk = ringB.next()
            C = len(srcs)
            for c in range(C):
                q, qk = sqring.next()
                ACT(q, srcs[c], AF.Square, r=[skeys[c]], w=[qk])
                MM(ps, lhs if lhs is not None else ONES, q, c == 0, c == C - 1, r=[qk, lkey], w=[pk])
            rstd_from(ps, pk, 1.0 / n_feat, eps)
            for c in range(C):
                STT(dsts[c], srcs[c], gains[c], rstd, ALU.mult, ALU.mult, r=[skeys[c], "rstd", "pv"], w=[dkeys[c]])

        def sl(t):
            return slice(t * TW, (t + 1) * TW)

        def norm_x_to_h(l, vidx):
            for t in range(NT):
                rmsnorm_tile([xT[:, c, sl(t)] for c in range(8)], [("xT", c, t) for c in range(8)],
                             [hT[:, c, sl(t)] for c in range(8)], [("hT", c, t) for c in range(8)],
                             [vd[:, l, vidx, c:c + 1] for c in range(8)], D)

        def proj(ps, pk, wv, wkey, cs, rhs_fn, rkeys_fn, nk=8):
            for kc in range(nk):
                MM(ps, wv[:, kc, cs], rhs_fn(kc), kc == 0, kc == nk - 1, r=[wkey] + rkeys_fn(kc), w=[pk], inc=(kc == nk - 1))

        def hrhs(t):
            return (lambda kc: hT[:, kc, sl(t)]), (lambda kc: [("hT", kc, t)])

        for l in range(nlayers):
            S.barrier()
            A.release(PH)
            catF = A.bf16(8 * SEQ)
            catT = catF.rearrange("p (c s) -> p c s", c=8)
            ropeC = A.bf16(SEQ)
            ropeS = A.bf16(SEQ)
            wslots = [A.bf16(8 * 256).rearrange("p (k n) -> p k n", k=8) for _ in range(4)]
            MX = A.mark()
            winv = w_in_d[l].rearrange("(k p) n -> p k n", p=128)
            woutv = w_out_d[l].rearrange("(k p) n -> p k n", p=128)
            mplan = [(lambda v, k, bi=bi: wload(v, winv[:, :, bi * 256:(bi + 1) * 256], k)) for bi in (0, 1, 2, 7, 8, 9, 3, 4, 5, 6)]
            mplan += [(lambda v, k, db=db: wload(v, woutv[:, :, db * 256:(db + 1) * 256], k)) for _t in range(NT) for db in range(4)]
            mstream = WStream([(wslots[i], ("ws", i)) for i in range(4)], mplan, auto=False)

            def win_block(bi):
                return mstream.get()

            def rope_tables(which):
                posi = catF[:, 2 * SEQ:4 * SEQ].bitcast(I32)
                v = catF[:, 4 * SEQ:6 * SEQ].bitcast(F32)
                ki = catF[:, 6 * SEQ:8 * SEQ].bitcast(I32)
                DMA("sp", posi, pos_d[0:1, :].broadcast_to([128, SEQ]), "ld_pos", r=[], w=["posi"])
                for tab, shift, key in ((ropeS, 0.0, "ropeS"), (ropeC, 0.25, "ropeC")):
                    TS("dve", v, posi, INVF[which], shift, ALU.mult, ALU.add, r=["posi", "cf"], w=["ropev"])
                    CP("dve", ki, v, r=["ropev"], w=["ropek"])
                    TTo("dve", v, v, ki, ALU.subtract, r=["ropev", "ropek"], w=["ropev"])
                    ACT(tab, v, AF.Sin, r=["ropev"], w=[key], scale=2 * math.pi * (1 - 1e-6))

            def rope_apply(ps, pk, t, RM, zbr, t1, t2):
                zb, zk = zbr.next()
                ACT(zb, ps, AF.Copy, r=[pk], w=[zk])
                ps2, pk2 = ringA.next()
                MM(ps2, RM, zb, True, True, r=[zk, "cmat"], w=[pk2])
                TTo("dve", t1[0], zb, ropeC[:, sl(t)], ALU.mult, r=[zk, "ropeC"], w=[t1[1]])
                TTo("dve", t2[0], ps2, ropeS[:, sl(t)], ALU.mult, r=[pk2, "ropeS"], w=[t2[1]])

            def attn_finalize_recip(O, ok, dst_rden):
                S.op("dve", lambda: nc.vector.reciprocal(out=dst_rden[0][0:64, :], in_=O[64:128, :]), r=[ok], w=[dst_rden[1]])

            def vproj(wv, wk, cs, vaug):
                for g in range(4):
                    ps, pk = ringA.next()
                    for s4 in range(4):
                        st = g * 4 + s4
                        for kc in range(8):
                            MM(ps[:, s4 * 128:(s4 + 1) * 128], hT[:, kc, st * 128:(st + 1) * 128], wv[:, kc, cs], kc == 0, kc == 7,
                               r=[wk, ("hT", kc, st // 4)], w=[pk], inc=(kc == 7 and s4 == 3))
                    ACT(vaug[:, g * 4:(g + 1) * 4, :, 0:64], ps.rearrange("p (s h d) -> p s h d", s=4, h=2), AF.Copy, r=[pk], w=[("V", g)])

            rope_tables(0)
            norm_x_to_h(l, 0)
            if l == 0:
                dump("hT0", hT[:, 0, 0:512], [("hT", 0, 0)])

            m0 = A.mark()
            Kaug = [A.bf16(SEQ) for _ in range(2)]
            vaug = A.bf16(16 * 2 * 128).rearrange("p (s h d) -> p s h d", s=16, h=2)
            Qa = [[A.bf16(512) for _ in range(2)] for _ in range(NT)]
            Pr = Ring([(A.bf16(512), ("P", i)) for i in range(4)])
            zbr = Ring([(A.bf16(512), ("zb", i)) for i in range(2)])
            t1 = (A.f32(512), "t1")
            t2 = (A.f32(512), "t2")
            rden = (A.f32(512), "rden")
            bstg4 = A.bf16(4 * 72).rearrange("p (j n) -> p j n", j=4)
            gm = A.f32(64)
            top8 = A.f32(32)
            kmf = A.f32(16)
            kmb = A.bf16(16)
            MSET("dve", vaug[:, :, :, 64:128], 1.0, w=[("V", g) for g in range(4)])
            MSET("dve", bstg4, 0.0, w=["bstg"])
            for hh in range(2):
                DMA("pool", Kaug[hh][64:72, :], ind_d[:, :], "ld_ind", r=[], w=[("Kind", hh)])
            wq, wqk = win_block(0)
            wk_, wkk = win_block(1)
            wv_, wvk = win_block(2)
            precast_ffn(l)
            for c in range(2):
                cs = slice(c * 128, (c + 1) * 128)
                for t in range(NT):
                    ps, pk = ringA.next()
                    rf, kf = hrhs(t)
                    proj(ps, pk, wk_, wkk, cs, rf, kf)
                    rope_apply(ps, pk, t, R64, zbr, t1, t2)
                    for hh in range(2):
                        TTo("dve", Kaug[hh][0:64, sl(t)], t1[0][hh * 64:(hh + 1) * 64, :], t2[0][hh * 64:(hh + 1) * 64, :], ALU.add,
                            r=[t1[1], t2[1]], w=[("K", hh, t)])
                for hh in range(2):
                    S.op("dve", lambda hh=hh: nc.vector.tensor_reduce(out=kmf[0:64, hh * 8:(hh + 1) * 8], in_=Kaug[hh][0:64, :].rearrange("p (n k) -> p n k", k=256), axis=AX.X, op=ALU.add),
                         r=[("K", hh, t) for t in range(NT)], w=[("kmf", hh)])
                    TS("dve", kmb[0:64, hh * 8:(hh + 1) * 8], kmf[0:64, hh * 8:(hh + 1) * 8], 1.0 / 256, None, ALU.mult, None, r=[("kmf", hh)], w=[("kmb", hh)])
                vproj(wv_, wvk, cs, vaug)
                for qt in range(NT):
                    qb = Qa[qt]
                    ps, pk = ringA.next()
                    rf, kf = hrhs(qt)
                    proj(ps, pk, wq, wqk, cs, rf, kf)
                    rope_apply(ps, pk, qt, R64, zbr, t1, t2)
                    for hh in range(2):
                        TTo("dve", qb[hh][0:64, :], t1[0][hh * 64:(hh + 1) * 64, :], t2[0][hh * 64:(hh + 1) * 64, :], ALU.add,
                            r=[t1[1], t2[1]], w=[("Qa", qt, hh)])
                    for hh in range(2):
                        gps, gk = ringA.next()
                        for j in range(4):
                            MM(gps[:, j * 8:(j + 1) * 8], qb[hh][0:64, j * 128:(j + 1) * 128], kmb[0:64, hh * 8:(hh + 1) * 8], True, True,
                               r=[("Qa", qt, hh), ("kmb", hh)], w=[gk], inc=(j == 3))
                        gmv = gm[:, 0:32].rearrange("p (j n) -> p j n", n=8)
                        TTo("dve", gmv, gps[:, 0:32].rearrange("p (j n) -> p j n", n=8), PMASK[:, qt * 4:(qt + 1) * 4, :], ALU.add, r=[gk, "cf"], w=["gm"])
                        tps, tk = ringA.next()
                        tpsb = tps.bitcast(BF16)
                        top8v = top8.rearrange("p (j n) -> p j n", n=8)
                        for j in range(4):
                            S.op("dve", lambda j=j: nc.vector.max(out=top8[:, j * 8:(j + 1) * 8], in_=gm[:, j * 8:(j + 1) * 8]), r=["gm"], w=["top8"])
                        TS("dve", top8v[:, :, 3:4], top8v[:, :, 3:4], -BIGG, None, ALU.max, None, r=["top8"], w=["top8"])
                        TTo("dve", bstg4[:, :, 64:72], gmv, top8v[:, :, 3:4].broadcast_to([128, 4, 8]), ALU.is_lt, r=["gm", "top8"], w=["bstg"])
                        for j in range(4):
                            S.op("pe", lambda j=j, tpsb=tpsb: nc.tensor.transpose(tpsb[0:72, j * 128:(j + 1) * 128], bstg4[:, j, :], IDENT), r=["bstg", "cmat"], w=[tk], inc=(j == 3))
                        ACT(qb[hh][64:72, :], tpsb[64:72, 0:512], AF.Copy, r=[tk], w=[("Qb", qt, hh)])
                if c == 1:
                    rope_tables(1)
                defer = []
                for qt in range(NT):
                    qb = Qa[qt]
                    for hh in range(2):
                        O, ok = ringB.next()
                        nk = 4 * qt + 4
                        pend = []
                        for kt in range(nk):
                            jd = kt - 4 * qt
                            c0 = 128 * jd if jd > 0 else 0
                            sp_, sk = ringA.next()
                            MM(sp_[:, c0:512], Kaug[hh][0:72, kt * 128:(kt + 1) * 128], qb[hh][0:72, c0:512], True, jd < 0,
                               r=[("K", hh, kt // 4), ("Kind", hh), ("Qa", qt, hh), ("Qb", qt, hh)], w=[sk], inc=(jd < 0))
                            if jd >= 0:
                                MM(sp_[:, c0:c0 + 128], IDENT, TRI, False, True, r=["cmat"], w=[sk])
                            P, Pk = Pr.next()
                            ACT(P[:, c0:512], sp_[:, c0:512], AF.Exp, r=[sk], w=[Pk], scale=0.125)

                            def pv_(kt=kt, c0=c0, P=P, Pk=Pk, O=O, ok=ok, hh=hh, nk=nk):
                                MM(O[:, c0:512], vaug[:, kt, hh, :], P[:, c0:512], kt == 0, kt == nk - 1, r=[Pk, ("V", kt // 4)], w=[ok])
                            pend.append(pv_)
                            if len(pend) > 2:
                                pend.pop(0)()
                            if kt == 1:
                                for f in defer:
                                    f()
                                defer = []
                        for f in pend:
                            f()
                        def fin_(O=O, ok=ok, hh=hh, c=c, qt=qt):
                            attn_finalize_recip(O, ok, rden)
                            TTo("dve", catT[hh * 64:(hh + 1) * 64, c, sl(qt)], O[0:64, :], rden[0][0:64, :], ALU.mult, r=[ok, rden[1]], w=[("cat", c, qt)])
                        defer.append(fin_)
                for f in defer:
                    f()
                defer = []
            if l == 0:
                dump("oa_pre", catT[:, 0, 0:512], [("cat", 0, 0)])
            if l == 0:
                dump("oa", catT[:, 0, 0:512], [("cat", 0, 0)])
            mstream.done()
            S.barrier()
            A.release(m0)

            lam_init = 0.8 - 0.6 * math.exp(-0.3 * l)
            lt = small[:, 0:8]
            prod = A.f32(64)
            TTo("dve", prod[:, 0:32], lamv[:, l, 0, :], lamv[:, l, 1, :], ALU.mult, r=["pv"], w=["prod"])
            S.op("dve", lambda: nc.vector.tensor_reduce(out=lt[:, 0:1], in_=prod[:, 0:32], axis=AX.X, op=ALU.add), r=["prod"], w=["lt"])
            TTo("dve", prod[:, 32:64], lamv[:, l, 2, :], lamv[:, l, 3, :], ALU.mult, r=["pv"], w=["prod2"])
            S.op("dve", lambda: nc.vector.tensor_reduce(out=lt[:, 1:2], in_=prod[:, 32:64], axis=AX.X, op=ALU.add), r=["prod2"], w=["lt"])
            ACT(lt[:, 2:4], lt[:, 0:2], AF.Exp, r=["lt"], w=["lt"])
            TTo("dve", lt[:, 4:5], lt[:, 3:4], lt[:, 2:3], ALU.subtract, r=["lt"], w=["lt"])
            TS("dve", lt[:, 5:6], lt[:, 4:5], -lam_init, None, ALU.add, None, r=["lt"], w=["lt"])
            TS("dve", lt[:, 6:7], gsub[:, l:l + 1], 1.0 - lam_init, None, ALU.mult, None, r=["pv", "lt"], w=["lt"])
            NEGLAM = lt[:, 5:6]
            GS = lt[:, 6:7]

            m0 = A.mark()
            Kd = A.bf16(SEQ)
            vaug = A.bf16(16 * 2 * 128).rearrange("p (s h d) -> p s h d", s=16, h=2)
            Qd = [A.bf16(512) for _ in range(NT)]
            Pr = Ring([(A.bf16(512), ("P", i)) for i in range(4)])
            zbr = Ring([(A.bf16(512), ("zb", i)) for i in range(2)])
            t1 = (A.f32(512), "t1")
            t2 = (A.f32(512), "t2")
            rden = (A.f32(512), "rden")
            ods = [A.f32(512) for _ in range(2)]
            Qm = [[A.bf16(512) for _ in range(4)] for _ in range(2)]
            MSET("dve", vaug[:, :, :, 64:128], 1.0, w=[("V", g) for g in range(4)])
            wq, wqk = win_block(7)
            wk_, wkk = win_block(8)
            wv_, wvk = win_block(9)
            dscale = 32.0 ** -0.5
            for c in range(2):
                cs = slice(c * 128, (c + 1) * 128)
                for t in range(NT):
                    ps, pk = ringA.next()
                    rf, kf = hrhs(t)
                    proj(ps, pk, wk_, wkk, cs, rf, kf)
                    rope_apply(ps, pk, t, R32, zbr, t1, t2)
                    TTo("dve", Kd[:, sl(t)], t1[0], t2[0], ALU.add, r=[t1[1], t2[1]], w=[("Kd", t)])
                vproj(wv_, wvk, cs, vaug)
                for qt in range(NT):
                    qd = Qd[qt]
                    ps, pk = ringA.next()
                    rf, kf = hrhs(qt)
                    proj(ps, pk, wq, wqk, cs, rf, kf)
                    rope_apply(ps, pk, qt, R32, zbr, t1, t2)
                    TTo("dve", qd, t1[0], t2[0], ALU.add, r=[t1[1], t2[1]], w=[("Qd", qt)])
                defer = []
                defer2 = []

                def qmask(qt):
                    for g in range(4):
                        TS("dve", Qm[qt % 2][g], Qd[qt], GMASK[:, g:g + 1], None, ALU.mult, None, r=[("Qd", qt), "cf"], w=[("Qm", qt % 2, g)])
                qmask(0)
                for qt in range(NT):
                    qd = Qd[qt]
                    od = ods[qt % 2]
                    for hh in range(2):
                        Os = [ringB.next(), ringB.next()]
                        nk = 4 * qt + 4
                        pend = []
                        for kt in range(nk):
                            jd = kt - 4 * qt
                            c0 = 128 * jd if jd > 0 else 0
                            cur = []
                            for m_ in range(2):
                                g = hh * 2 + m_
                                sp_, sk = ringA.next()
                                MM(sp_[:, c0:512], Kd[:, kt * 128:(kt + 1) * 128], Qm[qt % 2][g][:, c0:512], True, jd < 0,
                                   r=[("Kd", kt // 4), ("Qm", qt % 2, g)], w=[sk], inc=(jd < 0))
                                if jd >= 0:
                                    MM(sp_[:, c0:c0 + 128], IDENT, TRI, False, True, r=["cmat"], w=[sk])
                                cur.append((sp_, sk))
                            for f in pend:
                                f()
                            pend = []
                            if kt == 1:
                                for f in defer:
                                    f()
                                defer = defer2
                                defer2 = []
                            for m_ in range(2):
                                sp_, sk = cur[m_]
                                P, Pk = Pr.next()
                                ACT(P[:, c0:512], sp_[:, c0:512], AF.Exp, r=[sk], w=[Pk], scale=dscale)

                                def pv_(kt=kt, c0=c0, P=P, Pk=Pk, Oo=Os[m_], hh=hh, nk=nk):
                                    MM(Oo[0][:, c0:512], vaug[:, kt, hh, :], P[:, c0:512], kt == 0, kt == nk - 1, r=[Pk, ("V", kt // 4)], w=[Oo[1]])
                                pend.append(pv_)
                        for f in pend:
                            f()
                        def fin_(Os=Os, hh=hh, qt=qt, od=od):
                            hs = slice(hh * 64, (hh + 1) * 64)
                            attn_finalize_recip(Os[0][0], Os[0][1], rden)
                            TTo("dve", t1[0][0:64, :], Os[0][0][0:64, :], rden[0][0:64, :], ALU.mult, r=[Os[0][1], rden[1]], w=[t1[1]])
                            attn_finalize_recip(Os[1][0], Os[1][1], (lnv, "lnv"))
                            TTo("dve", t2[0][0:64, :], Os[1][0][0:64, :], lnv[0:64, :], ALU.mult, r=[Os[1][1], "lnv"], w=[t2[1]])
                            STT(od[hs, :], t2[0][0:64, :], NEGLAM[0:64, :], t1[0][0:64, :], ALU.mult, ALU.add, r=[t1[1], t2[1], "lt"], w=[("od", qt % 2, hh)])
                        defer.append(fin_)
                        if hh == 0 and qt + 1 < NT:
                            qmask(qt + 1)
                    def norm_(qt=qt, od=od, c=c):
                        ps, pk = ringA.next()
                        q_, qk_ = sqring.next()
                        ACT(q_, od, AF.Square, r=[("od", qt % 2, 0), ("od", qt % 2, 1)], w=[qk_])
                        MM(ps, BLK2, q_, True, True, r=[qk_, "cmat"], w=[pk])
                        rstd_from(ps, pk, 1.0 / 64, 1e-5)
                        STT(catT[:, 6 + c, sl(qt)], od, GS, rstd, ALU.mult, ALU.mult, r=[("od", qt % 2, 0), ("od", qt % 2, 1), "rstd", "lt"], w=[("cat", 6 + c, qt)])
                    defer2.append(norm_)
                for f in defer + defer2:
                    f()
                defer = []
                defer2 = []
            if l == 0:
                dump("od", catT[:, 6, 0:512], [("cat", 6, 0)])
            mstream.done()
            S.barrier()
            A.release(m0)

            m0 = A.mark()
            uT = A.bf16(2 * SEQ).rearrange("p (c s) -> p c s", c=2)
            vgl = A.bf16(16 * 256).rearrange("p (n d) -> p n d", n=16)
            glnb = A.f32(512).rearrange("p (v d) -> p v d", v=2)
            gb = A.f32(1024).rearrange("p (c s) -> p c s", c=2)
            wsf = A.f32(512).rearrange("p (h i) -> p h i", h=4)
            wsb = A.bf16(512).rearrange("p (h i) -> p h i", h=4)
            stats = A.f32(16 * 6)
            mv = A.f32(16 * 2).rearrange("p (n k) -> p n k", k=2)
            rsd = A.f32(16)
            vtmp = A.f32(256)
            vln = [A.bf16(256) for _ in range(2)]
            stmp = A.f32(512)
            DMA("sp", glnb, glnb_d[l], "ld_g1", r=[], w=["glnb"])
            DMA("sp", gb, gb_d[l].rearrange("c p s -> p c s"), "ld_g2", r=[], w=["gb"])
            DMA("sp", wsf, wsT_d[l].rearrange("h j i -> j h i"), "ld_g3", r=[], w=["wsf"])
            for h in range(4):
                TTo("dve", wsb[:, h, :], wsf[:, h, :], TRIL, ALU.mult, r=["wsf", "cf"], w=["wsb"])
            wu, wuk = win_block(3)
            wvv, wvvk = win_block(4)
            for t in range(NT):
                for cc in range(2):
                    ps, pk = ringA.next()
                    rf, kf = hrhs(t)
                    proj(ps, pk, wu, wuk, slice(cc * 128, (cc + 1) * 128), rf, kf)
                    ACT(uT[:, cc, sl(t)], ps, AF.Gelu_apprx_tanh, r=[pk], w=[("uT", cc, t)])
            for n2 in range(8):
                ps, pk = ringA.next()
                for s2 in range(2):
                    n = n2 * 2 + s2
                    for kc in range(8):
                        MM(ps[:, s2 * 256:(s2 + 1) * 256], hT[:, kc, n * 128:(n + 1) * 128], wvv[:, kc, :], kc == 0, kc == 7,
                           r=[wvvk, ("hT", kc, n // 4)], w=[pk], inc=(kc == 7 and s2 == 1))
                ACT(vgl[:, n2 * 2:(n2 + 1) * 2, :], ps.rearrange("p (s d) -> p s d", s=2), AF.Gelu_apprx_tanh, r=[pk], w=[("vgl", n2)])
            for n in range(16):
                S.op("dve", lambda n=n: nc.vector.bn_stats(out=stats[:, n * 6:(n + 1) * 6], in_=vgl[:, n, :]), r=[("vgl", n // 2)], w=[("st", n)])
                S.op("dve", lambda n=n: nc.vector.bn_aggr(out=mv[:, n, :], in_=stats[:, n * 6:(n + 1) * 6]), r=[("st", n)], w=["mv"])
            ACT(rsd, mv[:, :, 1], AF.Ln, r=["mv"], w=["rsd"], bias=EPS)
            ACT(rsd, rsd, AF.Exp, r=["rsd"], w=["rsd"], scale=-0.5)
            for t in range(NT):
                pss = [ringA.next(), ringA.next()]
                for s4 in range(4):
                    n = t * 4 + s4
                    vl = vln[n % 2]
                    vk = ("vln", n % 2)
                    TS("dve", vtmp, vgl[:, n, :], mv[:, n, 0:1], rsd[:, n:n + 1], ALU.subtract, ALU.mult, r=[("vgl", n // 2), "mv", "rsd"], w=["vtmp"])
                    TTo("dve", vtmp, vtmp, glnb[:, 0, :], ALU.mult, r=["vtmp", "glnb"], w=["vtmp"])
                    TTo("dve", vl, vtmp, glnb[:, 1, :], ALU.add, r=["vtmp", "glnb"], w=[vk])
                    for cc in range(2):
                        for hh in range(2):
                            h = 2 * cc + hh
                            MM(pss[cc][0][hh * 64:(hh + 1) * 64, s4 * 128:(s4 + 1) * 128], vl[:, h * 64:(h + 1) * 64], wsb[:, h, :], True, True,
                               r=[vk, "wsb"], w=[pss[cc][1]], tp=(0, hh * 64))
                for cc in range(2):
                    TTo("dve", stmp, pss[cc][0], gb[:, cc, :], ALU.add, r=[pss[cc][1], "gb"], w=["stmp"])
                    TTo("dve", catT[:, 2 + cc, sl(t)], stmp, uT[:, cc, sl(t)], ALU.mult, r=["stmp", ("uT", cc, t)], w=[("cat", 2 + cc, t)])
            if l == 0:
                dump("ob_pre", catT[:, 2, 0:512], [("cat", 2, 0)])
            mstream.done()
            S.barrier()
            A.release(m0)

            m0 = A.mark()
            ybuf = A.bf16(32 + SEQ)
            diag = A.bf16(31 * 128).rearrange("p (k n) -> p k n", k=31)
            cy = A.bf16(2 * SEQ).rearrange("p (c s) -> p c s", c=2)
            sg = A.f32(512)
            wpw = A.bf16(2 * 256).rearrange("p (k n) -> p k n", k=2)
            mstat = A.f32(512)
            m2 = A.f32(512)
            yn = A.f32(512)
            sil = A.bf16(1024).rearrange("p (c s) -> p c s", c=2)
            Y0 = 2
            wa, wak = win_block(5)
            wg_, wgk = win_block(6)
            wload(wpw, w_pw_d[l].rearrange("(k p) n -> p k n", p=128), ("wpw",))
            MSET("dve", ybuf[:, 0:32], 0.0, w=["ypad"])
            for cc in range(2):
                cs = slice(cc * 128, (cc + 1) * 128)
                for k in range(31):
                    TS("dve", diag[:, k, :], IDENT, wdw[:, l, cc, k:k + 1], None, ALU.mult, None, r=["cmat", "pv"], w=[("diag", k)])
                for t in range(NT):
                    pa, pak = ringA.next()
                    rf, kf = hrhs(t)
                    proj(pa, pak, wa, wak, cs, rf, kf)
                    pg, pgk = ringA.next()
                    proj(pg, pgk, wg_, wgk, cs, rf, kf)
                    ACT(sg, pg, AF.Sigmoid, r=[pgk], w=["sg"])
                    TTo("dve", ybuf[:, 32 + t * TW:32 + (t + 1) * TW], pa, sg, ALU.mult, r=[pak, "sg"], w=[("yb", t)])
                for t in range(NT):
                    ps, pk = ringB.next()
                    for k in range(31):
                        o = Y0 + t * TW + k
                        rk = ["ypad", ("diag", k), ("yb", t)] + ([("yb", t - 1)] if t > 0 else [])
                        MM(ps, diag[:, k, :], ybuf[:, o:o + TW], k == 0, k == 30, r=rk, w=[pk], inc=(k == 30))
                    ACT(cy[:, cc, sl(t)], ps, AF.Identity, r=[pk, "pv"], w=[("cy", cc, t)], bias=vg[:, l, 0, cc:cc + 1])
            if l == 0:
                dump("cy", cy[:, 0, 0:512], [("cy", 0, 0)])
            for t in range(NT):
                ps1, pk1 = ringB.next()
                ps2, pk2 = ringB.next()
                for cc in range(2):
                    MM(ps1, ONES, cy[:, cc, sl(t)], cc == 0, cc == 1, r=[("cy", cc, t), "cmat"], w=[pk1])
                for cc in range(2):
                    q_, qk_ = sqring.next()
                    ACT(q_, cy[:, cc, sl(t)], AF.Square, r=[("cy", cc, t)], w=[qk_])
                    MM(ps2, ONES, q_, cc == 0, cc == 1, r=[qk_, "cmat"], w=[pk2])
                TS("dve", mstat, ps1, 1.0 / GW, None, ALU.mult, None, r=[pk1], w=["mstat"])
                TTo("dve", m2, mstat, mstat, ALU.mult, r=["mstat"], w=["m2"])
                STT(m2, ps2, 1.0 / GW, m2, ALU.mult, ALU.subtract, r=[pk2, "m2"], w=["m2"])
                ACT(lnv, m2, AF.Ln, r=["m2"], w=["lnv"], bias=EPS)
                ACT(rstd, lnv, AF.Exp, r=["lnv"], w=["rstd"], scale=-0.5)
                for cc in range(2):
                    TTo("dve", yn, cy[:, cc, sl(t)], mstat, ALU.subtract, r=[("cy", cc, t), "mstat"], w=["yn"])
                    TTo("dve", yn, yn, rstd, ALU.mult, r=["yn", "rstd"], w=["yn"])
                    ACT(sil[:, cc, :], yn, AF.Silu, r=["yn", "pv"], w=[("sil", cc)], scale=vg[:, l, 1, cc:cc + 1], bias=vg[:, l, 2, cc:cc + 1])
                for co in range(2):
                    ps, pk = ringA.next()
                    for ci in range(2):
                        MM(ps, wpw[:, ci, co * 128:(co + 1) * 128], sil[:, ci, :], ci == 0, ci == 1, r=[("wpw",), ("sil", ci)], w=[pk])
                    ACT(catT[:, 4 + co, sl(t)], ps, AF.Identity, r=[pk, "pv"], w=[("cat", 4 + co, t)], bias=vg[:, l, 3, co:co + 1])
            if l == 0:
                dump("oc_pre", catT[:, 4, 0:512], [("cat", 4, 0)])
            mstream.done()
            S.barrier()
            A.release(m0)

            def group_norms(t):
                for gi_, vi_ in ((0, 4), (1, 5), (2, 6)):
                    rmsnorm_tile([catT[:, 2 * gi_ + c, sl(t)] for c in range(2)], [("cat", 2 * gi_ + c, t) for c in range(2)],
                                 [catT[:, 2 * gi_ + c, sl(t)] for c in range(2)], [("cat", 2 * gi_ + c, t) for c in range(2)],
                                 [vg[:, l, vi_, c:c + 1] for c in range(2)], GW)

            def out_proj_norm_res(nkc, wsrc, rhs_fn, rkeys_fn, vidx, wr, ytile):
                group_norms(0)
                for t in range(NT):
                    for db in range(4):
                        wv, wk = wr.get()
                        for dd in range(2):
                            dc = db * 2 + dd
                            ps, pk = ringA.next()
                            for kc in range(nkc):
                                MM(ps, wv[:, kc, dd * 128:(dd + 1) * 128], rhs_fn(kc, t), kc == 0, kc == nkc - 1, r=[wk] + rkeys_fn(kc, t), w=[pk], inc=(kc == nkc - 1))
                            if dc % 2 == 0:
                                ACT(ytile[:, dc, :], ps, AF.Copy, r=[pk], w=[("yt", dc)])
                            else:
                                CP("dve", ytile[:, dc, :], ps, r=[pk], w=[("yt", dc)])
                        wr.done()
                    if t + 1 < NT:
                        group_norms(t + 1)
                    rmsnorm_tile([ytile[:, dc, :] for dc in range(8)], [("yt", dc) for dc in range(8)],
                                 [ytile[:, dc, :] for dc in range(8)], [("yt", dc) for dc in range(8)],
                                 [vd[:, l, vidx, dc:dc + 1] for dc in range(8)], D)
                    for dc in range(8):
                        TTo("dve", xT[:, dc, sl(t)], xT[:, dc, sl(t)], ytile[:, dc, :], ALU.add, r=[("xT", dc, t), ("yt", dc)], w=[("xT", dc, t)])

            m0 = A.mark()
            ytile = A.f32(8 * 512).rearrange("p (c s) -> p c s", c=8)
            out_proj_norm_res(8, w_out_d[l].rearrange("(k p) n -> p k n", p=128), lambda kc, t: catT[:, kc, sl(t)], lambda kc, t: [("cat", kc, t)], 1, mstream, ytile)
            if l == 0:
                dump("x1", xT[:, 0, 0:512], [("xT", 0, 0)])
            S.barrier()
            A.release(MX)
            A.release(PH)

            norm_x_to_h(l, 2)
            ytile = A.f32(8 * 512).rearrange("p (c s) -> p c s", c=8)
            fT = A.bf16(NJ * 512).rearrange("p (j s) -> p j s", j=NJ)
            ub = [[A.f32(2 + 512) for _ in range(2)] for _ in range(2)]
            halo = A.f32(44 * 2).rearrange("p (j k) -> p j k", k=2)
            tas = [A.f32(512) for _ in range(2)]
            tbs = [A.f32(512) for _ in range(2)]
            cgs = [A.f32(512) for _ in range(2)]
            cvs = [A.f32(512) for _ in range(2)]
            wups = [A.bf16(8 * 2 * 128).rearrange("p (k g n) -> p k g n", k=8, g=2) for _ in range(3)]
            wdns = [A.bf16(NJ * 128).rearrange("p (j n) -> p j n", j=NJ) for _ in range(3)]
            MSET("dve", halo, 0.0, w=["halo"])
            wupv = w_up_d[l].rearrange("(k p) (g n) -> p k g n", p=128, g=2)
            wdnv = w_down_d[l].rearrange("(j p) n -> p j n", p=128)

            def ld_up(v, k, j):
                DMA("sp", v.rearrange("p k g n -> p (k g n)"), wup_s[l, j], ("wsem",) + k, r=[("pcu", l, j, 0), ("pcu", l, j, 1)], w=[k])

            def ld_dn(v, k, dc):
                DMA("sp", v.rearrange("p j n -> p (j n)"), wdn_s[l, dc], ("wsem",) + k, r=[("pcd", l, dc)], w=[k])

            wur = WStream([(wups[i], ("wu", i)) for i in range(3)], [(lambda v, k, j=j: ld_up(v, k, j)) for _t in range(NT) for j in range(NJ)])
            wdr = WStream([(wdns[i], ("wd", i)) for i in range(3)], [(lambda v, k, dc=dc: ld_dn(v, k, dc)) for _t in range(NT) for dc in range(8)])
            wur.top()
            wdr.top()
            pend_f = None
            for t in range(NT):
                for j in range(NJ):
                    wv, wk = wur.get()
                    u = ub[j % 2]
                    b2 = j % 2
                    ta, tb, cg, cv = tas[b2], tbs[b2], cgs[b2], cvs[b2]
                    for gi in range(2):
                        jj = j + gi * NJ
                        ps, pk = ringA.next()
                        for kc in range(8):
                            MM(ps, wv[:, kc, gi, :], hT[:, kc, sl(t)], kc == 0, kc == 7, r=[wk, ("hT", kc, t)], w=[pk], inc=(kc == 7))
                        uk = ("ub", b2, gi)
                        ACT(u[gi][:, 2:514], ps, AF.Copy, r=[pk], w=[uk])
                        CP("dve", u[gi][:, 0:2], halo[:, jj, :], r=["halo", ("halo", jj)], w=[uk])
                        tmp = ta if gi == 0 else tb
                        tk_ = ("ta", b2) if gi == 0 else ("tb", b2)
                        ACT(tmp, ps, AF.Identity, r=[pk, "pv"], w=[tk_], scale=fcw[:, l, jj, 2:3], bias=fcb[:, l, jj:jj + 1])
                        ACT(halo[:, jj, :], ps[:, 510:512], AF.Copy, r=[pk], w=[("halo", jj)])
                        dst = cg if gi == 0 else cv
                        dk = ("cg", b2) if gi == 0 else ("cv", b2)
                        STT(tmp, u[gi][:, 1:513], fcw[:, l, jj, 1:2], tmp, ALU.mult, ALU.add, r=[uk, tk_, "pv"], w=[tk_])
                        STT(dst, u[gi][:, 0:512], fcw[:, l, jj, 0:1], tmp, ALU.mult, ALU.add, r=[uk, tk_, "pv"], w=[dk])
                    if pend_f is not None:
                        pend_f()

                    def fin(j=j, b2=b2, cg=cg, cv=cv):
                        ACT(cg, cg, AF.Gelu_apprx_tanh, r=[("cg", b2)], w=[("cg", b2)])
                        TTo("dve", fT[:, j, :], cg, cv, ALU.mult, r=[("cg", b2), ("cv", b2)], w=[("fT", j)])
                    pend_f = fin
                pend_f()
                pend_f = None
                if l == 0 and t == 0:
                    dump("fT", fT[:, 0, :], [("fT", 0)])
                for dc in range(8):
                    wv, wk = wdr.get()
                    ps, pk = ringB.next()
                    for j in range(NJ):
                        MM(ps, wv[:, j, :], fT[:, j, :], j == 0, j == NJ - 1, r=[wk, ("fT", j)], w=[pk], inc=(j == NJ - 1))
                    if dc % 2 == 0:
                        ACT(ytile[:, dc, :], ps, AF.Copy, r=[pk], w=[("yt", dc)])
                    else:
                        CP("dve", ytile[:, dc, :], ps, r=[pk], w=[("yt", dc)])
                rmsnorm_tile([ytile[:, dc, :] for dc in range(8)], [("yt", dc) for dc in range(8)],
                             [ytile[:, dc, :] for dc in range(8)], [("yt", dc) for dc in range(8)],
                             [vd[:, l, 3, dc:dc + 1] for dc in range(8)], D)
                for dc in range(8):
                    TTo("dve", xT[:, dc, sl(t)], xT[:, dc, sl(t)], ytile[:, dc, :], ALU.add, r=[("xT", dc, t), ("yt", dc)], w=[("xT", dc, t)])
            if l == 0:
                dump("x2", xT[:, 0, 0:512], [("xT", 0, 0)])
            S.barrier()
            A.release(PH)

            norm_x_to_h(l, 4)
            pTb = A.bf16(2 * SEQ).rearrange("p (c s) -> p c s", c=2)
            wgs = [A.bf16(8 * 256).rearrange("p (k n) -> p k n", k=8) for _ in range(3)]
            wpj = A.bf16(2 * D).rearrange("p (k n) -> p k n", k=2)
            sgt = [A.f32(512) for _ in range(2)]
            pjt = [A.f32(512) for _ in range(2)]
            wload(pTb, pT_d[l].rearrange("(k p) s -> p k s", p=128), ("pTb",))
            wload(wpj, w_proj_d[l].rearrange("(k p) n -> p k n", p=128), ("wpj",))
            wgv = w_gate_d[l].rearrange("(k p) n -> p k n", p=128)
            wgr = WStream([(wgs[i], ("wg", i)) for i in range(3)], [(lambda v, k, db=db: wload(v, wgv[:, :, db * 256:(db + 1) * 256], k)) for db in range(4)])
            wgr.top()
            it = 0
            for db in range(4):
                wv, wk = wgr.get()
                for dd in range(2):
                    dc = db * 2 + dd
                    for t in range(NT):
                        b = it % 2
                        it += 1
                        ps, pk = ringA.next()
                        rf, kf = hrhs(t)
                        proj(ps, pk, wv, wk, slice(dd * 128, (dd + 1) * 128), rf, kf)
                        ACT(sgt[b], ps, AF.Sigmoid, r=[pk], w=[("sgt", b)])
                        ps2, pk2 = ringB.next()
                        for kc in range(2):
                            MM(ps2, wpj[:, kc, dc * 128:(dc + 1) * 128], pTb[:, kc, sl(t)], kc == 0, kc == 1, r=[("wpj",), ("pTb",)], w=[pk2], inc=(kc == 1))
                        TTo("dve", pjt[b], ps2, sgt[b], ALU.mult, r=[pk2, ("sgt", b)], w=[("pjt", b)])
                        TTo("dve", xT[:, dc, sl(t)], xT[:, dc, sl(t)], pjt[b], ALU.add, r=[("xT", dc, t), ("pjt", b)], w=[("xT", dc, t)])
            if l == 0:
                dump("x3", xT[:, 0, 0:512], [("xT", 0, 0)])

        for c in range(8):
            DMA("sp", out_d[c * 128:(c + 1) * 128, :], xT[:, c, :], "st_out", r=[("xT", c, t) for t in range(NT)], w=[("out", c)])
        S.barrier()
        S.emit()
        build.arena_hi = A.hi
    return nc


_CACHE = {}


def _prep_inputs(inp):
    cmat, cf, ind = _host_consts()
    pv, glnb, gb, wsT = _host_params(inp)
    x = np.asarray(inp["x"], np.float32)
    p = np.asarray(inp["p"], np.float32)
    pos = np.asarray(inp["positions"], np.int32)
    shared = dict(cmat=cmat, cf=cf, ind=ind, pv=pv, glnb=glnb, gb=gb, wsT=wsT)
    for k in ("w_in", "w_out", "w_up", "w_down", "w_pe_gate", "w_pe_proj", "conv_pw_w"):
        shared[k] = np.ascontiguousarray(np.asarray(inp[k], np.float32))
    maps = []
    for b in range(8):
        m = dict(shared)
        m["xT"] = np.ascontiguousarray(x[b].T)
        m["pT"] = np.ascontiguousarray(p[:, b].transpose(0, 2, 1))
        m["pos"] = np.ascontiguousarray(pos[b][None, :])
        maps.append(m)
    return maps


def kernel(**inputs):
    if "nc" not in _CACHE:
        _CACHE["nc"] = build()
    nc = _CACHE["nc"]
    maps = _prep_inputs(inputs)
    res = run_bass_kernel_spmd(nc, maps, core_ids=list(range(8)))
    out = np.stack([np.asarray(r["outT"], np.float32).T for r in res.results], axis=0)
    return np.ascontiguousarray(out)
```
